# Optimizing a Trainium2 kernel written in Bass

```python
import math
import jax, jax.numpy as jnp
from jax import lax
import numpy as np

D_MODEL = 1024
BATCH = 8
SEQ = 4096
DEPTH = 4

GRID_W = 64
CTX_LEN = 256
N_MIXERS = 2
NA_HEADS = 16
NA_HEAD_DIM = D_MODEL // NA_HEADS
NA_KR = 8
NA_KC = 16
NA_QCB = 16
NA_BAND = NA_QCB + NA_KC
GDN_HEADS = 8
GDN_HEAD_DIM = D_MODEL // GDN_HEADS
GDN_CONV = 5
GDN_CHUNK = 64
N_EXPERTS = 16
EC_CAPACITY_FACTOR = 2
D_EXPERT = 2048
DEEPNORM_ALPHA = (2.0 * DEPTH) ** 0.25
DEEPNORM_BETA = (8.0 * DEPTH) ** -0.25
LN_EPS = 1e-6
NEG_INF = -1e30

kernel_name = "hybrid_natten_gdn_ecmoe_dit"


def _layernorm(x, g, b):
    xf = x.astype(jnp.float32)
    mu = jnp.mean(xf, -1, keepdims=True)
    var = jnp.mean(jnp.square(xf - mu), -1, keepdims=True)
    y = (xf - mu) * lax.rsqrt(var + LN_EPS) * g.astype(jnp.float32) + b.astype(jnp.float32)
    return y.astype(x.dtype)


def _modulate(x, shift, scale):
    return x * (1 + scale) + shift


def _natten_tables(rows):
    kr = min(NA_KR, rows)
    r = np.arange(rows)
    row_start = np.clip(r - kr // 2, 0, rows - kr)
    dr_idx = row_start[:, None] + np.arange(kr)[None, :] - r[:, None] + NA_KR - 1
    n_cb = GRID_W // NA_QCB
    band_start = np.clip(np.arange(n_cb) * NA_QCB - NA_KC // 2, 0, GRID_W - NA_BAND)
    band_cols = band_start[:, None] + np.arange(NA_BAND)[None, :]
    qcol = np.arange(GRID_W).reshape(n_cb, NA_QCB)
    col_start = np.clip(qcol - NA_KC // 2, 0, GRID_W - NA_KC)
    kc = band_cols[:, None, :]
    valid = (kc >= col_start[..., None]) & (kc < col_start[..., None] + NA_KC)
    dc_idx = np.clip(kc - qcol[..., None], -(NA_KC - 1), NA_KC - 1) + NA_KC - 1
    return (kr, row_start.astype(np.int32), dr_idx.astype(np.int32),
            band_cols.astype(np.int32), valid, dc_idx.astype(np.int32))


def _natten_mixer(h, hc, w_qkv, w_o, rpb, need_ctx):
    B, N, D = h.shape
    L = hc.shape[1]
    H, dh = NA_HEADS, NA_HEAD_DIM
    rows = N // GRID_W
    n_cb = GRID_W // NA_QCB
    kr, row_start, dr_idx, band_cols, valid, dc_idx = _natten_tables(rows)
    scale = dh ** -0.5
    qkv = (h @ w_qkv).reshape(B, rows, GRID_W, 3, H, dh)
    q, k, v = qkv[..., 0, :, :], qkv[..., 1, :, :], qkv[..., 2, :, :]
    qkvc = (hc @ w_qkv).reshape(B, L, 3, H, dh)
    qc, kc, vc = qkvc[:, :, 0], qkvc[:, :, 1], qkvc[:, :, 2]
    mask_add = jnp.where(jnp.asarray(valid), 0.0, NEG_INF).astype(jnp.float32)
    bias_tab = rpb.astype(jnp.float32)[:, :, dc_idx] + mask_add[None, None]
    q_rows = jnp.moveaxis(q, 1, 0).reshape(rows, B, n_cb, NA_QCB, H, dh)

    def row_block(args):
        q_r, rs, dr = args
        k_rows = lax.dynamic_slice_in_dim(k, rs, kr, axis=1)
        v_rows = lax.dynamic_slice_in_dim(v, rs, kr, axis=1)
        k_band = k_rows[:, :, band_cols]
        v_band = v_rows[:, :, band_cols]
        s_win = jnp.einsum('bjqhd,bijkhd->bhjqik', q_r, k_band).astype(jnp.float32) * scale
        bias = jnp.take(bias_tab, dr, axis=1).transpose(0, 2, 3, 1, 4)
        s_win = (s_win + bias[None]).reshape(B, H, n_cb, NA_QCB, kr * NA_BAND)
        s_ctx = jnp.einsum('bjqhd,blhd->bhjql', q_r, kc).astype(jnp.float32) * scale
        p = jax.nn.softmax(jnp.concatenate([s_win, s_ctx], -1), axis=-1).astype(v.dtype)
        p_win = p[..., :kr * NA_BAND].reshape(B, H, n_cb, NA_QCB, kr, NA_BAND)
        p_ctx = p[..., kr * NA_BAND:]
        o = (jnp.einsum('bhjqik,bijkhd->bjqhd', p_win, v_band)
             + jnp.einsum('bhjql,blhd->bjqhd', p_ctx, vc))
        return o.reshape(B, GRID_W, H * dh)

    o = lax.map(row_block, (q_rows, jnp.asarray(row_start), jnp.asarray(dr_idx)))
    y = jnp.moveaxis(o, 0, 1).reshape(B, N, D) @ w_o
    yc = None
    if need_ctx:
        sc = jnp.einsum('blhd,bmhd->bhlm', qc, kc).astype(jnp.float32) * scale
        pc = jax.nn.softmax(sc, axis=-1).astype(vc.dtype)
        yc = jnp.einsum('bhlm,bmhd->blhd', pc, vc).reshape(B, L, D) @ w_o
    return y, yc


def _short_conv(u, w):
    C = u.shape[-1]
    pad = GDN_CONV // 2
    return lax.conv_general_dilated(u, w[:, None, :], window_strides=(1,), padding=[(pad, pad)],
                                    dimension_numbers=('NWC', 'WIO', 'NWC'), feature_group_count=C)


def _l2norm(a):
    return a * lax.rsqrt(jnp.sum(a * a, -1, keepdims=True) + 1e-6)


def _gated_delta_chunked(q, k, v, beta, g, s0):
    B, T, H, dk = q.shape
    dv = v.shape[-1]
    C = GDN_CHUNK
    n = T // C
    chunks = lambda a: a.reshape(B, n, C, H, *a.shape[3:]).swapaxes(2, 3)
    q = chunks(q) * dk ** -0.5
    k, v, beta, g = chunks(k), chunks(v), chunks(beta), chunks(g)
    G = jnp.cumsum(g, axis=-1)
    incl = jnp.tril(jnp.ones((C, C), bool))
    strict = jnp.tril(jnp.ones((C, C), bool), -1)
    decay = jnp.exp(jnp.where(incl, G[..., :, None] - G[..., None, :], -jnp.inf))
    kb = k * beta[..., None]
    A = jnp.where(strict, jnp.einsum('bnhcd,bnhsd->bnhcs', kb, k) * decay, 0.0)
    rhs = jnp.concatenate([kb * jnp.exp(G)[..., None], v * beta[..., None]], -1)
    wu = lax.linalg.triangular_solve(A + jnp.eye(C, dtype=A.dtype), rhs, left_side=True,
                                     lower=True, unit_diagonal=True)
    w, u = wu[..., :dk], wu[..., dk:]
    attn = jnp.einsum('bnhcd,bnhsd->bnhcs', q, k) * decay
    q_dec = q * jnp.exp(G)[..., None]
    k_tail = k * jnp.exp(G[..., -1:] - G)[..., None]
    g_tot = jnp.exp(G[..., -1])
    xs = tuple(jnp.moveaxis(a, 1, 0) for a in (w, u, attn, q_dec, k_tail, g_tot))

    def step(S, inp):
        w_c, u_c, a_c, qd_c, kt_c, gt_c = inp
        v_new = u_c - jnp.einsum('bhcd,bhde->bhce', w_c, S)
        o_c = jnp.einsum('bhcd,bhde->bhce', qd_c, S) + jnp.einsum('bhcs,bhse->bhce', a_c, v_new)
        S = S * gt_c[..., None, None] + jnp.einsum('bhcd,bhce->bhde', kt_c, v_new)
        return S, o_c

    S, o = lax.scan(step, s0, xs)
    return o.transpose(1, 0, 3, 2, 4).reshape(B, T, H, dv), S


def _gdn_project(u, w_in, conv_w, a_log, dt_bias):
    B, T, D = u.shape
    H, dk = GDN_HEADS, GDN_HEAD_DIM
    p = u @ w_in
    qkv = jax.nn.silu(_short_conv(p[..., :3 * D], conv_w)).astype(jnp.float32)
    q = _l2norm(qkv[..., :D].reshape(B, T, H, dk))
    k = _l2norm(qkv[..., D:2 * D].reshape(B, T, H, dk))
    v = qkv[..., 2 * D:].reshape(B, T, H, dk)
    z = p[..., 3 * D:4 * D]
    ab = p[..., 4 * D:].astype(jnp.float32).reshape(B, T, 2, 2, H)
    g = -jnp.exp(a_log.astype(jnp.float32)) * jax.nn.softplus(ab[:, :, 0] + dt_bias.astype(jnp.float32))
    beta = jax.nn.sigmoid(ab[:, :, 1])
    return q, k, v, g, beta, z


def _gdn_out(o, z, norm_w):
    B, T, H, dv = o.shape
    zf = z.astype(jnp.float32).reshape(B, T, H, dv)
    y = o * lax.rsqrt(jnp.mean(o * o, -1, keepdims=True) + LN_EPS) * norm_w.astype(jnp.float32)
    return (y * jax.nn.silu(zf)).reshape(B, T, H * dv).astype(z.dtype)


def _gdn_mixer(h, hc, w_in, conv_w, a_log, dt_bias, norm_w, w_o, need_ctx):
    B = h.shape[0]
    q, k, v, g, beta, z = _gdn_project(h, w_in, conv_w, a_log, dt_bias)
    qc, kc, vc, gc, betac, zc = _gdn_project(hc, w_in, conv_w, a_log, dt_bias)
    s0 = jnp.zeros((B, GDN_HEADS, GDN_HEAD_DIM, GDN_HEAD_DIM), jnp.float32)
    rev = lambda a: jnp.flip(a, axis=1)
    oc_f, s_f = _gated_delta_chunked(qc, kc, vc, betac[:, :, 0], gc[:, :, 0], s0)
    o_f, _ = _gated_delta_chunked(q, k, v, beta[:, :, 0], g[:, :, 0], s_f)
    oc_b, s_b = _gated_delta_chunked(rev(qc), rev(kc), rev(vc), rev(betac[:, :, 1]), rev(gc[:, :, 1]), s0)
    o_b, _ = _gated_delta_chunked(rev(q), rev(k), rev(v), rev(beta[:, :, 1]), rev(g[:, :, 1]), s_b)
    y = _gdn_out(o_f + rev(o_b), z, norm_w) @ w_o
    yc = _gdn_out(oc_f + rev(oc_b), zc, norm_w) @ w_o if need_ctx else None
    return y, yc


def _ec_moe(h, w_router, w_gate, w_up, w_down):
    B, n, D = h.shape
    cap = EC_CAPACITY_FACTOR * n // N_EXPERTS
    aff = jax.nn.softmax((h @ w_router).astype(jnp.float32), axis=-1)
    gate, idx = lax.top_k(jnp.swapaxes(aff, 1, 2), cap)
    xs = jax.vmap(lambda hb, ib: hb[ib])(h, idx)
    a = jnp.einsum('becd,edf->becf', xs, w_gate)
    u = jnp.einsum('becd,edf->becf', xs, w_up)
    ye = jnp.einsum('becf,efd->becd', jax.nn.silu(a) * u, w_down) * gate[..., None].astype(h.dtype)
    return jax.vmap(lambda yb, ib: jnp.zeros((n, D), h.dtype).at[ib.reshape(-1)].add(yb.reshape(-1, D)))(ye, idx)


def setup_inputs(seed: int = 0) -> dict:
    key = jax.random.key(seed)
    ks = jax.random.split(key, 24)
    D, H = D_MODEL, GDN_HEADS
    nA, nB = (DEPTH + 1) // 2, DEPTH // 2
    f32 = jnp.float32
    nrm = lambda k, shape, std: jax.random.normal(k, shape, f32) * std
    dt = jnp.exp(jax.random.uniform(ks[13], (nB, 2, H), f32, math.log(1e-3), math.log(1e-1)))
    return {
        "x": nrm(ks[0], (BATCH, SEQ, D), 1.0),
        "c": nrm(ks[1], (BATCH, D), 1.0),
        "ctx": nrm(ks[2], (BATCH, CTX_LEN, D), 1.0),
        "c_ctx": nrm(ks[3], (D,), 1.0),
        "ada_w": nrm(ks[4], (DEPTH, D, 6 * D), D ** -0.5),
        "ada_b": nrm(ks[5], (DEPTH, 6 * D), 0.02),
        "ln_g": 1.0 + nrm(ks[6], (DEPTH, 2, D), 0.02),
        "ln_b": nrm(ks[7], (DEPTH, 2, D), 0.02),
        "na_w_qkv": nrm(ks[8], (nA, D, 3 * D), D ** -0.5),
        "na_w_o": nrm(ks[9], (nA, D, D), D ** -0.5 * DEEPNORM_BETA),
        "na_rpb": nrm(ks[10], (nA, NA_HEADS, 2 * NA_KR - 1, 2 * NA_KC - 1), 0.02),
        "gdn_w_in": nrm(ks[11], (nB, D, 4 * D + 4 * H), D ** -0.5),
        "gdn_conv_w": nrm(ks[12], (nB, GDN_CONV, 3 * D), GDN_CONV ** -0.5),
        "gdn_a_log": jnp.log(jax.random.uniform(ks[14], (nB, 2, H), f32, 1.0, 16.0)),
        "gdn_dt_bias": dt + jnp.log(-jnp.expm1(-dt)),
        "gdn_norm_w": 1.0 + nrm(ks[15], (nB, GDN_HEAD_DIM), 0.02),
        "gdn_w_o": nrm(ks[16], (nB, D, D), D ** -0.5 * DEEPNORM_BETA),
        "moe_w_router": nrm(ks[17], (DEPTH, D, N_EXPERTS), D ** -0.5),
        "moe_w_gate": nrm(ks[18], (DEPTH, N_EXPERTS, D, D_EXPERT), D ** -0.5),
        "moe_w_up": nrm(ks[19], (DEPTH, N_EXPERTS, D, D_EXPERT), D ** -0.5),
        "moe_w_down": nrm(ks[20], (DEPTH, N_EXPERTS, D_EXPERT, D), D_EXPERT ** -0.5 * DEEPNORM_BETA),
    }


def reference(x, c, ctx, c_ctx, ada_w, ada_b, ln_g, ln_b, na_w_qkv, na_w_o, na_rpb,
              gdn_w_in, gdn_conv_w, gdn_a_log, gdn_dt_bias, gdn_norm_w, gdn_w_o,
              moe_w_router, moe_w_gate, moe_w_up, moe_w_down):
    alpha = DEEPNORM_ALPHA
    xc = ctx
    silu_c = jax.nn.silu(c)[:, None, :]
    silu_cc = jax.nn.silu(c_ctx)[None, None, :]
    for l in range(DEPTH):
        last = l == DEPTH - 1
        i = l // N_MIXERS
        mod = jnp.split(silu_c @ ada_w[l] + ada_b[l], 6, axis=-1)
        modc = jnp.split(silu_cc @ ada_w[l] + ada_b[l], 6, axis=-1)
        h = _modulate(x, mod[0], mod[1])
        hc = _modulate(xc, modc[0], modc[1])
        if l % N_MIXERS == 0:
            y, yc = _natten_mixer(h, hc, na_w_qkv[i], na_w_o[i], na_rpb[i], not last)
        else:
            y, yc = _gdn_mixer(h, hc, gdn_w_in[i], gdn_conv_w[i], gdn_a_log[i], gdn_dt_bias[i],
                               gdn_norm_w[i], gdn_w_o[i], not last)
        x = _layernorm(alpha * x + mod[2] * y, ln_g[l, 0], ln_b[l, 0])
        h = _modulate(x, mod[3], mod[4])
        x = _layernorm(alpha * x + mod[5] * _ec_moe(h, moe_w_router[l], moe_w_gate[l], moe_w_up[l], moe_w_down[l]),
                       ln_g[l, 1], ln_b[l, 1])
        if not last:
            xc = _layernorm(alpha * xc + modc[2] * yc, ln_g[l, 0], ln_b[l, 0])
            hc = _modulate(xc, modc[3], modc[4])
            xc = _layernorm(alpha * xc + modc[5] * _ec_moe(hc, moe_w_router[l], moe_w_gate[l], moe_w_up[l], moe_w_down[l]),
                            ln_g[l, 1], ln_b[l, 1])
    return x
```

```python
import contextlib
import numpy as np
import concourse.bass as bass
import concourse.mybir as mybir
from concourse.bass_utils import run_bass_kernel_spmd

F32 = mybir.dt.float32
BF16 = mybir.dt.bfloat16
I32 = mybir.dt.int32
U8 = mybir.dt.uint8
AF = mybir.ActivationFunctionType
ALU = mybir.AluOpType
AX = mybir.AxisListType

PE, ACT, DVE, POOL, SP = "tensor", "scalar", "vector", "gpsimd", "sync"
ENGS = [PE, ACT, DVE, POOL, SP]
N_DMA_SEMS = 20
SEM_EPOCH = 20000

D = 1024
NT = 34
TT = NT * 128
NE = 16
DEPTH = 4
ALPHA = (2.0 * DEPTH) ** 0.25
LN_EPS = 1e-6
DSZ = {F32: 4, BF16: 2, I32: 4, U8: 1}


class Res:
    __slots__ = ("name", "last_w", "readers", "excl")

    def __init__(self, name=""):
        self.name = name
        self.last_w = None
        self.readers = []
        self.excl = False


class Op:
    __slots__ = ("eng", "fn", "deps", "is_dma", "sem", "val", "sig")

    def __init__(self, eng, fn, is_dma):
        self.eng = eng
        self.fn = fn
        self.deps = ()
        self.is_dma = is_dma
        self.sem = None
        self.val = None
        self.sig = False


class Prog:
    def __init__(self, nc):
        self.nc = nc
        self.q = {e: [] for e in ENGS}
        self.dmas_since_barrier = []
        self.nops = 0

    def _add(self, eng, fn, reads, writes, is_dma):
        op = Op(eng, fn, is_dma)
        deps = set()
        for r in reads:
            if r.last_w is not None:
                deps.add(r.last_w)
            if r.excl:
                deps.update(x for x in r.readers if x.eng != eng)
        for w in writes:
            if w.last_w is not None:
                deps.add(w.last_w)
            deps.update(w.readers)
        op.deps = tuple(deps)
        for r in reads:
            r.readers.append(op)
        for w in writes:
            w.last_w = op
            w.readers = []
        self.q[eng].append(op)
        self.nops += 1
        if is_dma:
            self.dmas_since_barrier.append(op)
        return op

    def op(self, eng, fn, reads=(), writes=()):
        return self._add(eng, fn, reads, writes, False)

    def dma(self, eng, out, in_, reads=(), writes=(), **kw):
        return self._add(eng, lambda e: e.dma_start(out=out, in_=in_, **kw), reads, writes, True)

    def barrier(self):
        b = Op(SP, lambda e: e.nop(), False)
        deps = set(self.dmas_since_barrier)
        for e in ENGS:
            if self.q[e]:
                deps.add(self.q[e][-1])
        b.deps = tuple(deps)
        self.q[SP].append(b)
        self.dmas_since_barrier = []
        for e in ENGS:
            if e == SP:
                continue
            o = Op(e, lambda en: en.nop(), False)
            o.deps = (b,)
            self.q[e].append(o)

    def emit(self):
        nc = self.nc
        for e in ENGS:
            for op in self.q[e]:
                for d in op.deps:
                    if d.is_dma or d.eng != op.eng or op.is_dma or d.eng != PE:
                        d.sig = True
        with contextlib.ExitStack() as st:
            nsig = {e: sum(1 for o in self.q[e] if o.sig and not o.is_dma) for e in ENGS}
            csem = {e: [st.enter_context(nc.semaphore("cs_%s_%d" % (e, i)))
                        for i in range(nsig[e] // SEM_EPOCH + 1)] for e in ENGS}
            dsem = {e: [st.enter_context(nc.semaphore("ds_%s_%d" % (e, i))) for i in range(N_DMA_SEMS)]
                    for e in (SP, ACT, POOL)}
            for e in ENGS:
                cnt = 0
                dcnt = 0
                for op in self.q[e]:
                    if op.is_dma:
                        op.sem = dsem[e][dcnt % N_DMA_SEMS]
                        op.val = 16 * (dcnt // N_DMA_SEMS + 1)
                        op.sig = True
                        dcnt += 1
                    elif op.sig:
                        op.sem = csem[e][cnt // SEM_EPOCH]
                        op.val = cnt % SEM_EPOCH + 1
                        cnt += 1
            block = st.enter_context(nc.Block())

            def gen(e):
                def body(eng):
                    waited = {}
                    for op in self.q[e]:
                        needs = {}
                        for d in op.deps:
                            if not d.sig:
                                continue
                            if (not d.is_dma) and d.eng == e and e == PE and not op.is_dma:
                                continue
                            k = id(d.sem)
                            if needs.get(k, (None, 0))[1] < d.val:
                                needs[k] = (d.sem, d.val)
                        if op.is_dma and op.val > 16:
                            k = id(op.sem)
                            if needs.get(k, (None, 0))[1] < op.val - 16:
                                needs[k] = (op.sem, op.val - 16)
                        for k, (s, v) in needs.items():
                            if waited.get(k, 0) >= v:
                                continue
                            eng.wait_ge(s, v)
                            waited[k] = v
                        ins = op.fn(eng)
                        if op.sig:
                            ins.then_inc(op.sem, 16 if op.is_dma else 1)
                return body

            for e in ENGS:
                if self.q[e]:
                    getattr(block, e)(gen(e))


class Tl:
    def __init__(self, ap, name="", nsub=0):
        self.ap = ap
        self.r = Res(name)
        self.sub = [Res(name + str(i)) for i in range(nsub)]


ARENA = 206000


class K:
    def __init__(self, nc, cfg):
        self.nc = nc
        self.cfg = cfg
        self.P = Prog(nc)
        global ARENA
        ARENA = (int(nc.sbuf_bytes_remaining) - 256) // 64 * 64
        self.big = nc.alloc_sbuf_tensor("arena", [128, ARENA], U8)
        self.off = 0
        self.ps = []
        for i in range(8):
            t = nc.alloc_psum_tensor("psb%d" % i, [128, 512], F32)
            self.ps.append(Tl(t[:], "ps%d" % i, nsub=4))
            for r_ in [self.ps[-1].r] + self.ps[-1].sub:
                r_.excl = True
        self.psn = 0
        self.uid = 0

    def tile(self, shape, dt, name="t", nsub=0):
        n = int(np.prod(shape[1:])) * DSZ[dt]
        if self.off + n > ARENA:
            raise RuntimeError("SBUF arena overflow at %s: %d + %d" % (name, self.off, n))
        a = self.big[0:shape[0], self.off:self.off + n].bitcast(dt)
        self.off += (n + 63) // 64 * 64
        if len(shape) == 3:
            a = a.rearrange("p (a b) -> p a b", a=shape[1])
        elif len(shape) == 4:
            a = a.rearrange("p (a b c) -> p a b c", a=shape[1], b=shape[2])
        self.uid += 1
        return Tl(a, "%s_%d" % (name, self.uid), nsub)

    def psum(self):
        t = self.ps[self.psn % 8]
        self.psn += 1
        return t

    def dram(self, name, shape, dt, kind="Internal"):
        return self.nc.dram_tensor(name, list(shape), dt, kind=kind).ap()

    def op(self, eng, fn, reads=(), writes=()):
        return self.P.op(eng, fn, reads, writes)

    def dma(self, eng, out, in_, reads=(), writes=(), **kw):
        return self.P.dma(eng, out, in_, reads, writes, **kw)

    def declare_io(self):
        nc, cfg = self.nc, self.cfg
        ein = lambda n, s: self.dram(n, s, F32, kind="ExternalInput")
        self.x_in = ein("x", [4096, D])
        self.c_in = ein("c", [1, D])
        self.ctx_in = ein("ctx", [256, D])
        self.cc_in = ein("c_ctx", [1, D])
        self.ada_w = ein("ada_w", [DEPTH, D, 6 * D])
        self.ada_b = ein("ada_b", [DEPTH, 6 * D])
        self.ln_g = ein("ln_g", [DEPTH, 2, D])
        self.ln_b = ein("ln_b", [DEPTH, 2, D])
        self.na_w_qkv = ein("na_w_qkv", [2, D, 3 * D])
        self.na_w_o = ein("na_w_o", [2, D, D])
        self.na_rpb = ein("na_rpb", [2, 16, 15, 31])
        self.gdn_w_in = ein("gdn_w_in", [2, D, 4 * D + 32])
        self.gdn_conv_w = ein("gdn_conv_w", [2, 5, 3 * D])
        self.gdn_a_log = ein("gdn_a_log", [2, 2, 8])
        self.gdn_dt_bias = ein("gdn_dt_bias", [2, 2, 8])
        self.gdn_norm_w = ein("gdn_norm_w", [2, 128])
        self.gdn_w_o = ein("gdn_w_o", [2, D, D])
        if cfg.get("test") not in ("mix", "gdn1", "gdn2"):
            self.moe_w_router = ein("moe_w_router", [DEPTH, D, NE])
            self.moe_w_gate = ein("moe_w_gate", [DEPTH, NE, D, 2048])
            self.moe_w_up = ein("moe_w_up", [DEPTH, NE, D, 2048])
            self.moe_w_down = ein("moe_w_down", [DEPTH, NE, 2048, D])
        if cfg.get("test") in ("gdn1", "gdn2"):
            self.dbg2 = self.dram("dbg2", [4, D, TT], F32, kind="ExternalOutput")
            self.dbg3 = self.dram("dbg3", [128, 2, NT, 16], F32, kind="ExternalOutput")
        self.rpb_pad = ein("rpb_pad", [2, 16, 15, 128])
        self.cmask_in = ein("cmask", [128, 128])
        self.gmask_in = ein("gmask", [4, 128, 128])
        self.out = self.dram("out", [4096, D], F32, kind="ExternalOutput")
        tk = cfg.get("test")
        self.XS = Tl(self.dram("XS", [TT, D], F32), "XS", NT)
        self.Y = Tl(self.dram("Y", [TT, D], F32), "Y", NT)
        if tk:
            self.xs_in = self.dram("xs_in", [TT, D], F32, kind="ExternalInput")
            self.y_in = self.dram("y_in", [TT, D], F32, kind="ExternalInput")
        if tk:
            self.dbg = Tl(self.dram("dbg", [TT, D], F32, kind="ExternalOutput"), "dbg", NT)
        self.AFFT = Tl(self.dram("AFFT", [NE, TT], F32), "AFFT")
        self.POST = Tl(self.dram("POST", [NE, TT], F32), "POST")
        self.YE = Tl(self.dram("YE", [NE, 4, 128, D], BF16), "YE", NE)
        self.YEC = Tl(self.dram("YEC", [NE, 32, D], BF16), "YEC", NE)
        self.SELT = Tl(self.dram("SELT", [32, 128, 64, 128], BF16), "SELT", 32)
        self.SELTC = Tl(self.dram("SELTC", [NE, 32, 256], BF16), "SELTC")
        self.outr = Res("out")

    def setup(self):
        op, dma = self.op, self.dma
        self.ident = self.tile([128, 128], BF16, "ident")
        self.ident32 = self.tile([128, 128], F32, "ident32")
        self.ones = self.tile([128, 128], BF16, "ones")
        self.triu = self.tile([128, 128], BF16, "triu")
        tmp = self.tile([128, 128], F32, "tmp")
        self.iota512 = self.tile([128, 512], F32, "iota512")
        self.iotacol = self.tile([128, 4], F32, "iotacol")
        i32, idb, on, tu, io, ic = self.ident32, self.ident, self.ones, self.triu, self.iota512, self.iotacol
        op(POOL, lambda e: e.memset(i32.ap, 1.0), writes=[i32.r])
        op(POOL, lambda e: e.affine_select(out=i32.ap, in_=i32.ap, pattern=[[-1, 128]], compare_op=ALU.is_equal,
                                           fill=0.0, base=0, channel_multiplier=1), reads=[i32.r], writes=[i32.r])
        op(DVE, lambda e: e.tensor_copy(out=idb.ap, in_=i32.ap), reads=[i32.r], writes=[idb.r])
        op(DVE, lambda e: e.memset(on.ap, 1.0), writes=[on.r])
        op(POOL, lambda e: e.memset(tmp.ap, 1.0), writes=[tmp.r])
        op(POOL, lambda e: e.affine_select(out=tmp.ap, in_=tmp.ap, pattern=[[1, 128]], compare_op=ALU.is_ge,
                                           fill=0.0, base=0, channel_multiplier=-1), reads=[tmp.r], writes=[tmp.r])
        op(DVE, lambda e: e.tensor_copy(out=tu.ap, in_=tmp.ap), reads=[tmp.r], writes=[tu.r])
        op(POOL, lambda e: e.iota(io.ap, pattern=[[1, 512]], base=0, channel_multiplier=0,
                                  allow_small_or_imprecise_dtypes=True), writes=[io.r])
        op(POOL, lambda e: e.iota(ic.ap, pattern=[[128, 4]], base=0, channel_multiplier=1,
                                  allow_small_or_imprecise_dtypes=True), writes=[ic.r])
        self.scb = self.tile([128, 8, 128], F32, "scb")
        self.sccb = self.tile([128, 8, 128], F32, "sccb")
        for src, dst in ((self.c_in, self.scb), (self.cc_in, self.sccb)):
            cv = self.tile([128, 8], F32, "cv")
            dma(SP, cv.ap, src.rearrange("o (k p) -> p (o k)", p=128), writes=[cv.r], allow_slow_non_contiguous=True)
            op(ACT, lambda e, cv=cv: e.activation(out=cv.ap, in_=cv.ap, func=AF.Silu), reads=[cv.r], writes=[cv.r])
            op(DVE, lambda e, cv=cv, dst=dst: e.tensor_copy(out=dst.ap, in_=cv.ap.unsqueeze(2).to_broadcast([128, 8, 128])),
               reads=[cv.r], writes=[dst.r])
        self.base_off = self.off
        if not self.cfg.get("test"):
            dma(SP, self.XS.ap[0:256, :], self.ctx_in, writes=self.XS.sub[0:2])
            dma(SP, self.XS.ap[256:TT, :], self.x_in, writes=self.XS.sub[2:NT])
        else:
            dma(SP, self.XS.ap, self.xs_in, writes=self.XS.sub)
            dma(SP, self.Y.ap, self.y_in, writes=self.Y.sub)

    def phase_begin(self):
        self.P.barrier()
        self.off = self.base_off

    def mod_tiles(self, l, idxs, plus1=()):
        op, dma = self.op, self.dma
        res = {}
        for idx in idxs:
            res[idx] = (self.tile([128, D], F32, "modl"), self.tile([128, D], F32, "modc"))
        mark = self.off
        wbuf = [self.tile([128, 8, 512], F32, "adaw") for _ in range(2)]
        bbuf = [self.tile([128, 1024], F32, "adab") for _ in range(2)]
        n = 0
        for ii, idx in enumerate(idxs):
            lat, ctx = res[idx]
            bb = bbuf[ii % 2]
            dma(SP, bb.ap, self.ada_b[l:l + 1, idx * D:(idx + 1) * D].to_broadcast([128, D]), writes=[bb.r])
            for half in range(2):
                wb = wbuf[n % 2]
                n += 1
                c0 = idx * D + half * 512
                dma(SP, wb.ap, self.ada_w[l, :, c0:c0 + 512].rearrange("(k p) n -> p k n", p=128), writes=[wb.r])
                for lhs, dst in ((self.scb, lat), (self.sccb, ctx)):
                    ps = self.psum()
                    for k in range(8):
                        op(PE, lambda e, ps=ps, lhs=lhs, wb=wb, k=k: e.matmul(ps.ap, lhsT=lhs.ap[:, k, :], rhs=wb.ap[:, k, :],
                                                                              start=(k == 0), stop=(k == 7)),
                           reads=[lhs.r, wb.r], writes=[ps.r])
                    sl = slice(half * 512, (half + 1) * 512)
                    op(DVE, lambda e, ps=ps, dst=dst, bb=bb, sl=sl: e.tensor_tensor(out=dst.ap[:, sl], in0=ps.ap, in1=bb.ap[:, sl], op=ALU.add),
                       reads=[ps.r, bb.r], writes=[dst.r])
            if idx in plus1:
                for t in (lat, ctx):
                    op(DVE, lambda e, t=t: e.tensor_scalar(out=t.ap, in0=t.ap, scalar1=1.0, scalar2=None, op0=ALU.add),
                       reads=[t.r], writes=[t.r])
        self.P.barrier()
        self.off = mark
        return res

    def ln_vec(self, src_row):
        t = self.tile([128, D], F32, "lnv")
        self.dma(SP, t.ap, src_row.to_broadcast([128, D]), writes=[t.r])
        return t

    def layernorm_(self, r, st, mv):
        op = self.op
        for h in range(2):
            op(DVE, lambda e, h=h: e.bn_stats(out=st.ap[:, h, :], in_=r.ap[:, h * 512:(h + 1) * 512]), reads=[r.r], writes=[st.r])
        op(DVE, lambda e: e.bn_aggr(out=mv.ap[:, 0:2], in_=st.ap.rearrange("p a b -> p (a b)")), reads=[st.r], writes=[mv.r])
        op(DVE, lambda e: e.tensor_scalar(out=mv.ap[:, 2:3], in0=mv.ap[:, 1:2], scalar1=LN_EPS, scalar2=None, op0=ALU.add),
           reads=[mv.r], writes=[mv.r])
        op(ACT, lambda e: e.activation(out=mv.ap[:, 2:3], in_=mv.ap[:, 2:3], func=AF.Ln), reads=[mv.r], writes=[mv.r])
        op(ACT, lambda e: e.activation(out=mv.ap[:, 2:3], in_=mv.ap[:, 2:3], func=AF.Exp, scale=-0.5), reads=[mv.r], writes=[mv.r])
        op(DVE, lambda e: e.tensor_scalar(out=r.ap, in0=r.ap, scalar1=mv.ap[:, 0:1], scalar2=mv.ap[:, 2:3],
                                          op0=ALU.subtract, op1=ALU.mult), reads=[r.r, mv.r], writes=[r.r])

    def premix(self, l):
        op, dma = self.op, self.dma
        self.phase_begin()
        hT = self.tile([128, 8, TT], BF16, "hT", nsub=NT)
        self.hT = hT
        self.mix_keep = self.off
        mods = self.mod_tiles(l, [0, 1], plus1=(1,))
        xb = [self.tile([128, D], F32, "xb") for _ in range(3)]
        hb = [self.tile([128, D], BF16, "hb") for _ in range(2)]
        for i in range(NT):
            x = xb[i % 3]
            h = hb[i % 2]
            c = 1 if i < 2 else 0
            dma(SP, x.ap, self.XS.ap[i * 128:(i + 1) * 128, :], reads=[self.XS.sub[i]], writes=[x.r])
            sc, sh = mods[1][c], mods[0][c]
            op(DVE, lambda e, x=x, sc=sc: e.tensor_tensor(out=x.ap, in0=x.ap, in1=sc.ap, op=ALU.mult), reads=[x.r, sc.r], writes=[x.r])
            op(POOL, lambda e, x=x, sh=sh, h=h: e.tensor_tensor(out=h.ap, in0=x.ap, in1=sh.ap, op=ALU.add), reads=[x.r, sh.r], writes=[h.r])
            self.transpose_to(h, hT, i)

    def transpose_to(self, h, hT, i, evac=ACT):
        op = self.op
        ps = self.psum()
        pb = ps.ap.bitcast(BF16)
        for k in range(8):
            op(PE, lambda e, k=k, pb=pb, h=h: e.transpose(out=pb[:, k * 128:(k + 1) * 128], in_=h.ap[:, k * 128:(k + 1) * 128],
                                                          identity=self.ident.ap), reads=[h.r, self.ident.r], writes=[ps.r])
        dst = hT.ap[:, :, i * 128:(i + 1) * 128]
        if evac == ACT:
            op(ACT, lambda e, pb=pb, dst=dst: e.copy(out=dst, in_=pb.rearrange("p (k n) -> p k n", k=8)), reads=[ps.r], writes=[hT.sub[i]])
        else:
            op(evac, lambda e, pb=pb, dst=dst: e.tensor_copy(out=dst, in_=pb.rearrange("p (k n) -> p k n", k=8)), reads=[ps.r], writes=[hT.sub[i]])

    def natten(self, l):
        op, dma = self.op, self.dma
        li = l // 2
        hT = self.hT
        self.P.barrier()
        self.off = self.mix_keep
        if not hasattr(self, "QT"):
            self.QT = Tl(self.dram("QT", [D, TT], BF16), "QT")
            self.KT = Tl(self.dram("KT", [D, TT], BF16), "KT")
            self.VX = Tl(self.dram("VX", [TT, 16, 65], BF16), "VX")
            self.OD = Tl(self.dram("OD", [TT, D], BF16), "OD", NT)
        QT, KT, VX, OD = self.QT, self.KT, self.VX, self.OD
        wq = self.na_w_qkv[li]
        wb = [self.tile([128, 8, 512], BF16, "wqk") for _ in range(2)]
        stq = [self.tile([128, 512], BF16, "stq") for _ in range(3)]
        blocks = [(0, 256)] + [(256 + 512 * b, 512) for b in range(8)]
        n = 0
        for cg in range(4):
            w = wb[cg % 2]
            dma(POOL, w.ap, wq[:, cg * 512:(cg + 1) * 512].rearrange("(k p) n -> p k n", p=128), writes=[w.r])
            for cl in range(4):
                ct = cg * 4 + cl
                dst = QT if ct < 8 else KT
                crow = (ct % 8) * 128
                for (t0, nt_) in blocks:
                    ps = self.psum()
                    for k in range(8):
                        op(PE, lambda e, ps=ps, w=w, k=k, cl=cl, t0=t0, nt_=nt_: e.matmul(ps.ap[:, 0:nt_], lhsT=w.ap[:, k, cl * 128:(cl + 1) * 128], rhs=hT.ap[:, k, t0:t0 + nt_],
                                                                                    start=(k == 0), stop=(k == 7)),
                           reads=[w.r] + hT.sub[t0 // 128:(t0 + nt_) // 128], writes=[ps.r])
                    sq = stq[n % 3]
                    n += 1
                    if ct < 8:
                        op(ACT, lambda e, ps=ps, sq=sq, nt_=nt_: e.mul(out=sq.ap[:, 0:nt_], in_=ps.ap[:, 0:nt_], mul=0.125), reads=[ps.r], writes=[sq.r])
                    else:
                        op(DVE, lambda e, ps=ps, sq=sq, nt_=nt_: e.tensor_copy(out=sq.ap[:, 0:nt_], in_=ps.ap[:, 0:nt_]), reads=[ps.r], writes=[sq.r])
                    dma(SP, dst.ap[crow:crow + 128, t0:t0 + nt_], sq.ap[:, 0:nt_], reads=[sq.r], writes=[dst.r])
        wv = self.tile([128, 8, D], BF16, "wv")
        dma(POOL, wv.ap, wq[:, 2 * D:3 * D].rearrange("(k p) n -> p k n", p=128), writes=[wv.r])
        vst = [self.tile([128, 16, 65], BF16, "vst") for _ in range(2)]
        for v in vst:
            op(DVE, lambda e, v=v: e.memset(v.ap, 1.0), writes=[v.r])
        for i in range(NT):
            v = vst[i % 2]
            for half in range(2):
                ps = self.psum()
                for k in range(8):
                    op(PE, lambda e, ps=ps, k=k, i=i, half=half: e.matmul(ps.ap, lhsT=hT.ap[:, k, i * 128:(i + 1) * 128], rhs=wv.ap[:, k, half * 512:(half + 1) * 512],
                                                                       start=(k == 0), stop=(k == 7)),
                       reads=[wv.r, hT.sub[i]], writes=[ps.r])
                op(ACT, lambda e, ps=ps, v=v, half=half: e.copy(out=v.ap[:, half * 8:(half + 1) * 8, 0:64], in_=ps.ap.rearrange("p (h d) -> p h d", d=64)),
                   reads=[ps.r], writes=[v.r])
            dma(SP, VX.ap[i * 128:(i + 1) * 128], v.ap, reads=[v.r], writes=[VX.r])
        self.P.barrier()
        self.off = self.mix_keep - 0
        self.off = self.base_off
        rs_ = np.clip(np.arange(64) - 4, 0, 56)
        tiles, tindex, per_rp = [], {}, []
        for rp in range(32):
            r0 = 2 * rp
            lst = []
            for kp in range(rs_[r0] // 2, (rs_[r0 + 1] + 7) // 2 + 1):
                spec = []
                for i2 in range(2):
                    for j2 in range(2):
                        kr, r = 2 * kp + i2, r0 + j2
                        spec.append(int(kr - r + 7) if rs_[r] <= kr < rs_[r] + 8 else None)
                spec = tuple(spec)
                key = (rp if rp in (0, 1, 30, 31) else -1, spec)
                if key not in tindex:
                    tindex[key] = len(tiles)
                    tiles.append(spec)
                lst.append((kp, tindex[key]))
            assert all(lst[j + 1][1] == lst[j][1] + 1 for j in range(len(lst) - 1))
            per_rp.append(lst)
        NTL = len(tiles)
        jm = self.tile([64, 128], F32, "jm")
        op(POOL, lambda e: e.memset(jm.ap, 1.0), writes=[jm.r])
        op(POOL, lambda e: e.affine_select(out=jm.ap[:, 0:64], in_=jm.ap[:, 0:64], pattern=[[1, 64]], compare_op=ALU.is_equal, fill=0.0, base=-63, channel_multiplier=1),
           reads=[jm.r], writes=[jm.r])
        op(POOL, lambda e: e.affine_select(out=jm.ap[:, 64:128], in_=jm.ap[:, 64:128], pattern=[[1, 64]], compare_op=ALU.is_equal, fill=0.0, base=-63, channel_multiplier=1),
           reads=[jm.r], writes=[jm.r])
        cm = self.tile([128, 128], F32, "cmask")
        dma(SP, cm.ap, self.cmask_in, writes=[cm.r])
        hk = self.tile([64, 15, 64], F32, "hk")
        toe = self.tile([128, 15, 64], F32, "toe")
        bias = self.tile([128, NTL, 128], F32, "bias")
        qt = [self.tile([64, TT], BF16, "qt") for _ in range(2)]
        kt = [self.tile([64, TT], BF16, "kt") for _ in range(2)]
        vh = [self.tile([128, NT, 65], BF16, "vh") for _ in range(2)]
        sb = [self.tile([128, 640], F32, "sb") for _ in range(2)]
        pb = [self.tile([128, 896], BF16, "pb") for _ in range(2)]
        ost = [self.tile([128, NT, 64], BF16, "ost") for _ in range(2)]
        rc = self.tile([128, 4], F32, "rc")
        rpb = self.rpb_pad[li]
        it = 0
        for h in range(16):
            q_, k_, v_, o_ = qt[h % 2], kt[h % 2], vh[h % 2], ost[h % 2]
            dma(SP, q_.ap, QT.ap[h * 64:(h + 1) * 64, :], reads=[QT.r], writes=[q_.r])
            dma(SP, k_.ap, KT.ap[h * 64:(h + 1) * 64, :], reads=[KT.r], writes=[k_.r])
            dma(SP, v_.ap, VX.ap.rearrange("(i p) h d -> p i h d", p=128)[:, :, h, :], reads=[VX.r], writes=[v_.r])
            src = bass.AP(tensor=rpb.tensor, offset=rpb[h].offset, ap=[[1, 64], [128, 15], [1, 64]])
            dma(SP, hk.ap, src, writes=[hk.r])
            for (d0, d1) in ((0, 8), (8, 15)):
                ps = self.psum()
                nn = (d1 - d0) * 64
                op(PE, lambda e, ps=ps, d0=d0, d1=d1, nn=nn: e.matmul(ps.ap[:, 0:nn], lhsT=jm.ap, rhs=hk.ap[:, d0:d1, :].rearrange("p a b -> p (a b)"), start=True, stop=True),
                   reads=[jm.r, hk.r], writes=[ps.r])
                op(ACT, lambda e, ps=ps, d0=d0, d1=d1, nn=nn: e.copy(out=toe.ap[:, d0:d1, :].rearrange("p a b -> p (a b)"), in_=ps.ap[:, 0:nn]), reads=[ps.r], writes=[toe.r])
            for t, spec in enumerate(tiles):
                for bi, dr in enumerate(spec):
                    i2, j2 = bi // 2, bi % 2
                    prt = slice(i2 * 64, (i2 + 1) * 64)
                    fr = slice(j2 * 64, (j2 + 1) * 64)
                    if dr is None:
                        op(POOL, lambda e, t=t, prt=prt, fr=fr: e.memset(bias.ap[prt, t, fr], -1e30), writes=[bias.r])
                    else:
                        op(POOL, lambda e, t=t, prt=prt, fr=fr, dr=dr: e.tensor_tensor(out=bias.ap[prt, t, fr], in0=toe.ap[prt, dr, :], in1=cm.ap[prt, fr], op=ALU.add),
                           reads=[toe.r, cm.r], writes=[bias.r])
            for qi in range(NT):
                if qi < 2:
                    wins = []
                    ctxk = [0, 1]
                else:
                    wins = per_rp[qi - 2]
                    ctxk = [0, 1]
                s_, p_ = sb[it % 2], pb[it % 2]
                it += 1
                nw = len(wins)
                qs = slice(qi * 128, (qi + 1) * 128)
                psA, psB = self.psum(), self.psum()
                slots = []
                for j in range(nw):
                    slots.append((psA, j * 128) if j < 4 else (psB, 0))
                cb = 128 if nw == 5 else 0
                for j, kk in enumerate(ctxk):
                    slots.append((psB, cb + j * 128))
                ktl = [2 + kp for (kp, _) in wins] + ctxk
                for (pst, co), kti in zip(slots, ktl):
                    op(PE, lambda e, pst=pst, co=co, kti=kti, qs=qs, k_=k_, q_=q_: e.matmul(pst.ap[:, co:co + 128], lhsT=k_.ap[:, kti * 128:(kti + 1) * 128], rhs=q_.ap[:, qs],
                                                                                         start=True, stop=True),
                       reads=[k_.r, q_.r], writes=[pst.r])
                if nw:
                    t0 = wins[0][1]
                    na = min(nw, 4)
                    op(DVE, lambda e, s_=s_, t0=t0, na=na, psA=psA: e.tensor_tensor(out=s_.ap[:, 0:na * 128], in0=psA.ap[:, 0:na * 128],
                                                                                   in1=bias.ap[:, t0:t0 + na, :].rearrange("p a b -> p (a b)"), op=ALU.add),
                       reads=[psA.r, bias.r], writes=[s_.r])
                    if nw == 5:
                        op(DVE, lambda e, s_=s_, t0=t0, psB=psB: e.tensor_tensor(out=s_.ap[:, 512:640], in0=psB.ap[:, 0:128], in1=bias.ap[:, t0 + 4, :], op=ALU.add),
                           reads=[psB.r, bias.r], writes=[s_.r])
                    op(ACT, lambda e, s_=s_, p_=p_, nw=nw: e.activation(out=p_.ap[:, 0:nw * 128], in_=s_.ap[:, 0:nw * 128], func=AF.Exp), reads=[s_.r], writes=[p_.r])
                op(ACT, lambda e, p_=p_, nw=nw, cb=cb, psB=psB: e.activation(out=p_.ap[:, nw * 128:nw * 128 + 256], in_=psB.ap[:, cb:cb + 256], func=AF.Exp),
                   reads=[psB.r], writes=[p_.r])
                pso = self.psum()
                nk = len(ktl)
                for j, kti in enumerate(ktl):
                    op(PE, lambda e, pso=pso, p_=p_, j=j, kti=kti, v_=v_, nk=nk: e.matmul(pso.ap[:, 0:65], lhsT=p_.ap[:, j * 128:(j + 1) * 128], rhs=v_.ap[:, kti, :],
                                                                                       start=(j == 0), stop=(j == nk - 1)),
                       reads=[p_.r, v_.r], writes=[pso.r])
                op(DVE, lambda e, pso=pso: e.reciprocal(out=rc.ap[:, 0:1], in_=pso.ap[:, 64:65]), reads=[pso.r], writes=[rc.r])
                op(DVE, lambda e, pso=pso, o_=o_, qi=qi: e.tensor_scalar(out=o_.ap[:, qi, :], in0=pso.ap[:, 0:64], scalar1=rc.ap[:, 0:1], scalar2=None, op0=ALU.mult),
                   reads=[pso.r, rc.r], writes=[o_.r])
            dma(SP, OD.ap.rearrange("(i p) d -> p i d", p=128)[:, :, h * 64:(h + 1) * 64], o_.ap, reads=[o_.r], writes=OD.sub)
        self.out_proj(self.na_w_o[li])

    def gdn(self, l):
        op, dma = self.op, self.dma
        li = l // 2
        hT = self.hT
        self.P.barrier()
        self.off = self.mix_keep
        if not hasattr(self, "QF"):
            for nm in ("QF", "KF", "VF", "ZF", "OF"):
                setattr(self, nm, Tl(self.dram(nm, [D, TT], F32), nm, NT))
        QF, KF, VF, ZF, OF = self.QF, self.KF, self.VF, self.ZF, self.OF
        w_in = self.gdn_w_in[li]
        ones_a = self.tile([128, 128], F32, "ones_a")
        op(DVE, lambda e: e.memset(ones_a.ap, 1.0), writes=[ones_a.r])
        gT_a = self.tile([128, NT, 16], F32, "gT_a")
        bT_a = self.tile([128, NT, 16], F32, "bT_a")
        g_keep = self.off
        cw = self.tile([128, 24, 5], F32, "cw")
        for j in range(5):
            dma(SP, cw.ap[:, :, j], self.gdn_conv_w[li, j:j + 1, :].rearrange("o (c p) -> p (o c)", p=128), writes=[cw.r], allow_slow_non_contiguous=True)
        nwv = self.tile([128, 1], F32, "nwv")
        dma(SP, nwv.ap, self.gdn_norm_w[li:li + 1, :].rearrange("o p -> p o"), writes=[nwv.r], allow_slow_non_contiguous=True)
        pbuf = [self.tile([128, TT + 8], F32, "pbuf") for _ in range(2)]
        cbuf = [self.tile([128, TT], F32, "cbuf") for _ in range(2)]
        for pb in pbuf:
            op(POOL, lambda e, pb=pb: e.memset(pb.ap, 0.0), writes=[pb.r])
        wb = [self.tile([128, 8, 512], BF16, "win") for _ in range(2)]
        sqb = [self.tile([128, 512], F32, "sqb") for _ in range(2)]
        rnb = [self.tile([128, 512], F32, "rnb") for _ in range(2)]
        zsb = [self.tile([128, 512], F32, "zsb") for _ in range(2)]
        blocks = [(0, 256)] + [(256 + 512 * b, 512) for b in range(8)]
        nb = 0
        for cg in range(8):
            w = wb[cg % 2]
            dma(POOL, w.ap, w_in[:, cg * 512:(cg + 1) * 512].rearrange("(k p) n -> p k n", p=128), writes=[w.r])
            for cl in range(4):
                ct = cg * 4 + cl
                pb, cb = pbuf[ct % 2], cbuf[ct % 2]
                for (t0, nt_) in blocks:
                    ps = self.psum()
                    for k in range(8):
                        op(PE, lambda e, ps=ps, w=w, k=k, cl=cl, t0=t0, nt_=nt_: e.matmul(ps.ap[:, 0:nt_], lhsT=w.ap[:, k, cl * 128:(cl + 1) * 128], rhs=hT.ap[:, k, t0:t0 + nt_],
                                                                                    start=(k == 0), stop=(k == 7)),
                           reads=[w.r] + hT.sub[t0 // 128:(t0 + nt_) // 128], writes=ps.sub)
                    if ct < 24:
                        o0 = (2 if t0 == 0 else 6) + t0
                        op(ACT, lambda e, ps=ps, pb=pb, o0=o0, nt_=nt_: e.copy(out=pb.ap[:, o0:o0 + nt_], in_=ps.ap[:, 0:nt_]), reads=ps.sub, writes=[pb.r])
                    else:
                        zs = zsb[nb % 2]
                        nb += 1
                        op(ACT, lambda e, ps=ps, zs=zs, nt_=nt_: e.activation(out=zs.ap[:, 0:nt_], in_=ps.ap[:, 0:nt_], func=AF.Silu), reads=ps.sub, writes=[zs.r])
                        op(DVE, lambda e, zs=zs, nt_=nt_: e.tensor_scalar(out=zs.ap[:, 0:nt_], in0=zs.ap[:, 0:nt_], scalar1=nwv.ap[:, 0:1], scalar2=None, op0=ALU.mult),
                           reads=[zs.r, nwv.r], writes=[zs.r])
                        r0 = (ct - 24) * 128
                        dma(SP, ZF.ap[r0:r0 + 128, t0:t0 + nt_], zs.ap[:, 0:nt_], reads=[zs.r], writes=ZF.sub[t0 // 128:(t0 + nt_) // 128])
                if ct >= 24:
                    continue
                for (c0, ln, base) in ((0, 256, 0), (256, 4096, 260)):
                    op(DVE, lambda e, cb=cb, pb=pb, c0=c0, ln=ln, base=base, ct=ct: e.tensor_scalar(out=cb.ap[:, c0:c0 + ln], in0=pb.ap[:, base:base + ln], scalar1=cw.ap[:, ct, 0:1],
                                                                                             scalar2=None, op0=ALU.mult), reads=[pb.r, cw.r], writes=[cb.r])
                    for j in range(1, 5):
                        op(DVE, lambda e, cb=cb, pb=pb, c0=c0, ln=ln, base=base, ct=ct, j=j: e.scalar_tensor_tensor(out=cb.ap[:, c0:c0 + ln], in0=pb.ap[:, base + j:base + j + ln],
                                                                                                            scalar=cw.ap[:, ct, j:j + 1], in1=cb.ap[:, c0:c0 + ln], op0=ALU.mult, op1=ALU.add),
                           reads=[pb.r, cw.r, cb.r], writes=[cb.r])
                op(ACT, lambda e, cb=cb: e.activation(out=cb.ap, in_=cb.ap, func=AF.Silu), reads=[cb.r], writes=[cb.r])
                if ct < 16:
                    qsc = (128.0 ** -0.5) if ct < 8 else 1.0
                    for (t0, nt_) in blocks:
                        sq, rn = sqb[nb % 2], rnb[nb % 2]
                        nb += 1
                        op(POOL, lambda e, sq=sq, cb=cb, t0=t0, nt_=nt_: e.tensor_tensor(out=sq.ap[:, 0:nt_], in0=cb.ap[:, t0:t0 + nt_], in1=cb.ap[:, t0:t0 + nt_], op=ALU.mult),
                           reads=[cb.r], writes=[sq.r])
                        ps = self.psum()
                        op(PE, lambda e, ps=ps, sq=sq, nt_=nt_, o32=ones_a: e.matmul(ps.ap[:, 0:nt_], lhsT=o32.ap, rhs=sq.ap[:, 0:nt_], start=True, stop=True), reads=[ones_a.r, sq.r], writes=ps.sub)
                        op(DVE, lambda e, ps=ps, rn=rn, nt_=nt_: e.tensor_scalar(out=rn.ap[:, 0:nt_], in0=ps.ap[:, 0:nt_], scalar1=1e-6, scalar2=None, op0=ALU.add), reads=ps.sub, writes=[rn.r])
                        op(ACT, lambda e, rn=rn, nt_=nt_: e.activation(out=rn.ap[:, 0:nt_], in_=rn.ap[:, 0:nt_], func=AF.Ln), reads=[rn.r], writes=[rn.r])
                        op(ACT, lambda e, rn=rn, nt_=nt_: e.activation(out=rn.ap[:, 0:nt_], in_=rn.ap[:, 0:nt_], func=AF.Exp, scale=-0.5), reads=[rn.r], writes=[rn.r])
                        op(DVE, lambda e, rn=rn, cb=cb, t0=t0, nt_=nt_, qsc=qsc: e.scalar_tensor_tensor(out=cb.ap[:, t0:t0 + nt_], in0=cb.ap[:, t0:t0 + nt_], scalar=qsc, in1=rn.ap[:, 0:nt_],
                                                                                                 op0=ALU.mult, op1=ALU.mult), reads=[cb.r, rn.r], writes=[cb.r])
                dst = (QF, KF, VF)[ct // 8]
                r0 = (ct % 8) * 128
                dma(SP, dst.ap[r0:r0 + 128, :], cb.ap, reads=[cb.r], writes=dst.sub)
        wab = self.tile([128, 8, 32], BF16, "wab")
        dma(POOL, wab.ap, w_in[:, 4096:4128].rearrange("(k p) n -> p k n", p=128), writes=[wab.r])
        coef = self.tile([128, 16], F32, "coef")
        dtb = self.tile([128, 16], F32, "dtb")
        dma(SP, coef.ap, self.gdn_a_log[li:li + 1].rearrange("o a b -> o (a b)").to_broadcast([128, 16]), writes=[coef.r])
        dma(SP, dtb.ap, self.gdn_dt_bias[li:li + 1].rearrange("o a b -> o (a b)").to_broadcast([128, 16]), writes=[dtb.r])
        op(ACT, lambda e: e.activation(out=coef.ap, in_=coef.ap, func=AF.Exp), reads=[coef.r], writes=[coef.r])
        op(DVE, lambda e: e.tensor_scalar(out=coef.ap, in0=coef.ap, scalar1=-1.0, scalar2=None, op0=ALU.mult), reads=[coef.r], writes=[coef.r])
        psl = []
        for i in range(NT):
            if i % 16 == 0:
                ps = self.psum()
                psl.append(ps)
            c0 = (i % 16) * 32
            for k in range(8):
                op(PE, lambda e, ps=ps, i=i, k=k, c0=c0: e.matmul(ps.ap[:, c0:c0 + 32], lhsT=hT.ap[:, k, i * 128:(i + 1) * 128], rhs=wab.ap[:, k, :], start=(k == 0), stop=(k == 7)),
                   reads=[wab.r, hT.sub[i]], writes=ps.sub)
        for gi, ps in enumerate(psl):
            n_ = min(16, NT - gi * 16)
            pv = ps.ap[:, 0:n_ * 32].rearrange("p (i c) -> p i c", c=32)
            gs = gT_a.ap[:, gi * 16:gi * 16 + n_, :]
            bs = bT_a.ap[:, gi * 16:gi * 16 + n_, :]
            op(DVE, lambda e, pv=pv, gs=gs, n_=n_: e.tensor_tensor(out=gs, in0=pv[:, :, 0:16], in1=dtb.ap.unsqueeze(1).to_broadcast([128, n_, 16]), op=ALU.add), reads=ps.sub + [dtb.r], writes=[gT_a.r])
            op(ACT, lambda e, gs=gs: e.activation(out=gs, in_=gs, func=AF.Exp), reads=[gT_a.r], writes=[gT_a.r])
            op(DVE, lambda e, gs=gs: e.tensor_scalar(out=gs, in0=gs, scalar1=1.0, scalar2=None, op0=ALU.add), reads=[gT_a.r], writes=[gT_a.r])
            op(ACT, lambda e, gs=gs: e.activation(out=gs, in_=gs, func=AF.Ln), reads=[gT_a.r], writes=[gT_a.r])
            op(DVE, lambda e, gs=gs, n_=n_: e.tensor_tensor(out=gs, in0=gs, in1=coef.ap.unsqueeze(1).to_broadcast([128, n_, 16]), op=ALU.mult), reads=[gT_a.r, coef.r], writes=[gT_a.r])
            op(ACT, lambda e, pv=pv, bs=bs: e.activation(out=bs, in_=pv[:, :, 16:32], func=AF.Exp, scale=-1.0), reads=ps.sub, writes=[bT_a.r])
            op(DVE, lambda e, bs=bs: e.tensor_scalar(out=bs, in0=bs, scalar1=1.0, scalar2=None, op0=ALU.add), reads=[bT_a.r], writes=[bT_a.r])
            op(DVE, lambda e, bs=bs: e.reciprocal(out=bs, in_=bs), reads=[bT_a.r], writes=[bT_a.r])
        self.P.barrier()
        if self.cfg.get("test") == "gdn1":
            rr = Res("dbg2")
            for i_, t_ in enumerate((QF, KF, VF, ZF)):
                dma(SP, self.dbg2[i_], t_.ap, reads=t_.sub, writes=[rr])
            dma(SP, self.dbg3[:, 0], gT_a.ap, reads=[gT_a.r], writes=[rr])
            dma(SP, self.dbg3[:, 1], bT_a.ap, reads=[bT_a.r], writes=[rr])
            self.dbg.sub.append(rr)
            return
        self.off = self.base_off
        gT_o, bT_o = gT_a, bT_a
        ones32 = self.tile([128, 128], F32, "ones32b")
        gT = self.tile([128, NT, 16], F32, "gTb")
        bT = self.tile([128, NT, 16], F32, "bTb")
        op(DVE, lambda e: e.memset(ones32.ap, 1.0), writes=[ones32.r])
        op(DVE, lambda e: e.tensor_copy(out=gT.ap, in_=gT_o.ap), reads=[gT_o.r], writes=[gT.r])
        op(DVE, lambda e: e.tensor_copy(out=bT.ap, in_=bT_o.ap), reads=[bT_o.r], writes=[bT.r])
        self.P.barrier()
        def cmat(name, fill_in, pattern, cmult, cmp, fill):
            t = self.tile([128, 128], F32, name)
            op(POOL, lambda e: e.memset(t.ap, fill_in), writes=[t.r])
            op(POOL, lambda e: e.affine_select(out=t.ap, in_=t.ap, pattern=[[pattern, 128]], compare_op=cmp, fill=fill, base=0, channel_multiplier=cmult), reads=[t.r], writes=[t.r])
            return t
        triu32 = cmat("triu32", 1.0, 1, -1, ALU.is_ge, 0.0)
        tril32 = cmat("tril32", 1.0, -1, 1, ALU.is_ge, 0.0)
        sup = cmat("sup", 1.0, 1, -1, ALU.is_gt, 0.0)
        slo = cmat("slo", 1.0, -1, 1, ALU.is_gt, 0.0)
        mup = cmat("mup", 0.0, 1, -1, ALU.is_ge, -1e30)
        mlo = cmat("mlo", 0.0, -1, 1, ALU.is_ge, -1e30)
        i32 = self.ident32
        wo = self.tile([128, 8, D], BF16, "wo")
        dma(POOL, wo.ap, self.gdn_w_o[li].rearrange("(k p) n -> p k n", p=128), writes=[wo.r])
        T4 = lambda nm: self.tile([128, 8, 128], F32, nm)
        qfb = [T4("qf") for _ in range(2)]
        kfb = [T4("kf") for _ in range(2)]
        vfb = [T4("vf") for _ in range(2)]
        xfb = [T4("xf") for _ in range(2)]
        zfb = [T4("zf") for _ in range(2)]
        Rt, dTt, d2t, dec, decT, eGbc, AmT, An = (T4(nm) for nm in ("R", "dT", "d2", "dec", "decT", "eGbc", "AmT", "An"))
        kbg, ktail, vb = T4("kbg"), T4("ktail"), T4("vb")
        Mb = [T4("M0"), T4("M1")]
        Nb = [T4("N0"), T4("N1")]
        Pm, attnT, qdT, nwT, vnew, St = T4("P"), T4("attnT"), T4("qdT"), T4("nwT"), T4("vnew"), T4("S")
        PT = T4("PT")
        Mc, Nc = [Rt], [d2t]
        gm = []
        for gi_ in range(4):
            gt_ = self.tile([128, 128], F32, "gmask")
            dma(SP, gt_.ap, self.gmask_in[gi_], writes=[gt_.r])
            gm.append(gt_)
        Gtok = self.tile([128, 8], F32, "Gtok")
        eGtok = self.tile([128, 8], F32, "eGtok")
        bg = self.tile([128, 8], F32, "bg")
        yT = self.tile([128, 8, 128], BF16, "yT")
        ytile = [self.tile([128, D], F32, "ytile") for _ in range(2)]
        fl = lambda t, j: t.ap[:, 4 * j:4 * j + 4, :]
        pv4 = lambda ps: ps.ap.rearrange("p (a b) -> p a b", a=4)
        bc_h = lambda ap2, j: ap2[:, 4 * j:4 * j + 4].unsqueeze(2).to_broadcast([128, 4, 128])
        bc_m = lambda m, n_=4: m.ap.unsqueeze(1).to_broadcast([128, n_, 128])
        hview = lambda ap: ap.rearrange("(h p) t -> p h t", p=128)
        nch = 0
        for d in range(self.cfg.get("gdn_dirs", 2)):
            TRI, maskT, maskN, strT, strN, last = ((triu32, mup, mlo, sup, slo, 127), (tril32, mlo, mup, slo, sup, 0))[d]
            order = list(range(NT)) if d == 0 else [1, 0] + list(range(NT - 1, 1, -1))
            order = order[:self.cfg.get("gdn_nch", NT)]
            op(DVE, lambda e: e.memset(St.ap, 0.0), writes=[St.r])
            for n in order:
                cols = slice(n * 128, (n + 1) * 128)
                qf, kf, vf, xf, zf = qfb[nch % 2], kfb[nch % 2], vfb[nch % 2], xfb[nch % 2], zfb[nch % 2]
                nch += 1
                dma(SP, qf.ap, hview(QF.ap)[:, :, cols], reads=[QF.sub[n]], writes=[qf.r])
                dma(SP, kf.ap, hview(KF.ap)[:, :, cols], reads=[KF.sub[n]], writes=[kf.r])
                dma(SP, vf.ap, hview(VF.ap)[:, :, cols], reads=[VF.sub[n]], writes=[vf.r])
                if d == 1:
                    dma(SP, xf.ap, hview(OF.ap)[:, :, cols], reads=[OF.sub[n]], writes=[xf.r])
                    dma(SP, zf.ap, hview(ZF.ap)[:, :, cols], reads=[ZF.sub[n]], writes=[zf.r])
                gsl = gT.ap[:, n, d * 8:(d + 1) * 8]
                bsl = bT.ap[:, n, d * 8:(d + 1) * 8]
                psG = self.psum()
                op(PE, lambda e, psG=psG, TRI=TRI, gsl=gsl: e.matmul(psG.ap[:, 0:8], lhsT=TRI.ap, rhs=gsl, start=True, stop=True), reads=[TRI.r, gT.r], writes=psG.sub)
                op(DVE, lambda e, gsl=gsl, TRI=TRI: e.tensor_tensor(out=Rt.ap, in0=gsl.unsqueeze(2).to_broadcast([128, 8, 128]), in1=bc_m(TRI, 8), op=ALU.mult),
                   reads=[gT.r, TRI.r], writes=[Rt.r])
                psGb = [self.psum(), self.psum()]
                for j in range(2):
                    op(PE, lambda e, j=j, p=psGb[j]: e.matmul(p.ap, lhsT=ones32.ap, rhs=fl(Rt, j).rearrange("p a b -> p (a b)"), start=True, stop=True),
                       reads=[ones32.r, Rt.r], writes=psGb[j].sub)
                op(ACT, lambda e, psG=psG: e.copy(out=Gtok.ap, in_=psG.ap[:, 0:8]), reads=psG.sub, writes=[Gtok.r])
                for j in range(2):
                    op(DVE, lambda e, j=j, p=psGb[j]: e.tensor_tensor(out=fl(dTt, j), in0=pv4(p), in1=bc_h(Gtok.ap, j), op=ALU.subtract), reads=psGb[j].sub + [Gtok.r], writes=[dTt.r])
                    op(ACT, lambda e, j=j, p=psGb[j]: e.activation(out=fl(eGbc, j), in_=pv4(p), func=AF.Exp), reads=psGb[j].sub, writes=[eGbc.r])
                op(DVE, lambda e, maskN=maskN: e.scalar_tensor_tensor(out=d2t.ap, in0=dTt.ap, scalar=-1.0, in1=bc_m(maskN, 8), op0=ALU.mult, op1=ALU.add),
                   reads=[dTt.r, maskN.r], writes=[d2t.r])
                op(POOL, lambda e, maskT=maskT: e.tensor_tensor(out=dTt.ap, in0=dTt.ap, in1=bc_m(maskT, 8), op=ALU.add), reads=[dTt.r, maskT.r, d2t.r], writes=[dTt.r])
                op(ACT, lambda e: e.activation(out=dec.ap, in_=d2t.ap, func=AF.Exp), reads=[d2t.r], writes=[dec.r])
                op(ACT, lambda e: e.activation(out=decT.ap, in_=dTt.ap, func=AF.Exp), reads=[dTt.r], writes=[decT.r])
                op(ACT, lambda e: e.activation(out=eGtok.ap, in_=Gtok.ap, func=AF.Exp), reads=[Gtok.r], writes=[eGtok.r])
                op(DVE, lambda e, bsl=bsl: e.tensor_tensor(out=bg.ap, in0=eGtok.ap, in1=bsl, op=ALU.mult), reads=[eGtok.r, bT.r], writes=[bg.r])
                op(DVE, lambda e, bsl=bsl: e.tensor_tensor(out=Rt.ap, in0=bsl.unsqueeze(2).to_broadcast([128, 8, 128]), in1=bc_m(i32, 8), op=ALU.mult),
                   reads=[bT.r, i32.r], writes=[Rt.r])
                psBb = [self.psum(), self.psum()]
                for j in range(2):
                    op(PE, lambda e, j=j, p=psBb[j]: e.matmul(p.ap, lhsT=ones32.ap, rhs=fl(Rt, j).rearrange("p a b -> p (a b)"), start=True, stop=True),
                       reads=[ones32.r, Rt.r], writes=psBb[j].sub)
                for j in range(2):
                    op(DVE, lambda e, j=j, p=psBb[j]: e.tensor_tensor(out=fl(AmT, j), in0=fl(decT, j), in1=pv4(p), op=ALU.mult), reads=psBb[j].sub + [decT.r], writes=[AmT.r])
                op(POOL, lambda e, strT=strT: e.tensor_tensor(out=AmT.ap, in0=AmT.ap, in1=bc_m(strT, 8), op=ALU.mult), reads=[AmT.r, strT.r], writes=[AmT.r])
                op(POOL, lambda e, bsl=bsl: e.tensor_tensor(out=An.ap, in0=dec.ap, in1=bsl.unsqueeze(2).to_broadcast([128, 8, 128]), op=ALU.mult), reads=[dec.r, bT.r], writes=[An.r])
                op(POOL, lambda e, strN=strN: e.tensor_tensor(out=An.ap, in0=An.ap, in1=bc_m(strN, 8), op=ALU.mult), reads=[An.r, strN.r], writes=[An.r])
                op(POOL, lambda e, qf=qf: e.tensor_tensor(out=qdT.ap, in0=qf.ap, in1=eGbc.ap, op=ALU.mult), reads=[qf.r, eGbc.r], writes=[qdT.r])
                if self.cfg.get('gdn_stage', 99) < 1:
                    continue
                psK = [self.psum(), self.psum()]
                psV = [self.psum(), self.psum()]
                for h in range(8):
                    j, q4 = h // 4, (h % 4) * 128
                    op(PE, lambda e, h=h, p=psK[j], q4=q4, kf=kf: e.transpose(out=p.ap[:, q4:q4 + 128], in_=kf.ap[:, h, :], identity=i32.ap), reads=[kf.r, i32.r], writes=[psK[j].sub[h % 4]])
                    op(PE, lambda e, h=h, p=psV[j], q4=q4, vf=vf: e.transpose(out=p.ap[:, q4:q4 + 128], in_=vf.ap[:, h, :], identity=i32.ap), reads=[vf.r, i32.r], writes=[psV[j].sub[h % 4]])
                for j in range(2):
                    op(DVE, lambda e, j=j, p=psK[j]: e.tensor_tensor(out=fl(kbg, j), in0=pv4(p), in1=bc_h(bg.ap, j), op=ALU.mult), reads=psK[j].sub + [bg.r], writes=[kbg.r])
                    op(DVE, lambda e, j=j, p=psK[j], last=last: e.tensor_tensor(out=fl(ktail, j), in0=pv4(p), in1=decT.ap[:, 4 * j:4 * j + 4, last:last + 1].to_broadcast([128, 4, 128]), op=ALU.mult),
                       reads=psK[j].sub + [decT.r], writes=[ktail.r])
                    op(DVE, lambda e, j=j, p=psV[j], bsl=bsl: e.tensor_tensor(out=fl(vb, j), in0=pv4(p), in1=bc_h(bsl, j), op=ALU.mult), reads=psV[j].sub + [bT.r], writes=[vb.r])
                if self.cfg.get('gdn_stage', 99) < 2:
                    continue
                psKK = [self.psum(), self.psum()]
                psKQ = [self.psum(), self.psum()]
                for h in range(8):
                    j, q4 = h // 4, (h % 4) * 128
                    op(PE, lambda e, h=h, p=psKK[j], q4=q4, kf=kf: e.matmul(p.ap[:, q4:q4 + 128], lhsT=kf.ap[:, h, :], rhs=kf.ap[:, h, :], start=True, stop=True), reads=[kf.r], writes=[psKK[j].sub[h % 4]])
                    op(PE, lambda e, h=h, p=psKQ[j], q4=q4, kf=kf, qf=qf: e.matmul(p.ap[:, q4:q4 + 128], lhsT=kf.ap[:, h, :], rhs=qf.ap[:, h, :], start=True, stop=True), reads=[kf.r, qf.r], writes=[psKQ[j].sub[h % 4]])
                M0, N0 = Mb[0], Nb[0]
                for j in range(2):
                    op(DVE, lambda e, j=j, p=psKK[j]: e.scalar_tensor_tensor(out=fl(M0, j), in0=pv4(p), scalar=-1.0, in1=fl(AmT, j), op0=ALU.mult, op1=ALU.mult), reads=psKK[j].sub + [AmT.r], writes=[M0.r])
                    op(DVE, lambda e, j=j, p=psKK[j]: e.scalar_tensor_tensor(out=fl(N0, j), in0=pv4(p), scalar=-1.0, in1=fl(An, j), op0=ALU.mult, op1=ALU.mult), reads=psKK[j].sub + [An.r], writes=[N0.r])
                    op(DVE, lambda e, j=j, p=psKQ[j]: e.tensor_tensor(out=fl(attnT, j), in0=pv4(p), in1=fl(decT, j), op=ALU.mult), reads=psKQ[j].sub + [decT.r], writes=[attnT.r])
                MA, NA, MB, NB = Mb[1], Nb[1], Mc[0], Nc[0]
                op(DVE, lambda e: e.tensor_tensor(out=MA.ap, in0=M0.ap, in1=bc_m(gm[0], 8), op=ALU.mult), reads=[M0.r, gm[0].r], writes=[MA.r])
                op(POOL, lambda e: e.tensor_tensor(out=NA.ap, in0=N0.ap, in1=bc_m(gm[0], 8), op=ALU.mult), reads=[N0.r, gm[0].r], writes=[NA.r])
                op(POOL, lambda e: e.tensor_tensor(out=Pm.ap, in0=MA.ap, in1=bc_m(i32, 8), op=ALU.add), reads=[MA.r, i32.r], writes=[Pm.r])
                op(POOL, lambda e: e.tensor_tensor(out=PT.ap, in0=NA.ap, in1=bc_m(i32, 8), op=ALU.add), reads=[NA.r, i32.r], writes=[PT.r])
                cur = (MA, NA)
                nxt = (MB, NB)
                for kk in range(1, 4):
                    Mp, Np = cur
                    Mn, Nn = nxt
                    psN = [self.psum(), self.psum()]
                    psM = [self.psum(), self.psum()]
                    for h in range(8):
                        j, q4 = h // 4, (h % 4) * 128
                        op(PE, lambda e, h=h, p=psN[j], q4=q4, Mp=Mp, Np=Np: e.matmul(p.ap[:, q4:q4 + 128], lhsT=Mp.ap[:, h, :], rhs=Np.ap[:, h, :], start=True, stop=True),
                           reads=[Mp.r, Np.r], writes=[psN[j].sub[h % 4]])
                        op(PE, lambda e, h=h, p=psM[j], q4=q4, Mp=Mp, Np=Np: e.matmul(p.ap[:, q4:q4 + 128], lhsT=Np.ap[:, h, :], rhs=Mp.ap[:, h, :], start=True, stop=True),
                           reads=[Mp.r, Np.r], writes=[psM[j].sub[h % 4]])
                    for j in range(2):
                        op(ACT, lambda e, j=j, p=psN[j], Nn=Nn: e.copy(out=fl(Nn, j), in_=pv4(p)), reads=psN[j].sub, writes=[Nn.r])
                        op(DVE, lambda e, j=j, p=psM[j], Mn=Mn: e.tensor_copy(out=fl(Mn, j), in_=pv4(p)), reads=psM[j].sub, writes=[Mn.r])
                    psP = [self.psum(), self.psum()]
                    psQ_ = [self.psum(), self.psum()]
                    for h in range(8):
                        j, q4 = h // 4, (h % 4) * 128
                        op(PE, lambda e, h=h, p=psP[j], q4=q4, Nn=Nn: e.matmul(p.ap[:, q4:q4 + 128], lhsT=Nn.ap[:, h, :], rhs=Pm.ap[:, h, :], start=True, stop=True),
                           reads=[Nn.r, Pm.r], writes=[psP[j].sub[h % 4]])
                        op(PE, lambda e, h=h, p=psQ_[j], q4=q4, Mn=Mn: e.matmul(p.ap[:, q4:q4 + 128], lhsT=Mn.ap[:, h, :], rhs=PT.ap[:, h, :], start=True, stop=True),
                           reads=[Mn.r, PT.r], writes=[psQ_[j].sub[h % 4]])
                    for j in range(2):
                        op(DVE, lambda e, j=j, p=psP[j]: e.tensor_tensor(out=fl(Pm, j), in0=fl(Pm, j), in1=pv4(p), op=ALU.add), reads=psP[j].sub + [Pm.r], writes=[Pm.r])
                        op(DVE, lambda e, j=j, p=psQ_[j]: e.tensor_tensor(out=fl(PT, j), in0=fl(PT, j), in1=pv4(p), op=ALU.add), reads=psQ_[j].sub + [PT.r], writes=[PT.r])
                    cur, nxt = nxt, cur
                for lv in range(3):
                    UoT, Yt = Mc[0], Nc[0]
                    op(DVE, lambda e, lv=lv, UoT=UoT: e.scalar_tensor_tensor(out=UoT.ap, in0=N0.ap, scalar=-1.0, in1=bc_m(gm[1 + lv], 8), op0=ALU.mult, op1=ALU.mult),
                       reads=[N0.r, gm[1 + lv].r], writes=[UoT.r])
                    psY = [self.psum(), self.psum()]
                    for h in range(8):
                        j, q4 = h // 4, (h % 4) * 128
                        op(PE, lambda e, h=h, p=psY[j], q4=q4, UoT=UoT: e.matmul(p.ap[:, q4:q4 + 128], lhsT=UoT.ap[:, h, :], rhs=Pm.ap[:, h, :], start=True, stop=True),
                           reads=[UoT.r, Pm.r], writes=[psY[j].sub[h % 4]])
                    for j in range(2):
                        op(ACT, lambda e, j=j, p=psY[j], Yt=Yt: e.copy(out=fl(Yt, j), in_=pv4(p)), reads=psY[j].sub, writes=[Yt.r])
                    psX = [self.psum(), self.psum()]
                    psXT = [self.psum(), self.psum()]
                    for h in range(8):
                        j, q4 = h // 4, (h % 4) * 128
                        op(PE, lambda e, h=h, p=psX[j], q4=q4, Yt=Yt: e.matmul(p.ap[:, q4:q4 + 128], lhsT=PT.ap[:, h, :], rhs=Yt.ap[:, h, :], start=True, stop=True),
                           reads=[PT.r, Yt.r], writes=[psX[j].sub[h % 4]])
                        if lv < 2:
                            op(PE, lambda e, h=h, p=psXT[j], q4=q4, Yt=Yt: e.matmul(p.ap[:, q4:q4 + 128], lhsT=Yt.ap[:, h, :], rhs=PT.ap[:, h, :], start=True, stop=True),
                               reads=[PT.r, Yt.r], writes=[psXT[j].sub[h % 4]])
                    for j in range(2):
                        op(DVE, lambda e, j=j, p=psX[j]: e.tensor_tensor(out=fl(Pm, j), in0=fl(Pm, j), in1=pv4(p), op=ALU.subtract), reads=psX[j].sub + [Pm.r], writes=[Pm.r])
                        if lv < 2:
                            op(DVE, lambda e, j=j, p=psXT[j]: e.tensor_tensor(out=fl(PT, j), in0=fl(PT, j), in1=pv4(p), op=ALU.subtract), reads=psXT[j].sub + [PT.r], writes=[PT.r])
                if self.cfg.get('gdn_stage', 99) < 4:
                    continue
                psW = [self.psum(), self.psum()]
                for h in range(8):
                    j, q4 = h // 4, (h % 4) * 128
                    op(PE, lambda e, h=h, p=psW[j], q4=q4: e.matmul(p.ap[:, q4:q4 + 128], lhsT=kbg.ap[:, h, :], rhs=Pm.ap[:, h, :], start=True, stop=True), reads=[kbg.r, Pm.r], writes=[psW[j].sub[h % 4]])
                for j in range(2):
                    op(ACT, lambda e, j=j, p=psW[j]: e.mul(out=fl(nwT, j), in_=pv4(p), mul=-1.0), reads=psW[j].sub, writes=[nwT.r])
                psVn = [self.psum(), self.psum()]
                for h in range(8):
                    j, q4 = h // 4, (h % 4) * 128
                    op(PE, lambda e, h=h, p=psVn[j], q4=q4: e.matmul(p.ap[:, q4:q4 + 128], lhsT=Pm.ap[:, h, :], rhs=vb.ap[:, h, :], start=True, stop=False), reads=[Pm.r, vb.r], writes=[psVn[j].sub[h % 4]])
                    op(PE, lambda e, h=h, p=psVn[j], q4=q4: e.matmul(p.ap[:, q4:q4 + 128], lhsT=nwT.ap[:, h, :], rhs=St.ap[:, h, :], start=False, stop=True), reads=[nwT.r, St.r], writes=[psVn[j].sub[h % 4]])
                for j in range(2):
                    op(ACT, lambda e, j=j, p=psVn[j]: e.copy(out=fl(vnew, j), in_=pv4(p)), reads=psVn[j].sub, writes=[vnew.r])
                if self.cfg.get('gdn_stage', 99) < 5:
                    continue
                psO = [self.psum(), self.psum()]
                psS = [self.psum(), self.psum()]
                for h in range(8):
                    j, q4 = h // 4, (h % 4) * 128
                    op(PE, lambda e, h=h, p=psO[j], q4=q4: e.matmul(p.ap[:, q4:q4 + 128], lhsT=St.ap[:, h, :], rhs=qdT.ap[:, h, :], start=True, stop=False), reads=[St.r, qdT.r], writes=[psO[j].sub[h % 4]])
                    op(PE, lambda e, h=h, p=psO[j], q4=q4: e.matmul(p.ap[:, q4:q4 + 128], lhsT=vnew.ap[:, h, :], rhs=attnT.ap[:, h, :], start=False, stop=True), reads=[vnew.r, attnT.r], writes=[psO[j].sub[h % 4]])
                    op(PE, lambda e, h=h, p=psS[j], q4=q4: e.matmul(p.ap[:, q4:q4 + 128], lhsT=ktail.ap[:, h, :], rhs=vnew.ap[:, h, :], start=True, stop=True), reads=[ktail.r, vnew.r], writes=[psS[j].sub[h % 4]])
                for j in range(2):
                    op(DVE, lambda e, j=j, last=last: e.tensor_tensor(out=fl(St, j), in0=fl(St, j), in1=eGbc.ap[:, 4 * j:4 * j + 4, last:last + 1].to_broadcast([128, 4, 128]), op=ALU.mult),
                       reads=[St.r, eGbc.r], writes=[St.r])
                    op(DVE, lambda e, j=j, p=psS[j]: e.tensor_tensor(out=fl(St, j), in0=fl(St, j), in1=pv4(p), op=ALU.add), reads=psS[j].sub + [St.r], writes=[St.r])
                if d == 0:
                    for j in range(2):
                        op(ACT, lambda e, j=j, p=psO[j], xf=xf: e.copy(out=fl(xf, j), in_=pv4(p)), reads=psO[j].sub, writes=[xf.r])
                    dma(SP, hview(OF.ap)[:, :, cols], xf.ap, reads=[xf.r], writes=[OF.sub[n]])
                    continue
                for j in range(2):
                    op(DVE, lambda e, j=j, p=psO[j], xf=xf: e.tensor_tensor(out=fl(xf, j), in0=fl(xf, j), in1=pv4(p), op=ALU.add), reads=psO[j].sub + [xf.r], writes=[xf.r])
                op(POOL, lambda e, xf=xf: e.tensor_tensor(out=Rt.ap, in0=xf.ap, in1=xf.ap, op=ALU.mult), reads=[xf.r], writes=[Rt.r])
                psQ = [self.psum(), self.psum()]
                for j in range(2):
                    op(PE, lambda e, j=j, p=psQ[j]: e.matmul(p.ap, lhsT=ones32.ap, rhs=fl(Rt, j).rearrange("p a b -> p (a b)"), start=True, stop=True), reads=[ones32.r, Rt.r], writes=psQ[j].sub)
                for j in range(2):
                    op(DVE, lambda e, j=j, p=psQ[j]: e.tensor_scalar(out=fl(d2t, j), in0=pv4(p), scalar1=1.0 / 128.0, scalar2=LN_EPS, op0=ALU.mult, op1=ALU.add), reads=psQ[j].sub, writes=[d2t.r])
                op(ACT, lambda e: e.activation(out=d2t.ap, in_=d2t.ap, func=AF.Ln), reads=[d2t.r], writes=[d2t.r])
                op(ACT, lambda e: e.activation(out=d2t.ap, in_=d2t.ap, func=AF.Exp, scale=-0.5), reads=[d2t.r], writes=[d2t.r])
                op(POOL, lambda e, xf=xf: e.tensor_tensor(out=xf.ap, in0=xf.ap, in1=d2t.ap, op=ALU.mult), reads=[xf.r, d2t.r], writes=[xf.r])
                op(POOL, lambda e, xf=xf, zf=zf: e.tensor_tensor(out=yT.ap, in0=xf.ap, in1=zf.ap, op=ALU.mult), reads=[xf.r, zf.r], writes=[yT.r])
                yt = ytile[n % 2]
                for half in range(2):
                    ps = self.psum()
                    for h in range(8):
                        op(PE, lambda e, ps=ps, h=h, half=half: e.matmul(ps.ap, lhsT=yT.ap[:, h, :], rhs=wo.ap[:, h, half * 512:(half + 1) * 512], start=(h == 0), stop=(h == 7)),
                           reads=[yT.r, wo.r], writes=ps.sub)
                    op(ACT, lambda e, ps=ps, yt=yt, half=half: e.copy(out=yt.ap[:, half * 512:(half + 1) * 512], in_=ps.ap), reads=ps.sub, writes=[yt.r])
                dma(SP, self.Y.ap[cols, :], yt.ap, reads=[yt.r], writes=[self.Y.sub[n]])
                if self.cfg.get("test") == "mix":
                    dma(SP, self.dbg.ap[cols, :], yt.ap, reads=[yt.r], writes=[self.dbg.sub[n]])
        if self.cfg.get("test") == "gdn2":
            self.P.barrier()
            rr = Res("dbg2")
            dma(SP, self.dbg2[0], VF.ap if self.cfg.get('gdn_nch') == 1 else OF.ap, reads=OF.sub + VF.sub, writes=[rr])
            dma(SP, self.dbg2[1, 0:128, 0:1024], St.ap.rearrange("p a b -> p (a b)"), reads=[St.r], writes=[rr])
            for ii, tt in enumerate((decT, dec, AmT, An, Pm, vnew, attnT, kbg, vb, ktail, qdT, eGbc, nwT, qfb[0], kfb[0], vfb[0])):
                dma(SP, self.dbg2[2 + ii // 8, (ii % 8) * 128:(ii % 8 + 1) * 128, 0:1024], tt.ap.rearrange("p a b -> p (a b)"), reads=[tt.r], writes=[rr])
            dma(SP, self.dbg3[:, 0], gT.ap, reads=[gT.r], writes=[rr])
            dma(SP, self.dbg3[:, 1], bT.ap, reads=[bT.r], writes=[rr])
            self.dbg.sub.append(rr)

    def out_proj(self, w_o):
        op, dma = self.op, self.dma
        self.P.barrier()
        self.off = self.base_off
        OD = self.OD
        wo = self.tile([128, 8, D], BF16, "wo")
        dma(POOL, wo.ap, w_o.rearrange("(k p) n -> p k n", p=128), writes=[wo.r])
        ob = [self.tile([128, D], BF16, "ob") for _ in range(3)]
        oT = [self.tile([128, 8, 128], BF16, "oT", nsub=1) for _ in range(2)]
        yb = [self.tile([128, D], F32, "yb") for _ in range(2)]
        for i in range(NT):
            o, t, y = ob[i % 3], oT[i % 2], yb[i % 2]
            rows = slice(i * 128, (i + 1) * 128)
            dma(SP, o.ap, OD.ap[rows, :], reads=[OD.sub[i]], writes=[o.r])
            self.transpose_to(o, t, 0)
            for half in range(2):
                ps = self.psum()
                for k in range(8):
                    op(PE, lambda e, ps=ps, t=t, k=k, half=half: e.matmul(ps.ap, lhsT=t.ap[:, k, :], rhs=wo.ap[:, k, half * 512:(half + 1) * 512], start=(k == 0), stop=(k == 7)),
                       reads=[t.sub[0], wo.r], writes=[ps.r])
                op(ACT if half else DVE, (lambda e, ps=ps, y=y, half=half: e.copy(out=y.ap[:, half * 512:(half + 1) * 512], in_=ps.ap)) if half else
                   (lambda e, ps=ps, y=y, half=half: e.tensor_copy(out=y.ap[:, half * 512:(half + 1) * 512], in_=ps.ap)), reads=[ps.r], writes=[y.r])
            dma(SP, self.Y.ap[rows, :], y.ap, reads=[y.r], writes=[self.Y.sub[i]])
            if self.cfg.get("test") == "mix":
                dma(SP, self.dbg.ap[rows, :], y.ap, reads=[y.r], writes=[self.dbg.sub[i]])

    def postmix(self, l):
        op, dma = self.op, self.dma
        self.phase_begin()
        self.h2tok = self.tile([128, NT, D], BF16, "h2tok", nsub=NT)
        self.affTok = self.tile([128, NT, NE], F32, "affTok")
        h2tok, affTok = self.h2tok, self.affTok
        self.post_keep = self.off
        mods = self.mod_tiles(l, [2, 3, 4], plus1=(4,))
        g0 = self.ln_vec(self.ln_g[l, 0:1, :])
        b0 = self.ln_vec(self.ln_b[l, 0:1, :])
        wr = self.tile([128, 8, NE], BF16, "wr")
        dma(POOL, wr.ap, self.moe_w_router[l].rearrange("(k p) n -> p k n", p=128), writes=[wr.r])
        xb = [self.tile([128, D], F32, "xb") for _ in range(2)]
        yb = [self.tile([128, D], F32, "yb") for _ in range(2)]
        ob = [self.tile([128, D], F32, "ob") for _ in range(2)]
        h2T = [self.tile([128, 8, 128], BF16, "h2T", nsub=1) for _ in range(2)]
        st = self.tile([128, 2, 6], F32, "st")
        mv = self.tile([128, 4], F32, "mv")
        sm = self.tile([128, 4], F32, "sm")
        ex = self.tile([128, NE], F32, "ex")
        for i in range(NT):
            x, y, o = xb[i % 2], yb[i % 2], ob[i % 2]
            c = 1 if i < 2 else 0
            rows = slice(i * 128, (i + 1) * 128)
            dma(SP, x.ap, self.XS.ap[rows, :], reads=[self.XS.sub[i]], writes=[x.r])
            dma(SP, y.ap, self.Y.ap[rows, :], reads=[self.Y.sub[i]], writes=[y.r])
            m2, m3, m4 = mods[2][c], mods[3][c], mods[4][c]
            op(POOL, lambda e, y=y, m2=m2: e.tensor_tensor(out=y.ap, in0=y.ap, in1=m2.ap, op=ALU.mult), reads=[y.r, m2.r], writes=[y.r])
            op(DVE, lambda e, x=x, y=y: e.scalar_tensor_tensor(out=y.ap, in0=x.ap, scalar=ALPHA, in1=y.ap, op0=ALU.mult, op1=ALU.add),
               reads=[x.r, y.r], writes=[y.r])
            self.layernorm_(y, st, mv)
            op(POOL, lambda e, y=y, o=o: e.tensor_tensor(out=o.ap, in0=y.ap, in1=g0.ap, op=ALU.mult), reads=[y.r, g0.r], writes=[o.r])
            op(POOL, lambda e, o=o: e.tensor_tensor(out=o.ap, in0=o.ap, in1=b0.ap, op=ALU.add), reads=[o.r, b0.r], writes=[o.r])
            dma(SP, self.XS.ap[rows, :], o.ap, reads=[o.r], writes=[self.XS.sub[i]])
            op(DVE, lambda e, o=o, x=x, m4=m4: e.tensor_tensor(out=x.ap, in0=o.ap, in1=m4.ap, op=ALU.mult), reads=[o.r, m4.r], writes=[x.r])
            hv = Tl(h2tok.ap[:, i, :])
            hv.r = h2tok.sub[i]
            op(DVE, lambda e, x=x, m3=m3, hv=hv: e.tensor_tensor(out=hv.ap, in0=x.ap, in1=m3.ap, op=ALU.add), reads=[x.r, m3.r], writes=[hv.r])
            ht = h2T[i % 2]
            self.transpose_to(hv, ht, 0)
            ps = self.psum()
            for k in range(8):
                op(PE, lambda e, ps=ps, ht=ht, k=k: e.matmul(ps.ap[:, 0:NE], lhsT=ht.ap[:, k, :], rhs=wr.ap[:, k, :], start=(k == 0), stop=(k == 7)),
                   reads=[ht.sub[0], wr.r], writes=[ps.r])
            op(DVE, lambda e, ps=ps: e.reduce_max(out=sm.ap[:, 0:1], in_=ps.ap[:, 0:NE], axis=AX.X), reads=[ps.r], writes=[sm.r])
            op(DVE, lambda e: e.tensor_scalar(out=sm.ap[:, 1:2], in0=sm.ap[:, 0:1], scalar1=-1.0, scalar2=None, op0=ALU.mult), reads=[sm.r], writes=[sm.r])
            op(ACT, lambda e, ps=ps: e.activation(out=ex.ap, in_=ps.ap[:, 0:NE], func=AF.Exp, bias=sm.ap[:, 1:2], scale=1.0, accum_out=sm.ap[:, 2:3]),
               reads=[ps.r, sm.r], writes=[ex.r, sm.r])
            op(DVE, lambda e: e.reciprocal(out=sm.ap[:, 3:4], in_=sm.ap[:, 2:3]), reads=[sm.r], writes=[sm.r])
            op(DVE, lambda e, i=i: e.tensor_scalar(out=affTok.ap[:, i, :], in0=ex.ap, scalar1=sm.ap[:, 3:4], scalar2=None, op0=ALU.mult),
               reads=[ex.r, sm.r], writes=[affTok.r])

    def moe_select(self, l):
        op, dma = self.op, self.dma
        affTok = self.affTok
        self.P.barrier()
        self.off = self.post_keep
        self.posm = self.tile([128, NT, NE], F32, "posm")
        posm = self.posm
        keep2 = self.off
        affT = self.tile([NE, TT], F32, "affT")
        work = self.tile([NE, 4096], F32, "work")
        m8 = self.tile([NE, 8], F32, "m8")
        thr = self.tile([NE, 2], F32, "thr")
        maskT = self.tile([NE, TT], BF16, "maskT")
        for g in range(9):
            ps = self.psum()
            tl = list(range(g * 4, min(g * 4 + 4, NT)))
            for j, i in enumerate(tl):
                op(PE, lambda e, ps=ps, i=i, j=j: e.transpose(out=ps.ap[0:NE, j * 128:(j + 1) * 128], in_=affTok.ap[:, i, :], identity=self.ident32.ap),
                   reads=[affTok.r, self.ident32.r], writes=[ps.r])
            n = len(tl) * 128
            op(ACT, lambda e, ps=ps, g=g, n=n: e.copy(out=affT.ap[:, g * 512:g * 512 + n], in_=ps.ap[0:NE, 0:n]), reads=[ps.r], writes=[affT.r])
        dma(SP, self.AFFT.ap, affT.ap, reads=[affT.r], writes=[self.AFFT.r])
        for (lo, n, iters, col) in ((0, 256, 4, 0), (256, 4096, 64, 1)):
            op(DVE, lambda e, lo=lo, n=n: e.tensor_copy(out=work.ap[:, 0:n], in_=affT.ap[:, lo:lo + n]), reads=[affT.r], writes=[work.r])
            for it in range(iters):
                op(DVE, lambda e, n=n: e.max(out=m8.ap, in_=work.ap[:, 0:n]), reads=[work.r], writes=[m8.r])
                if it < iters - 1:
                    op(DVE, lambda e, n=n: e.match_replace(out=work.ap[:, 0:n], in_to_replace=m8.ap, in_values=work.ap[:, 0:n], imm_value=-1.0),
                       reads=[work.r, m8.r], writes=[work.r])
            op(DVE, lambda e, col=col: e.tensor_copy(out=thr.ap[:, col:col + 1], in_=m8.ap[:, 7:8]), reads=[m8.r], writes=[thr.r])
            op(DVE, lambda e, lo=lo, n=n, col=col: e.tensor_scalar(out=maskT.ap[:, lo:lo + n], in0=affT.ap[:, lo:lo + n], scalar1=thr.ap[:, col:col + 1],
                                                                   scalar2=None, op0=ALU.is_ge), reads=[affT.r, thr.r], writes=[maskT.r])
        maskTok = self.tile([128, NT, NE], BF16, "maskTok")
        maskF = self.tile([128, NT, NE], F32, "maskF")
        ps = self.psum()
        pb = ps.ap.bitcast(BF16)
        for i in range(NT):
            op(PE, lambda e, i=i, pb=pb: e.transpose(out=pb[:, i * NE:(i + 1) * NE], in_=maskT.ap[:, i * 128:(i + 1) * 128], identity=self.ident.ap[0:NE, 0:NE]),
               reads=[maskT.r, self.ident.r], writes=[ps.r])
        op(DVE, lambda e, pb=pb: e.tensor_copy(out=maskTok.ap.rearrange("p a b -> p (a b)"), in_=pb[:, 0:NT * NE]), reads=[ps.r], writes=[maskTok.r])
        op(DVE, lambda e: e.tensor_copy(out=maskF.ap, in_=maskTok.ap), reads=[maskTok.r], writes=[maskF.r])
        mflat = maskTok.ap.rearrange("p a b -> p (a b)")
        ps_w, ps_t, ps_c = self.psum(), self.psum(), self.psum()
        op(PE, lambda e: e.matmul(ps_w.ap, lhsT=self.triu.ap, rhs=mflat[:, 2 * NE:NT * NE], start=True, stop=True), reads=[maskTok.r, self.triu.r], writes=[ps_w.r])
        op(PE, lambda e: e.matmul(ps_t.ap, lhsT=self.ones.ap, rhs=mflat[:, 2 * NE:NT * NE], start=True, stop=True), reads=[maskTok.r, self.ones.r], writes=[ps_t.r])
        op(PE, lambda e: e.matmul(ps_c.ap[:, 0:2 * NE], lhsT=self.triu.ap, rhs=mflat[:, 0:2 * NE], start=True, stop=True), reads=[maskTok.r, self.triu.r], writes=[ps_c.r])
        op(PE, lambda e: e.matmul(ps_c.ap[:, 2 * NE:4 * NE], lhsT=self.ones.ap, rhs=mflat[:, 0:2 * NE], start=True, stop=True), reads=[maskTok.r, self.ones.r], writes=[ps_c.r])
        eoff = self.tile([128, NT, NE], F32, "eoff")
        tot = self.tile([128, NT, NE], F32, "tot")
        op(DVE, lambda e: e.tensor_copy(out=tot.ap[:, 2:NT, :].rearrange("p a b -> p (a b)"), in_=ps_t.ap), reads=[ps_t.r], writes=[tot.r])
        op(DVE, lambda e: e.tensor_copy(out=tot.ap[:, 0:2, :].rearrange("p a b -> p (a b)"), in_=ps_c.ap[:, 2 * NE:4 * NE]), reads=[ps_c.r], writes=[tot.r])
        op(DVE, lambda e: e.memset(eoff.ap, 0.0), writes=[eoff.r])
        op(DVE, lambda e: e.tensor_copy(out=eoff.ap[:, 1, :], in_=tot.ap[:, 0, :]), reads=[tot.r], writes=[eoff.r])
        for i in range(3, NT):
            op(DVE, lambda e, i=i: e.tensor_tensor(out=eoff.ap[:, i, :], in0=eoff.ap[:, i - 1, :], in1=tot.ap[:, i - 1, :], op=ALU.add),
               reads=[eoff.r, tot.r], writes=[eoff.r])
        op(DVE, lambda e: e.tensor_tensor(out=posm.ap[:, 2:NT, :].rearrange("p a b -> p (a b)"), in0=ps_w.ap,
                                          in1=eoff.ap[:, 2:NT, :].rearrange("p a b -> p (a b)"), op=ALU.add), reads=[ps_w.r, eoff.r], writes=[posm.r])
        op(DVE, lambda e: e.tensor_tensor(out=posm.ap[:, 0:2, :].rearrange("p a b -> p (a b)"), in0=ps_c.ap[:, 0:2 * NE],
                                          in1=eoff.ap[:, 0:2, :].rearrange("p a b -> p (a b)"), op=ALU.add), reads=[ps_c.r, eoff.r], writes=[posm.r])
        op(DVE, lambda e: e.tensor_tensor(out=posm.ap, in0=posm.ap, in1=maskF.ap, op=ALU.mult), reads=[posm.r, maskF.r], writes=[posm.r])
        op(DVE, lambda e: e.tensor_scalar(out=posm.ap, in0=posm.ap, scalar1=-1.0, scalar2=None, op0=ALU.add), reads=[posm.r], writes=[posm.r])
        posT = self.tile([NE, TT], F32, "posT")
        for g in range(9):
            ps = self.psum()
            tl = list(range(g * 4, min(g * 4 + 4, NT)))
            for j, i in enumerate(tl):
                op(PE, lambda e, ps=ps, i=i, j=j: e.transpose(out=ps.ap[0:NE, j * 128:(j + 1) * 128], in_=posm.ap[:, i, :], identity=self.ident32.ap),
                   reads=[posm.r, self.ident32.r], writes=[ps.r])
            n = len(tl) * 128
            op(DVE, lambda e, ps=ps, g=g, n=n: e.tensor_scalar(out=posT.ap[:, g * 512:g * 512 + n], in0=ps.ap[0:NE, 0:n], scalar1=12582912.0, scalar2=12582912.0,
                                                               op0=ALU.add, op1=ALU.subtract), reads=[ps.r], writes=[posT.r])
        dma(SP, self.POST.ap, posT.ap, reads=[posT.r], writes=[self.POST.r])
        self.P.barrier()
        self.off = keep2

    def moe_passA(self, l):
        op, dma = self.op, self.dma
        h2tok, posm = self.h2tok, self.posm
        sel = self.tile([128, 32, 512], BF16, "sel")
        selc = self.tile([128, 2, 32], BF16, "selc")
        xsT = self.tile([128, 8, 544], BF16, "xsT")
        hact = self.tile([128, 16, 544], BF16, "hact")
        wgb = [self.tile([128, 8, 256], BF16, "wg") for _ in range(2)]
        wub = [self.tile([128, 8, 256], BF16, "wu") for _ in range(2)]
        wdb = [self.tile([128, 2, 512], BF16, "wd") for _ in range(3)]
        sa = [self.tile([128, 512], F32, "sa") for _ in range(2)]
        sac = self.tile([128, 32], F32, "sac")
        yes = [self.tile([128, D], BF16, "yes") for _ in range(4)]
        yec = self.tile([32, D], BF16, "yec")
        posbc = [self.tile([128, 1024], F32, "posbc") for _ in range(2)]
        affbc = [self.tile([128, 1024], F32, "affbc") for _ in range(2)]
        posc = self.tile([32, 256], F32, "posc")
        affc = self.tile([32, 256], F32, "affc")
        stg = [self.tile([128, 1024], BF16, "stg") for _ in range(2)]
        nq = 0
        stgc = self.tile([32, 256], BF16, "stgc")
        nw = [0, 0]
        ns = 0
        for ex in range(NE):
            for i in range(2, NT):
                eng = DVE if i % 2 == 0 else POOL
                op(DVE, lambda e, i=i, ex=ex: e.tensor_scalar(out=sel.ap[:, i - 2, :], in0=self.iota512.ap, scalar1=posm.ap[:, i, ex:ex + 1], scalar2=None,
                                                              op0=ALU.is_equal), reads=[self.iota512.r, posm.r], writes=[sel.r])
            for i in range(2):
                op(DVE, lambda e, i=i, ex=ex: e.tensor_scalar(out=selc.ap[:, i, :], in0=self.iota512.ap[:, 0:32], scalar1=posm.ap[:, i, ex:ex + 1], scalar2=None,
                                                              op0=ALU.is_equal), reads=[self.iota512.r, posm.r], writes=[selc.r])
            for k in range(8):
                ps = self.psum()
                ks = slice(k * 128, (k + 1) * 128)
                for i in range(2, NT):
                    op(PE, lambda e, ps=ps, i=i, ks=ks: e.matmul(ps.ap, lhsT=h2tok.ap[:, i, ks], rhs=sel.ap[:, i - 2, :], start=(i == 2), stop=(i == NT - 1)),
                       reads=[h2tok.sub[i], sel.r], writes=[ps.r])
                op(ACT, lambda e, ps=ps, k=k: e.copy(out=xsT.ap[:, k, 0:512], in_=ps.ap), reads=[ps.r], writes=[xsT.r])
                ps2 = self.psum()
                for i in range(2):
                    op(PE, lambda e, ps2=ps2, i=i, ks=ks: e.matmul(ps2.ap[:, 0:32], lhsT=h2tok.ap[:, i, ks], rhs=selc.ap[:, i, :], start=(i == 0), stop=(i == 1)),
                       reads=[h2tok.sub[i], selc.r], writes=[ps2.r])
                op(ACT, lambda e, ps2=ps2, k=k: e.copy(out=xsT.ap[:, k, 512:544], in_=ps2.ap[:, 0:32]), reads=[ps2.r], writes=[xsT.r])
            for q in range(8):
                wg, wu = wgb[nw[0] % 2], wub[nw[0] % 2]
                nw[0] += 1
                cs = slice(q * 256, (q + 1) * 256)
                dma(POOL, wg.ap, self.moe_w_gate[l, ex][:, cs].rearrange("(k p) n -> p k n", p=128), writes=[wg.r])
                dma(POOL, wu.ap, self.moe_w_up[l, ex][:, cs].rearrange("(k p) n -> p k n", p=128), writes=[wu.r])
                for jl in range(2):
                    j = q * 2 + jl
                    js = slice(jl * 128, (jl + 1) * 128)
                    ps_a, ps_u, ps_c = self.psum(), self.psum(), self.psum()
                    for (w, ps) in ((wg, ps_a), (wu, ps_u)):
                        for k in range(8):
                            op(PE, lambda e, w=w, ps=ps, k=k, js=js: e.matmul(ps.ap, lhsT=w.ap[:, k, js], rhs=xsT.ap[:, k, 0:512], start=(k == 0), stop=(k == 7)),
                               reads=[w.r, xsT.r], writes=[ps.r])
                    for (w, c0) in ((wg, 0), (wu, 32)):
                        for k in range(8):
                            op(PE, lambda e, w=w, c0=c0, k=k, js=js, ps_c=ps_c: e.matmul(ps_c.ap[:, c0:c0 + 32], lhsT=w.ap[:, k, js], rhs=xsT.ap[:, k, 512:544],
                                                                                         start=(k == 0), stop=(k == 7)),
                               reads=[w.r, xsT.r], writes=[ps_c.r])
                    s = sa[ns % 2]
                    ns += 1
                    op(ACT, lambda e, s=s, ps_a=ps_a: e.activation(out=s.ap, in_=ps_a.ap, func=AF.Silu), reads=[ps_a.r], writes=[s.r])
                    op(DVE, lambda e, s=s, ps_u=ps_u, j=j: e.tensor_tensor(out=hact.ap[:, j, 0:512], in0=s.ap, in1=ps_u.ap, op=ALU.mult),
                       reads=[s.r, ps_u.r], writes=[hact.r])
                    op(ACT, lambda e, ps_c=ps_c: e.activation(out=sac.ap, in_=ps_c.ap[:, 0:32], func=AF.Silu), reads=[ps_c.r], writes=[sac.r])
                    op(DVE, lambda e, ps_c=ps_c, j=j: e.tensor_tensor(out=hact.ap[:, j, 512:544], in0=sac.ap, in1=ps_c.ap[:, 32:64], op=ALU.mult),
                       reads=[sac.r, ps_c.r], writes=[hact.r])
            for half in range(2):
                hs = slice(half * 512, (half + 1) * 512)
                psd = [self.psum() for _ in range(5)]
                for jj in range(8):
                    wd = wdb[nw[1] % 3]
                    nw[1] += 1
                    dma(POOL, wd.ap, self.moe_w_down[l, ex][jj * 256:(jj + 1) * 256, hs].rearrange("(a p) n -> p a n", p=128), writes=[wd.r])
                    for a in range(2):
                        j = jj * 2 + a
                        first, last = (j == 0), (j == 15)
                        for c in range(4):
                            op(PE, lambda e, c=c, j=j, a=a, wd=wd, first=first, last=last, p=psd[c]: e.matmul(p.ap, lhsT=hact.ap[:, j, c * 128:(c + 1) * 128], rhs=wd.ap[:, a, :],
                                                                                                                  start=first, stop=last),
                               reads=[hact.r, wd.r], writes=[psd[c].r])
                        op(PE, lambda e, j=j, a=a, wd=wd, first=first, last=last, p=psd[4]: e.matmul(p.ap[0:32, :], lhsT=hact.ap[:, j, 512:544], rhs=wd.ap[:, a, :],
                                                                                                      start=first, stop=last),
                           reads=[hact.r, wd.r], writes=[psd[4].r])
                for c in range(4):
                    op(ACT, lambda e, c=c, p=psd[c], hs=hs: e.copy(out=yes[c].ap[:, hs], in_=p.ap), reads=[psd[c].r], writes=[yes[c].r])
                op(ACT, lambda e, p=psd[4], hs=hs: e.copy(out=yec.ap[:, hs], in_=p.ap[0:32, :]), reads=[psd[4].r], writes=[yec.r])
            for c in range(4):
                dma(SP, self.YE.ap[ex, c], yes[c].ap, reads=[yes[c].r], writes=[self.YE.sub[ex]])
            dma(SP, self.YEC.ap[ex], yec.ap, reads=[yec.r], writes=[self.YEC.sub[ex]])
            dma(SP, posc.ap, self.POST.ap[ex:ex + 1, 0:256].to_broadcast([32, 256]), reads=[self.POST.r], writes=[posc.r])
            dma(SP, affc.ap, self.AFFT.ap[ex:ex + 1, 0:256].to_broadcast([32, 256]), reads=[self.AFFT.r], writes=[affc.r])
            op(DVE, lambda e: e.scalar_tensor_tensor(out=stgc.ap, in0=posc.ap, scalar=self.iotacol.ap[0:32, 0:1], in1=affc.ap,
                                                      op0=ALU.is_equal, op1=ALU.mult), reads=[posc.r, affc.r, self.iotacol.r], writes=[stgc.r])
            dma(SP, self.SELTC.ap[ex], stgc.ap, reads=[stgc.r], writes=[self.SELTC.r])
            for qd in range(4):
                pb_, ab_ = posbc[nq % 2], affbc[nq % 2]
                nq += 1
                t0 = 256 + qd * 1024
                dma(SP, pb_.ap, self.POST.ap[ex:ex + 1, t0:t0 + 1024].to_broadcast([128, 1024]), reads=[self.POST.r], writes=[pb_.r])
                dma(SP, ab_.ap, self.AFFT.ap[ex:ex + 1, t0:t0 + 1024].to_broadcast([128, 1024]), reads=[self.AFFT.r], writes=[ab_.r])
                for c in range(4):
                    sg = stg[c % 2]
                    ecx = ex * 4 + c
                    op(DVE, lambda e, sg=sg, c=c, pb_=pb_, ab_=ab_: e.scalar_tensor_tensor(
                        out=sg.ap, in0=pb_.ap, scalar=self.iotacol.ap[:, c:c + 1], in1=ab_.ap, op0=ALU.is_equal, op1=ALU.mult),
                       reads=[pb_.r, ab_.r, self.iotacol.r], writes=[sg.r])
                    dma(SP, self.SELT.ap[qd * 8:(qd + 1) * 8, :, ecx, :].rearrange("i s t -> s i t"), sg.ap.rearrange("s (i t) -> s i t", t=128),
                        reads=[sg.r], writes=self.SELT.sub[qd * 8:(qd + 1) * 8])

    def moe_passB(self, l):
        op, dma = self.op, self.dma
        self.phase_begin()
        last = (l == DEPTH - 1)
        yeall = self.tile([128, 64, D], BF16, "yeall")
        for ex in range(NE):
            dma(SP, yeall.ap[:, ex * 4:(ex + 1) * 4, :], self.YE.ap[ex].rearrange("c s d -> s c d"), reads=[self.YE.sub[ex]], writes=[yeall.r])
        yecall = self.tile([128, 4, D], BF16, "yecall")
        dma(SP, yecall.ap, self.YEC.ap.rearrange("(g e) s d -> (e s) g d", e=4), reads=self.YEC.sub, writes=[yecall.r])
        seltc = self.tile([128, 4, 256], BF16, "seltc")
        dma(SP, seltc.ap, self.SELTC.ap.rearrange("(g e) s t -> (e s) g t", e=4), reads=[self.SELTC.r], writes=[seltc.r])
        mods = self.mod_tiles(l, [5])
        g1 = self.ln_vec(self.ln_g[l, 1:2, :])
        b1 = self.ln_vec(self.ln_b[l, 1:2, :])
        selt = [self.tile([128, 32, 128], BF16, "selt") for _ in range(2)]
        xb = [self.tile([128, D], F32, "xb") for _ in range(2)]
        yb = [self.tile([128, D], F32, "yb") for _ in range(2)]
        st = self.tile([128, 2, 6], F32, "st")
        mv = self.tile([128, 4], F32, "mv")
        nsl = 0
        for i in range(NT):
            c = 1 if i < 2 else 0
            rows = slice(i * 128, (i + 1) * 128)
            x, y = xb[i % 2], yb[i % 2]
            dma(SP, x.ap, self.XS.ap[rows, :], reads=[self.XS.sub[i]], writes=[x.r])
            m5 = mods[5][c]
            pss = [self.psum(), self.psum()]
            if c:
                for g in range(4):
                    for half in range(2):
                        hs = slice(half * 512, (half + 1) * 512)
                        op(PE, lambda e, ps=pss[half], g=g, hs=hs, i=i: e.matmul(ps.ap, lhsT=seltc.ap[:, g, i * 128:(i + 1) * 128], rhs=yecall.ap[:, g, hs],
                                                                                 start=(g == 0), stop=(g == 3)),
                           reads=[seltc.r, yecall.r], writes=[pss[half].r])
            else:
                for hh in range(2):
                    sl = selt[nsl % 2]
                    nsl += 1
                    dma(SP, sl.ap, self.SELT.ap[i - 2, :, hh * 32:(hh + 1) * 32, :], reads=[self.SELT.sub[i - 2]], writes=[sl.r])
                    for b in range(32):
                        ec = hh * 32 + b
                        for half in range(2):
                            hs = slice(half * 512, (half + 1) * 512)
                            op(PE, lambda e, ps=pss[half], sl=sl, b=b, ec=ec, hs=hs: e.matmul(ps.ap, lhsT=sl.ap[:, b, :], rhs=yeall.ap[:, ec, hs],
                                                                                              start=(ec == 0), stop=(ec == 63)),
                               reads=[sl.r, yeall.r], writes=[pss[half].r])
            for half in range(2):
                hs = slice(half * 512, (half + 1) * 512)
                op(DVE, lambda e, ps=pss[half], y=y, m5=m5, hs=hs: e.tensor_tensor(out=y.ap[:, hs], in0=ps.ap, in1=m5.ap[:, hs], op=ALU.mult),
                   reads=[pss[half].r, m5.r], writes=[y.r])
            op(DVE, lambda e, x=x, y=y: e.scalar_tensor_tensor(out=y.ap, in0=x.ap, scalar=ALPHA, in1=y.ap, op0=ALU.mult, op1=ALU.add),
               reads=[x.r, y.r], writes=[y.r])
            self.layernorm_(y, st, mv)
            op(POOL, lambda e, y=y, x=x: e.tensor_tensor(out=x.ap, in0=y.ap, in1=g1.ap, op=ALU.mult), reads=[y.r, g1.r], writes=[x.r])
            op(POOL, lambda e, x=x: e.tensor_tensor(out=x.ap, in0=x.ap, in1=b1.ap, op=ALU.add), reads=[x.r, b1.r], writes=[x.r])
            dma(SP, self.XS.ap[rows, :], x.ap, reads=[x.r], writes=[self.XS.sub[i]])
            if last and not c:
                dma(SP, self.out[(i - 2) * 128:(i - 1) * 128, :], x.ap, reads=[x.r], writes=[self.outr])
            if self.cfg.get("test"):
                dma(SP, self.dbg.ap[rows, :], x.ap, reads=[x.r], writes=[self.dbg.sub[i]])

    def finish(self):
        rs = [self.outr]
        if self.cfg.get("test"):
            rs = rs + [self.dbg.r] + self.dbg.sub
        self.P.barrier()
        self.op(SP, lambda e: e.nop(), reads=rs)
        self.P.emit()


def Tl_view(t):
    return t


def build(nc, cfg):
    k = K(nc, cfg)
    k.declare_io()
    k.setup()
    tk = cfg.get("test")
    if tk in ("mix", "gdn1", "gdn2"):
        l = cfg["layer"]
        k.premix(l)
        if l % 2 == 0:
            k.natten(l)
        else:
            k.gdn(l)
    if tk == "post":
        l = cfg["layer"]
        k.postmix(l)
        k.moe_select(l)
        k.moe_passA(l)
        k.moe_passB(l)
    if not tk:
        for l in range(cfg.get("depth", DEPTH)):
            k.premix(l)
            if l % 2 == 0:
                k.natten(l)
            else:
                k.gdn(l)
            k.postmix(l)
            k.moe_select(l)
            k.moe_passA(l)
            k.moe_passB(l)
    k.finish()
    return k


WNAMES = ["c_ctx", "ada_w", "ada_b", "ln_g", "ln_b", "na_w_qkv", "na_w_o", "na_rpb", "gdn_w_in", "gdn_conv_w", "gdn_a_log",
          "gdn_dt_bias", "gdn_norm_w", "gdn_w_o", "moe_w_router", "moe_w_gate", "moe_w_up", "moe_w_down"]


def make_in_maps(inputs, cores):
    maps = []
    shared = {}
    for n in WNAMES:
        a = np.ascontiguousarray(inputs[n], dtype=np.float32)
        if n == "c_ctx":
            a = a.reshape(1, D)
        shared[n] = a
    rp = np.zeros((2, 16, 15, 128), np.float32)
    rp[..., 48:79] = np.asarray(inputs["na_rpb"], np.float32)[..., ::-1]
    shared["rpb_pad"] = rp
    qc = np.arange(64)
    cs = np.clip(qc - 8, 0, 48)
    kc = np.arange(64)[:, None]
    cmv = np.where((kc >= cs[None, :]) & (kc < cs[None, :] + 16), 0.0, -1e30).astype(np.float32)
    shared["cmask"] = np.tile(cmv, (2, 2))
    pp = np.arange(128)
    bd = lambda b: (pp[:, None] // b == pp[None, :] // b).astype(np.float32)
    shared["gmask"] = np.stack([bd(16), bd(32) - bd(16), bd(64) - bd(32), 1.0 - bd(64)]).astype(np.float32)
    for b in cores:
        m = dict(shared)
        m["x"] = np.ascontiguousarray(inputs["x"][b], dtype=np.float32)
        m["c"] = np.ascontiguousarray(inputs["c"][b:b + 1], dtype=np.float32)
        m["ctx"] = np.ascontiguousarray(inputs["ctx"][b], dtype=np.float32)
        maps.append(m)
    return maps


def kernel(**inputs):
    nc = bass.Bass("TRN2", target_bir_lowering=False)
    build(nc, {})
    maps = make_in_maps(inputs, list(range(8)))
    res = run_bass_kernel_spmd(nc, maps, core_ids=list(range(8)))
    return np.stack([np.asarray(r["out"], dtype=np.float32) for r in res.results], axis=0)
```

```python
import contextlib
import numpy as np
import concourse.bass as bass
import concourse.mybir as mybir
from concourse.bass_utils import run_bass_kernel_spmd

F32 = mybir.dt.float32
BF16 = mybir.dt.bfloat16
I32 = mybir.dt.int32
U8 = mybir.dt.uint8
AF = mybir.ActivationFunctionType
ALU = mybir.AluOpType
AX = mybir.AxisListType

PE, ACT, DVE, POOL, SP = "tensor", "scalar", "vector", "gpsimd", "sync"
ENGS = [PE, ACT, DVE, POOL, SP]
N_DMA_SEMS = 20
SEM_EPOCH = 20000

D = 1024
NT = 34
TT = NT * 128
NE = 16
DEPTH = 4
ALPHA = (2.0 * DEPTH) ** 0.25
LN_EPS = 1e-6
DSZ = {F32: 4, BF16: 2, I32: 4, U8: 1}


class Res:
    __slots__ = ("name", "last_w", "readers", "excl")

    def __init__(self, name=""):
        self.name = name
        self.last_w = None
        self.readers = []
        self.excl = False


class GRes:
    def __init__(self, name=""):
        self.subs = [Res(name + "a"), Res(name + "b")]


def _grp(fn):
    d = fn.__defaults__
    if not d:
        return None
    c = fn.__code__
    names = c.co_varnames[:c.co_argcount]
    m = dict(zip(names[len(names) - len(d):], d))
    if isinstance(m.get("j"), int):
        return m["j"]
    if isinstance(m.get("h"), int):
        return m["h"] // 4
    return None


def _expand(rs, g):
    out = []
    for r in rs:
        if isinstance(r, GRes):
            out.extend(r.subs if g is None else [r.subs[g]])
        else:
            out.append(r)
    return out


class Op:
    __slots__ = ("eng", "fn", "deps", "is_dma", "sem", "val", "sig")

    def __init__(self, eng, fn, is_dma):
        self.eng = eng
        self.fn = fn
        self.deps = ()
        self.is_dma = is_dma
        self.sem = None
        self.val = None
        self.sig = False


class Prog:
    def __init__(self, nc):
        self.nc = nc
        self.q = {e: [] for e in ENGS}
        self.dmas_since_barrier = []
        self.nops = 0

    def _add(self, eng, fn, reads, writes, is_dma):
        if any(isinstance(r, GRes) for r in reads) or any(isinstance(r, GRes) for r in writes):
            g = _grp(fn)
            reads, writes = _expand(reads, g), _expand(writes, g)
        op = Op(eng, fn, is_dma)
        deps = set()
        for r in reads:
            if r.last_w is not None:
                deps.add(r.last_w)
            if r.excl:
                deps.update(x for x in r.readers if x.eng != eng)
        for w in writes:
            if w.last_w is not None:
                deps.add(w.last_w)
            deps.update(w.readers)
        op.deps = tuple(deps)
        for r in reads:
            r.readers.append(op)
        for w in writes:
            w.last_w = op
            w.readers = []
        self.q[eng].append(op)
        self.nops += 1
        if is_dma:
            self.dmas_since_barrier.append(op)
        return op

    def op(self, eng, fn, reads=(), writes=()):
        return self._add(eng, fn, reads, writes, False)

    def dma(self, eng, out, in_, reads=(), writes=(), **kw):
        return self._add(eng, lambda e: e.dma_start(out=out, in_=in_, **kw), reads, writes, True)

    def barrier(self):
        b = Op(SP, lambda e: e.nop(), False)
        deps = set(self.dmas_since_barrier)
        for e in ENGS:
            if self.q[e]:
                deps.add(self.q[e][-1])
        b.deps = tuple(deps)
        self.q[SP].append(b)
        self.dmas_since_barrier = []
        for e in ENGS:
            if e == SP:
                continue
            o = Op(e, lambda en: en.nop(), False)
            o.deps = (b,)
            self.q[e].append(o)

    def emit(self):
        nc = self.nc
        for e in ENGS:
            for op in self.q[e]:
                for d in op.deps:
                    if d.is_dma or d.eng != op.eng or op.is_dma or d.eng != PE:
                        d.sig = True
        with contextlib.ExitStack() as st:
            nsig = {e: sum(1 for o in self.q[e] if o.sig and not o.is_dma) for e in ENGS}
            csem = {e: [st.enter_context(nc.semaphore("cs_%s_%d" % (e, i)))
                        for i in range(nsig[e] // SEM_EPOCH + 1)] for e in ENGS}
            dsem = {e: [st.enter_context(nc.semaphore("ds_%s_%d" % (e, i))) for i in range(N_DMA_SEMS)]
                    for e in (SP, ACT, POOL)}
            for e in ENGS:
                cnt = 0
                dcnt = 0
                for op in self.q[e]:
                    if op.is_dma:
                        op.sem = dsem[e][dcnt % N_DMA_SEMS]
                        op.val = 16 * (dcnt // N_DMA_SEMS + 1)
                        op.sig = True
                        dcnt += 1
                    elif op.sig:
                        op.sem = csem[e][cnt // SEM_EPOCH]
                        op.val = cnt % SEM_EPOCH + 1
                        cnt += 1
            block = st.enter_context(nc.Block())

            def gen(e):
                def body(eng):
                    waited = {}
                    for op in self.q[e]:
                        needs = {}
                        for d in op.deps:
                            if not d.sig:
                                continue
                            if (not d.is_dma) and d.eng == e and e == PE and not op.is_dma:
                                continue
                            k = id(d.sem)
                            if needs.get(k, (None, 0))[1] < d.val:
                                needs[k] = (d.sem, d.val)
                        if op.is_dma and op.val > 16:
                            k = id(op.sem)
                            if needs.get(k, (None, 0))[1] < op.val - 16:
                                needs[k] = (op.sem, op.val - 16)
                        for k, (s, v) in needs.items():
                            if waited.get(k, 0) >= v:
                                continue
                            eng.wait_ge(s, v)
                            waited[k] = v
                        ins = op.fn(eng)
                        if op.sig:
                            ins.then_inc(op.sem, 16 if op.is_dma else 1)
                return body

            for e in ENGS:
                if self.q[e]:
                    getattr(block, e)(gen(e))


class Tl:
    def __init__(self, ap, name="", nsub=0):
        self.ap = ap
        self.r = Res(name)
        self.sub = [Res(name + str(i)) for i in range(nsub)]


ARENA = 206000


class K:
    def __init__(self, nc, cfg):
        self.nc = nc
        self.cfg = cfg
        self.P = Prog(nc)
        global ARENA
        ARENA = (int(nc.sbuf_bytes_remaining) - 256) // 64 * 64
        self.big = nc.alloc_sbuf_tensor("arena", [128, ARENA], U8)
        self.off = 0
        self.ps = []
        for i in range(8):
            t = nc.alloc_psum_tensor("psb%d" % i, [128, 512], F32)
            self.ps.append(Tl(t[:], "ps%d" % i, nsub=4))
            for r_ in [self.ps[-1].r] + self.ps[-1].sub:
                r_.excl = True
        self.psn = 0
        self.uid = 0

    def tile(self, shape, dt, name="t", nsub=0):
        n = int(np.prod(shape[1:])) * DSZ[dt]
        if self.off + n > ARENA:
            raise RuntimeError("SBUF arena overflow at %s: %d + %d" % (name, self.off, n))
        a = self.big[0:shape[0], self.off:self.off + n].bitcast(dt)
        self.off += (n + 63) // 64 * 64
        if len(shape) == 3:
            a = a.rearrange("p (a b) -> p a b", a=shape[1])
        elif len(shape) == 4:
            a = a.rearrange("p (a b c) -> p a b c", a=shape[1], b=shape[2])
        self.uid += 1
        return Tl(a, "%s_%d" % (name, self.uid), nsub)

    def psum(self):
        t = self.ps[self.psn % 8]
        self.psn += 1
        return t

    def dram(self, name, shape, dt, kind="Internal"):
        return self.nc.dram_tensor(name, list(shape), dt, kind=kind).ap()

    def op(self, eng, fn, reads=(), writes=()):
        return self.P.op(eng, fn, reads, writes)

    def dma(self, eng, out, in_, reads=(), writes=(), **kw):
        return self.P.dma(eng, out, in_, reads, writes, **kw)

    def declare_io(self):
        nc, cfg = self.nc, self.cfg
        ein = lambda n, s: self.dram(n, s, F32, kind="ExternalInput")
        self.x_in = ein("x", [4096, D])
        self.c_in = ein("c", [1, D])
        self.ctx_in = ein("ctx", [256, D])
        self.cc_in = ein("c_ctx", [1, D])
        self.ada_w = ein("ada_w", [DEPTH, D, 6 * D])
        self.ada_b = ein("ada_b", [DEPTH, 6 * D])
        self.ln_g = ein("ln_g", [DEPTH, 2, D])
        self.ln_b = ein("ln_b", [DEPTH, 2, D])
        self.na_w_qkv = ein("na_w_qkv", [2, D, 3 * D])
        self.na_w_o = ein("na_w_o", [2, D, D])
        self.na_rpb = ein("na_rpb", [2, 16, 15, 31])
        self.gdn_w_in = ein("gdn_w_in", [2, D, 4 * D + 32])
        self.gdn_conv_w = ein("gdn_conv_w", [2, 5, 3 * D])
        self.gdn_a_log = ein("gdn_a_log", [2, 2, 8])
        self.gdn_dt_bias = ein("gdn_dt_bias", [2, 2, 8])
        self.gdn_norm_w = ein("gdn_norm_w", [2, 128])
        self.gdn_w_o = ein("gdn_w_o", [2, D, D])
        if cfg.get("test") not in ("mix", "gdn1", "gdn2"):
            self.moe_w_router = ein("moe_w_router", [DEPTH, D, NE])
            self.moe_w_gate = ein("moe_w_gate", [DEPTH, NE, D, 2048])
            self.moe_w_up = ein("moe_w_up", [DEPTH, NE, D, 2048])
            self.moe_w_down = ein("moe_w_down", [DEPTH, NE, 2048, D])
        if cfg.get("test") in ("gdn1", "gdn2"):
            self.dbg2 = self.dram("dbg2", [4, D, TT], F32, kind="ExternalOutput")
            self.dbg3 = self.dram("dbg3", [128, 2, NT, 16], F32, kind="ExternalOutput")
        self.rpb_pad = ein("rpb_pad", [2, 16, 15, 128])
        self.cmask_in = ein("cmask", [128, 128])
        self.gmask_in = ein("gmask", [4, 128, 128])
        self.out = self.dram("out", [4096, D], F32, kind="ExternalOutput")
        tk = cfg.get("test")
        self.XS = Tl(self.dram("XS", [TT, D], F32), "XS", NT)
        self.Y = Tl(self.dram("Y", [TT, D], F32), "Y", NT)
        if tk:
            self.xs_in = self.dram("xs_in", [TT, D], F32, kind="ExternalInput")
            self.y_in = self.dram("y_in", [TT, D], F32, kind="ExternalInput")
        if tk:
            self.dbg = Tl(self.dram("dbg", [TT, D], F32, kind="ExternalOutput"), "dbg", NT)
        self.AFFT = Tl(self.dram("AFFT", [NE, TT], F32), "AFFT")
        self.POST = Tl(self.dram("POST", [NE, TT], F32), "POST")
        self.YE = Tl(self.dram("YE", [NE, 4, 128, D], BF16), "YE", NE)
        self.YEC = Tl(self.dram("YEC", [NE, 32, D], BF16), "YEC", NE)
        self.SELT = Tl(self.dram("SELT", [32, 128, 64, 128], BF16), "SELT", 32)
        self.SELTC = Tl(self.dram("SELTC", [NE, 32, 256], BF16), "SELTC")
        self.outr = Res("out")

    def setup(self):
        op, dma = self.op, self.dma
        self.ident = self.tile([128, 128], BF16, "ident")
        self.ident32 = self.tile([128, 128], F32, "ident32")
        self.ones = self.tile([128, 128], BF16, "ones")
        self.triu = self.tile([128, 128], BF16, "triu")
        tmp = self.tile([128, 128], F32, "tmp")
        self.iota512 = self.tile([128, 512], F32, "iota512")
        self.iotacol = self.tile([128, 4], F32, "iotacol")
        i32, idb, on, tu, io, ic = self.ident32, self.ident, self.ones, self.triu, self.iota512, self.iotacol
        op(POOL, lambda e: e.memset(i32.ap, 1.0), writes=[i32.r])
        op(POOL, lambda e: e.affine_select(out=i32.ap, in_=i32.ap, pattern=[[-1, 128]], compare_op=ALU.is_equal,
                                           fill=0.0, base=0, channel_multiplier=1), reads=[i32.r], writes=[i32.r])
        op(DVE, lambda e: e.tensor_copy(out=idb.ap, in_=i32.ap), reads=[i32.r], writes=[idb.r])
        op(DVE, lambda e: e.memset(on.ap, 1.0), writes=[on.r])
        op(POOL, lambda e: e.memset(tmp.ap, 1.0), writes=[tmp.r])
        op(POOL, lambda e: e.affine_select(out=tmp.ap, in_=tmp.ap, pattern=[[1, 128]], compare_op=ALU.is_ge,
                                           fill=0.0, base=0, channel_multiplier=-1), reads=[tmp.r], writes=[tmp.r])
        op(DVE, lambda e: e.tensor_copy(out=tu.ap, in_=tmp.ap), reads=[tmp.r], writes=[tu.r])
        op(POOL, lambda e: e.iota(io.ap, pattern=[[1, 512]], base=0, channel_multiplier=0,
                                  allow_small_or_imprecise_dtypes=True), writes=[io.r])
        op(POOL, lambda e: e.iota(ic.ap, pattern=[[128, 4]], base=0, channel_multiplier=1,
                                  allow_small_or_imprecise_dtypes=True), writes=[ic.r])
        self.scb = self.tile([128, 8, 128], F32, "scb")
        self.sccb = self.tile([128, 8, 128], F32, "sccb")
        for src, dst in ((self.c_in, self.scb), (self.cc_in, self.sccb)):
            cv = self.tile([128, 8], F32, "cv")
            dma(SP, cv.ap, src.rearrange("o (k p) -> p (o k)", p=128), writes=[cv.r], allow_slow_non_contiguous=True)
            op(ACT, lambda e, cv=cv: e.activation(out=cv.ap, in_=cv.ap, func=AF.Silu), reads=[cv.r], writes=[cv.r])
            op(DVE, lambda e, cv=cv, dst=dst: e.tensor_copy(out=dst.ap, in_=cv.ap.unsqueeze(2).to_broadcast([128, 8, 128])),
               reads=[cv.r], writes=[dst.r])
        self.base_off = self.off
        if not self.cfg.get("test"):
            dma(SP, self.XS.ap[0:256, :], self.ctx_in, writes=self.XS.sub[0:2])
            dma(SP, self.XS.ap[256:TT, :], self.x_in, writes=self.XS.sub[2:NT])
        else:
            dma(SP, self.XS.ap, self.xs_in, writes=self.XS.sub)
            dma(SP, self.Y.ap, self.y_in, writes=self.Y.sub)

    def phase_begin(self):
        self.P.barrier()
        self.off = self.base_off

    def mod_tiles(self, l, idxs, plus1=()):
        op, dma = self.op, self.dma
        res = {}
        for idx in idxs:
            res[idx] = (self.tile([128, D], F32, "modl"), self.tile([128, D], F32, "modc"))
        mark = self.off
        wbuf = [self.tile([128, 8, 512], F32, "adaw") for _ in range(2)]
        bbuf = [self.tile([128, 1024], F32, "adab") for _ in range(2)]
        n = 0
        for ii, idx in enumerate(idxs):
            lat, ctx = res[idx]
            bb = bbuf[ii % 2]
            dma(SP, bb.ap, self.ada_b[l:l + 1, idx * D:(idx + 1) * D].to_broadcast([128, D]), writes=[bb.r])
            for half in range(2):
                wb = wbuf[n % 2]
                n += 1
                c0 = idx * D + half * 512
                dma(SP, wb.ap, self.ada_w[l, :, c0:c0 + 512].rearrange("(k p) n -> p k n", p=128), writes=[wb.r])
                for lhs, dst in ((self.scb, lat), (self.sccb, ctx)):
                    ps = self.psum()
                    for k in range(8):
                        op(PE, lambda e, ps=ps, lhs=lhs, wb=wb, k=k: e.matmul(ps.ap, lhsT=lhs.ap[:, k, :], rhs=wb.ap[:, k, :],
                                                                              start=(k == 0), stop=(k == 7)),
                           reads=[lhs.r, wb.r], writes=[ps.r])
                    sl = slice(half * 512, (half + 1) * 512)
                    op(DVE, lambda e, ps=ps, dst=dst, bb=bb, sl=sl: e.tensor_tensor(out=dst.ap[:, sl], in0=ps.ap, in1=bb.ap[:, sl], op=ALU.add),
                       reads=[ps.r, bb.r], writes=[dst.r])
            if idx in plus1:
                for t in (lat, ctx):
                    op(DVE, lambda e, t=t: e.tensor_scalar(out=t.ap, in0=t.ap, scalar1=1.0, scalar2=None, op0=ALU.add),
                       reads=[t.r], writes=[t.r])
        self.P.barrier()
        self.off = mark
        return res

    def ln_vec(self, src_row):
        t = self.tile([128, D], F32, "lnv")
        self.dma(SP, t.ap, src_row.to_broadcast([128, D]), writes=[t.r])
        return t

    def layernorm_(self, r, st, mv):
        op = self.op
        for h in range(2):
            op(DVE, lambda e, h=h: e.bn_stats(out=st.ap[:, h, :], in_=r.ap[:, h * 512:(h + 1) * 512]), reads=[r.r], writes=[st.r])
        op(DVE, lambda e: e.bn_aggr(out=mv.ap[:, 0:2], in_=st.ap.rearrange("p a b -> p (a b)")), reads=[st.r], writes=[mv.r])
        op(DVE, lambda e: e.tensor_scalar(out=mv.ap[:, 2:3], in0=mv.ap[:, 1:2], scalar1=LN_EPS, scalar2=None, op0=ALU.add),
           reads=[mv.r], writes=[mv.r])
        op(ACT, lambda e: e.activation(out=mv.ap[:, 2:3], in_=mv.ap[:, 2:3], func=AF.Ln), reads=[mv.r], writes=[mv.r])
        op(ACT, lambda e: e.activation(out=mv.ap[:, 2:3], in_=mv.ap[:, 2:3], func=AF.Exp, scale=-0.5), reads=[mv.r], writes=[mv.r])
        op(DVE, lambda e: e.tensor_scalar(out=r.ap, in0=r.ap, scalar1=mv.ap[:, 0:1], scalar2=mv.ap[:, 2:3],
                                          op0=ALU.subtract, op1=ALU.mult), reads=[r.r, mv.r], writes=[r.r])

    def premix(self, l):
        op, dma = self.op, self.dma
        self.phase_begin()
        hT = self.tile([128, 8, TT], BF16, "hT", nsub=NT)
        self.hT = hT
        self.mix_keep = self.off
        mods = self.mod_tiles(l, [0, 1], plus1=(1,))
        xb = [self.tile([128, D], F32, "xb") for _ in range(3)]
        hb = [self.tile([128, D], BF16, "hb") for _ in range(2)]
        for i in range(NT):
            x = xb[i % 3]
            h = hb[i % 2]
            c = 1 if i < 2 else 0
            dma(SP, x.ap, self.XS.ap[i * 128:(i + 1) * 128, :], reads=[self.XS.sub[i]], writes=[x.r])
            sc, sh = mods[1][c], mods[0][c]
            op(DVE, lambda e, x=x, sc=sc: e.tensor_tensor(out=x.ap, in0=x.ap, in1=sc.ap, op=ALU.mult), reads=[x.r, sc.r], writes=[x.r])
            op(POOL, lambda e, x=x, sh=sh, h=h: e.tensor_tensor(out=h.ap, in0=x.ap, in1=sh.ap, op=ALU.add), reads=[x.r, sh.r], writes=[h.r])
            self.transpose_to(h, hT, i)

    def transpose_to(self, h, hT, i, evac=ACT):
        op = self.op
        ps = self.psum()
        pb = ps.ap.bitcast(BF16)
        for k in range(8):
            op(PE, lambda e, k=k, pb=pb, h=h: e.transpose(out=pb[:, k * 128:(k + 1) * 128], in_=h.ap[:, k * 128:(k + 1) * 128],
                                                          identity=self.ident.ap), reads=[h.r, self.ident.r], writes=[ps.r])
        dst = hT.ap[:, :, i * 128:(i + 1) * 128]
        if evac == ACT:
            op(ACT, lambda e, pb=pb, dst=dst: e.copy(out=dst, in_=pb.rearrange("p (k n) -> p k n", k=8)), reads=[ps.r], writes=[hT.sub[i]])
        else:
            op(evac, lambda e, pb=pb, dst=dst: e.tensor_copy(out=dst, in_=pb.rearrange("p (k n) -> p k n", k=8)), reads=[ps.r], writes=[hT.sub[i]])

    def natten(self, l):
        op, dma = self.op, self.dma
        li = l // 2
        hT = self.hT
        self.P.barrier()
        self.off = self.mix_keep
        if not hasattr(self, "QT"):
            self.QT = Tl(self.dram("QT", [D, TT], BF16), "QT")
            self.KT = Tl(self.dram("KT", [D, TT], BF16), "KT")
            self.VX = Tl(self.dram("VX", [TT, 16, 65], BF16), "VX")
            self.OD = Tl(self.dram("OD", [TT, D], BF16), "OD", NT)
        QT, KT, VX, OD = self.QT, self.KT, self.VX, self.OD
        wq = self.na_w_qkv[li]
        wb = [self.tile([128, 8, 512], BF16, "wqk") for _ in range(2)]
        stq = [self.tile([128, 512], BF16, "stq") for _ in range(3)]
        blocks = [(0, 256)] + [(256 + 512 * b, 512) for b in range(8)]
        n = 0
        for cg in range(4):
            w = wb[cg % 2]
            dma(POOL, w.ap, wq[:, cg * 512:(cg + 1) * 512].rearrange("(k p) n -> p k n", p=128), writes=[w.r])
            for cl in range(4):
                ct = cg * 4 + cl
                dst = QT if ct < 8 else KT
                crow = (ct % 8) * 128
                for (t0, nt_) in blocks:
                    ps = self.psum()
                    for k in range(8):
                        op(PE, lambda e, ps=ps, w=w, k=k, cl=cl, t0=t0, nt_=nt_: e.matmul(ps.ap[:, 0:nt_], lhsT=w.ap[:, k, cl * 128:(cl + 1) * 128], rhs=hT.ap[:, k, t0:t0 + nt_],
                                                                                    start=(k == 0), stop=(k == 7)),
                           reads=[w.r] + hT.sub[t0 // 128:(t0 + nt_) // 128], writes=[ps.r])
                    sq = stq[n % 3]
                    n += 1
                    if ct < 8:
                        op(ACT, lambda e, ps=ps, sq=sq, nt_=nt_: e.mul(out=sq.ap[:, 0:nt_], in_=ps.ap[:, 0:nt_], mul=0.125), reads=[ps.r], writes=[sq.r])
                    else:
                        op(DVE, lambda e, ps=ps, sq=sq, nt_=nt_: e.tensor_copy(out=sq.ap[:, 0:nt_], in_=ps.ap[:, 0:nt_]), reads=[ps.r], writes=[sq.r])
                    dma(SP, dst.ap[crow:crow + 128, t0:t0 + nt_], sq.ap[:, 0:nt_], reads=[sq.r], writes=[dst.r])
        wv = self.tile([128, 8, D], BF16, "wv")
        dma(POOL, wv.ap, wq[:, 2 * D:3 * D].rearrange("(k p) n -> p k n", p=128), writes=[wv.r])
        vst = [self.tile([128, 16, 65], BF16, "vst") for _ in range(2)]
        for v in vst:
            op(DVE, lambda e, v=v: e.memset(v.ap, 1.0), writes=[v.r])
        for i in range(NT):
            v = vst[i % 2]
            for half in range(2):
                ps = self.psum()
                for k in range(8):
                    op(PE, lambda e, ps=ps, k=k, i=i, half=half: e.matmul(ps.ap, lhsT=hT.ap[:, k, i * 128:(i + 1) * 128], rhs=wv.ap[:, k, half * 512:(half + 1) * 512],
                                                                       start=(k == 0), stop=(k == 7)),
                       reads=[wv.r, hT.sub[i]], writes=[ps.r])
                op(ACT, lambda e, ps=ps, v=v, half=half: e.copy(out=v.ap[:, half * 8:(half + 1) * 8, 0:64], in_=ps.ap.rearrange("p (h d) -> p h d", d=64)),
                   reads=[ps.r], writes=[v.r])
            dma(SP, VX.ap[i * 128:(i + 1) * 128], v.ap, reads=[v.r], writes=[VX.r])
        self.P.barrier()
        self.off = self.mix_keep - 0
        self.off = self.base_off
        rs_ = np.clip(np.arange(64) - 4, 0, 56)
        tiles, tindex, per_rp = [], {}, []
        for rp in range(32):
            r0 = 2 * rp
            lst = []
            for kp in range(rs_[r0] // 2, (rs_[r0 + 1] + 7) // 2 + 1):
                spec = []
                for i2 in range(2):
                    for j2 in range(2):
                        kr, r = 2 * kp + i2, r0 + j2
                        spec.append(int(kr - r + 7) if rs_[r] <= kr < rs_[r] + 8 else None)
                spec = tuple(spec)
                key = (rp if rp in (0, 1, 30, 31) else -1, spec)
                if key not in tindex:
                    tindex[key] = len(tiles)
                    tiles.append(spec)
                lst.append((kp, tindex[key]))
            assert all(lst[j + 1][1] == lst[j][1] + 1 for j in range(len(lst) - 1))
            per_rp.append(lst)
        NTL = len(tiles)
        jm = self.tile([64, 128], F32, "jm")
        op(POOL, lambda e: e.memset(jm.ap, 1.0), writes=[jm.r])
        op(POOL, lambda e: e.affine_select(out=jm.ap[:, 0:64], in_=jm.ap[:, 0:64], pattern=[[1, 64]], compare_op=ALU.is_equal, fill=0.0, base=-63, channel_multiplier=1),
           reads=[jm.r], writes=[jm.r])
        op(POOL, lambda e: e.affine_select(out=jm.ap[:, 64:128], in_=jm.ap[:, 64:128], pattern=[[1, 64]], compare_op=ALU.is_equal, fill=0.0, base=-63, channel_multiplier=1),
           reads=[jm.r], writes=[jm.r])
        cm = self.tile([128, 128], F32, "cmask")
        dma(SP, cm.ap, self.cmask_in, writes=[cm.r])
        hk = self.tile([64, 15, 64], F32, "hk")
        toe = self.tile([128, 15, 64], F32, "toe")
        bias = self.tile([128, NTL, 128], F32, "bias")
        qt = [self.tile([64, TT], BF16, "qt") for _ in range(2)]
        kt = [self.tile([64, TT], BF16, "kt") for _ in range(2)]
        vh = [self.tile([128, NT, 65], BF16, "vh") for _ in range(2)]
        sb = [self.tile([128, 640], F32, "sb") for _ in range(2)]
        pb = [self.tile([128, 896], BF16, "pb") for _ in range(2)]
        ost = [self.tile([128, NT, 64], BF16, "ost") for _ in range(2)]
        rc = self.tile([128, 4], F32, "rc")
        rpb = self.rpb_pad[li]
        it = 0
        for h in range(16):
            q_, k_, v_, o_ = qt[h % 2], kt[h % 2], vh[h % 2], ost[h % 2]
            dma(SP, q_.ap, QT.ap[h * 64:(h + 1) * 64, :], reads=[QT.r], writes=[q_.r])
            dma(SP, k_.ap, KT.ap[h * 64:(h + 1) * 64, :], reads=[KT.r], writes=[k_.r])
            dma(SP, v_.ap, VX.ap.rearrange("(i p) h d -> p i h d", p=128)[:, :, h, :], reads=[VX.r], writes=[v_.r])
            src = bass.AP(tensor=rpb.tensor, offset=rpb[h].offset, ap=[[1, 64], [128, 15], [1, 64]])
            dma(SP, hk.ap, src, writes=[hk.r])
            for (d0, d1) in ((0, 8), (8, 15)):
                ps = self.psum()
                nn = (d1 - d0) * 64
                op(PE, lambda e, ps=ps, d0=d0, d1=d1, nn=nn: e.matmul(ps.ap[:, 0:nn], lhsT=jm.ap, rhs=hk.ap[:, d0:d1, :].rearrange("p a b -> p (a b)"), start=True, stop=True),
                   reads=[jm.r, hk.r], writes=[ps.r])
                op(ACT, lambda e, ps=ps, d0=d0, d1=d1, nn=nn: e.copy(out=toe.ap[:, d0:d1, :].rearrange("p a b -> p (a b)"), in_=ps.ap[:, 0:nn]), reads=[ps.r], writes=[toe.r])
            for t, spec in enumerate(tiles):
                for bi, dr in enumerate(spec):
                    i2, j2 = bi // 2, bi % 2
                    prt = slice(i2 * 64, (i2 + 1) * 64)
                    fr = slice(j2 * 64, (j2 + 1) * 64)
                    if dr is None:
                        op(POOL, lambda e, t=t, prt=prt, fr=fr: e.memset(bias.ap[prt, t, fr], -1e30), writes=[bias.r])
                    else:
                        op(POOL, lambda e, t=t, prt=prt, fr=fr, dr=dr: e.tensor_tensor(out=bias.ap[prt, t, fr], in0=toe.ap[prt, dr, :], in1=cm.ap[prt, fr], op=ALU.add),
                           reads=[toe.r, cm.r], writes=[bias.r])
            for qi in range(NT):
                if qi < 2:
                    wins = []
                    ctxk = [0, 1]
                else:
                    wins = per_rp[qi - 2]
                    ctxk = [0, 1]
                s_, p_ = sb[it % 2], pb[it % 2]
                it += 1
                nw = len(wins)
                qs = slice(qi * 128, (qi + 1) * 128)
                psA, psB = self.psum(), self.psum()
                slots = []
                for j in range(nw):
                    slots.append((psA, j * 128) if j < 4 else (psB, 0))
                cb = 128 if nw == 5 else 0
                for j, kk in enumerate(ctxk):
                    slots.append((psB, cb + j * 128))
                ktl = [2 + kp for (kp, _) in wins] + ctxk
                for (pst, co), kti in zip(slots, ktl):
                    op(PE, lambda e, pst=pst, co=co, kti=kti, qs=qs, k_=k_, q_=q_: e.matmul(pst.ap[:, co:co + 128], lhsT=k_.ap[:, kti * 128:(kti + 1) * 128], rhs=q_.ap[:, qs],
                                                                                         start=True, stop=True),
                       reads=[k_.r, q_.r], writes=[pst.r])
                if nw:
                    t0 = wins[0][1]
                    na = min(nw, 4)
                    op(DVE, lambda e, s_=s_, t0=t0, na=na, psA=psA: e.tensor_tensor(out=s_.ap[:, 0:na * 128], in0=psA.ap[:, 0:na * 128],
                                                                                   in1=bias.ap[:, t0:t0 + na, :].rearrange("p a b -> p (a b)"), op=ALU.add),
                       reads=[psA.r, bias.r], writes=[s_.r])
                    if nw == 5:
                        op(DVE, lambda e, s_=s_, t0=t0, psB=psB: e.tensor_tensor(out=s_.ap[:, 512:640], in0=psB.ap[:, 0:128], in1=bias.ap[:, t0 + 4, :], op=ALU.add),
                           reads=[psB.r, bias.r], writes=[s_.r])
                    op(ACT, lambda e, s_=s_, p_=p_, nw=nw: e.activation(out=p_.ap[:, 0:nw * 128], in_=s_.ap[:, 0:nw * 128], func=AF.Exp), reads=[s_.r], writes=[p_.r])
                op(ACT, lambda e, p_=p_, nw=nw, cb=cb, psB=psB: e.activation(out=p_.ap[:, nw * 128:nw * 128 + 256], in_=psB.ap[:, cb:cb + 256], func=AF.Exp),
                   reads=[psB.r], writes=[p_.r])
                pso = self.psum()
                nk = len(ktl)
                for j, kti in enumerate(ktl):
                    op(PE, lambda e, pso=pso, p_=p_, j=j, kti=kti, v_=v_, nk=nk: e.matmul(pso.ap[:, 0:65], lhsT=p_.ap[:, j * 128:(j + 1) * 128], rhs=v_.ap[:, kti, :],
                                                                                       start=(j == 0), stop=(j == nk - 1)),
                       reads=[p_.r, v_.r], writes=[pso.r])
                op(DVE, lambda e, pso=pso: e.reciprocal(out=rc.ap[:, 0:1], in_=pso.ap[:, 64:65]), reads=[pso.r], writes=[rc.r])
                op(DVE, lambda e, pso=pso, o_=o_, qi=qi: e.tensor_scalar(out=o_.ap[:, qi, :], in0=pso.ap[:, 0:64], scalar1=rc.ap[:, 0:1], scalar2=None, op0=ALU.mult),
                   reads=[pso.r, rc.r], writes=[o_.r])
            dma(SP, OD.ap.rearrange("(i p) d -> p i d", p=128)[:, :, h * 64:(h + 1) * 64], o_.ap, reads=[o_.r], writes=OD.sub)
        self.out_proj(self.na_w_o[li])

    def gdn(self, l):
        op, dma = self.op, self.dma
        li = l // 2
        hT = self.hT
        self.P.barrier()
        self.off = self.mix_keep
        if not hasattr(self, "QF"):
            for nm in ("QF", "KF", "VF", "ZF", "OF"):
                setattr(self, nm, Tl(self.dram(nm, [D, TT], F32), nm, NT))
        QF, KF, VF, ZF, OF = self.QF, self.KF, self.VF, self.ZF, self.OF
        w_in = self.gdn_w_in[li]
        ones_a = self.tile([128, 128], F32, "ones_a")
        op(DVE, lambda e: e.memset(ones_a.ap, 1.0), writes=[ones_a.r])
        gT_a = self.tile([128, NT, 16], F32, "gT_a")
        bT_a = self.tile([128, NT, 16], F32, "bT_a")
        g_keep = self.off
        cw = self.tile([128, 24, 5], F32, "cw")
        for j in range(5):
            dma(SP, cw.ap[:, :, j], self.gdn_conv_w[li, j:j + 1, :].rearrange("o (c p) -> p (o c)", p=128), writes=[cw.r], allow_slow_non_contiguous=True)
        nwv = self.tile([128, 1], F32, "nwv")
        dma(SP, nwv.ap, self.gdn_norm_w[li:li + 1, :].rearrange("o p -> p o"), writes=[nwv.r], allow_slow_non_contiguous=True)
        pbuf = [self.tile([128, TT + 8], F32, "pbuf") for _ in range(2)]
        cbuf = [self.tile([128, TT], F32, "cbuf") for _ in range(2)]
        for pb in pbuf:
            op(POOL, lambda e, pb=pb: e.memset(pb.ap, 0.0), writes=[pb.r])
        wb = [self.tile([128, 8, 512], BF16, "win") for _ in range(2)]
        sqb = [self.tile([128, 512], F32, "sqb") for _ in range(2)]
        rnb = [self.tile([128, 512], F32, "rnb") for _ in range(2)]
        zsb = [self.tile([128, 512], F32, "zsb") for _ in range(2)]
        blocks = [(0, 256)] + [(256 + 512 * b, 512) for b in range(8)]
        nb = 0
        for cg in range(8):
            w = wb[cg % 2]
            dma(POOL, w.ap, w_in[:, cg * 512:(cg + 1) * 512].rearrange("(k p) n -> p k n", p=128), writes=[w.r])
            for cl in range(4):
                ct = cg * 4 + cl
                pb, cb = pbuf[ct % 2], cbuf[ct % 2]
                for (t0, nt_) in blocks:
                    ps = self.psum()
                    for k in range(8):
                        op(PE, lambda e, ps=ps, w=w, k=k, cl=cl, t0=t0, nt_=nt_: e.matmul(ps.ap[:, 0:nt_], lhsT=w.ap[:, k, cl * 128:(cl + 1) * 128], rhs=hT.ap[:, k, t0:t0 + nt_],
                                                                                    start=(k == 0), stop=(k == 7)),
                           reads=[w.r] + hT.sub[t0 // 128:(t0 + nt_) // 128], writes=ps.sub)
                    if ct < 24:
                        o0 = (2 if t0 == 0 else 6) + t0
                        op(ACT, lambda e, ps=ps, pb=pb, o0=o0, nt_=nt_: e.copy(out=pb.ap[:, o0:o0 + nt_], in_=ps.ap[:, 0:nt_]), reads=ps.sub, writes=[pb.r])
                    else:
                        zs = zsb[nb % 2]
                        nb += 1
                        op(ACT, lambda e, ps=ps, zs=zs, nt_=nt_: e.activation(out=zs.ap[:, 0:nt_], in_=ps.ap[:, 0:nt_], func=AF.Silu), reads=ps.sub, writes=[zs.r])
                        op(DVE, lambda e, zs=zs, nt_=nt_: e.tensor_scalar(out=zs.ap[:, 0:nt_], in0=zs.ap[:, 0:nt_], scalar1=nwv.ap[:, 0:1], scalar2=None, op0=ALU.mult),
                           reads=[zs.r, nwv.r], writes=[zs.r])
                        r0 = (ct - 24) * 128
                        dma(SP, ZF.ap[r0:r0 + 128, t0:t0 + nt_], zs.ap[:, 0:nt_], reads=[zs.r], writes=ZF.sub[t0 // 128:(t0 + nt_) // 128])
                if ct >= 24:
                    continue
                for (c0, ln, base) in ((0, 256, 0), (256, 4096, 260)):
                    op(DVE, lambda e, cb=cb, pb=pb, c0=c0, ln=ln, base=base, ct=ct: e.tensor_scalar(out=cb.ap[:, c0:c0 + ln], in0=pb.ap[:, base:base + ln], scalar1=cw.ap[:, ct, 0:1],
                                                                                             scalar2=None, op0=ALU.mult), reads=[pb.r, cw.r], writes=[cb.r])
                    for j in range(1, 5):
                        op(DVE, lambda e, cb=cb, pb=pb, c0=c0, ln=ln, base=base, ct=ct, j=j: e.scalar_tensor_tensor(out=cb.ap[:, c0:c0 + ln], in0=pb.ap[:, base + j:base + j + ln],
                                                                                                            scalar=cw.ap[:, ct, j:j + 1], in1=cb.ap[:, c0:c0 + ln], op0=ALU.mult, op1=ALU.add),
                           reads=[pb.r, cw.r, cb.r], writes=[cb.r])
                op(ACT, lambda e, cb=cb: e.activation(out=cb.ap, in_=cb.ap, func=AF.Silu), reads=[cb.r], writes=[cb.r])
                if ct < 16:
                    qsc = (128.0 ** -0.5) if ct < 8 else 1.0
                    for (t0, nt_) in blocks:
                        sq, rn = sqb[nb % 2], rnb[nb % 2]
                        nb += 1
                        op(POOL, lambda e, sq=sq, cb=cb, t0=t0, nt_=nt_: e.tensor_tensor(out=sq.ap[:, 0:nt_], in0=cb.ap[:, t0:t0 + nt_], in1=cb.ap[:, t0:t0 + nt_], op=ALU.mult),
                           reads=[cb.r], writes=[sq.r])
                        ps = self.psum()
                        op(PE, lambda e, ps=ps, sq=sq, nt_=nt_, o32=ones_a: e.matmul(ps.ap[:, 0:nt_], lhsT=o32.ap, rhs=sq.ap[:, 0:nt_], start=True, stop=True), reads=[ones_a.r, sq.r], writes=ps.sub)
                        op(DVE, lambda e, ps=ps, rn=rn, nt_=nt_: e.tensor_scalar(out=rn.ap[:, 0:nt_], in0=ps.ap[:, 0:nt_], scalar1=1e-6, scalar2=None, op0=ALU.add), reads=ps.sub, writes=[rn.r])
                        op(ACT, lambda e, rn=rn, nt_=nt_: e.activation(out=rn.ap[:, 0:nt_], in_=rn.ap[:, 0:nt_], func=AF.Ln), reads=[rn.r], writes=[rn.r])
                        op(ACT, lambda e, rn=rn, nt_=nt_: e.activation(out=rn.ap[:, 0:nt_], in_=rn.ap[:, 0:nt_], func=AF.Exp, scale=-0.5), reads=[rn.r], writes=[rn.r])
                        op(DVE, lambda e, rn=rn, cb=cb, t0=t0, nt_=nt_, qsc=qsc: e.scalar_tensor_tensor(out=cb.ap[:, t0:t0 + nt_], in0=cb.ap[:, t0:t0 + nt_], scalar=qsc, in1=rn.ap[:, 0:nt_],
                                                                                                 op0=ALU.mult, op1=ALU.mult), reads=[cb.r, rn.r], writes=[cb.r])
                dst = (QF, KF, VF)[ct // 8]
                r0 = (ct % 8) * 128
                dma(SP, dst.ap[r0:r0 + 128, :], cb.ap, reads=[cb.r], writes=dst.sub)
        wab = self.tile([128, 8, 32], BF16, "wab")
        dma(POOL, wab.ap, w_in[:, 4096:4128].rearrange("(k p) n -> p k n", p=128), writes=[wab.r])
        coef = self.tile([128, 16], F32, "coef")
        dtb = self.tile([128, 16], F32, "dtb")
        dma(SP, coef.ap, self.gdn_a_log[li:li + 1].rearrange("o a b -> o (a b)").to_broadcast([128, 16]), writes=[coef.r])
        dma(SP, dtb.ap, self.gdn_dt_bias[li:li + 1].rearrange("o a b -> o (a b)").to_broadcast([128, 16]), writes=[dtb.r])
        op(ACT, lambda e: e.activation(out=coef.ap, in_=coef.ap, func=AF.Exp), reads=[coef.r], writes=[coef.r])
        op(DVE, lambda e: e.tensor_scalar(out=coef.ap, in0=coef.ap, scalar1=-1.0, scalar2=None, op0=ALU.mult), reads=[coef.r], writes=[coef.r])
        psl = []
        for i in range(NT):
            if i % 16 == 0:
                ps = self.psum()
                psl.append(ps)
            c0 = (i % 16) * 32
            for k in range(8):
                op(PE, lambda e, ps=ps, i=i, k=k, c0=c0: e.matmul(ps.ap[:, c0:c0 + 32], lhsT=hT.ap[:, k, i * 128:(i + 1) * 128], rhs=wab.ap[:, k, :], start=(k == 0), stop=(k == 7)),
                   reads=[wab.r, hT.sub[i]], writes=ps.sub)
        for gi, ps in enumerate(psl):
            n_ = min(16, NT - gi * 16)
            pv = ps.ap[:, 0:n_ * 32].rearrange("p (i c) -> p i c", c=32)
            gs = gT_a.ap[:, gi * 16:gi * 16 + n_, :]
            bs = bT_a.ap[:, gi * 16:gi * 16 + n_, :]
            op(DVE, lambda e, pv=pv, gs=gs, n_=n_: e.tensor_tensor(out=gs, in0=pv[:, :, 0:16], in1=dtb.ap.unsqueeze(1).to_broadcast([128, n_, 16]), op=ALU.add), reads=ps.sub + [dtb.r], writes=[gT_a.r])
            op(ACT, lambda e, gs=gs: e.activation(out=gs, in_=gs, func=AF.Exp), reads=[gT_a.r], writes=[gT_a.r])
            op(DVE, lambda e, gs=gs: e.tensor_scalar(out=gs, in0=gs, scalar1=1.0, scalar2=None, op0=ALU.add), reads=[gT_a.r], writes=[gT_a.r])
            op(ACT, lambda e, gs=gs: e.activation(out=gs, in_=gs, func=AF.Ln), reads=[gT_a.r], writes=[gT_a.r])
            op(DVE, lambda e, gs=gs, n_=n_: e.tensor_tensor(out=gs, in0=gs, in1=coef.ap.unsqueeze(1).to_broadcast([128, n_, 16]), op=ALU.mult), reads=[gT_a.r, coef.r], writes=[gT_a.r])
            op(ACT, lambda e, pv=pv, bs=bs: e.activation(out=bs, in_=pv[:, :, 16:32], func=AF.Exp, scale=-1.0), reads=ps.sub, writes=[bT_a.r])
            op(DVE, lambda e, bs=bs: e.tensor_scalar(out=bs, in0=bs, scalar1=1.0, scalar2=None, op0=ALU.add), reads=[bT_a.r], writes=[bT_a.r])
            op(DVE, lambda e, bs=bs: e.reciprocal(out=bs, in_=bs), reads=[bT_a.r], writes=[bT_a.r])
        self.P.barrier()
        if self.cfg.get("test") == "gdn1":
            rr = Res("dbg2")
            for i_, t_ in enumerate((QF, KF, VF, ZF)):
                dma(SP, self.dbg2[i_], t_.ap, reads=t_.sub, writes=[rr])
            dma(SP, self.dbg3[:, 0], gT_a.ap, reads=[gT_a.r], writes=[rr])
            dma(SP, self.dbg3[:, 1], bT_a.ap, reads=[bT_a.r], writes=[rr])
            self.dbg.sub.append(rr)
            return
        self.off = self.base_off
        gT_o, bT_o = gT_a, bT_a
        ones32 = self.tile([128, 128], F32, "ones32b")
        gT = self.tile([128, NT, 16], F32, "gTb")
        bT = self.tile([128, NT, 16], F32, "bTb")
        op(DVE, lambda e: e.memset(ones32.ap, 1.0), writes=[ones32.r])
        op(DVE, lambda e: e.tensor_copy(out=gT.ap, in_=gT_o.ap), reads=[gT_o.r], writes=[gT.r])
        op(DVE, lambda e: e.tensor_copy(out=bT.ap, in_=bT_o.ap), reads=[bT_o.r], writes=[bT.r])
        self.P.barrier()
        def cmat(name, fill_in, pattern, cmult, cmp, fill):
            t = self.tile([128, 128], F32, name)
            op(POOL, lambda e: e.memset(t.ap, fill_in), writes=[t.r])
            op(POOL, lambda e: e.affine_select(out=t.ap, in_=t.ap, pattern=[[pattern, 128]], compare_op=cmp, fill=fill, base=0, channel_multiplier=cmult), reads=[t.r], writes=[t.r])
            return t
        triu32 = cmat("triu32", 1.0, 1, -1, ALU.is_ge, 0.0)
        tril32 = cmat("tril32", 1.0, -1, 1, ALU.is_ge, 0.0)
        sup = cmat("sup", 1.0, 1, -1, ALU.is_gt, 0.0)
        slo = cmat("slo", 1.0, -1, 1, ALU.is_gt, 0.0)
        mup = cmat("mup", 0.0, 1, -1, ALU.is_ge, -1e30)
        mlo = cmat("mlo", 0.0, -1, 1, ALU.is_ge, -1e30)
        i32 = self.ident32
        wo = self.tile([128, 8, D], BF16, "wo")
        dma(POOL, wo.ap, self.gdn_w_o[li].rearrange("(k p) n -> p k n", p=128), writes=[wo.r])
        def T4(nm):
            t_ = self.tile([128, 8, 128], F32, nm)
            t_.r = GRes(nm)
            return t_
        qfb = [T4("qf") for _ in range(2)]
        kfb = [T4("kf") for _ in range(2)]
        vfb = [T4("vf") for _ in range(2)]
        xfb = [T4("xf") for _ in range(2)]
        zfb = [T4("zf") for _ in range(2)]
        Rt, dTt, d2t, dec, decT, eGbc, AmT, An = (T4(nm) for nm in ("R", "dT", "d2", "dec", "decT", "eGbc", "AmT", "An"))
        kbg, ktail, vb = T4("kbg"), T4("ktail"), T4("vb")
        Mb = [T4("M0"), T4("M1")]
        Nb = [T4("N0"), T4("N1")]
        Pm, attnT, qdT, nwT, vnew, St = T4("P"), T4("attnT"), T4("qdT"), T4("nwT"), T4("vnew"), T4("S")
        PT = T4("PT")
        Mc, Nc = [Rt], [d2t]
        gm = []
        for gi_ in range(4):
            gt_ = self.tile([128, 128], F32, "gmask")
            dma(SP, gt_.ap, self.gmask_in[gi_], writes=[gt_.r])
            gm.append(gt_)
        Gtok = self.tile([128, 8], F32, "Gtok")
        eGtok = self.tile([128, 8], F32, "eGtok")
        bg = self.tile([128, 8], F32, "bg")
        yT = self.tile([128, 8, 128], BF16, "yT")
        ytile = [self.tile([128, D], F32, "ytile") for _ in range(2)]
        fl = lambda t, j: t.ap[:, 4 * j:4 * j + 4, :]
        pv4 = lambda ps: ps.ap.rearrange("p (a b) -> p a b", a=4)
        bc_h = lambda ap2, j: ap2[:, 4 * j:4 * j + 4].unsqueeze(2).to_broadcast([128, 4, 128])
        bc_m = lambda m, n_=4: m.ap.unsqueeze(1).to_broadcast([128, n_, 128])
        hview = lambda ap: ap.rearrange("(h p) t -> p h t", p=128)
        nch = 0
        for d in range(self.cfg.get("gdn_dirs", 2)):
            TRI, maskT, maskN, strT, strN, last = ((triu32, mup, mlo, sup, slo, 127), (tril32, mlo, mup, slo, sup, 0))[d]
            order = list(range(NT)) if d == 0 else [1, 0] + list(range(NT - 1, 1, -1))
            order = order[:self.cfg.get("gdn_nch", NT)]
            op(DVE, lambda e: e.memset(St.ap, 0.0), writes=[St.r])
            for n in order:
                cols = slice(n * 128, (n + 1) * 128)
                qf, kf, vf, xf, zf = qfb[nch % 2], kfb[nch % 2], vfb[nch % 2], xfb[nch % 2], zfb[nch % 2]
                nch += 1
                dma(SP, qf.ap, hview(QF.ap)[:, :, cols], reads=[QF.sub[n]], writes=[qf.r])
                dma(SP, kf.ap, hview(KF.ap)[:, :, cols], reads=[KF.sub[n]], writes=[kf.r])
                dma(SP, vf.ap, hview(VF.ap)[:, :, cols], reads=[VF.sub[n]], writes=[vf.r])
                if d == 1:
                    dma(SP, xf.ap, hview(OF.ap)[:, :, cols], reads=[OF.sub[n]], writes=[xf.r])
                    dma(SP, zf.ap, hview(ZF.ap)[:, :, cols], reads=[ZF.sub[n]], writes=[zf.r])
                gsl = gT.ap[:, n, d * 8:(d + 1) * 8]
                bsl = bT.ap[:, n, d * 8:(d + 1) * 8]
                psG = self.psum()
                op(PE, lambda e, psG=psG, TRI=TRI, gsl=gsl: e.matmul(psG.ap[:, 0:8], lhsT=TRI.ap, rhs=gsl, start=True, stop=True), reads=[TRI.r, gT.r], writes=psG.sub)
                op(DVE, lambda e, gsl=gsl, TRI=TRI: e.tensor_tensor(out=Rt.ap, in0=gsl.unsqueeze(2).to_broadcast([128, 8, 128]), in1=bc_m(TRI, 8), op=ALU.mult),
                   reads=[gT.r, TRI.r], writes=[Rt.r])
                psGb = [self.psum(), self.psum()]
                for j in range(2):
                    op(PE, lambda e, j=j, p=psGb[j]: e.matmul(p.ap, lhsT=ones32.ap, rhs=fl(Rt, j).rearrange("p a b -> p (a b)"), start=True, stop=True),
                       reads=[ones32.r, Rt.r], writes=psGb[j].sub)
                op(ACT, lambda e, psG=psG: e.copy(out=Gtok.ap, in_=psG.ap[:, 0:8]), reads=psG.sub, writes=[Gtok.r])
                for j in range(2):
                    op(DVE, lambda e, j=j, p=psGb[j]: e.tensor_tensor(out=fl(dTt, j), in0=pv4(p), in1=bc_h(Gtok.ap, j), op=ALU.subtract), reads=psGb[j].sub + [Gtok.r], writes=[dTt.r])
                    op(ACT, lambda e, j=j, p=psGb[j]: e.activation(out=fl(eGbc, j), in_=pv4(p), func=AF.Exp), reads=psGb[j].sub, writes=[eGbc.r])
                op(DVE, lambda e, maskN=maskN: e.scalar_tensor_tensor(out=d2t.ap, in0=dTt.ap, scalar=-1.0, in1=bc_m(maskN, 8), op0=ALU.mult, op1=ALU.add),
                   reads=[dTt.r, maskN.r], writes=[d2t.r])
                op(POOL, lambda e, maskT=maskT: e.tensor_tensor(out=dTt.ap, in0=dTt.ap, in1=bc_m(maskT, 8), op=ALU.add), reads=[dTt.r, maskT.r, d2t.r], writes=[dTt.r])
                op(ACT, lambda e: e.activation(out=dec.ap, in_=d2t.ap, func=AF.Exp), reads=[d2t.r], writes=[dec.r])
                op(ACT, lambda e: e.activation(out=decT.ap, in_=dTt.ap, func=AF.Exp), reads=[dTt.r], writes=[decT.r])
                op(ACT, lambda e: e.activation(out=eGtok.ap, in_=Gtok.ap, func=AF.Exp), reads=[Gtok.r], writes=[eGtok.r])
                op(DVE, lambda e, bsl=bsl: e.tensor_tensor(out=bg.ap, in0=eGtok.ap, in1=bsl, op=ALU.mult), reads=[eGtok.r, bT.r], writes=[bg.r])
                op(DVE, lambda e, bsl=bsl: e.tensor_tensor(out=Rt.ap, in0=bsl.unsqueeze(2).to_broadcast([128, 8, 128]), in1=bc_m(i32, 8), op=ALU.mult),
                   reads=[bT.r, i32.r], writes=[Rt.r])
                psBb = [self.psum(), self.psum()]
                for j in range(2):
                    op(PE, lambda e, j=j, p=psBb[j]: e.matmul(p.ap, lhsT=ones32.ap, rhs=fl(Rt, j).rearrange("p a b -> p (a b)"), start=True, stop=True),
                       reads=[ones32.r, Rt.r], writes=psBb[j].sub)
                for j in range(2):
                    op(DVE, lambda e, j=j, p=psBb[j]: e.tensor_tensor(out=fl(AmT, j), in0=fl(decT, j), in1=pv4(p), op=ALU.mult), reads=psBb[j].sub + [decT.r], writes=[AmT.r])
                op(POOL, lambda e, strT=strT: e.tensor_tensor(out=AmT.ap, in0=AmT.ap, in1=bc_m(strT, 8), op=ALU.mult), reads=[AmT.r, strT.r], writes=[AmT.r])
                op(POOL, lambda e, bsl=bsl: e.tensor_tensor(out=An.ap, in0=dec.ap, in1=bsl.unsqueeze(2).to_broadcast([128, 8, 128]), op=ALU.mult), reads=[dec.r, bT.r], writes=[An.r])
                op(POOL, lambda e, strN=strN: e.tensor_tensor(out=An.ap, in0=An.ap, in1=bc_m(strN, 8), op=ALU.mult), reads=[An.r, strN.r], writes=[An.r])
                op(POOL, lambda e, qf=qf: e.tensor_tensor(out=qdT.ap, in0=qf.ap, in1=eGbc.ap, op=ALU.mult), reads=[qf.r, eGbc.r], writes=[qdT.r])
                if self.cfg.get('gdn_stage', 99) < 1:
                    continue
                psK = [self.psum(), self.psum()]
                psV = [self.psum(), self.psum()]
                for h in range(8):
                    j, q4 = h // 4, (h % 4) * 128
                    op(PE, lambda e, h=h, p=psK[j], q4=q4, kf=kf: e.transpose(out=p.ap[:, q4:q4 + 128], in_=kf.ap[:, h, :], identity=i32.ap), reads=[kf.r, i32.r], writes=[psK[j].sub[h % 4]])
                    op(PE, lambda e, h=h, p=psV[j], q4=q4, vf=vf: e.transpose(out=p.ap[:, q4:q4 + 128], in_=vf.ap[:, h, :], identity=i32.ap), reads=[vf.r, i32.r], writes=[psV[j].sub[h % 4]])
                for j in range(2):
                    op(DVE, lambda e, j=j, p=psK[j]: e.tensor_tensor(out=fl(kbg, j), in0=pv4(p), in1=bc_h(bg.ap, j), op=ALU.mult), reads=psK[j].sub + [bg.r], writes=[kbg.r])
                    op(DVE, lambda e, j=j, p=psK[j], last=last: e.tensor_tensor(out=fl(ktail, j), in0=pv4(p), in1=decT.ap[:, 4 * j:4 * j + 4, last:last + 1].to_broadcast([128, 4, 128]), op=ALU.mult),
                       reads=psK[j].sub + [decT.r], writes=[ktail.r])
                    op(DVE, lambda e, j=j, p=psV[j], bsl=bsl: e.tensor_tensor(out=fl(vb, j), in0=pv4(p), in1=bc_h(bsl, j), op=ALU.mult), reads=psV[j].sub + [bT.r], writes=[vb.r])
                if self.cfg.get('gdn_stage', 99) < 2:
                    continue
                psKK = [self.psum(), self.psum()]
                psKQ = [self.psum(), self.psum()]
                for h in range(8):
                    j, q4 = h // 4, (h % 4) * 128
                    op(PE, lambda e, h=h, p=psKK[j], q4=q4, kf=kf: e.matmul(p.ap[:, q4:q4 + 128], lhsT=kf.ap[:, h, :], rhs=kf.ap[:, h, :], start=True, stop=True), reads=[kf.r], writes=[psKK[j].sub[h % 4]])
                    op(PE, lambda e, h=h, p=psKQ[j], q4=q4, kf=kf, qf=qf: e.matmul(p.ap[:, q4:q4 + 128], lhsT=kf.ap[:, h, :], rhs=qf.ap[:, h, :], start=True, stop=True), reads=[kf.r, qf.r], writes=[psKQ[j].sub[h % 4]])
                M0, N0 = Mb[0], Nb[0]
                for j in range(2):
                    op(DVE, lambda e, j=j, p=psKK[j]: e.scalar_tensor_tensor(out=fl(M0, j), in0=pv4(p), scalar=-1.0, in1=fl(AmT, j), op0=ALU.mult, op1=ALU.mult), reads=psKK[j].sub + [AmT.r], writes=[M0.r])
                    op(DVE, lambda e, j=j, p=psKK[j]: e.scalar_tensor_tensor(out=fl(N0, j), in0=pv4(p), scalar=-1.0, in1=fl(An, j), op0=ALU.mult, op1=ALU.mult), reads=psKK[j].sub + [An.r], writes=[N0.r])
                    op(DVE, lambda e, j=j, p=psKQ[j]: e.tensor_tensor(out=fl(attnT, j), in0=pv4(p), in1=fl(decT, j), op=ALU.mult), reads=psKQ[j].sub + [decT.r], writes=[attnT.r])
                MA, NA, MB, NB = Mb[1], Nb[1], Mc[0], Nc[0]
                op(DVE, lambda e: e.tensor_tensor(out=MA.ap, in0=M0.ap, in1=bc_m(gm[0], 8), op=ALU.mult), reads=[M0.r, gm[0].r], writes=[MA.r])
                op(POOL, lambda e: e.tensor_tensor(out=NA.ap, in0=N0.ap, in1=bc_m(gm[0], 8), op=ALU.mult), reads=[N0.r, gm[0].r], writes=[NA.r])
                op(POOL, lambda e: e.tensor_tensor(out=Pm.ap, in0=MA.ap, in1=bc_m(i32, 8), op=ALU.add), reads=[MA.r, i32.r], writes=[Pm.r])
                op(POOL, lambda e: e.tensor_tensor(out=PT.ap, in0=NA.ap, in1=bc_m(i32, 8), op=ALU.add), reads=[NA.r, i32.r], writes=[PT.r])
                cur = (MA, NA)
                nxt = (MB, NB)
                for kk in range(1, 4):
                    Mp, Np = cur
                    Mn, Nn = nxt
                    psN = [self.psum(), self.psum()]
                    psM = [self.psum(), self.psum()]
                    for h in range(8):
                        j, q4 = h // 4, (h % 4) * 128
                        op(PE, lambda e, h=h, p=psN[j], q4=q4, Mp=Mp, Np=Np: e.matmul(p.ap[:, q4:q4 + 128], lhsT=Mp.ap[:, h, :], rhs=Np.ap[:, h, :], start=True, stop=True),
                           reads=[Mp.r, Np.r], writes=[psN[j].sub[h % 4]])
                        op(PE, lambda e, h=h, p=psM[j], q4=q4, Mp=Mp, Np=Np: e.matmul(p.ap[:, q4:q4 + 128], lhsT=Np.ap[:, h, :], rhs=Mp.ap[:, h, :], start=True, stop=True),
                           reads=[Mp.r, Np.r], writes=[psM[j].sub[h % 4]])
                    op(ACT, lambda e, j=0, p=psN[0], Nn=Nn: e.copy(out=fl(Nn, j), in_=pv4(p)), reads=psN[0].sub, writes=[Nn.r])
                    op(DVE, lambda e, j=1, p=psN[1], Nn=Nn: e.tensor_copy(out=fl(Nn, j), in_=pv4(p)), reads=psN[1].sub, writes=[Nn.r])
                    op(DVE, lambda e, j=0, p=psM[0], Mn=Mn: e.tensor_copy(out=fl(Mn, j), in_=pv4(p)), reads=psM[0].sub, writes=[Mn.r])
                    op(ACT, lambda e, j=1, p=psM[1], Mn=Mn: e.copy(out=fl(Mn, j), in_=pv4(p)), reads=psM[1].sub, writes=[Mn.r])
                    psP = [self.psum(), self.psum()]
                    psQ_ = [self.psum(), self.psum()]
                    for h in range(8):
                        j, q4 = h // 4, (h % 4) * 128
                        op(PE, lambda e, h=h, p=psP[j], q4=q4, Nn=Nn: e.matmul(p.ap[:, q4:q4 + 128], lhsT=Nn.ap[:, h, :], rhs=Pm.ap[:, h, :], start=True, stop=True),
                           reads=[Nn.r, Pm.r], writes=[psP[j].sub[h % 4]])
                        op(PE, lambda e, h=h, p=psQ_[j], q4=q4, Mn=Mn: e.matmul(p.ap[:, q4:q4 + 128], lhsT=Mn.ap[:, h, :], rhs=PT.ap[:, h, :], start=True, stop=True),
                           reads=[Mn.r, PT.r], writes=[psQ_[j].sub[h % 4]])
                    for j in range(2):
                        op(DVE, lambda e, j=j, p=psP[j]: e.tensor_tensor(out=fl(Pm, j), in0=fl(Pm, j), in1=pv4(p), op=ALU.add), reads=psP[j].sub + [Pm.r], writes=[Pm.r])
                        op(DVE, lambda e, j=j, p=psQ_[j]: e.tensor_tensor(out=fl(PT, j), in0=fl(PT, j), in1=pv4(p), op=ALU.add), reads=psQ_[j].sub + [PT.r], writes=[PT.r])
                    cur, nxt = nxt, cur
                for lv in range(3):
                    UoT, Yt = Mc[0], Nc[0]
                    op(DVE, lambda e, lv=lv, UoT=UoT: e.scalar_tensor_tensor(out=UoT.ap, in0=N0.ap, scalar=-1.0, in1=bc_m(gm[1 + lv], 8), op0=ALU.mult, op1=ALU.mult),
                       reads=[N0.r, gm[1 + lv].r], writes=[UoT.r])
                    psY = [self.psum(), self.psum()]
                    for h in range(8):
                        j, q4 = h // 4, (h % 4) * 128
                        op(PE, lambda e, h=h, p=psY[j], q4=q4, UoT=UoT: e.matmul(p.ap[:, q4:q4 + 128], lhsT=UoT.ap[:, h, :], rhs=Pm.ap[:, h, :], start=True, stop=True),
                           reads=[UoT.r, Pm.r], writes=[psY[j].sub[h % 4]])
                    op(ACT, lambda e, j=0, p=psY[0], Yt=Yt: e.copy(out=fl(Yt, j), in_=pv4(p)), reads=psY[0].sub, writes=[Yt.r])
                    op(DVE, lambda e, j=1, p=psY[1], Yt=Yt: e.tensor_copy(out=fl(Yt, j), in_=pv4(p)), reads=psY[1].sub, writes=[Yt.r])
                    psX = [self.psum(), self.psum()]
                    psXT = [self.psum(), self.psum()]
                    for h in range(8):
                        j, q4 = h // 4, (h % 4) * 128
                        op(PE, lambda e, h=h, p=psX[j], q4=q4, Yt=Yt: e.matmul(p.ap[:, q4:q4 + 128], lhsT=PT.ap[:, h, :], rhs=Yt.ap[:, h, :], start=True, stop=True),
                           reads=[PT.r, Yt.r], writes=[psX[j].sub[h % 4]])
                        if lv < 2:
                            op(PE, lambda e, h=h, p=psXT[j], q4=q4, Yt=Yt: e.matmul(p.ap[:, q4:q4 + 128], lhsT=Yt.ap[:, h, :], rhs=PT.ap[:, h, :], start=True, stop=True),
                               reads=[PT.r, Yt.r], writes=[psXT[j].sub[h % 4]])
                    for j in range(2):
                        op(DVE, lambda e, j=j, p=psX[j]: e.tensor_tensor(out=fl(Pm, j), in0=fl(Pm, j), in1=pv4(p), op=ALU.subtract), reads=psX[j].sub + [Pm.r], writes=[Pm.r])
                        if lv < 2:
                            op(DVE, lambda e, j=j, p=psXT[j]: e.tensor_tensor(out=fl(PT, j), in0=fl(PT, j), in1=pv4(p), op=ALU.subtract), reads=psXT[j].sub + [PT.r], writes=[PT.r])
                if self.cfg.get('gdn_stage', 99) < 4:
                    continue
                psW = [self.psum(), self.psum()]
                for h in range(8):
                    j, q4 = h // 4, (h % 4) * 128
                    op(PE, lambda e, h=h, p=psW[j], q4=q4: e.matmul(p.ap[:, q4:q4 + 128], lhsT=kbg.ap[:, h, :], rhs=Pm.ap[:, h, :], start=True, stop=True), reads=[kbg.r, Pm.r], writes=[psW[j].sub[h % 4]])
                for j in range(2):
                    op(ACT, lambda e, j=j, p=psW[j]: e.mul(out=fl(nwT, j), in_=pv4(p), mul=-1.0), reads=psW[j].sub, writes=[nwT.r])
                psVn = [self.psum(), self.psum()]
                for h in range(8):
                    j, q4 = h // 4, (h % 4) * 128
                    op(PE, lambda e, h=h, p=psVn[j], q4=q4: e.matmul(p.ap[:, q4:q4 + 128], lhsT=Pm.ap[:, h, :], rhs=vb.ap[:, h, :], start=True, stop=False), reads=[Pm.r, vb.r], writes=[psVn[j].sub[h % 4]])
                    op(PE, lambda e, h=h, p=psVn[j], q4=q4: e.matmul(p.ap[:, q4:q4 + 128], lhsT=nwT.ap[:, h, :], rhs=St.ap[:, h, :], start=False, stop=True), reads=[nwT.r, St.r], writes=[psVn[j].sub[h % 4]])
                op(ACT, lambda e, j=0, p=psVn[0]: e.copy(out=fl(vnew, j), in_=pv4(p)), reads=psVn[0].sub, writes=[vnew.r])
                op(DVE, lambda e, j=1, p=psVn[1]: e.tensor_copy(out=fl(vnew, j), in_=pv4(p)), reads=psVn[1].sub, writes=[vnew.r])
                if self.cfg.get('gdn_stage', 99) < 5:
                    continue
                psO = [self.psum(), self.psum()]
                psS = [self.psum(), self.psum()]
                for h in range(8):
                    j, q4 = h // 4, (h % 4) * 128
                    op(PE, lambda e, h=h, p=psO[j], q4=q4: e.matmul(p.ap[:, q4:q4 + 128], lhsT=St.ap[:, h, :], rhs=qdT.ap[:, h, :], start=True, stop=False), reads=[St.r, qdT.r], writes=[psO[j].sub[h % 4]])
                    op(PE, lambda e, h=h, p=psO[j], q4=q4: e.matmul(p.ap[:, q4:q4 + 128], lhsT=vnew.ap[:, h, :], rhs=attnT.ap[:, h, :], start=False, stop=True), reads=[vnew.r, attnT.r], writes=[psO[j].sub[h % 4]])
                    op(PE, lambda e, h=h, p=psS[j], q4=q4: e.matmul(p.ap[:, q4:q4 + 128], lhsT=ktail.ap[:, h, :], rhs=vnew.ap[:, h, :], start=True, stop=True), reads=[ktail.r, vnew.r], writes=[psS[j].sub[h % 4]])
                for j in range(2):
                    op(DVE, lambda e, j=j, last=last: e.tensor_tensor(out=fl(St, j), in0=fl(St, j), in1=eGbc.ap[:, 4 * j:4 * j + 4, last:last + 1].to_broadcast([128, 4, 128]), op=ALU.mult),
                       reads=[St.r, eGbc.r], writes=[St.r])
                    op(DVE, lambda e, j=j, p=psS[j]: e.tensor_tensor(out=fl(St, j), in0=fl(St, j), in1=pv4(p), op=ALU.add), reads=psS[j].sub + [St.r], writes=[St.r])
                if d == 0:
                    for j in range(2):
                        op(ACT, lambda e, j=j, p=psO[j], xf=xf: e.copy(out=fl(xf, j), in_=pv4(p)), reads=psO[j].sub, writes=[xf.r])
                    dma(SP, hview(OF.ap)[:, :, cols], xf.ap, reads=[xf.r], writes=[OF.sub[n]])
                    continue
                for j in range(2):
                    op(DVE, lambda e, j=j, p=psO[j], xf=xf: e.tensor_tensor(out=fl(xf, j), in0=fl(xf, j), in1=pv4(p), op=ALU.add), reads=psO[j].sub + [xf.r], writes=[xf.r])
                op(POOL, lambda e, xf=xf: e.tensor_tensor(out=Rt.ap, in0=xf.ap, in1=xf.ap, op=ALU.mult), reads=[xf.r], writes=[Rt.r])
                psQ = [self.psum(), self.psum()]
                for j in range(2):
                    op(PE, lambda e, j=j, p=psQ[j]: e.matmul(p.ap, lhsT=ones32.ap, rhs=fl(Rt, j).rearrange("p a b -> p (a b)"), start=True, stop=True), reads=[ones32.r, Rt.r], writes=psQ[j].sub)
                for j in range(2):
                    op(DVE, lambda e, j=j, p=psQ[j]: e.tensor_scalar(out=fl(d2t, j), in0=pv4(p), scalar1=1.0 / 128.0, scalar2=LN_EPS, op0=ALU.mult, op1=ALU.add), reads=psQ[j].sub, writes=[d2t.r])
                op(ACT, lambda e: e.activation(out=d2t.ap, in_=d2t.ap, func=AF.Ln), reads=[d2t.r], writes=[d2t.r])
                op(ACT, lambda e: e.activation(out=d2t.ap, in_=d2t.ap, func=AF.Exp, scale=-0.5), reads=[d2t.r], writes=[d2t.r])
                op(POOL, lambda e, xf=xf: e.tensor_tensor(out=xf.ap, in0=xf.ap, in1=d2t.ap, op=ALU.mult), reads=[xf.r, d2t.r], writes=[xf.r])
                op(POOL, lambda e, xf=xf, zf=zf: e.tensor_tensor(out=yT.ap, in0=xf.ap, in1=zf.ap, op=ALU.mult), reads=[xf.r, zf.r], writes=[yT.r])
                yt = ytile[n % 2]
                for half in range(2):
                    ps = self.psum()
                    for h in range(8):
                        op(PE, lambda e, ps=ps, h=h, half=half: e.matmul(ps.ap, lhsT=yT.ap[:, h, :], rhs=wo.ap[:, h, half * 512:(half + 1) * 512], start=(h == 0), stop=(h == 7)),
                           reads=[yT.r, wo.r], writes=ps.sub)
                    op(ACT, lambda e, ps=ps, yt=yt, half=half: e.copy(out=yt.ap[:, half * 512:(half + 1) * 512], in_=ps.ap), reads=ps.sub, writes=[yt.r])
                dma(SP, self.Y.ap[cols, :], yt.ap, reads=[yt.r], writes=[self.Y.sub[n]])
                if self.cfg.get("test") == "mix":
                    dma(SP, self.dbg.ap[cols, :], yt.ap, reads=[yt.r], writes=[self.dbg.sub[n]])
        if self.cfg.get("test") == "gdn2":
            self.P.barrier()
            rr = Res("dbg2")
            dma(SP, self.dbg2[0], VF.ap if self.cfg.get('gdn_nch') == 1 else OF.ap, reads=OF.sub + VF.sub, writes=[rr])
            dma(SP, self.dbg2[1, 0:128, 0:1024], St.ap.rearrange("p a b -> p (a b)"), reads=[St.r], writes=[rr])
            for ii, tt in enumerate((decT, dec, AmT, An, Pm, vnew, attnT, kbg, vb, ktail, qdT, eGbc, nwT, qfb[0], kfb[0], vfb[0])):
                dma(SP, self.dbg2[2 + ii // 8, (ii % 8) * 128:(ii % 8 + 1) * 128, 0:1024], tt.ap.rearrange("p a b -> p (a b)"), reads=[tt.r], writes=[rr])
            dma(SP, self.dbg3[:, 0], gT.ap, reads=[gT.r], writes=[rr])
            dma(SP, self.dbg3[:, 1], bT.ap, reads=[bT.r], writes=[rr])
            self.dbg.sub.append(rr)

    def out_proj(self, w_o):
        op, dma = self.op, self.dma
        self.P.barrier()
        self.off = self.base_off
        OD = self.OD
        wo = self.tile([128, 8, D], BF16, "wo")
        dma(POOL, wo.ap, w_o.rearrange("(k p) n -> p k n", p=128), writes=[wo.r])
        ob = [self.tile([128, D], BF16, "ob") for _ in range(3)]
        oT = [self.tile([128, 8, 128], BF16, "oT", nsub=1) for _ in range(2)]
        yb = [self.tile([128, D], F32, "yb") for _ in range(2)]
        for i in range(NT):
            o, t, y = ob[i % 3], oT[i % 2], yb[i % 2]
            rows = slice(i * 128, (i + 1) * 128)
            dma(SP, o.ap, OD.ap[rows, :], reads=[OD.sub[i]], writes=[o.r])
            self.transpose_to(o, t, 0)
            for half in range(2):
                ps = self.psum()
                for k in range(8):
                    op(PE, lambda e, ps=ps, t=t, k=k, half=half: e.matmul(ps.ap, lhsT=t.ap[:, k, :], rhs=wo.ap[:, k, half * 512:(half + 1) * 512], start=(k == 0), stop=(k == 7)),
                       reads=[t.sub[0], wo.r], writes=[ps.r])
                op(ACT if half else DVE, (lambda e, ps=ps, y=y, half=half: e.copy(out=y.ap[:, half * 512:(half + 1) * 512], in_=ps.ap)) if half else
                   (lambda e, ps=ps, y=y, half=half: e.tensor_copy(out=y.ap[:, half * 512:(half + 1) * 512], in_=ps.ap)), reads=[ps.r], writes=[y.r])
            dma(SP, self.Y.ap[rows, :], y.ap, reads=[y.r], writes=[self.Y.sub[i]])
            if self.cfg.get("test") == "mix":
                dma(SP, self.dbg.ap[rows, :], y.ap, reads=[y.r], writes=[self.dbg.sub[i]])

    def postmix(self, l):
        op, dma = self.op, self.dma
        self.phase_begin()
        self.h2tok = self.tile([128, NT, D], BF16, "h2tok", nsub=NT)
        self.affTok = self.tile([128, NT, NE], F32, "affTok")
        h2tok, affTok = self.h2tok, self.affTok
        self.post_keep = self.off
        mods = self.mod_tiles(l, [2, 3, 4], plus1=(4,))
        g0 = self.ln_vec(self.ln_g[l, 0:1, :])
        b0 = self.ln_vec(self.ln_b[l, 0:1, :])
        wr = self.tile([128, 8, NE], BF16, "wr")
        dma(POOL, wr.ap, self.moe_w_router[l].rearrange("(k p) n -> p k n", p=128), writes=[wr.r])
        xb = [self.tile([128, D], F32, "xb") for _ in range(2)]
        yb = [self.tile([128, D], F32, "yb") for _ in range(2)]
        ob = [self.tile([128, D], F32, "ob") for _ in range(2)]
        h2T = [self.tile([128, 8, 128], BF16, "h2T", nsub=1) for _ in range(2)]
        st = self.tile([128, 2, 6], F32, "st")
        mv = self.tile([128, 4], F32, "mv")
        sm = self.tile([128, 4], F32, "sm")
        ex = self.tile([128, NE], F32, "ex")
        for i in range(NT):
            x, y, o = xb[i % 2], yb[i % 2], ob[i % 2]
            c = 1 if i < 2 else 0
            rows = slice(i * 128, (i + 1) * 128)
            dma(SP, x.ap, self.XS.ap[rows, :], reads=[self.XS.sub[i]], writes=[x.r])
            dma(SP, y.ap, self.Y.ap[rows, :], reads=[self.Y.sub[i]], writes=[y.r])
            m2, m3, m4 = mods[2][c], mods[3][c], mods[4][c]
            op(POOL, lambda e, y=y, m2=m2: e.tensor_tensor(out=y.ap, in0=y.ap, in1=m2.ap, op=ALU.mult), reads=[y.r, m2.r], writes=[y.r])
            op(DVE, lambda e, x=x, y=y: e.scalar_tensor_tensor(out=y.ap, in0=x.ap, scalar=ALPHA, in1=y.ap, op0=ALU.mult, op1=ALU.add),
               reads=[x.r, y.r], writes=[y.r])
            self.layernorm_(y, st, mv)
            op(POOL, lambda e, y=y, o=o: e.tensor_tensor(out=o.ap, in0=y.ap, in1=g0.ap, op=ALU.mult), reads=[y.r, g0.r], writes=[o.r])
            op(POOL, lambda e, o=o: e.tensor_tensor(out=o.ap, in0=o.ap, in1=b0.ap, op=ALU.add), reads=[o.r, b0.r], writes=[o.r])
            dma(SP, self.XS.ap[rows, :], o.ap, reads=[o.r], writes=[self.XS.sub[i]])
            op(DVE, lambda e, o=o, x=x, m4=m4: e.tensor_tensor(out=x.ap, in0=o.ap, in1=m4.ap, op=ALU.mult), reads=[o.r, m4.r], writes=[x.r])
            hv = Tl(h2tok.ap[:, i, :])
            hv.r = h2tok.sub[i]
            op(DVE, lambda e, x=x, m3=m3, hv=hv: e.tensor_tensor(out=hv.ap, in0=x.ap, in1=m3.ap, op=ALU.add), reads=[x.r, m3.r], writes=[hv.r])
            ht = h2T[i % 2]
            self.transpose_to(hv, ht, 0)
            ps = self.psum()
            for k in range(8):
                op(PE, lambda e, ps=ps, ht=ht, k=k: e.matmul(ps.ap[:, 0:NE], lhsT=ht.ap[:, k, :], rhs=wr.ap[:, k, :], start=(k == 0), stop=(k == 7)),
                   reads=[ht.sub[0], wr.r], writes=[ps.r])
            op(DVE, lambda e, ps=ps: e.reduce_max(out=sm.ap[:, 0:1], in_=ps.ap[:, 0:NE], axis=AX.X), reads=[ps.r], writes=[sm.r])
            op(DVE, lambda e: e.tensor_scalar(out=sm.ap[:, 1:2], in0=sm.ap[:, 0:1], scalar1=-1.0, scalar2=None, op0=ALU.mult), reads=[sm.r], writes=[sm.r])
            op(ACT, lambda e, ps=ps: e.activation(out=ex.ap, in_=ps.ap[:, 0:NE], func=AF.Exp, bias=sm.ap[:, 1:2], scale=1.0, accum_out=sm.ap[:, 2:3]),
               reads=[ps.r, sm.r], writes=[ex.r, sm.r])
            op(DVE, lambda e: e.reciprocal(out=sm.ap[:, 3:4], in_=sm.ap[:, 2:3]), reads=[sm.r], writes=[sm.r])
            op(DVE, lambda e, i=i: e.tensor_scalar(out=affTok.ap[:, i, :], in0=ex.ap, scalar1=sm.ap[:, 3:4], scalar2=None, op0=ALU.mult),
               reads=[ex.r, sm.r], writes=[affTok.r])

    def moe_select(self, l):
        op, dma = self.op, self.dma
        affTok = self.affTok
        self.P.barrier()
        self.off = self.post_keep
        self.posm = self.tile([128, NT, NE], F32, "posm")
        posm = self.posm
        keep2 = self.off
        affT = self.tile([NE, TT], F32, "affT")
        work = self.tile([NE, 4096], F32, "work")
        m8 = self.tile([NE, 8], F32, "m8")
        thr = self.tile([NE, 2], F32, "thr")
        maskT = self.tile([NE, TT], BF16, "maskT")
        for g in range(9):
            ps = self.psum()
            tl = list(range(g * 4, min(g * 4 + 4, NT)))
            for j, i in enumerate(tl):
                op(PE, lambda e, ps=ps, i=i, j=j: e.transpose(out=ps.ap[0:NE, j * 128:(j + 1) * 128], in_=affTok.ap[:, i, :], identity=self.ident32.ap),
                   reads=[affTok.r, self.ident32.r], writes=[ps.r])
            n = len(tl) * 128
            op(ACT, lambda e, ps=ps, g=g, n=n: e.copy(out=affT.ap[:, g * 512:g * 512 + n], in_=ps.ap[0:NE, 0:n]), reads=[ps.r], writes=[affT.r])
        dma(SP, self.AFFT.ap, affT.ap, reads=[affT.r], writes=[self.AFFT.r])
        op(DVE, lambda e: e.tensor_copy(out=work.ap[:, 0:256], in_=affT.ap[:, 0:256]), reads=[affT.r], writes=[work.r])
        for it in range(4):
            op(DVE, lambda e: e.max(out=m8.ap, in_=work.ap[:, 0:256]), reads=[work.r], writes=[m8.r])
            if it < 3:
                op(DVE, lambda e: e.match_replace(out=work.ap[:, 0:256], in_to_replace=m8.ap, in_values=work.ap[:, 0:256], imm_value=-1.0),
                   reads=[work.r, m8.r], writes=[work.r])
        op(DVE, lambda e: e.tensor_copy(out=thr.ap[:, 0:1], in_=m8.ap[:, 7:8]), reads=[m8.r], writes=[thr.r])
        if not hasattr(self, "CAND"):
            self.CAND = Tl(self.dram("CAND", [128, 128], F32), "CAND")
        w1 = self.tile([128, 512], F32, "w1")
        c1 = self.tile([128, 128], F32, "c1")
        for e_ in range(NE):
            dma(SP, w1.ap[e_ * 8:(e_ + 1) * 8, :], self.AFFT.ap[e_, 256:TT].rearrange("(s t) -> s t", s=8), reads=[self.AFFT.r], writes=[w1.r])
        for it in range(16):
            op(DVE, lambda e, it=it: e.max(out=c1.ap[:, it * 8:(it + 1) * 8], in_=w1.ap), reads=[w1.r], writes=[c1.r])
            if it < 15:
                op(DVE, lambda e, it=it: e.match_replace(out=w1.ap, in_to_replace=c1.ap[:, it * 8:(it + 1) * 8], in_values=w1.ap, imm_value=-1.0),
                   reads=[w1.r, c1.r], writes=[w1.r])
        dma(SP, self.CAND.ap, c1.ap, reads=[c1.r], writes=[self.CAND.r])
        dma(SP, work.ap[:, 0:1024], self.CAND.ap.rearrange("(e s) c -> e (s c)", s=8), reads=[self.CAND.r], writes=[work.r])
        for it in range(64):
            op(DVE, lambda e: e.max(out=m8.ap, in_=work.ap[:, 0:1024]), reads=[work.r], writes=[m8.r])
            if it < 63:
                op(DVE, lambda e: e.match_replace(out=work.ap[:, 0:1024], in_to_replace=m8.ap, in_values=work.ap[:, 0:1024], imm_value=-1.0),
                   reads=[work.r, m8.r], writes=[work.r])
        op(DVE, lambda e: e.tensor_copy(out=thr.ap[:, 1:2], in_=m8.ap[:, 7:8]), reads=[m8.r], writes=[thr.r])
        for (lo, n, col) in ((0, 256, 0), (256, 4096, 1)):
            op(DVE, lambda e, lo=lo, n=n, col=col: e.tensor_scalar(out=maskT.ap[:, lo:lo + n], in0=affT.ap[:, lo:lo + n], scalar1=thr.ap[:, col:col + 1],
                                                                   scalar2=None, op0=ALU.is_ge), reads=[affT.r, thr.r], writes=[maskT.r])
        maskTok = self.tile([128, NT, NE], BF16, "maskTok")
        maskF = self.tile([128, NT, NE], F32, "maskF")
        ps = self.psum()
        pb = ps.ap.bitcast(BF16)
        for i in range(NT):
            op(PE, lambda e, i=i, pb=pb: e.transpose(out=pb[:, i * NE:(i + 1) * NE], in_=maskT.ap[:, i * 128:(i + 1) * 128], identity=self.ident.ap[0:NE, 0:NE]),
               reads=[maskT.r, self.ident.r], writes=[ps.r])
        op(DVE, lambda e, pb=pb: e.tensor_copy(out=maskTok.ap.rearrange("p a b -> p (a b)"), in_=pb[:, 0:NT * NE]), reads=[ps.r], writes=[maskTok.r])
        op(DVE, lambda e: e.tensor_copy(out=maskF.ap, in_=maskTok.ap), reads=[maskTok.r], writes=[maskF.r])
        mflat = maskTok.ap.rearrange("p a b -> p (a b)")
        ps_w, ps_t, ps_c = self.psum(), self.psum(), self.psum()
        op(PE, lambda e: e.matmul(ps_w.ap, lhsT=self.triu.ap, rhs=mflat[:, 2 * NE:NT * NE], start=True, stop=True), reads=[maskTok.r, self.triu.r], writes=[ps_w.r])
        op(PE, lambda e: e.matmul(ps_t.ap, lhsT=self.ones.ap, rhs=mflat[:, 2 * NE:NT * NE], start=True, stop=True), reads=[maskTok.r, self.ones.r], writes=[ps_t.r])
        op(PE, lambda e: e.matmul(ps_c.ap[:, 0:2 * NE], lhsT=self.triu.ap, rhs=mflat[:, 0:2 * NE], start=True, stop=True), reads=[maskTok.r, self.triu.r], writes=[ps_c.r])
        op(PE, lambda e: e.matmul(ps_c.ap[:, 2 * NE:4 * NE], lhsT=self.ones.ap, rhs=mflat[:, 0:2 * NE], start=True, stop=True), reads=[maskTok.r, self.ones.r], writes=[ps_c.r])
        eoff = self.tile([128, NT, NE], F32, "eoff")
        tot = self.tile([128, NT, NE], F32, "tot")
        op(DVE, lambda e: e.tensor_copy(out=tot.ap[:, 2:NT, :].rearrange("p a b -> p (a b)"), in_=ps_t.ap), reads=[ps_t.r], writes=[tot.r])
        op(DVE, lambda e: e.tensor_copy(out=tot.ap[:, 0:2, :].rearrange("p a b -> p (a b)"), in_=ps_c.ap[:, 2 * NE:4 * NE]), reads=[ps_c.r], writes=[tot.r])
        op(DVE, lambda e: e.memset(eoff.ap, 0.0), writes=[eoff.r])
        op(DVE, lambda e: e.tensor_copy(out=eoff.ap[:, 1, :], in_=tot.ap[:, 0, :]), reads=[tot.r], writes=[eoff.r])
        for i in range(3, NT):
            op(DVE, lambda e, i=i: e.tensor_tensor(out=eoff.ap[:, i, :], in0=eoff.ap[:, i - 1, :], in1=tot.ap[:, i - 1, :], op=ALU.add),
               reads=[eoff.r, tot.r], writes=[eoff.r])
        op(DVE, lambda e: e.tensor_tensor(out=posm.ap[:, 2:NT, :].rearrange("p a b -> p (a b)"), in0=ps_w.ap,
                                          in1=eoff.ap[:, 2:NT, :].rearrange("p a b -> p (a b)"), op=ALU.add), reads=[ps_w.r, eoff.r], writes=[posm.r])
        op(DVE, lambda e: e.tensor_tensor(out=posm.ap[:, 0:2, :].rearrange("p a b -> p (a b)"), in0=ps_c.ap[:, 0:2 * NE],
                                          in1=eoff.ap[:, 0:2, :].rearrange("p a b -> p (a b)"), op=ALU.add), reads=[ps_c.r, eoff.r], writes=[posm.r])
        op(DVE, lambda e: e.tensor_tensor(out=posm.ap, in0=posm.ap, in1=maskF.ap, op=ALU.mult), reads=[posm.r, maskF.r], writes=[posm.r])
        op(DVE, lambda e: e.tensor_scalar(out=posm.ap, in0=posm.ap, scalar1=-1.0, scalar2=None, op0=ALU.add), reads=[posm.r], writes=[posm.r])
        posT = self.tile([NE, TT], F32, "posT")
        for g in range(9):
            ps = self.psum()
            tl = list(range(g * 4, min(g * 4 + 4, NT)))
            for j, i in enumerate(tl):
                op(PE, lambda e, ps=ps, i=i, j=j: e.transpose(out=ps.ap[0:NE, j * 128:(j + 1) * 128], in_=posm.ap[:, i, :], identity=self.ident32.ap),
                   reads=[posm.r, self.ident32.r], writes=[ps.r])
            n = len(tl) * 128
            op(DVE, lambda e, ps=ps, g=g, n=n: e.tensor_scalar(out=posT.ap[:, g * 512:g * 512 + n], in0=ps.ap[0:NE, 0:n], scalar1=12582912.0, scalar2=12582912.0,
                                                               op0=ALU.add, op1=ALU.subtract), reads=[ps.r], writes=[posT.r])
        dma(SP, self.POST.ap, posT.ap, reads=[posT.r], writes=[self.POST.r])
        self.P.barrier()
        self.off = keep2

    def moe_passA(self, l):
        op, dma = self.op, self.dma
        h2tok, posm = self.h2tok, self.posm
        sel = self.tile([128, 32, 512], BF16, "sel", nsub=32)
        selc = self.tile([128, 2, 32], BF16, "selc")
        xsT = self.tile([128, 8, 544], BF16, "xsT")
        hact = self.tile([128, 16, 544], BF16, "hact")
        wgb = [self.tile([128, 8, 256], BF16, "wg") for _ in range(2)]
        wub = [self.tile([128, 8, 256], BF16, "wu") for _ in range(2)]
        wdb = [self.tile([128, 2, 512], BF16, "wd") for _ in range(3)]
        sa = [self.tile([128, 512], F32, "sa") for _ in range(2)]
        sac = self.tile([128, 32], F32, "sac")
        yes = [self.tile([128, D], BF16, "yes") for _ in range(4)]
        yec = self.tile([32, D], BF16, "yec")
        posbc = [self.tile([128, 1024], F32, "posbc") for _ in range(2)]
        affbc = [self.tile([128, 1024], F32, "affbc") for _ in range(2)]
        posc = self.tile([32, 256], F32, "posc")
        affc = self.tile([32, 256], F32, "affc")
        stg = [self.tile([128, 1024], BF16, "stg") for _ in range(2)]
        nq = 0
        stgc = self.tile([32, 256], BF16, "stgc")
        nw = [0, 0]
        ns = 0
        for ex in range(NE):
            for i in range(2, NT):
                eng = DVE if i % 2 == 0 else POOL
                op(DVE, lambda e, i=i, ex=ex: e.tensor_scalar(out=sel.ap[:, i - 2, :], in0=self.iota512.ap, scalar1=posm.ap[:, i, ex:ex + 1], scalar2=None,
                                                              op0=ALU.is_equal), reads=[self.iota512.r, posm.r], writes=[sel.sub[i - 2]])
            for i in range(2):
                op(DVE, lambda e, i=i, ex=ex: e.tensor_scalar(out=selc.ap[:, i, :], in0=self.iota512.ap[:, 0:32], scalar1=posm.ap[:, i, ex:ex + 1], scalar2=None,
                                                              op0=ALU.is_equal), reads=[self.iota512.r, posm.r], writes=[selc.r])
            for k in range(8):
                ps = self.psum()
                ks = slice(k * 128, (k + 1) * 128)
                for i in range(2, NT):
                    op(PE, lambda e, ps=ps, i=i, ks=ks: e.matmul(ps.ap, lhsT=h2tok.ap[:, i, ks], rhs=sel.ap[:, i - 2, :], start=(i == 2), stop=(i == NT - 1)),
                       reads=[h2tok.sub[i], sel.sub[i - 2]], writes=[ps.r])
                op(ACT, lambda e, ps=ps, k=k: e.copy(out=xsT.ap[:, k, 0:512], in_=ps.ap), reads=[ps.r], writes=[xsT.r])
                ps2 = self.psum()
                for i in range(2):
                    op(PE, lambda e, ps2=ps2, i=i, ks=ks: e.matmul(ps2.ap[:, 0:32], lhsT=h2tok.ap[:, i, ks], rhs=selc.ap[:, i, :], start=(i == 0), stop=(i == 1)),
                       reads=[h2tok.sub[i], selc.r], writes=[ps2.r])
                op(ACT, lambda e, ps2=ps2, k=k: e.copy(out=xsT.ap[:, k, 512:544], in_=ps2.ap[:, 0:32]), reads=[ps2.r], writes=[xsT.r])
            for q in range(8):
                wg, wu = wgb[nw[0] % 2], wub[nw[0] % 2]
                nw[0] += 1
                cs = slice(q * 256, (q + 1) * 256)
                if not (self.cfg.get('nowdma') and ex > 0):
                    dma(POOL, wg.ap, self.moe_w_gate[l, ex][:, cs].rearrange("(k p) n -> p k n", p=128), writes=[wg.r])
                    dma(POOL, wu.ap, self.moe_w_up[l, ex][:, cs].rearrange("(k p) n -> p k n", p=128), writes=[wu.r])
                for jl in range(2):
                    j = q * 2 + jl
                    js = slice(jl * 128, (jl + 1) * 128)
                    ps_a, ps_u, ps_c = self.psum(), self.psum(), self.psum()
                    for (w, ps) in ((wg, ps_a), (wu, ps_u)):
                        for k in range(8):
                            op(PE, lambda e, w=w, ps=ps, k=k, js=js: e.matmul(ps.ap, lhsT=w.ap[:, k, js], rhs=xsT.ap[:, k, 0:512], start=(k == 0), stop=(k == 7)),
                               reads=[w.r, xsT.r], writes=[ps.r])
                    for (w, c0) in ((wg, 0), (wu, 32)):
                        for k in range(8):
                            op(PE, lambda e, w=w, c0=c0, k=k, js=js, ps_c=ps_c: e.matmul(ps_c.ap[:, c0:c0 + 32], lhsT=w.ap[:, k, js], rhs=xsT.ap[:, k, 512:544],
                                                                                         start=(k == 0), stop=(k == 7)),
                               reads=[w.r, xsT.r], writes=[ps_c.r])
                    s = sa[ns % 2]
                    ns += 1
                    op(ACT, lambda e, s=s, ps_a=ps_a: e.activation(out=s.ap, in_=ps_a.ap, func=AF.Silu), reads=[ps_a.r], writes=[s.r])
                    op(DVE, lambda e, s=s, ps_u=ps_u, j=j: e.tensor_tensor(out=hact.ap[:, j, 0:512], in0=s.ap, in1=ps_u.ap, op=ALU.mult),
                       reads=[s.r, ps_u.r], writes=[hact.r])
                    op(ACT, lambda e, ps_c=ps_c: e.activation(out=sac.ap, in_=ps_c.ap[:, 0:32], func=AF.Silu), reads=[ps_c.r], writes=[sac.r])
                    op(DVE, lambda e, ps_c=ps_c, j=j: e.tensor_tensor(out=hact.ap[:, j, 512:544], in0=sac.ap, in1=ps_c.ap[:, 32:64], op=ALU.mult),
                       reads=[sac.r, ps_c.r], writes=[hact.r])
            for half in range(2):
                hs = slice(half * 512, (half + 1) * 512)
                psd = [self.psum() for _ in range(5)]
                for jj in range(8):
                    wd = wdb[nw[1] % 3]
                    nw[1] += 1
                    if not (self.cfg.get('nowdma') and ex > 0):
                        dma(POOL, wd.ap, self.moe_w_down[l, ex][jj * 256:(jj + 1) * 256, hs].rearrange("(a p) n -> p a n", p=128), writes=[wd.r])
                    for a in range(2):
                        j = jj * 2 + a
                        first, last = (j == 0), (j == 15)
                        for c in range(4):
                            op(PE, lambda e, c=c, j=j, a=a, wd=wd, first=first, last=last, p=psd[c]: e.matmul(p.ap, lhsT=hact.ap[:, j, c * 128:(c + 1) * 128], rhs=wd.ap[:, a, :],
                                                                                                                  start=first, stop=last),
                               reads=[hact.r, wd.r], writes=[psd[c].r])
                        op(PE, lambda e, j=j, a=a, wd=wd, first=first, last=last, p=psd[4]: e.matmul(p.ap[0:32, :], lhsT=hact.ap[:, j, 512:544], rhs=wd.ap[:, a, :],
                                                                                                      start=first, stop=last),
                           reads=[hact.r, wd.r], writes=[psd[4].r])
                for c in range(4):
                    op(ACT, lambda e, c=c, p=psd[c], hs=hs: e.copy(out=yes[c].ap[:, hs], in_=p.ap), reads=[psd[c].r], writes=[yes[c].r])
                op(ACT, lambda e, p=psd[4], hs=hs: e.copy(out=yec.ap[:, hs], in_=p.ap[0:32, :]), reads=[psd[4].r], writes=[yec.r])
            for c in range(4):
                dma(SP, self.YE.ap[ex, c], yes[c].ap, reads=[yes[c].r], writes=[self.YE.sub[ex]])
            dma(SP, self.YEC.ap[ex], yec.ap, reads=[yec.r], writes=[self.YEC.sub[ex]])
            dma(SP, posc.ap, self.POST.ap[ex:ex + 1, 0:256].to_broadcast([32, 256]), reads=[self.POST.r], writes=[posc.r])
            dma(SP, affc.ap, self.AFFT.ap[ex:ex + 1, 0:256].to_broadcast([32, 256]), reads=[self.AFFT.r], writes=[affc.r])
            op(DVE, lambda e: e.scalar_tensor_tensor(out=stgc.ap, in0=posc.ap, scalar=self.iotacol.ap[0:32, 0:1], in1=affc.ap,
                                                      op0=ALU.is_equal, op1=ALU.mult), reads=[posc.r, affc.r, self.iotacol.r], writes=[stgc.r])
            dma(SP, self.SELTC.ap[ex], stgc.ap, reads=[stgc.r], writes=[self.SELTC.r])
            for qd in range(4):
                pb_, ab_ = posbc[nq % 2], affbc[nq % 2]
                nq += 1
                t0 = 256 + qd * 1024
                dma(SP, pb_.ap, self.POST.ap[ex:ex + 1, t0:t0 + 1024].to_broadcast([128, 1024]), reads=[self.POST.r], writes=[pb_.r])
                dma(SP, ab_.ap, self.AFFT.ap[ex:ex + 1, t0:t0 + 1024].to_broadcast([128, 1024]), reads=[self.AFFT.r], writes=[ab_.r])
                for c in range(4):
                    sg = stg[c % 2]
                    ecx = ex * 4 + c
                    op(DVE, lambda e, sg=sg, c=c, pb_=pb_, ab_=ab_: e.scalar_tensor_tensor(
                        out=sg.ap, in0=pb_.ap, scalar=self.iotacol.ap[:, c:c + 1], in1=ab_.ap, op0=ALU.is_equal, op1=ALU.mult),
                       reads=[pb_.r, ab_.r, self.iotacol.r], writes=[sg.r])
                    dma(SP, self.SELT.ap[qd * 8:(qd + 1) * 8, :, ecx, :].rearrange("i s t -> s i t"), sg.ap.rearrange("s (i t) -> s i t", t=128),
                        reads=[sg.r], writes=self.SELT.sub[qd * 8:(qd + 1) * 8])

    def moe_passB(self, l):
        op, dma = self.op, self.dma
        self.phase_begin()
        last = (l == DEPTH - 1)
        yeall = self.tile([128, 64, D], BF16, "yeall")
        for ex in range(NE):
            dma(SP, yeall.ap[:, ex * 4:(ex + 1) * 4, :], self.YE.ap[ex].rearrange("c s d -> s c d"), reads=[self.YE.sub[ex]], writes=[yeall.r])
        yecall = self.tile([128, 4, D], BF16, "yecall")
        dma(SP, yecall.ap, self.YEC.ap.rearrange("(g e) s d -> (e s) g d", e=4), reads=self.YEC.sub, writes=[yecall.r])
        seltc = self.tile([128, 4, 256], BF16, "seltc")
        dma(SP, seltc.ap, self.SELTC.ap.rearrange("(g e) s t -> (e s) g t", e=4), reads=[self.SELTC.r], writes=[seltc.r])
        mods = self.mod_tiles(l, [5])
        g1 = self.ln_vec(self.ln_g[l, 1:2, :])
        b1 = self.ln_vec(self.ln_b[l, 1:2, :])
        selt = [self.tile([128, 32, 128], BF16, "selt") for _ in range(3)]
        xb = [self.tile([128, D], F32, "xb") for _ in range(2)]
        yb = [self.tile([128, D], F32, "yb") for _ in range(2)]
        st = self.tile([128, 2, 6], F32, "st")
        mv = self.tile([128, 4], F32, "mv")
        nsl = 0
        for i in range(NT):
            c = 1 if i < 2 else 0
            rows = slice(i * 128, (i + 1) * 128)
            x, y = xb[i % 2], yb[i % 2]
            dma(SP, x.ap, self.XS.ap[rows, :], reads=[self.XS.sub[i]], writes=[x.r])
            m5 = mods[5][c]
            pss = [self.psum(), self.psum()]
            if c:
                for g in range(4):
                    for half in range(2):
                        hs = slice(half * 512, (half + 1) * 512)
                        op(PE, lambda e, ps=pss[half], g=g, hs=hs, i=i: e.matmul(ps.ap, lhsT=seltc.ap[:, g, i * 128:(i + 1) * 128], rhs=yecall.ap[:, g, hs],
                                                                                 start=(g == 0), stop=(g == 3)),
                           reads=[seltc.r, yecall.r], writes=[pss[half].r])
            else:
                for hh in range(2):
                    sl = selt[nsl % 3]
                    nsl += 1
                    dma(SP, sl.ap, self.SELT.ap[i - 2, :, hh * 32:(hh + 1) * 32, :], reads=[self.SELT.sub[i - 2]], writes=[sl.r])
                    for b in range(32):
                        ec = hh * 32 + b
                        for half in range(2):
                            hs = slice(half * 512, (half + 1) * 512)
                            op(PE, lambda e, ps=pss[half], sl=sl, b=b, ec=ec, hs=hs: e.matmul(ps.ap, lhsT=sl.ap[:, b, :], rhs=yeall.ap[:, ec, hs],
                                                                                              start=(ec == 0), stop=(ec == 63)),
                               reads=[sl.r, yeall.r], writes=[pss[half].r])
            for half in range(2):
                hs = slice(half * 512, (half + 1) * 512)
                op(DVE, lambda e, ps=pss[half], y=y, m5=m5, hs=hs: e.tensor_tensor(out=y.ap[:, hs], in0=ps.ap, in1=m5.ap[:, hs], op=ALU.mult),
                   reads=[pss[half].r, m5.r], writes=[y.r])
            op(DVE, lambda e, x=x, y=y: e.scalar_tensor_tensor(out=y.ap, in0=x.ap, scalar=ALPHA, in1=y.ap, op0=ALU.mult, op1=ALU.add),
               reads=[x.r, y.r], writes=[y.r])
            self.layernorm_(y, st, mv)
            op(POOL, lambda e, y=y, x=x: e.tensor_tensor(out=x.ap, in0=y.ap, in1=g1.ap, op=ALU.mult), reads=[y.r, g1.r], writes=[x.r])
            op(POOL, lambda e, x=x: e.tensor_tensor(out=x.ap, in0=x.ap, in1=b1.ap, op=ALU.add), reads=[x.r, b1.r], writes=[x.r])
            dma(SP, self.XS.ap[rows, :], x.ap, reads=[x.r], writes=[self.XS.sub[i]])
            if last and not c:
                dma(SP, self.out[(i - 2) * 128:(i - 1) * 128, :], x.ap, reads=[x.r], writes=[self.outr])
            if self.cfg.get("test"):
                dma(SP, self.dbg.ap[rows, :], x.ap, reads=[x.r], writes=[self.dbg.sub[i]])

    def finish(self):
        rs = [self.outr]
        if self.cfg.get("test"):
            rs = rs + [self.dbg.r] + self.dbg.sub
        self.P.barrier()
        self.op(SP, lambda e: e.nop(), reads=rs)
        self.P.emit()


def Tl_view(t):
    return t


def build(nc, cfg):
    k = K(nc, cfg)
    k.declare_io()
    k.setup()
    tk = cfg.get("test")
    if tk in ("mix", "gdn1", "gdn2"):
        l = cfg["layer"]
        k.premix(l)
        if l % 2 == 0:
            k.natten(l)
        else:
            k.gdn(l)
    if tk == "post":
        l = cfg["layer"]
        k.postmix(l)
        k.moe_select(l)
        k.moe_passA(l)
        k.moe_passB(l)
    if not tk:
        for l in range(cfg.get("depth", DEPTH)):
            k.premix(l)
            if l % 2 == 0:
                k.natten(l)
            else:
                k.gdn(l)
            k.postmix(l)
            k.moe_select(l)
            k.moe_passA(l)
            k.moe_passB(l)
    k.finish()
    return k


WNAMES = ["c_ctx", "ada_w", "ada_b", "ln_g", "ln_b", "na_w_qkv", "na_w_o", "na_rpb", "gdn_w_in", "gdn_conv_w", "gdn_a_log",
          "gdn_dt_bias", "gdn_norm_w", "gdn_w_o", "moe_w_router", "moe_w_gate", "moe_w_up", "moe_w_down"]


def make_in_maps(inputs, cores):
    maps = []
    shared = {}
    for n in WNAMES:
        a = np.ascontiguousarray(inputs[n], dtype=np.float32)
        if n == "c_ctx":
            a = a.reshape(1, D)
        shared[n] = a
    rp = np.zeros((2, 16, 15, 128), np.float32)
    rp[..., 48:79] = np.asarray(inputs["na_rpb"], np.float32)[..., ::-1]
    shared["rpb_pad"] = rp
    qc = np.arange(64)
    cs = np.clip(qc - 8, 0, 48)
    kc = np.arange(64)[:, None]
    cmv = np.where((kc >= cs[None, :]) & (kc < cs[None, :] + 16), 0.0, -1e30).astype(np.float32)
    shared["cmask"] = np.tile(cmv, (2, 2))
    pp = np.arange(128)
    bd = lambda b: (pp[:, None] // b == pp[None, :] // b).astype(np.float32)
    shared["gmask"] = np.stack([bd(16), bd(32) - bd(16), bd(64) - bd(32), 1.0 - bd(64)]).astype(np.float32)
    for b in cores:
        m = dict(shared)
        m["x"] = np.ascontiguousarray(inputs["x"][b], dtype=np.float32)
        m["c"] = np.ascontiguousarray(inputs["c"][b:b + 1], dtype=np.float32)
        m["ctx"] = np.ascontiguousarray(inputs["ctx"][b], dtype=np.float32)
        maps.append(m)
    return maps


def kernel(**inputs):
    nc = bass.Bass("TRN2", target_bir_lowering=False)
    build(nc, {})
    maps = make_in_maps(inputs, list(range(8)))
    res = run_bass_kernel_spmd(nc, maps, core_ids=list(range(8)))
    return np.stack([np.asarray(r["out"], dtype=np.float32) for r in res.results], axis=0)
```

```python
import contextlib
import numpy as np
import concourse.bass as bass
import concourse.mybir as mybir
from concourse.bass_utils import run_bass_kernel_spmd

F32 = mybir.dt.float32
BF16 = mybir.dt.bfloat16
I32 = mybir.dt.int32
U8 = mybir.dt.uint8
AF = mybir.ActivationFunctionType
ALU = mybir.AluOpType
AX = mybir.AxisListType

PE, ACT, DVE, POOL, SP = "tensor", "scalar", "vector", "gpsimd", "sync"
ENGS = [PE, ACT, DVE, POOL, SP]
N_DMA_SEMS = 20
SEM_EPOCH = 20000

D = 1024
NT = 34
TT = NT * 128
NE = 16
DEPTH = 4
ALPHA = (2.0 * DEPTH) ** 0.25
LN_EPS = 1e-6
DSZ = {F32: 4, BF16: 2, I32: 4, U8: 1}


class Res:
    __slots__ = ("name", "last_w", "readers", "excl")

    def __init__(self, name=""):
        self.name = name
        self.last_w = None
        self.readers = []
        self.excl = False


class GRes:
    def __init__(self, name=""):
        self.subs = [Res(name + "a"), Res(name + "b")]


def _grp(fn):
    d = fn.__defaults__
    if not d:
        return None
    c = fn.__code__
    names = c.co_varnames[:c.co_argcount]
    m = dict(zip(names[len(names) - len(d):], d))
    if isinstance(m.get("j"), int):
        return m["j"]
    if isinstance(m.get("h"), int):
        return m["h"] // 4
    return None


def _expand(rs, g):
    out = []
    for r in rs:
        if isinstance(r, GRes):
            out.extend(r.subs if g is None else [r.subs[g]])
        else:
            out.append(r)
    return out


class Op:
    __slots__ = ("eng", "fn", "deps", "is_dma", "sem", "val", "sig")

    def __init__(self, eng, fn, is_dma):
        self.eng = eng
        self.fn = fn
        self.deps = ()
        self.is_dma = is_dma
        self.sem = None
        self.val = None
        self.sig = False


class Prog:
    def __init__(self, nc):
        self.nc = nc
        self.q = {e: [] for e in ENGS}
        self.dmas_since_barrier = []
        self.nops = 0

    def _add(self, eng, fn, reads, writes, is_dma):
        if any(isinstance(r, GRes) for r in reads) or any(isinstance(r, GRes) for r in writes):
            g = _grp(fn)
            reads, writes = _expand(reads, g), _expand(writes, g)
        op = Op(eng, fn, is_dma)
        deps = set()
        for r in reads:
            if r.last_w is not None:
                deps.add(r.last_w)
            if r.excl:
                deps.update(x for x in r.readers if x.eng != eng)
        for w in writes:
            if w.last_w is not None:
                deps.add(w.last_w)
            deps.update(w.readers)
        op.deps = tuple(deps)
        for r in reads:
            r.readers.append(op)
        for w in writes:
            w.last_w = op
            w.readers = []
        self.q[eng].append(op)
        self.nops += 1
        if is_dma:
            self.dmas_since_barrier.append(op)
        return op

    def op(self, eng, fn, reads=(), writes=()):
        return self._add(eng, fn, reads, writes, False)

    def dma(self, eng, out, in_, reads=(), writes=(), **kw):
        return self._add(eng, lambda e: e.dma_start(out=out, in_=in_, **kw), reads, writes, True)

    def barrier(self):
        b = Op(SP, lambda e: e.nop(), False)
        deps = set(self.dmas_since_barrier)
        for e in ENGS:
            if self.q[e]:
                deps.add(self.q[e][-1])
        b.deps = tuple(deps)
        self.q[SP].append(b)
        self.dmas_since_barrier = []
        for e in ENGS:
            if e == SP:
                continue
            o = Op(e, lambda en: en.nop(), False)
            o.deps = (b,)
            self.q[e].append(o)

    def emit(self):
        nc = self.nc
        for e in ENGS:
            for op in self.q[e]:
                for d in op.deps:
                    if d.is_dma or d.eng != op.eng or op.is_dma or d.eng != PE:
                        d.sig = True
        with contextlib.ExitStack() as st:
            nsig = {e: sum(1 for o in self.q[e] if o.sig and not o.is_dma) for e in ENGS}
            csem = {e: [st.enter_context(nc.semaphore("cs_%s_%d" % (e, i)))
                        for i in range(nsig[e] // SEM_EPOCH + 1)] for e in ENGS}
            dsem = {e: [st.enter_context(nc.semaphore("ds_%s_%d" % (e, i))) for i in range(N_DMA_SEMS)]
                    for e in (SP, ACT, POOL)}
            for e in ENGS:
                cnt = 0
                dcnt = 0
                for op in self.q[e]:
                    if op.is_dma:
                        op.sem = dsem[e][dcnt % N_DMA_SEMS]
                        op.val = 16 * (dcnt // N_DMA_SEMS + 1)
                        op.sig = True
                        dcnt += 1
                    elif op.sig:
                        op.sem = csem[e][cnt // SEM_EPOCH]
                        op.val = cnt % SEM_EPOCH + 1
                        cnt += 1
            block = st.enter_context(nc.Block())

            def gen(e):
                def body(eng):
                    waited = {}
                    for op in self.q[e]:
                        needs = {}
                        for d in op.deps:
                            if not d.sig:
                                continue
                            if (not d.is_dma) and d.eng == e and e == PE and not op.is_dma:
                                continue
                            k = id(d.sem)
                            if needs.get(k, (None, 0))[1] < d.val:
                                needs[k] = (d.sem, d.val)
                        if op.is_dma and op.val > 16:
                            k = id(op.sem)
                            if needs.get(k, (None, 0))[1] < op.val - 16:
                                needs[k] = (op.sem, op.val - 16)
                        for k, (s, v) in needs.items():
                            if waited.get(k, 0) >= v:
                                continue
                            eng.wait_ge(s, v)
                            waited[k] = v
                        ins = op.fn(eng)
                        if op.sig:
                            ins.then_inc(op.sem, 16 if op.is_dma else 1)
                return body

            for e in ENGS:
                if self.q[e]:
                    getattr(block, e)(gen(e))


class Tl:
    def __init__(self, ap, name="", nsub=0):
        self.ap = ap
        self.r = Res(name)
        self.sub = [Res(name + str(i)) for i in range(nsub)]


ARENA = 206000


class K:
    def __init__(self, nc, cfg):
        self.nc = nc
        self.cfg = cfg
        self.P = Prog(nc)
        global ARENA
        ARENA = (int(nc.sbuf_bytes_remaining) - 256) // 64 * 64
        self.big = nc.alloc_sbuf_tensor("arena", [128, ARENA], U8)
        self.off = 0
        self.ps = []
        for i in range(8):
            t = nc.alloc_psum_tensor("psb%d" % i, [128, 512], F32)
            self.ps.append(Tl(t[:], "ps%d" % i, nsub=4))
            for r_ in [self.ps[-1].r] + self.ps[-1].sub:
                r_.excl = True
        self.psn = 0
        self.uid = 0

    def tile(self, shape, dt, name="t", nsub=0):
        n = int(np.prod(shape[1:])) * DSZ[dt]
        if self.off + n > ARENA:
            raise RuntimeError("SBUF arena overflow at %s: %d + %d" % (name, self.off, n))
        a = self.big[0:shape[0], self.off:self.off + n].bitcast(dt)
        self.off += (n + 63) // 64 * 64
        if len(shape) == 3:
            a = a.rearrange("p (a b) -> p a b", a=shape[1])
        elif len(shape) == 4:
            a = a.rearrange("p (a b c) -> p a b c", a=shape[1], b=shape[2])
        self.uid += 1
        return Tl(a, "%s_%d" % (name, self.uid), nsub)

    def psum(self):
        t = self.ps[self.psn % 8]
        self.psn += 1
        return t

    def dram(self, name, shape, dt, kind="Internal"):
        return self.nc.dram_tensor(name, list(shape), dt, kind=kind).ap()

    def op(self, eng, fn, reads=(), writes=()):
        return self.P.op(eng, fn, reads, writes)

    def dma(self, eng, out, in_, reads=(), writes=(), **kw):
        return self.P.dma(eng, out, in_, reads, writes, **kw)

    def declare_io(self):
        nc, cfg = self.nc, self.cfg
        ein = lambda n, s: self.dram(n, s, F32, kind="ExternalInput")
        self.x_in = ein("x", [4096, D])
        self.c_in = ein("c", [1, D])
        self.ctx_in = ein("ctx", [256, D])
        self.cc_in = ein("c_ctx", [1, D])
        self.ada_w = ein("ada_w", [DEPTH, D, 6 * D])
        self.ada_b = ein("ada_b", [DEPTH, 6 * D])
        self.ln_g = ein("ln_g", [DEPTH, 2, D])
        self.ln_b = ein("ln_b", [DEPTH, 2, D])
        self.na_w_qkv = ein("na_w_qkv", [2, D, 3 * D])
        self.na_w_o = ein("na_w_o", [2, D, D])
        self.na_rpb = ein("na_rpb", [2, 16, 15, 31])
        self.gdn_w_in = ein("gdn_w_in", [2, D, 4 * D + 32])
        self.gdn_conv_w = ein("gdn_conv_w", [2, 5, 3 * D])
        self.gdn_a_log = ein("gdn_a_log", [2, 2, 8])
        self.gdn_dt_bias = ein("gdn_dt_bias", [2, 2, 8])
        self.gdn_norm_w = ein("gdn_norm_w", [2, 128])
        self.gdn_w_o = ein("gdn_w_o", [2, D, D])
        if cfg.get("test") not in ("mix", "gdn1", "gdn2"):
            self.moe_w_router = ein("moe_w_router", [DEPTH, D, NE])
            self.moe_w_gate = ein("moe_w_gate", [DEPTH, NE, D, 2048])
            self.moe_w_up = ein("moe_w_up", [DEPTH, NE, D, 2048])
            self.moe_w_down = ein("moe_w_down", [DEPTH, NE, 2048, D])
        if cfg.get("test") in ("gdn1", "gdn2"):
            self.dbg2 = self.dram("dbg2", [4, D, TT], F32, kind="ExternalOutput")
            self.dbg3 = self.dram("dbg3", [128, 2, NT, 16], F32, kind="ExternalOutput")
        self.rpb_pad = ein("rpb_pad", [2, 16, 15, 128])
        self.cmask_in = ein("cmask", [128, 128])
        self.gmask_in = ein("gmask", [4, 128, 128])
        self.out = self.dram("out", [4096, D], F32, kind="ExternalOutput")
        tk = cfg.get("test")
        self.XS = Tl(self.dram("XS", [TT, D], F32), "XS", NT)
        self.Y = Tl(self.dram("Y", [TT, D], F32), "Y", NT)
        if tk:
            self.xs_in = self.dram("xs_in", [TT, D], F32, kind="ExternalInput")
            self.y_in = self.dram("y_in", [TT, D], F32, kind="ExternalInput")
        if tk:
            self.dbg = Tl(self.dram("dbg", [TT, D], F32, kind="ExternalOutput"), "dbg", NT)
        self.AFFT = Tl(self.dram("AFFT", [NE, TT], F32), "AFFT")
        self.POST = Tl(self.dram("POST", [NE, TT], F32), "POST")
        self.YE = Tl(self.dram("YE", [NE, 4, 128, D], BF16), "YE", NE)
        self.YEC = Tl(self.dram("YEC", [NE, 32, D], BF16), "YEC", NE)
        self.SELT = Tl(self.dram("SELT", [32, 128, 64, 128], BF16), "SELT", 32)
        self.SELTC = Tl(self.dram("SELTC", [NE, 32, 256], BF16), "SELTC")
        self.outr = Res("out")

    def setup(self):
        op, dma = self.op, self.dma
        self.ident = self.tile([128, 128], BF16, "ident")
        self.ident32 = self.tile([128, 128], F32, "ident32")
        self.ones = self.tile([128, 128], BF16, "ones")
        self.triu = self.tile([128, 128], BF16, "triu")
        tmp = self.tile([128, 128], F32, "tmp")
        self.iota512 = self.tile([128, 512], F32, "iota512")
        self.iotacol = self.tile([128, 4], F32, "iotacol")
        i32, idb, on, tu, io, ic = self.ident32, self.ident, self.ones, self.triu, self.iota512, self.iotacol
        op(POOL, lambda e: e.memset(i32.ap, 1.0), writes=[i32.r])
        op(POOL, lambda e: e.affine_select(out=i32.ap, in_=i32.ap, pattern=[[-1, 128]], compare_op=ALU.is_equal,
                                           fill=0.0, base=0, channel_multiplier=1), reads=[i32.r], writes=[i32.r])
        op(DVE, lambda e: e.tensor_copy(out=idb.ap, in_=i32.ap), reads=[i32.r], writes=[idb.r])
        op(DVE, lambda e: e.memset(on.ap, 1.0), writes=[on.r])
        op(POOL, lambda e: e.memset(tmp.ap, 1.0), writes=[tmp.r])
        op(POOL, lambda e: e.affine_select(out=tmp.ap, in_=tmp.ap, pattern=[[1, 128]], compare_op=ALU.is_ge,
                                           fill=0.0, base=0, channel_multiplier=-1), reads=[tmp.r], writes=[tmp.r])
        op(DVE, lambda e: e.tensor_copy(out=tu.ap, in_=tmp.ap), reads=[tmp.r], writes=[tu.r])
        op(POOL, lambda e: e.iota(io.ap, pattern=[[1, 512]], base=0, channel_multiplier=0,
                                  allow_small_or_imprecise_dtypes=True), writes=[io.r])
        op(POOL, lambda e: e.iota(ic.ap, pattern=[[128, 4]], base=0, channel_multiplier=1,
                                  allow_small_or_imprecise_dtypes=True), writes=[ic.r])
        self.scb = self.tile([128, 8, 128], F32, "scb")
        self.sccb = self.tile([128, 8, 128], F32, "sccb")
        for src, dst in ((self.c_in, self.scb), (self.cc_in, self.sccb)):
            cv = self.tile([128, 8], F32, "cv")
            dma(SP, cv.ap, src.rearrange("o (k p) -> p (o k)", p=128), writes=[cv.r], allow_slow_non_contiguous=True)
            op(ACT, lambda e, cv=cv: e.activation(out=cv.ap, in_=cv.ap, func=AF.Silu), reads=[cv.r], writes=[cv.r])
            op(DVE, lambda e, cv=cv, dst=dst: e.tensor_copy(out=dst.ap, in_=cv.ap.unsqueeze(2).to_broadcast([128, 8, 128])),
               reads=[cv.r], writes=[dst.r])
        self.base_off = self.off
        if not self.cfg.get("test"):
            dma(SP, self.XS.ap[0:256, :], self.ctx_in, writes=self.XS.sub[0:2])
            dma(SP, self.XS.ap[256:TT, :], self.x_in, writes=self.XS.sub[2:NT])
        else:
            dma(SP, self.XS.ap, self.xs_in, writes=self.XS.sub)
            dma(SP, self.Y.ap, self.y_in, writes=self.Y.sub)

    def phase_begin(self):
        self.P.barrier()
        self.off = self.base_off

    def mod_tiles(self, l, idxs, plus1=()):
        op, dma = self.op, self.dma
        res = {}
        for idx in idxs:
            res[idx] = (self.tile([128, D], F32, "modl"), self.tile([128, D], F32, "modc"))
        mark = self.off
        wbuf = [self.tile([128, 8, 512], F32, "adaw") for _ in range(2)]
        bbuf = [self.tile([128, 1024], F32, "adab") for _ in range(2)]
        n = 0
        for ii, idx in enumerate(idxs):
            lat, ctx = res[idx]
            bb = bbuf[ii % 2]
            dma(SP, bb.ap, self.ada_b[l:l + 1, idx * D:(idx + 1) * D].to_broadcast([128, D]), writes=[bb.r])
            for half in range(2):
                wb = wbuf[n % 2]
                n += 1
                c0 = idx * D + half * 512
                dma(SP, wb.ap, self.ada_w[l, :, c0:c0 + 512].rearrange("(k p) n -> p k n", p=128), writes=[wb.r])
                for lhs, dst in ((self.scb, lat), (self.sccb, ctx)):
                    ps = self.psum()
                    for k in range(8):
                        op(PE, lambda e, ps=ps, lhs=lhs, wb=wb, k=k: e.matmul(ps.ap, lhsT=lhs.ap[:, k, :], rhs=wb.ap[:, k, :],
                                                                              start=(k == 0), stop=(k == 7)),
                           reads=[lhs.r, wb.r], writes=[ps.r])
                    sl = slice(half * 512, (half + 1) * 512)
                    op(DVE, lambda e, ps=ps, dst=dst, bb=bb, sl=sl: e.tensor_tensor(out=dst.ap[:, sl], in0=ps.ap, in1=bb.ap[:, sl], op=ALU.add),
                       reads=[ps.r, bb.r], writes=[dst.r])
            if idx in plus1:
                for t in (lat, ctx):
                    op(DVE, lambda e, t=t: e.tensor_scalar(out=t.ap, in0=t.ap, scalar1=1.0, scalar2=None, op0=ALU.add),
                       reads=[t.r], writes=[t.r])
        self.P.barrier()
        self.off = mark
        return res

    def ln_vec(self, src_row):
        t = self.tile([128, D], F32, "lnv")
        self.dma(SP, t.ap, src_row.to_broadcast([128, D]), writes=[t.r])
        return t

    def layernorm_(self, r, st, mv):
        op = self.op
        for h in range(2):
            op(DVE, lambda e, h=h: e.bn_stats(out=st.ap[:, h, :], in_=r.ap[:, h * 512:(h + 1) * 512]), reads=[r.r], writes=[st.r])
        op(DVE, lambda e: e.bn_aggr(out=mv.ap[:, 0:2], in_=st.ap.rearrange("p a b -> p (a b)")), reads=[st.r], writes=[mv.r])
        op(DVE, lambda e: e.tensor_scalar(out=mv.ap[:, 2:3], in0=mv.ap[:, 1:2], scalar1=LN_EPS, scalar2=None, op0=ALU.add),
           reads=[mv.r], writes=[mv.r])
        op(ACT, lambda e: e.activation(out=mv.ap[:, 2:3], in_=mv.ap[:, 2:3], func=AF.Ln), reads=[mv.r], writes=[mv.r])
        op(ACT, lambda e: e.activation(out=mv.ap[:, 2:3], in_=mv.ap[:, 2:3], func=AF.Exp, scale=-0.5), reads=[mv.r], writes=[mv.r])
        op(DVE, lambda e: e.tensor_scalar(out=r.ap, in0=r.ap, scalar1=mv.ap[:, 0:1], scalar2=mv.ap[:, 2:3],
                                          op0=ALU.subtract, op1=ALU.mult), reads=[r.r, mv.r], writes=[r.r])

    def premix(self, l):
        op, dma = self.op, self.dma
        self.phase_begin()
        hT = self.tile([128, 8, TT], BF16, "hT", nsub=NT)
        self.hT = hT
        self.mix_keep = self.off
        mods = self.mod_tiles(l, [0, 1], plus1=(1,))
        xb = [self.tile([128, D], F32, "xb") for _ in range(3)]
        hb = [self.tile([128, D], BF16, "hb") for _ in range(2)]
        for i in range(NT):
            x = xb[i % 3]
            h = hb[i % 2]
            c = 1 if i < 2 else 0
            dma(SP, x.ap, self.XS.ap[i * 128:(i + 1) * 128, :], reads=[self.XS.sub[i]], writes=[x.r])
            sc, sh = mods[1][c], mods[0][c]
            op(DVE, lambda e, x=x, sc=sc: e.tensor_tensor(out=x.ap, in0=x.ap, in1=sc.ap, op=ALU.mult), reads=[x.r, sc.r], writes=[x.r])
            op(POOL, lambda e, x=x, sh=sh, h=h: e.tensor_tensor(out=h.ap, in0=x.ap, in1=sh.ap, op=ALU.add), reads=[x.r, sh.r], writes=[h.r])
            self.transpose_to(h, hT, i)

    def transpose_to(self, h, hT, i, evac=ACT):
        op = self.op
        ps = self.psum()
        pb = ps.ap.bitcast(BF16)
        for k in range(8):
            op(PE, lambda e, k=k, pb=pb, h=h: e.transpose(out=pb[:, k * 128:(k + 1) * 128], in_=h.ap[:, k * 128:(k + 1) * 128],
                                                          identity=self.ident.ap), reads=[h.r, self.ident.r], writes=[ps.r])
        dst = hT.ap[:, :, i * 128:(i + 1) * 128]
        if evac == ACT:
            op(ACT, lambda e, pb=pb, dst=dst: e.copy(out=dst, in_=pb.rearrange("p (k n) -> p k n", k=8)), reads=[ps.r], writes=[hT.sub[i]])
        else:
            op(evac, lambda e, pb=pb, dst=dst: e.tensor_copy(out=dst, in_=pb.rearrange("p (k n) -> p k n", k=8)), reads=[ps.r], writes=[hT.sub[i]])

    def natten(self, l):
        op, dma = self.op, self.dma
        li = l // 2
        hT = self.hT
        self.P.barrier()
        self.off = self.mix_keep
        if not hasattr(self, "QT"):
            self.QT = Tl(self.dram("QT", [D, TT], BF16), "QT")
            self.KT = Tl(self.dram("KT", [D, TT], BF16), "KT")
            self.VX = Tl(self.dram("VX", [TT, 16, 65], BF16), "VX")
            self.OD = Tl(self.dram("OD", [TT, D], BF16), "OD", NT)
        QT, KT, VX, OD = self.QT, self.KT, self.VX, self.OD
        wq = self.na_w_qkv[li]
        wb = [self.tile([128, 8, 512], BF16, "wqk") for _ in range(2)]
        stq = [self.tile([128, 512], BF16, "stq") for _ in range(3)]
        blocks = [(0, 256)] + [(256 + 512 * b, 512) for b in range(8)]
        n = 0
        for cg in range(4):
            w = wb[cg % 2]
            dma(POOL, w.ap, wq[:, cg * 512:(cg + 1) * 512].rearrange("(k p) n -> p k n", p=128), writes=[w.r])
            for cl in range(4):
                ct = cg * 4 + cl
                dst = QT if ct < 8 else KT
                crow = (ct % 8) * 128
                for (t0, nt_) in blocks:
                    ps = self.psum()
                    for k in range(8):
                        op(PE, lambda e, ps=ps, w=w, k=k, cl=cl, t0=t0, nt_=nt_: e.matmul(ps.ap[:, 0:nt_], lhsT=w.ap[:, k, cl * 128:(cl + 1) * 128], rhs=hT.ap[:, k, t0:t0 + nt_],
                                                                                    start=(k == 0), stop=(k == 7)),
                           reads=[w.r] + hT.sub[t0 // 128:(t0 + nt_) // 128], writes=[ps.r])
                    sq = stq[n % 3]
                    n += 1
                    if ct < 8:
                        op(ACT, lambda e, ps=ps, sq=sq, nt_=nt_: e.mul(out=sq.ap[:, 0:nt_], in_=ps.ap[:, 0:nt_], mul=0.125), reads=[ps.r], writes=[sq.r])
                    else:
                        op(DVE, lambda e, ps=ps, sq=sq, nt_=nt_: e.tensor_copy(out=sq.ap[:, 0:nt_], in_=ps.ap[:, 0:nt_]), reads=[ps.r], writes=[sq.r])
                    dma(SP, dst.ap[crow:crow + 128, t0:t0 + nt_], sq.ap[:, 0:nt_], reads=[sq.r], writes=[dst.r])
        wv = self.tile([128, 8, D], BF16, "wv")
        dma(POOL, wv.ap, wq[:, 2 * D:3 * D].rearrange("(k p) n -> p k n", p=128), writes=[wv.r])
        vst = [self.tile([128, 16, 65], BF16, "vst") for _ in range(2)]
        for v in vst:
            op(DVE, lambda e, v=v: e.memset(v.ap, 1.0), writes=[v.r])
        for i in range(NT):
            v = vst[i % 2]
            for half in range(2):
                ps = self.psum()
                for k in range(8):
                    op(PE, lambda e, ps=ps, k=k, i=i, half=half: e.matmul(ps.ap, lhsT=hT.ap[:, k, i * 128:(i + 1) * 128], rhs=wv.ap[:, k, half * 512:(half + 1) * 512],
                                                                       start=(k == 0), stop=(k == 7)),
                       reads=[wv.r, hT.sub[i]], writes=[ps.r])
                op(ACT, lambda e, ps=ps, v=v, half=half: e.copy(out=v.ap[:, half * 8:(half + 1) * 8, 0:64], in_=ps.ap.rearrange("p (h d) -> p h d", d=64)),
                   reads=[ps.r], writes=[v.r])
            dma(SP, VX.ap[i * 128:(i + 1) * 128], v.ap, reads=[v.r], writes=[VX.r])
        self.P.barrier()
        self.off = self.mix_keep - 0
        self.off = self.base_off
        rs_ = np.clip(np.arange(64) - 4, 0, 56)
        tiles, tindex, per_rp = [], {}, []
        for rp in range(32):
            r0 = 2 * rp
            lst = []
            for kp in range(rs_[r0] // 2, (rs_[r0 + 1] + 7) // 2 + 1):
                spec = []
                for i2 in range(2):
                    for j2 in range(2):
                        kr, r = 2 * kp + i2, r0 + j2
                        spec.append(int(kr - r + 7) if rs_[r] <= kr < rs_[r] + 8 else None)
                spec = tuple(spec)
                key = (rp if rp in (0, 1, 30, 31) else -1, spec)
                if key not in tindex:
                    tindex[key] = len(tiles)
                    tiles.append(spec)
                lst.append((kp, tindex[key]))
            assert all(lst[j + 1][1] == lst[j][1] + 1 for j in range(len(lst) - 1))
            per_rp.append(lst)
        NTL = len(tiles)
        jm = self.tile([64, 128], F32, "jm")
        op(POOL, lambda e: e.memset(jm.ap, 1.0), writes=[jm.r])
        op(POOL, lambda e: e.affine_select(out=jm.ap[:, 0:64], in_=jm.ap[:, 0:64], pattern=[[1, 64]], compare_op=ALU.is_equal, fill=0.0, base=-63, channel_multiplier=1),
           reads=[jm.r], writes=[jm.r])
        op(POOL, lambda e: e.affine_select(out=jm.ap[:, 64:128], in_=jm.ap[:, 64:128], pattern=[[1, 64]], compare_op=ALU.is_equal, fill=0.0, base=-63, channel_multiplier=1),
           reads=[jm.r], writes=[jm.r])
        cm = self.tile([128, 128], F32, "cmask")
        dma(SP, cm.ap, self.cmask_in, writes=[cm.r])
        hk = self.tile([64, 15, 64], F32, "hk")
        toe = self.tile([128, 15, 64], F32, "toe")
        bias = self.tile([128, NTL, 128], F32, "bias")
        qt = [self.tile([64, TT], BF16, "qt") for _ in range(2)]
        kt = [self.tile([64, TT], BF16, "kt") for _ in range(2)]
        vh = [self.tile([128, NT, 65], BF16, "vh") for _ in range(2)]
        sb = [self.tile([128, 640], F32, "sb") for _ in range(2)]
        pb = [self.tile([128, 896], BF16, "pb") for _ in range(2)]
        ost = [self.tile([128, NT, 64], BF16, "ost") for _ in range(2)]
        rc = self.tile([128, 4], F32, "rc")
        rpb = self.rpb_pad[li]
        it = 0
        pending = None
        for h in range(16):
            q_, k_, v_, o_ = qt[h % 2], kt[h % 2], vh[h % 2], ost[h % 2]
            dma(SP, q_.ap, QT.ap[h * 64:(h + 1) * 64, :], reads=[QT.r], writes=[q_.r])
            dma(SP, k_.ap, KT.ap[h * 64:(h + 1) * 64, :], reads=[KT.r], writes=[k_.r])
            dma(SP, v_.ap, VX.ap.rearrange("(i p) h d -> p i h d", p=128)[:, :, h, :], reads=[VX.r], writes=[v_.r])
            src = bass.AP(tensor=rpb.tensor, offset=rpb[h].offset, ap=[[1, 64], [128, 15], [1, 64]])
            dma(SP, hk.ap, src, writes=[hk.r])
            for (d0, d1) in ((0, 8), (8, 15)):
                ps = self.psum()
                nn = (d1 - d0) * 64
                op(PE, lambda e, ps=ps, d0=d0, d1=d1, nn=nn: e.matmul(ps.ap[:, 0:nn], lhsT=jm.ap, rhs=hk.ap[:, d0:d1, :].rearrange("p a b -> p (a b)"), start=True, stop=True),
                   reads=[jm.r, hk.r], writes=[ps.r])
                op(ACT, lambda e, ps=ps, d0=d0, d1=d1, nn=nn: e.copy(out=toe.ap[:, d0:d1, :].rearrange("p a b -> p (a b)"), in_=ps.ap[:, 0:nn]), reads=[ps.r], writes=[toe.r])
            for t, spec in enumerate(tiles):
                for bi, dr in enumerate(spec):
                    i2, j2 = bi // 2, bi % 2
                    prt = slice(i2 * 64, (i2 + 1) * 64)
                    fr = slice(j2 * 64, (j2 + 1) * 64)
                    if dr is None:
                        op(POOL, lambda e, t=t, prt=prt, fr=fr: e.memset(bias.ap[prt, t, fr], -1e30), writes=[bias.r])
                    else:
                        op(POOL, lambda e, t=t, prt=prt, fr=fr, dr=dr: e.tensor_tensor(out=bias.ap[prt, t, fr], in0=toe.ap[prt, dr, :], in1=cm.ap[prt, fr], op=ALU.add),
                           reads=[toe.r, cm.r], writes=[bias.r])
            for qi in range(NT):
                if qi < 2:
                    wins = []
                    ctxk = [0, 1]
                else:
                    wins = per_rp[qi - 2]
                    ctxk = [0, 1]
                s_, p_ = sb[it % 2], pb[it % 2]
                it += 1
                nw = len(wins)
                qs = slice(qi * 128, (qi + 1) * 128)
                psA, psB = self.psum(), self.psum()
                slots = []
                for j in range(nw):
                    slots.append((psA, j * 128) if j < 4 else (psB, 0))
                cb = 128 if nw == 5 else 0
                for j, kk in enumerate(ctxk):
                    slots.append((psB, cb + j * 128))
                ktl = [2 + kp for (kp, _) in wins] + ctxk
                for (pst, co), kti in zip(slots, ktl):
                    op(PE, lambda e, pst=pst, co=co, kti=kti, qs=qs, k_=k_, q_=q_: e.matmul(pst.ap[:, co:co + 128], lhsT=k_.ap[:, kti * 128:(kti + 1) * 128], rhs=q_.ap[:, qs],
                                                                                         start=True, stop=True),
                       reads=[k_.r, q_.r], writes=[pst.r])
                if nw:
                    t0 = wins[0][1]
                    na = min(nw, 4)
                    op(DVE, lambda e, s_=s_, t0=t0, na=na, psA=psA: e.tensor_tensor(out=s_.ap[:, 0:na * 128], in0=psA.ap[:, 0:na * 128],
                                                                                   in1=bias.ap[:, t0:t0 + na, :].rearrange("p a b -> p (a b)"), op=ALU.add),
                       reads=[psA.r, bias.r], writes=[s_.r])
                    if nw == 5:
                        op(DVE, lambda e, s_=s_, t0=t0, psB=psB: e.tensor_tensor(out=s_.ap[:, 512:640], in0=psB.ap[:, 0:128], in1=bias.ap[:, t0 + 4, :], op=ALU.add),
                           reads=[psB.r, bias.r], writes=[s_.r])
                    op(ACT, lambda e, s_=s_, p_=p_, nw=nw: e.activation(out=p_.ap[:, 0:nw * 128], in_=s_.ap[:, 0:nw * 128], func=AF.Exp), reads=[s_.r], writes=[p_.r])
                op(ACT, lambda e, p_=p_, nw=nw, cb=cb, psB=psB: e.activation(out=p_.ap[:, nw * 128:nw * 128 + 256], in_=psB.ap[:, cb:cb + 256], func=AF.Exp),
                   reads=[psB.r], writes=[p_.r])
                def stage2(p_=p_, ktl=ktl, v_=v_, o_=o_, qi=qi):
                    pso = self.psum()
                    nk = len(ktl)
                    for j, kti in enumerate(ktl):
                        op(PE, lambda e, pso=pso, p_=p_, j=j, kti=kti, v_=v_, nk=nk: e.matmul(pso.ap[:, 0:65], lhsT=p_.ap[:, j * 128:(j + 1) * 128], rhs=v_.ap[:, kti, :],
                                                                                           start=(j == 0), stop=(j == nk - 1)),
                           reads=[p_.r, v_.r], writes=[pso.r])
                    op(DVE, lambda e, pso=pso: e.reciprocal(out=rc.ap[:, 0:1], in_=pso.ap[:, 64:65]), reads=[pso.r], writes=[rc.r])
                    op(DVE, lambda e, pso=pso, o_=o_, qi=qi: e.tensor_scalar(out=o_.ap[:, qi, :], in0=pso.ap[:, 0:64], scalar1=rc.ap[:, 0:1], scalar2=None, op0=ALU.mult),
                       reads=[pso.r, rc.r], writes=[o_.r])
                if pending is not None:
                    pending()
                pending = stage2
            if pending is not None:
                pending()
                pending = None
            dma(SP, OD.ap.rearrange("(i p) d -> p i d", p=128)[:, :, h * 64:(h + 1) * 64], o_.ap, reads=[o_.r], writes=OD.sub)
        self.out_proj(self.na_w_o[li])

    def gdn(self, l):
        op, dma = self.op, self.dma
        li = l // 2
        hT = self.hT
        self.P.barrier()
        self.off = self.mix_keep
        if not hasattr(self, "QF"):
            for nm in ("QF", "KF", "VF", "ZF", "OF"):
                setattr(self, nm, Tl(self.dram(nm, [D, TT], F32), nm, NT))
        QF, KF, VF, ZF, OF = self.QF, self.KF, self.VF, self.ZF, self.OF
        w_in = self.gdn_w_in[li]
        ones_a = self.tile([128, 128], F32, "ones_a")
        op(DVE, lambda e: e.memset(ones_a.ap, 1.0), writes=[ones_a.r])
        gT_a = self.tile([128, NT, 16], F32, "gT_a")
        bT_a = self.tile([128, NT, 16], F32, "bT_a")
        g_keep = self.off
        cw = self.tile([128, 24, 5], F32, "cw")
        for j in range(5):
            dma(SP, cw.ap[:, :, j], self.gdn_conv_w[li, j:j + 1, :].rearrange("o (c p) -> p (o c)", p=128), writes=[cw.r], allow_slow_non_contiguous=True)
        nwv = self.tile([128, 1], F32, "nwv")
        dma(SP, nwv.ap, self.gdn_norm_w[li:li + 1, :].rearrange("o p -> p o"), writes=[nwv.r], allow_slow_non_contiguous=True)
        pbuf = [self.tile([128, TT + 8], F32, "pbuf") for _ in range(2)]
        cbuf = [self.tile([128, TT], F32, "cbuf") for _ in range(2)]
        for pb in pbuf:
            op(POOL, lambda e, pb=pb: e.memset(pb.ap, 0.0), writes=[pb.r])
        wb = [self.tile([128, 8, 512], BF16, "win") for _ in range(2)]
        sqb = [self.tile([128, 512], F32, "sqb") for _ in range(2)]
        rnb = [self.tile([128, 512], F32, "rnb") for _ in range(2)]
        zsb = [self.tile([128, 512], F32, "zsb") for _ in range(2)]
        blocks = [(0, 256)] + [(256 + 512 * b, 512) for b in range(8)]
        nb = 0
        for cg in range(8):
            w = wb[cg % 2]
            dma(POOL, w.ap, w_in[:, cg * 512:(cg + 1) * 512].rearrange("(k p) n -> p k n", p=128), writes=[w.r])
            for cl in range(4):
                ct = cg * 4 + cl
                pb, cb = pbuf[ct % 2], cbuf[ct % 2]
                for (t0, nt_) in blocks:
                    ps = self.psum()
                    for k in range(8):
                        op(PE, lambda e, ps=ps, w=w, k=k, cl=cl, t0=t0, nt_=nt_: e.matmul(ps.ap[:, 0:nt_], lhsT=w.ap[:, k, cl * 128:(cl + 1) * 128], rhs=hT.ap[:, k, t0:t0 + nt_],
                                                                                    start=(k == 0), stop=(k == 7)),
                           reads=[w.r] + hT.sub[t0 // 128:(t0 + nt_) // 128], writes=ps.sub)
                    if ct < 24:
                        o0 = (2 if t0 == 0 else 6) + t0
                        op(ACT, lambda e, ps=ps, pb=pb, o0=o0, nt_=nt_: e.copy(out=pb.ap[:, o0:o0 + nt_], in_=ps.ap[:, 0:nt_]), reads=ps.sub, writes=[pb.r])
                    else:
                        zs = zsb[nb % 2]
                        nb += 1
                        op(ACT, lambda e, ps=ps, zs=zs, nt_=nt_: e.activation(out=zs.ap[:, 0:nt_], in_=ps.ap[:, 0:nt_], func=AF.Silu), reads=ps.sub, writes=[zs.r])
                        op(DVE, lambda e, zs=zs, nt_=nt_: e.tensor_scalar(out=zs.ap[:, 0:nt_], in0=zs.ap[:, 0:nt_], scalar1=nwv.ap[:, 0:1], scalar2=None, op0=ALU.mult),
                           reads=[zs.r, nwv.r], writes=[zs.r])
                        r0 = (ct - 24) * 128
                        dma(SP, ZF.ap[r0:r0 + 128, t0:t0 + nt_], zs.ap[:, 0:nt_], reads=[zs.r], writes=ZF.sub[t0 // 128:(t0 + nt_) // 128])
                if ct >= 24:
                    continue
                for (c0, ln, base) in ((0, 256, 0), (256, 4096, 260)):
                    op(DVE, lambda e, cb=cb, pb=pb, c0=c0, ln=ln, base=base, ct=ct: e.tensor_scalar(out=cb.ap[:, c0:c0 + ln], in0=pb.ap[:, base:base + ln], scalar1=cw.ap[:, ct, 0:1],
                                                                                             scalar2=None, op0=ALU.mult), reads=[pb.r, cw.r], writes=[cb.r])
                    for j in range(1, 5):
                        op(DVE, lambda e, cb=cb, pb=pb, c0=c0, ln=ln, base=base, ct=ct, j=j: e.scalar_tensor_tensor(out=cb.ap[:, c0:c0 + ln], in0=pb.ap[:, base + j:base + j + ln],
                                                                                                            scalar=cw.ap[:, ct, j:j + 1], in1=cb.ap[:, c0:c0 + ln], op0=ALU.mult, op1=ALU.add),
                           reads=[pb.r, cw.r, cb.r], writes=[cb.r])
                op(ACT, lambda e, cb=cb: e.activation(out=cb.ap, in_=cb.ap, func=AF.Silu), reads=[cb.r], writes=[cb.r])
                if ct < 16:
                    qsc = (128.0 ** -0.5) if ct < 8 else 1.0
                    for (t0, nt_) in blocks:
                        sq, rn = sqb[nb % 2], rnb[nb % 2]
                        nb += 1
                        op(POOL, lambda e, sq=sq, cb=cb, t0=t0, nt_=nt_: e.tensor_tensor(out=sq.ap[:, 0:nt_], in0=cb.ap[:, t0:t0 + nt_], in1=cb.ap[:, t0:t0 + nt_], op=ALU.mult),
                           reads=[cb.r], writes=[sq.r])
                        ps = self.psum()
                        op(PE, lambda e, ps=ps, sq=sq, nt_=nt_, o32=ones_a: e.matmul(ps.ap[:, 0:nt_], lhsT=o32.ap, rhs=sq.ap[:, 0:nt_], start=True, stop=True), reads=[ones_a.r, sq.r], writes=ps.sub)
                        op(DVE, lambda e, ps=ps, rn=rn, nt_=nt_: e.tensor_scalar(out=rn.ap[:, 0:nt_], in0=ps.ap[:, 0:nt_], scalar1=1e-6, scalar2=None, op0=ALU.add), reads=ps.sub, writes=[rn.r])
                        op(ACT, lambda e, rn=rn, nt_=nt_: e.activation(out=rn.ap[:, 0:nt_], in_=rn.ap[:, 0:nt_], func=AF.Ln), reads=[rn.r], writes=[rn.r])
                        op(ACT, lambda e, rn=rn, nt_=nt_: e.activation(out=rn.ap[:, 0:nt_], in_=rn.ap[:, 0:nt_], func=AF.Exp, scale=-0.5), reads=[rn.r], writes=[rn.r])
                        op(DVE, lambda e, rn=rn, cb=cb, t0=t0, nt_=nt_, qsc=qsc: e.scalar_tensor_tensor(out=cb.ap[:, t0:t0 + nt_], in0=cb.ap[:, t0:t0 + nt_], scalar=qsc, in1=rn.ap[:, 0:nt_],
                                                                                                 op0=ALU.mult, op1=ALU.mult), reads=[cb.r, rn.r], writes=[cb.r])
                dst = (QF, KF, VF)[ct // 8]
                r0 = (ct % 8) * 128
                dma(SP, dst.ap[r0:r0 + 128, :], cb.ap, reads=[cb.r], writes=dst.sub)
        wab = self.tile([128, 8, 32], BF16, "wab")
        dma(POOL, wab.ap, w_in[:, 4096:4128].rearrange("(k p) n -> p k n", p=128), writes=[wab.r])
        coef = self.tile([128, 16], F32, "coef")
        dtb = self.tile([128, 16], F32, "dtb")
        dma(SP, coef.ap, self.gdn_a_log[li:li + 1].rearrange("o a b -> o (a b)").to_broadcast([128, 16]), writes=[coef.r])
        dma(SP, dtb.ap, self.gdn_dt_bias[li:li + 1].rearrange("o a b -> o (a b)").to_broadcast([128, 16]), writes=[dtb.r])
        op(ACT, lambda e: e.activation(out=coef.ap, in_=coef.ap, func=AF.Exp), reads=[coef.r], writes=[coef.r])
        op(DVE, lambda e: e.tensor_scalar(out=coef.ap, in0=coef.ap, scalar1=-1.0, scalar2=None, op0=ALU.mult), reads=[coef.r], writes=[coef.r])
        psl = []
        for i in range(NT):
            if i % 16 == 0:
                ps = self.psum()
                psl.append(ps)
            c0 = (i % 16) * 32
            for k in range(8):
                op(PE, lambda e, ps=ps, i=i, k=k, c0=c0: e.matmul(ps.ap[:, c0:c0 + 32], lhsT=hT.ap[:, k, i * 128:(i + 1) * 128], rhs=wab.ap[:, k, :], start=(k == 0), stop=(k == 7)),
                   reads=[wab.r, hT.sub[i]], writes=ps.sub)
        for gi, ps in enumerate(psl):
            n_ = min(16, NT - gi * 16)
            pv = ps.ap[:, 0:n_ * 32].rearrange("p (i c) -> p i c", c=32)
            gs = gT_a.ap[:, gi * 16:gi * 16 + n_, :]
            bs = bT_a.ap[:, gi * 16:gi * 16 + n_, :]
            op(DVE, lambda e, pv=pv, gs=gs, n_=n_: e.tensor_tensor(out=gs, in0=pv[:, :, 0:16], in1=dtb.ap.unsqueeze(1).to_broadcast([128, n_, 16]), op=ALU.add), reads=ps.sub + [dtb.r], writes=[gT_a.r])
            op(ACT, lambda e, gs=gs: e.activation(out=gs, in_=gs, func=AF.Exp), reads=[gT_a.r], writes=[gT_a.r])
            op(DVE, lambda e, gs=gs: e.tensor_scalar(out=gs, in0=gs, scalar1=1.0, scalar2=None, op0=ALU.add), reads=[gT_a.r], writes=[gT_a.r])
            op(ACT, lambda e, gs=gs: e.activation(out=gs, in_=gs, func=AF.Ln), reads=[gT_a.r], writes=[gT_a.r])
            op(DVE, lambda e, gs=gs, n_=n_: e.tensor_tensor(out=gs, in0=gs, in1=coef.ap.unsqueeze(1).to_broadcast([128, n_, 16]), op=ALU.mult), reads=[gT_a.r, coef.r], writes=[gT_a.r])
            op(ACT, lambda e, pv=pv, bs=bs: e.activation(out=bs, in_=pv[:, :, 16:32], func=AF.Exp, scale=-1.0), reads=ps.sub, writes=[bT_a.r])
            op(DVE, lambda e, bs=bs: e.tensor_scalar(out=bs, in0=bs, scalar1=1.0, scalar2=None, op0=ALU.add), reads=[bT_a.r], writes=[bT_a.r])
            op(DVE, lambda e, bs=bs: e.reciprocal(out=bs, in_=bs), reads=[bT_a.r], writes=[bT_a.r])
        self.P.barrier()
        if self.cfg.get("test") == "gdn1":
            rr = Res("dbg2")
            for i_, t_ in enumerate((QF, KF, VF, ZF)):
                dma(SP, self.dbg2[i_], t_.ap, reads=t_.sub, writes=[rr])
            dma(SP, self.dbg3[:, 0], gT_a.ap, reads=[gT_a.r], writes=[rr])
            dma(SP, self.dbg3[:, 1], bT_a.ap, reads=[bT_a.r], writes=[rr])
            self.dbg.sub.append(rr)
            return
        self.off = self.base_off
        gT_o, bT_o = gT_a, bT_a
        ones32 = self.tile([128, 128], F32, "ones32b")
        gT = self.tile([128, NT, 16], F32, "gTb")
        bT = self.tile([128, NT, 16], F32, "bTb")
        op(DVE, lambda e: e.memset(ones32.ap, 1.0), writes=[ones32.r])
        op(DVE, lambda e: e.tensor_copy(out=gT.ap, in_=gT_o.ap), reads=[gT_o.r], writes=[gT.r])
        op(DVE, lambda e: e.tensor_copy(out=bT.ap, in_=bT_o.ap), reads=[bT_o.r], writes=[bT.r])
        self.P.barrier()
        def cmat(name, fill_in, pattern, cmult, cmp, fill):
            t = self.tile([128, 128], F32, name)
            op(POOL, lambda e: e.memset(t.ap, fill_in), writes=[t.r])
            op(POOL, lambda e: e.affine_select(out=t.ap, in_=t.ap, pattern=[[pattern, 128]], compare_op=cmp, fill=fill, base=0, channel_multiplier=cmult), reads=[t.r], writes=[t.r])
            return t
        triu32 = cmat("triu32", 1.0, 1, -1, ALU.is_ge, 0.0)
        tril32 = cmat("tril32", 1.0, -1, 1, ALU.is_ge, 0.0)
        sup = cmat("sup", 1.0, 1, -1, ALU.is_gt, 0.0)
        slo = cmat("slo", 1.0, -1, 1, ALU.is_gt, 0.0)
        mup = cmat("mup", 0.0, 1, -1, ALU.is_ge, -1e30)
        mlo = cmat("mlo", 0.0, -1, 1, ALU.is_ge, -1e30)
        i32 = self.ident32
        wo = self.tile([128, 8, D], BF16, "wo")
        dma(POOL, wo.ap, self.gdn_w_o[li].rearrange("(k p) n -> p k n", p=128), writes=[wo.r])
        def T4(nm):
            t_ = self.tile([128, 8, 128], F32, nm)
            t_.r = GRes(nm)
            return t_
        qfb = [T4("qf") for _ in range(2)]
        kfb = [T4("kf") for _ in range(2)]
        vfb = [T4("vf") for _ in range(2)]
        xfb = [T4("xf") for _ in range(2)]
        zfb = [T4("zf") for _ in range(2)]
        Rt, dTt, d2t, dec, decT, eGbc, AmT, An = (T4(nm) for nm in ("R", "dT", "d2", "dec", "decT", "eGbc", "AmT", "An"))
        kbg, ktail, vb = T4("kbg"), T4("ktail"), T4("vb")
        Mb = [T4("M0"), T4("M1")]
        Nb = [T4("N0"), T4("N1")]
        Pm, attnT, qdT, nwT, vnew, St = T4("P"), T4("attnT"), T4("qdT"), T4("nwT"), T4("vnew"), T4("S")
        PT = T4("PT")
        Mc, Nc = [Rt], [d2t]
        gm = []
        for gi_ in range(4):
            gt_ = self.tile([128, 128], F32, "gmask")
            dma(SP, gt_.ap, self.gmask_in[gi_], writes=[gt_.r])
            gm.append(gt_)
        Gtok = self.tile([128, 8], F32, "Gtok")
        eGtok = self.tile([128, 8], F32, "eGtok")
        bg = self.tile([128, 8], F32, "bg")
        yT = self.tile([128, 8, 128], BF16, "yT")
        ytile = [self.tile([128, D], F32, "ytile") for _ in range(2)]
        fl = lambda t, j: t.ap[:, 4 * j:4 * j + 4, :]
        pv4 = lambda ps: ps.ap.rearrange("p (a b) -> p a b", a=4)
        bc_h = lambda ap2, j: ap2[:, 4 * j:4 * j + 4].unsqueeze(2).to_broadcast([128, 4, 128])
        bc_m = lambda m, n_=4: m.ap.unsqueeze(1).to_broadcast([128, n_, 128])
        hview = lambda ap: ap.rearrange("(h p) t -> p h t", p=128)
        nch = 0
        for d in range(self.cfg.get("gdn_dirs", 2)):
            TRI, maskT, maskN, strT, strN, last = ((triu32, mup, mlo, sup, slo, 127), (tril32, mlo, mup, slo, sup, 0))[d]
            order = list(range(NT)) if d == 0 else [1, 0] + list(range(NT - 1, 1, -1))
            order = order[:self.cfg.get("gdn_nch", NT)]
            op(DVE, lambda e: e.memset(St.ap, 0.0), writes=[St.r])
            for n in order:
                cols = slice(n * 128, (n + 1) * 128)
                qf, kf, vf, xf, zf = qfb[nch % 2], kfb[nch % 2], vfb[nch % 2], xfb[nch % 2], zfb[nch % 2]
                nch += 1
                dma(SP, qf.ap, hview(QF.ap)[:, :, cols], reads=[QF.sub[n]], writes=[qf.r])
                dma(SP, kf.ap, hview(KF.ap)[:, :, cols], reads=[KF.sub[n]], writes=[kf.r])
                dma(SP, vf.ap, hview(VF.ap)[:, :, cols], reads=[VF.sub[n]], writes=[vf.r])
                if d == 1:
                    dma(SP, xf.ap, hview(OF.ap)[:, :, cols], reads=[OF.sub[n]], writes=[xf.r])
                    dma(SP, zf.ap, hview(ZF.ap)[:, :, cols], reads=[ZF.sub[n]], writes=[zf.r])
                gsl = gT.ap[:, n, d * 8:(d + 1) * 8]
                bsl = bT.ap[:, n, d * 8:(d + 1) * 8]
                psG = self.psum()
                op(PE, lambda e, psG=psG, TRI=TRI, gsl=gsl: e.matmul(psG.ap[:, 0:8], lhsT=TRI.ap, rhs=gsl, start=True, stop=True), reads=[TRI.r, gT.r], writes=psG.sub)
                op(DVE, lambda e, gsl=gsl, TRI=TRI: e.tensor_tensor(out=Rt.ap, in0=gsl.unsqueeze(2).to_broadcast([128, 8, 128]), in1=bc_m(TRI, 8), op=ALU.mult),
                   reads=[gT.r, TRI.r], writes=[Rt.r])
                psGb = [self.psum(), self.psum()]
                for j in range(2):
                    op(PE, lambda e, j=j, p=psGb[j]: e.matmul(p.ap, lhsT=ones32.ap, rhs=fl(Rt, j).rearrange("p a b -> p (a b)"), start=True, stop=True),
                       reads=[ones32.r, Rt.r], writes=psGb[j].sub)
                op(ACT, lambda e, psG=psG: e.copy(out=Gtok.ap, in_=psG.ap[:, 0:8]), reads=psG.sub, writes=[Gtok.r])
                for j in range(2):
                    op(DVE, lambda e, j=j, p=psGb[j]: e.tensor_tensor(out=fl(dTt, j), in0=pv4(p), in1=bc_h(Gtok.ap, j), op=ALU.subtract), reads=psGb[j].sub + [Gtok.r], writes=[dTt.r])
                    op(ACT, lambda e, j=j, p=psGb[j]: e.activation(out=fl(eGbc, j), in_=pv4(p), func=AF.Exp), reads=psGb[j].sub, writes=[eGbc.r])
                op(DVE, lambda e, maskN=maskN: e.scalar_tensor_tensor(out=d2t.ap, in0=dTt.ap, scalar=-1.0, in1=bc_m(maskN, 8), op0=ALU.mult, op1=ALU.add),
                   reads=[dTt.r, maskN.r], writes=[d2t.r])
                op(POOL, lambda e, maskT=maskT: e.tensor_tensor(out=dTt.ap, in0=dTt.ap, in1=bc_m(maskT, 8), op=ALU.add), reads=[dTt.r, maskT.r, d2t.r], writes=[dTt.r])
                op(ACT, lambda e: e.activation(out=dec.ap, in_=d2t.ap, func=AF.Exp), reads=[d2t.r], writes=[dec.r])
                op(ACT, lambda e: e.activation(out=decT.ap, in_=dTt.ap, func=AF.Exp), reads=[dTt.r], writes=[decT.r])
                op(ACT, lambda e: e.activation(out=eGtok.ap, in_=Gtok.ap, func=AF.Exp), reads=[Gtok.r], writes=[eGtok.r])
                op(DVE, lambda e, bsl=bsl: e.tensor_tensor(out=bg.ap, in0=eGtok.ap, in1=bsl, op=ALU.mult), reads=[eGtok.r, bT.r], writes=[bg.r])
                op(DVE, lambda e, bsl=bsl: e.tensor_tensor(out=Rt.ap, in0=bsl.unsqueeze(2).to_broadcast([128, 8, 128]), in1=bc_m(i32, 8), op=ALU.mult),
                   reads=[bT.r, i32.r], writes=[Rt.r])
                psBb = [self.psum(), self.psum()]
                for j in range(2):
                    op(PE, lambda e, j=j, p=psBb[j]: e.matmul(p.ap, lhsT=ones32.ap, rhs=fl(Rt, j).rearrange("p a b -> p (a b)"), start=True, stop=True),
                       reads=[ones32.r, Rt.r], writes=psBb[j].sub)
                for j in range(2):
                    op(DVE, lambda e, j=j, p=psBb[j]: e.tensor_tensor(out=fl(AmT, j), in0=fl(decT, j), in1=pv4(p), op=ALU.mult), reads=psBb[j].sub + [decT.r], writes=[AmT.r])
                op(POOL, lambda e, strT=strT: e.tensor_tensor(out=AmT.ap, in0=AmT.ap, in1=bc_m(strT, 8), op=ALU.mult), reads=[AmT.r, strT.r], writes=[AmT.r])
                op(POOL, lambda e, bsl=bsl: e.tensor_tensor(out=An.ap, in0=dec.ap, in1=bsl.unsqueeze(2).to_broadcast([128, 8, 128]), op=ALU.mult), reads=[dec.r, bT.r], writes=[An.r])
                op(POOL, lambda e, strN=strN: e.tensor_tensor(out=An.ap, in0=An.ap, in1=bc_m(strN, 8), op=ALU.mult), reads=[An.r, strN.r], writes=[An.r])
                op(POOL, lambda e, qf=qf: e.tensor_tensor(out=qdT.ap, in0=qf.ap, in1=eGbc.ap, op=ALU.mult), reads=[qf.r, eGbc.r], writes=[qdT.r])
                if self.cfg.get('gdn_stage', 99) < 1:
                    continue
                psK = [self.psum(), self.psum()]
                psV = [self.psum(), self.psum()]
                for h in range(8):
                    j, q4 = h // 4, (h % 4) * 128
                    op(PE, lambda e, h=h, p=psK[j], q4=q4, kf=kf: e.transpose(out=p.ap[:, q4:q4 + 128], in_=kf.ap[:, h, :], identity=i32.ap), reads=[kf.r, i32.r], writes=[psK[j].sub[h % 4]])
                    op(PE, lambda e, h=h, p=psV[j], q4=q4, vf=vf: e.transpose(out=p.ap[:, q4:q4 + 128], in_=vf.ap[:, h, :], identity=i32.ap), reads=[vf.r, i32.r], writes=[psV[j].sub[h % 4]])
                for j in range(2):
                    op(DVE, lambda e, j=j, p=psK[j]: e.tensor_tensor(out=fl(kbg, j), in0=pv4(p), in1=bc_h(bg.ap, j), op=ALU.mult), reads=psK[j].sub + [bg.r], writes=[kbg.r])
                    op(DVE, lambda e, j=j, p=psK[j], last=last: e.tensor_tensor(out=fl(ktail, j), in0=pv4(p), in1=decT.ap[:, 4 * j:4 * j + 4, last:last + 1].to_broadcast([128, 4, 128]), op=ALU.mult),
                       reads=psK[j].sub + [decT.r], writes=[ktail.r])
                    op(DVE, lambda e, j=j, p=psV[j], bsl=bsl: e.tensor_tensor(out=fl(vb, j), in0=pv4(p), in1=bc_h(bsl, j), op=ALU.mult), reads=psV[j].sub + [bT.r], writes=[vb.r])
                if self.cfg.get('gdn_stage', 99) < 2:
                    continue
                psKK = [self.psum(), self.psum()]
                psKQ = [self.psum(), self.psum()]
                for h in range(8):
                    j, q4 = h // 4, (h % 4) * 128
                    op(PE, lambda e, h=h, p=psKK[j], q4=q4, kf=kf: e.matmul(p.ap[:, q4:q4 + 128], lhsT=kf.ap[:, h, :], rhs=kf.ap[:, h, :], start=True, stop=True), reads=[kf.r], writes=[psKK[j].sub[h % 4]])
                    op(PE, lambda e, h=h, p=psKQ[j], q4=q4, kf=kf, qf=qf: e.matmul(p.ap[:, q4:q4 + 128], lhsT=kf.ap[:, h, :], rhs=qf.ap[:, h, :], start=True, stop=True), reads=[kf.r, qf.r], writes=[psKQ[j].sub[h % 4]])
                M0, N0 = Mb[0], Nb[0]
                for j in range(2):
                    op(DVE, lambda e, j=j, p=psKK[j]: e.scalar_tensor_tensor(out=fl(M0, j), in0=pv4(p), scalar=-1.0, in1=fl(AmT, j), op0=ALU.mult, op1=ALU.mult), reads=psKK[j].sub + [AmT.r], writes=[M0.r])
                    op(DVE, lambda e, j=j, p=psKK[j]: e.scalar_tensor_tensor(out=fl(N0, j), in0=pv4(p), scalar=-1.0, in1=fl(An, j), op0=ALU.mult, op1=ALU.mult), reads=psKK[j].sub + [An.r], writes=[N0.r])
                    op(DVE, lambda e, j=j, p=psKQ[j]: e.tensor_tensor(out=fl(attnT, j), in0=pv4(p), in1=fl(decT, j), op=ALU.mult), reads=psKQ[j].sub + [decT.r], writes=[attnT.r])
                MA, NA, MB, NB = Mb[1], Nb[1], Mc[0], Nc[0]
                op(DVE, lambda e: e.tensor_tensor(out=MA.ap, in0=M0.ap, in1=bc_m(gm[0], 8), op=ALU.mult), reads=[M0.r, gm[0].r], writes=[MA.r])
                op(POOL, lambda e: e.tensor_tensor(out=NA.ap, in0=N0.ap, in1=bc_m(gm[0], 8), op=ALU.mult), reads=[N0.r, gm[0].r], writes=[NA.r])
                op(POOL, lambda e: e.tensor_tensor(out=Pm.ap, in0=MA.ap, in1=bc_m(i32, 8), op=ALU.add), reads=[MA.r, i32.r], writes=[Pm.r])
                op(POOL, lambda e: e.tensor_tensor(out=PT.ap, in0=NA.ap, in1=bc_m(i32, 8), op=ALU.add), reads=[NA.r, i32.r], writes=[PT.r])
                cur = (MA, NA)
                nxt = (MB, NB)
                for kk in range(1, 4):
                    Mp, Np = cur
                    Mn, Nn = nxt
                    psN = [self.psum(), self.psum()]
                    psM = [self.psum(), self.psum()]
                    for h in range(8):
                        j, q4 = h // 4, (h % 4) * 128
                        op(PE, lambda e, h=h, p=psN[j], q4=q4, Mp=Mp, Np=Np: e.matmul(p.ap[:, q4:q4 + 128], lhsT=Mp.ap[:, h, :], rhs=Np.ap[:, h, :], start=True, stop=True),
                           reads=[Mp.r, Np.r], writes=[psN[j].sub[h % 4]])
                        op(PE, lambda e, h=h, p=psM[j], q4=q4, Mp=Mp, Np=Np: e.matmul(p.ap[:, q4:q4 + 128], lhsT=Np.ap[:, h, :], rhs=Mp.ap[:, h, :], start=True, stop=True),
                           reads=[Mp.r, Np.r], writes=[psM[j].sub[h % 4]])
                    op(ACT, lambda e, j=0, p=psN[0], Nn=Nn: e.copy(out=fl(Nn, j), in_=pv4(p)), reads=psN[0].sub, writes=[Nn.r])
                    op(DVE, lambda e, j=1, p=psN[1], Nn=Nn: e.tensor_copy(out=fl(Nn, j), in_=pv4(p)), reads=psN[1].sub, writes=[Nn.r])
                    op(DVE, lambda e, j=0, p=psM[0], Mn=Mn: e.tensor_copy(out=fl(Mn, j), in_=pv4(p)), reads=psM[0].sub, writes=[Mn.r])
                    op(ACT, lambda e, j=1, p=psM[1], Mn=Mn: e.copy(out=fl(Mn, j), in_=pv4(p)), reads=psM[1].sub, writes=[Mn.r])
                    psP = [self.psum(), self.psum()]
                    psQ_ = [self.psum(), self.psum()]
                    for h in range(8):
                        j, q4 = h // 4, (h % 4) * 128
                        op(PE, lambda e, h=h, p=psP[j], q4=q4, Nn=Nn: e.matmul(p.ap[:, q4:q4 + 128], lhsT=Nn.ap[:, h, :], rhs=Pm.ap[:, h, :], start=True, stop=True),
                           reads=[Nn.r, Pm.r], writes=[psP[j].sub[h % 4]])
                        op(PE, lambda e, h=h, p=psQ_[j], q4=q4, Mn=Mn: e.matmul(p.ap[:, q4:q4 + 128], lhsT=Mn.ap[:, h, :], rhs=PT.ap[:, h, :], start=True, stop=True),
                           reads=[Mn.r, PT.r], writes=[psQ_[j].sub[h % 4]])
                    for j in range(2):
                        op(DVE, lambda e, j=j, p=psP[j]: e.tensor_tensor(out=fl(Pm, j), in0=fl(Pm, j), in1=pv4(p), op=ALU.add), reads=psP[j].sub + [Pm.r], writes=[Pm.r])
                        op(DVE, lambda e, j=j, p=psQ_[j]: e.tensor_tensor(out=fl(PT, j), in0=fl(PT, j), in1=pv4(p), op=ALU.add), reads=psQ_[j].sub + [PT.r], writes=[PT.r])
                    cur, nxt = nxt, cur
                for lv in range(3):
                    UoT, Yt = Mc[0], Nc[0]
                    op(DVE, lambda e, lv=lv, UoT=UoT: e.scalar_tensor_tensor(out=UoT.ap, in0=N0.ap, scalar=-1.0, in1=bc_m(gm[1 + lv], 8), op0=ALU.mult, op1=ALU.mult),
                       reads=[N0.r, gm[1 + lv].r], writes=[UoT.r])
                    psY = [self.psum(), self.psum()]
                    for h in range(8):
                        j, q4 = h // 4, (h % 4) * 128
                        op(PE, lambda e, h=h, p=psY[j], q4=q4, UoT=UoT: e.matmul(p.ap[:, q4:q4 + 128], lhsT=UoT.ap[:, h, :], rhs=Pm.ap[:, h, :], start=True, stop=True),
                           reads=[UoT.r, Pm.r], writes=[psY[j].sub[h % 4]])
                    op(ACT, lambda e, j=0, p=psY[0], Yt=Yt: e.copy(out=fl(Yt, j), in_=pv4(p)), reads=psY[0].sub, writes=[Yt.r])
                    op(DVE, lambda e, j=1, p=psY[1], Yt=Yt: e.tensor_copy(out=fl(Yt, j), in_=pv4(p)), reads=psY[1].sub, writes=[Yt.r])
                    psX = [self.psum(), self.psum()]
                    psXT = [self.psum(), self.psum()]
                    for h in range(8):
                        j, q4 = h // 4, (h % 4) * 128
                        op(PE, lambda e, h=h, p=psX[j], q4=q4, Yt=Yt: e.matmul(p.ap[:, q4:q4 + 128], lhsT=PT.ap[:, h, :], rhs=Yt.ap[:, h, :], start=True, stop=True),
                           reads=[PT.r, Yt.r], writes=[psX[j].sub[h % 4]])
                        if lv < 2:
                            op(PE, lambda e, h=h, p=psXT[j], q4=q4, Yt=Yt: e.matmul(p.ap[:, q4:q4 + 128], lhsT=Yt.ap[:, h, :], rhs=PT.ap[:, h, :], start=True, stop=True),
                               reads=[PT.r, Yt.r], writes=[psXT[j].sub[h % 4]])
                    for j in range(2):
                        op(DVE, lambda e, j=j, p=psX[j]: e.tensor_tensor(out=fl(Pm, j), in0=fl(Pm, j), in1=pv4(p), op=ALU.subtract), reads=psX[j].sub + [Pm.r], writes=[Pm.r])
                        if lv < 2:
                            op(DVE, lambda e, j=j, p=psXT[j]: e.tensor_tensor(out=fl(PT, j), in0=fl(PT, j), in1=pv4(p), op=ALU.subtract), reads=psXT[j].sub + [PT.r], writes=[PT.r])
                if self.cfg.get('gdn_stage', 99) < 4:
                    continue
                psW = [self.psum(), self.psum()]
                for h in range(8):
                    j, q4 = h // 4, (h % 4) * 128
                    op(PE, lambda e, h=h, p=psW[j], q4=q4: e.matmul(p.ap[:, q4:q4 + 128], lhsT=kbg.ap[:, h, :], rhs=Pm.ap[:, h, :], start=True, stop=True), reads=[kbg.r, Pm.r], writes=[psW[j].sub[h % 4]])
                for j in range(2):
                    op(ACT, lambda e, j=j, p=psW[j]: e.mul(out=fl(nwT, j), in_=pv4(p), mul=-1.0), reads=psW[j].sub, writes=[nwT.r])
                psVn = [self.psum(), self.psum()]
                for h in range(8):
                    j, q4 = h // 4, (h % 4) * 128
                    op(PE, lambda e, h=h, p=psVn[j], q4=q4: e.matmul(p.ap[:, q4:q4 + 128], lhsT=Pm.ap[:, h, :], rhs=vb.ap[:, h, :], start=True, stop=False), reads=[Pm.r, vb.r], writes=[psVn[j].sub[h % 4]])
                    op(PE, lambda e, h=h, p=psVn[j], q4=q4: e.matmul(p.ap[:, q4:q4 + 128], lhsT=nwT.ap[:, h, :], rhs=St.ap[:, h, :], start=False, stop=True), reads=[nwT.r, St.r], writes=[psVn[j].sub[h % 4]])
                op(ACT, lambda e, j=0, p=psVn[0]: e.copy(out=fl(vnew, j), in_=pv4(p)), reads=psVn[0].sub, writes=[vnew.r])
                op(DVE, lambda e, j=1, p=psVn[1]: e.tensor_copy(out=fl(vnew, j), in_=pv4(p)), reads=psVn[1].sub, writes=[vnew.r])
                if self.cfg.get('gdn_stage', 99) < 5:
                    continue
                psO = [self.psum(), self.psum()]
                psS = [self.psum(), self.psum()]
                for h in range(8):
                    j, q4 = h // 4, (h % 4) * 128
                    op(PE, lambda e, h=h, p=psO[j], q4=q4: e.matmul(p.ap[:, q4:q4 + 128], lhsT=St.ap[:, h, :], rhs=qdT.ap[:, h, :], start=True, stop=False), reads=[St.r, qdT.r], writes=[psO[j].sub[h % 4]])
                    op(PE, lambda e, h=h, p=psO[j], q4=q4: e.matmul(p.ap[:, q4:q4 + 128], lhsT=vnew.ap[:, h, :], rhs=attnT.ap[:, h, :], start=False, stop=True), reads=[vnew.r, attnT.r], writes=[psO[j].sub[h % 4]])
                    op(PE, lambda e, h=h, p=psS[j], q4=q4: e.matmul(p.ap[:, q4:q4 + 128], lhsT=ktail.ap[:, h, :], rhs=vnew.ap[:, h, :], start=True, stop=True), reads=[ktail.r, vnew.r], writes=[psS[j].sub[h % 4]])
                for j in range(2):
                    op(DVE, lambda e, j=j, last=last: e.tensor_tensor(out=fl(St, j), in0=fl(St, j), in1=eGbc.ap[:, 4 * j:4 * j + 4, last:last + 1].to_broadcast([128, 4, 128]), op=ALU.mult),
                       reads=[St.r, eGbc.r], writes=[St.r])
                    op(DVE, lambda e, j=j, p=psS[j]: e.tensor_tensor(out=fl(St, j), in0=fl(St, j), in1=pv4(p), op=ALU.add), reads=psS[j].sub + [St.r], writes=[St.r])
                if d == 0:
                    for j in range(2):
                        op(ACT, lambda e, j=j, p=psO[j], xf=xf: e.copy(out=fl(xf, j), in_=pv4(p)), reads=psO[j].sub, writes=[xf.r])
                    dma(SP, hview(OF.ap)[:, :, cols], xf.ap, reads=[xf.r], writes=[OF.sub[n]])
                    continue
                for j in range(2):
                    op(DVE, lambda e, j=j, p=psO[j], xf=xf: e.tensor_tensor(out=fl(xf, j), in0=fl(xf, j), in1=pv4(p), op=ALU.add), reads=psO[j].sub + [xf.r], writes=[xf.r])
                op(POOL, lambda e, xf=xf: e.tensor_tensor(out=Rt.ap, in0=xf.ap, in1=xf.ap, op=ALU.mult), reads=[xf.r], writes=[Rt.r])
                psQ = [self.psum(), self.psum()]
                for j in range(2):
                    op(PE, lambda e, j=j, p=psQ[j]: e.matmul(p.ap, lhsT=ones32.ap, rhs=fl(Rt, j).rearrange("p a b -> p (a b)"), start=True, stop=True), reads=[ones32.r, Rt.r], writes=psQ[j].sub)
                for j in range(2):
                    op(DVE, lambda e, j=j, p=psQ[j]: e.tensor_scalar(out=fl(d2t, j), in0=pv4(p), scalar1=1.0 / 128.0, scalar2=LN_EPS, op0=ALU.mult, op1=ALU.add), reads=psQ[j].sub, writes=[d2t.r])
                op(ACT, lambda e: e.activation(out=d2t.ap, in_=d2t.ap, func=AF.Ln), reads=[d2t.r], writes=[d2t.r])
                op(ACT, lambda e: e.activation(out=d2t.ap, in_=d2t.ap, func=AF.Exp, scale=-0.5), reads=[d2t.r], writes=[d2t.r])
                op(POOL, lambda e, xf=xf: e.tensor_tensor(out=xf.ap, in0=xf.ap, in1=d2t.ap, op=ALU.mult), reads=[xf.r, d2t.r], writes=[xf.r])
                op(POOL, lambda e, xf=xf, zf=zf: e.tensor_tensor(out=yT.ap, in0=xf.ap, in1=zf.ap, op=ALU.mult), reads=[xf.r, zf.r], writes=[yT.r])
                yt = ytile[n % 2]
                for half in range(2):
                    ps = self.psum()
                    for h in range(8):
                        op(PE, lambda e, ps=ps, h=h, half=half: e.matmul(ps.ap, lhsT=yT.ap[:, h, :], rhs=wo.ap[:, h, half * 512:(half + 1) * 512], start=(h == 0), stop=(h == 7)),
                           reads=[yT.r, wo.r], writes=ps.sub)
                    op(ACT, lambda e, ps=ps, yt=yt, half=half: e.copy(out=yt.ap[:, half * 512:(half + 1) * 512], in_=ps.ap), reads=ps.sub, writes=[yt.r])
                dma(SP, self.Y.ap[cols, :], yt.ap, reads=[yt.r], writes=[self.Y.sub[n]])
                if self.cfg.get("test") == "mix":
                    dma(SP, self.dbg.ap[cols, :], yt.ap, reads=[yt.r], writes=[self.dbg.sub[n]])
        if self.cfg.get("test") == "gdn2":
            self.P.barrier()
            rr = Res("dbg2")
            dma(SP, self.dbg2[0], VF.ap if self.cfg.get('gdn_nch') == 1 else OF.ap, reads=OF.sub + VF.sub, writes=[rr])
            dma(SP, self.dbg2[1, 0:128, 0:1024], St.ap.rearrange("p a b -> p (a b)"), reads=[St.r], writes=[rr])
            for ii, tt in enumerate((decT, dec, AmT, An, Pm, vnew, attnT, kbg, vb, ktail, qdT, eGbc, nwT, qfb[0], kfb[0], vfb[0])):
                dma(SP, self.dbg2[2 + ii // 8, (ii % 8) * 128:(ii % 8 + 1) * 128, 0:1024], tt.ap.rearrange("p a b -> p (a b)"), reads=[tt.r], writes=[rr])
            dma(SP, self.dbg3[:, 0], gT.ap, reads=[gT.r], writes=[rr])
            dma(SP, self.dbg3[:, 1], bT.ap, reads=[bT.r], writes=[rr])
            self.dbg.sub.append(rr)

    def out_proj(self, w_o):
        op, dma = self.op, self.dma
        self.P.barrier()
        self.off = self.base_off
        OD = self.OD
        wo = self.tile([128, 8, D], BF16, "wo")
        dma(POOL, wo.ap, w_o.rearrange("(k p) n -> p k n", p=128), writes=[wo.r])
        ob = [self.tile([128, D], BF16, "ob") for _ in range(3)]
        oT = [self.tile([128, 8, 128], BF16, "oT", nsub=1) for _ in range(2)]
        yb = [self.tile([128, D], F32, "yb") for _ in range(2)]
        for i in range(NT):
            o, t, y = ob[i % 3], oT[i % 2], yb[i % 2]
            rows = slice(i * 128, (i + 1) * 128)
            dma(SP, o.ap, OD.ap[rows, :], reads=[OD.sub[i]], writes=[o.r])
            self.transpose_to(o, t, 0)
            for half in range(2):
                ps = self.psum()
                for k in range(8):
                    op(PE, lambda e, ps=ps, t=t, k=k, half=half: e.matmul(ps.ap, lhsT=t.ap[:, k, :], rhs=wo.ap[:, k, half * 512:(half + 1) * 512], start=(k == 0), stop=(k == 7)),
                       reads=[t.sub[0], wo.r], writes=[ps.r])
                op(ACT if half else DVE, (lambda e, ps=ps, y=y, half=half: e.copy(out=y.ap[:, half * 512:(half + 1) * 512], in_=ps.ap)) if half else
                   (lambda e, ps=ps, y=y, half=half: e.tensor_copy(out=y.ap[:, half * 512:(half + 1) * 512], in_=ps.ap)), reads=[ps.r], writes=[y.r])
            dma(SP, self.Y.ap[rows, :], y.ap, reads=[y.r], writes=[self.Y.sub[i]])
            if self.cfg.get("test") == "mix":
                dma(SP, self.dbg.ap[rows, :], y.ap, reads=[y.r], writes=[self.dbg.sub[i]])

    def postmix(self, l):
        op, dma = self.op, self.dma
        self.phase_begin()
        self.h2tok = self.tile([128, NT, D], BF16, "h2tok", nsub=NT)
        self.affTok = self.tile([128, NT, NE], F32, "affTok")
        h2tok, affTok = self.h2tok, self.affTok
        self.post_keep = self.off
        mods = self.mod_tiles(l, [2, 3, 4], plus1=(4,))
        g0 = self.ln_vec(self.ln_g[l, 0:1, :])
        b0 = self.ln_vec(self.ln_b[l, 0:1, :])
        wr = self.tile([128, 8, NE], BF16, "wr")
        dma(POOL, wr.ap, self.moe_w_router[l].rearrange("(k p) n -> p k n", p=128), writes=[wr.r])
        xb = [self.tile([128, D], F32, "xb") for _ in range(2)]
        yb = [self.tile([128, D], F32, "yb") for _ in range(2)]
        ob = [self.tile([128, D], F32, "ob") for _ in range(2)]
        h2T = [self.tile([128, 8, 128], BF16, "h2T", nsub=1) for _ in range(2)]
        st = self.tile([128, 2, 6], F32, "st")
        mv = self.tile([128, 4], F32, "mv")
        sm = self.tile([128, 4], F32, "sm")
        ex = self.tile([128, NE], F32, "ex")
        for i in range(NT):
            x, y, o = xb[i % 2], yb[i % 2], ob[i % 2]
            c = 1 if i < 2 else 0
            rows = slice(i * 128, (i + 1) * 128)
            dma(SP, x.ap, self.XS.ap[rows, :], reads=[self.XS.sub[i]], writes=[x.r])
            dma(SP, y.ap, self.Y.ap[rows, :], reads=[self.Y.sub[i]], writes=[y.r])
            m2, m3, m4 = mods[2][c], mods[3][c], mods[4][c]
            op(POOL, lambda e, y=y, m2=m2: e.tensor_tensor(out=y.ap, in0=y.ap, in1=m2.ap, op=ALU.mult), reads=[y.r, m2.r], writes=[y.r])
            op(DVE, lambda e, x=x, y=y: e.scalar_tensor_tensor(out=y.ap, in0=x.ap, scalar=ALPHA, in1=y.ap, op0=ALU.mult, op1=ALU.add),
               reads=[x.r, y.r], writes=[y.r])
            self.layernorm_(y, st, mv)
            op(POOL, lambda e, y=y, o=o: e.tensor_tensor(out=o.ap, in0=y.ap, in1=g0.ap, op=ALU.mult), reads=[y.r, g0.r], writes=[o.r])
            op(POOL, lambda e, o=o: e.tensor_tensor(out=o.ap, in0=o.ap, in1=b0.ap, op=ALU.add), reads=[o.r, b0.r], writes=[o.r])
            dma(SP, self.XS.ap[rows, :], o.ap, reads=[o.r], writes=[self.XS.sub[i]])
            op(DVE, lambda e, o=o, x=x, m4=m4: e.tensor_tensor(out=x.ap, in0=o.ap, in1=m4.ap, op=ALU.mult), reads=[o.r, m4.r], writes=[x.r])
            hv = Tl(h2tok.ap[:, i, :])
            hv.r = h2tok.sub[i]
            op(DVE, lambda e, x=x, m3=m3, hv=hv: e.tensor_tensor(out=hv.ap, in0=x.ap, in1=m3.ap, op=ALU.add), reads=[x.r, m3.r], writes=[hv.r])
            ht = h2T[i % 2]
            self.transpose_to(hv, ht, 0)
            ps = self.psum()
            for k in range(8):
                op(PE, lambda e, ps=ps, ht=ht, k=k: e.matmul(ps.ap[:, 0:NE], lhsT=ht.ap[:, k, :], rhs=wr.ap[:, k, :], start=(k == 0), stop=(k == 7)),
                   reads=[ht.sub[0], wr.r], writes=[ps.r])
            op(DVE, lambda e, ps=ps: e.reduce_max(out=sm.ap[:, 0:1], in_=ps.ap[:, 0:NE], axis=AX.X), reads=[ps.r], writes=[sm.r])
            op(DVE, lambda e: e.tensor_scalar(out=sm.ap[:, 1:2], in0=sm.ap[:, 0:1], scalar1=-1.0, scalar2=None, op0=ALU.mult), reads=[sm.r], writes=[sm.r])
            op(ACT, lambda e, ps=ps: e.activation(out=ex.ap, in_=ps.ap[:, 0:NE], func=AF.Exp, bias=sm.ap[:, 1:2], scale=1.0, accum_out=sm.ap[:, 2:3]),
               reads=[ps.r, sm.r], writes=[ex.r, sm.r])
            op(DVE, lambda e: e.reciprocal(out=sm.ap[:, 3:4], in_=sm.ap[:, 2:3]), reads=[sm.r], writes=[sm.r])
            op(DVE, lambda e, i=i: e.tensor_scalar(out=affTok.ap[:, i, :], in0=ex.ap, scalar1=sm.ap[:, 3:4], scalar2=None, op0=ALU.mult),
               reads=[ex.r, sm.r], writes=[affTok.r])

    def moe_select(self, l):
        op, dma = self.op, self.dma
        affTok = self.affTok
        self.P.barrier()
        self.off = self.post_keep
        self.posm = self.tile([128, NT, NE], F32, "posm")
        posm = self.posm
        keep2 = self.off
        affT = self.tile([NE, TT], F32, "affT")
        work = self.tile([NE, 4096], F32, "work")
        m8 = self.tile([NE, 8], F32, "m8")
        thr = self.tile([NE, 2], F32, "thr")
        maskT = self.tile([NE, TT], BF16, "maskT")
        for g in range(9):
            ps = self.psum()
            tl = list(range(g * 4, min(g * 4 + 4, NT)))
            for j, i in enumerate(tl):
                op(PE, lambda e, ps=ps, i=i, j=j: e.transpose(out=ps.ap[0:NE, j * 128:(j + 1) * 128], in_=affTok.ap[:, i, :], identity=self.ident32.ap),
                   reads=[affTok.r, self.ident32.r], writes=[ps.r])
            n = len(tl) * 128
            op(ACT, lambda e, ps=ps, g=g, n=n: e.copy(out=affT.ap[:, g * 512:g * 512 + n], in_=ps.ap[0:NE, 0:n]), reads=[ps.r], writes=[affT.r])
        dma(SP, self.AFFT.ap, affT.ap, reads=[affT.r], writes=[self.AFFT.r])
        op(DVE, lambda e: e.tensor_copy(out=work.ap[:, 0:256], in_=affT.ap[:, 0:256]), reads=[affT.r], writes=[work.r])
        for it in range(4):
            op(DVE, lambda e: e.max(out=m8.ap, in_=work.ap[:, 0:256]), reads=[work.r], writes=[m8.r])
            if it < 3:
                op(DVE, lambda e: e.match_replace(out=work.ap[:, 0:256], in_to_replace=m8.ap, in_values=work.ap[:, 0:256], imm_value=-1.0),
                   reads=[work.r, m8.r], writes=[work.r])
        op(DVE, lambda e: e.tensor_copy(out=thr.ap[:, 0:1], in_=m8.ap[:, 7:8]), reads=[m8.r], writes=[thr.r])
        if not hasattr(self, "CAND"):
            self.CAND = Tl(self.dram("CAND", [128, 128], F32), "CAND")
        w1 = self.tile([128, 512], F32, "w1")
        c1 = self.tile([128, 128], F32, "c1")
        for e_ in range(NE):
            dma(SP, w1.ap[e_ * 8:(e_ + 1) * 8, :], self.AFFT.ap[e_, 256:TT].rearrange("(s t) -> s t", s=8), reads=[self.AFFT.r], writes=[w1.r])
        for it in range(16):
            op(DVE, lambda e, it=it: e.max(out=c1.ap[:, it * 8:(it + 1) * 8], in_=w1.ap), reads=[w1.r], writes=[c1.r])
            if it < 15:
                op(DVE, lambda e, it=it: e.match_replace(out=w1.ap, in_to_replace=c1.ap[:, it * 8:(it + 1) * 8], in_values=w1.ap, imm_value=-1.0),
                   reads=[w1.r, c1.r], writes=[w1.r])
        dma(SP, self.CAND.ap, c1.ap, reads=[c1.r], writes=[self.CAND.r])
        dma(SP, work.ap[:, 0:1024], self.CAND.ap.rearrange("(e s) c -> e (s c)", s=8), reads=[self.CAND.r], writes=[work.r])
        for it in range(64):
            op(DVE, lambda e: e.max(out=m8.ap, in_=work.ap[:, 0:1024]), reads=[work.r], writes=[m8.r])
            if it < 63:
                op(DVE, lambda e: e.match_replace(out=work.ap[:, 0:1024], in_to_replace=m8.ap, in_values=work.ap[:, 0:1024], imm_value=-1.0),
                   reads=[work.r, m8.r], writes=[work.r])
        op(DVE, lambda e: e.tensor_copy(out=thr.ap[:, 1:2], in_=m8.ap[:, 7:8]), reads=[m8.r], writes=[thr.r])
        for (lo, n, col) in ((0, 256, 0), (256, 4096, 1)):
            op(DVE, lambda e, lo=lo, n=n, col=col: e.tensor_scalar(out=maskT.ap[:, lo:lo + n], in0=affT.ap[:, lo:lo + n], scalar1=thr.ap[:, col:col + 1],
                                                                   scalar2=None, op0=ALU.is_ge), reads=[affT.r, thr.r], writes=[maskT.r])
        maskTok = self.tile([128, NT, NE], BF16, "maskTok")
        maskF = self.tile([128, NT, NE], F32, "maskF")
        ps = self.psum()
        pb = ps.ap.bitcast(BF16)
        for i in range(NT):
            op(PE, lambda e, i=i, pb=pb: e.transpose(out=pb[:, i * NE:(i + 1) * NE], in_=maskT.ap[:, i * 128:(i + 1) * 128], identity=self.ident.ap[0:NE, 0:NE]),
               reads=[maskT.r, self.ident.r], writes=[ps.r])
        op(DVE, lambda e, pb=pb: e.tensor_copy(out=maskTok.ap.rearrange("p a b -> p (a b)"), in_=pb[:, 0:NT * NE]), reads=[ps.r], writes=[maskTok.r])
        op(DVE, lambda e: e.tensor_copy(out=maskF.ap, in_=maskTok.ap), reads=[maskTok.r], writes=[maskF.r])
        mflat = maskTok.ap.rearrange("p a b -> p (a b)")
        ps_w, ps_t, ps_c = self.psum(), self.psum(), self.psum()
        op(PE, lambda e: e.matmul(ps_w.ap, lhsT=self.triu.ap, rhs=mflat[:, 2 * NE:NT * NE], start=True, stop=True), reads=[maskTok.r, self.triu.r], writes=[ps_w.r])
        op(PE, lambda e: e.matmul(ps_t.ap, lhsT=self.ones.ap, rhs=mflat[:, 2 * NE:NT * NE], start=True, stop=True), reads=[maskTok.r, self.ones.r], writes=[ps_t.r])
        op(PE, lambda e: e.matmul(ps_c.ap[:, 0:2 * NE], lhsT=self.triu.ap, rhs=mflat[:, 0:2 * NE], start=True, stop=True), reads=[maskTok.r, self.triu.r], writes=[ps_c.r])
        op(PE, lambda e: e.matmul(ps_c.ap[:, 2 * NE:4 * NE], lhsT=self.ones.ap, rhs=mflat[:, 0:2 * NE], start=True, stop=True), reads=[maskTok.r, self.ones.r], writes=[ps_c.r])
        eoff = self.tile([128, NT, NE], F32, "eoff")
        tot = self.tile([128, NT, NE], F32, "tot")
        op(DVE, lambda e: e.tensor_copy(out=tot.ap[:, 2:NT, :].rearrange("p a b -> p (a b)"), in_=ps_t.ap), reads=[ps_t.r], writes=[tot.r])
        op(DVE, lambda e: e.tensor_copy(out=tot.ap[:, 0:2, :].rearrange("p a b -> p (a b)"), in_=ps_c.ap[:, 2 * NE:4 * NE]), reads=[ps_c.r], writes=[tot.r])
        op(DVE, lambda e: e.memset(eoff.ap, 0.0), writes=[eoff.r])
        op(DVE, lambda e: e.tensor_copy(out=eoff.ap[:, 1, :], in_=tot.ap[:, 0, :]), reads=[tot.r], writes=[eoff.r])
        for i in range(3, NT):
            op(DVE, lambda e, i=i: e.tensor_tensor(out=eoff.ap[:, i, :], in0=eoff.ap[:, i - 1, :], in1=tot.ap[:, i - 1, :], op=ALU.add),
               reads=[eoff.r, tot.r], writes=[eoff.r])
        op(DVE, lambda e: e.tensor_tensor(out=posm.ap[:, 2:NT, :].rearrange("p a b -> p (a b)"), in0=ps_w.ap,
                                          in1=eoff.ap[:, 2:NT, :].rearrange("p a b -> p (a b)"), op=ALU.add), reads=[ps_w.r, eoff.r], writes=[posm.r])
        op(DVE, lambda e: e.tensor_tensor(out=posm.ap[:, 0:2, :].rearrange("p a b -> p (a b)"), in0=ps_c.ap[:, 0:2 * NE],
                                          in1=eoff.ap[:, 0:2, :].rearrange("p a b -> p (a b)"), op=ALU.add), reads=[ps_c.r, eoff.r], writes=[posm.r])
        op(DVE, lambda e: e.tensor_tensor(out=posm.ap, in0=posm.ap, in1=maskF.ap, op=ALU.mult), reads=[posm.r, maskF.r], writes=[posm.r])
        op(DVE, lambda e: e.tensor_scalar(out=posm.ap, in0=posm.ap, scalar1=-1.0, scalar2=None, op0=ALU.add), reads=[posm.r], writes=[posm.r])
        posT = self.tile([NE, TT], F32, "posT")
        for g in range(9):
            ps = self.psum()
            tl = list(range(g * 4, min(g * 4 + 4, NT)))
            for j, i in enumerate(tl):
                op(PE, lambda e, ps=ps, i=i, j=j: e.transpose(out=ps.ap[0:NE, j * 128:(j + 1) * 128], in_=posm.ap[:, i, :], identity=self.ident32.ap),
                   reads=[posm.r, self.ident32.r], writes=[ps.r])
            n = len(tl) * 128
            op(DVE, lambda e, ps=ps, g=g, n=n: e.tensor_scalar(out=posT.ap[:, g * 512:g * 512 + n], in0=ps.ap[0:NE, 0:n], scalar1=12582912.0, scalar2=12582912.0,
                                                               op0=ALU.add, op1=ALU.subtract), reads=[ps.r], writes=[posT.r])
        dma(SP, self.POST.ap, posT.ap, reads=[posT.r], writes=[self.POST.r])
        self.P.barrier()
        self.off = keep2

    def moe_passA(self, l):
        op, dma = self.op, self.dma
        h2tok, posm = self.h2tok, self.posm
        sel = self.tile([128, 32, 512], BF16, "sel", nsub=32)
        selc = self.tile([128, 2, 32], BF16, "selc")
        xsT = self.tile([128, 8, 544], BF16, "xsT")
        hact = self.tile([128, 16, 544], BF16, "hact")
        wgb = [self.tile([128, 8, 256], BF16, "wg") for _ in range(2)]
        wub = [self.tile([128, 8, 256], BF16, "wu") for _ in range(2)]
        wdb = [self.tile([128, 2, 512], BF16, "wd") for _ in range(3)]
        sa = [self.tile([128, 512], F32, "sa") for _ in range(2)]
        sac = self.tile([128, 32], F32, "sac")
        yes = [self.tile([128, D], BF16, "yes") for _ in range(4)]
        yec = self.tile([32, D], BF16, "yec")
        posbc = [self.tile([128, 1024], F32, "posbc") for _ in range(2)]
        affbc = [self.tile([128, 1024], F32, "affbc") for _ in range(2)]
        posc = self.tile([32, 256], F32, "posc")
        affc = self.tile([32, 256], F32, "affc")
        stg = [self.tile([128, 1024], BF16, "stg") for _ in range(2)]
        nq = 0
        stgc = self.tile([32, 256], BF16, "stgc")
        nw = [0, 0]
        ns = 0
        def build_selT(ex):
            nonlocal nq
            dma(SP, posc.ap, self.POST.ap[ex:ex + 1, 0:256].to_broadcast([32, 256]), reads=[self.POST.r], writes=[posc.r])
            dma(SP, affc.ap, self.AFFT.ap[ex:ex + 1, 0:256].to_broadcast([32, 256]), reads=[self.AFFT.r], writes=[affc.r])
            op(DVE, lambda e: e.scalar_tensor_tensor(out=stgc.ap, in0=posc.ap, scalar=self.iotacol.ap[0:32, 0:1], in1=affc.ap,
                                                      op0=ALU.is_equal, op1=ALU.mult), reads=[posc.r, affc.r, self.iotacol.r], writes=[stgc.r])
            dma(SP, self.SELTC.ap[ex], stgc.ap, reads=[stgc.r], writes=[self.SELTC.r])
            for qd in range(4):
                pb_, ab_ = posbc[nq % 2], affbc[nq % 2]
                nq += 1
                t0 = 256 + qd * 1024
                dma(SP, pb_.ap, self.POST.ap[ex:ex + 1, t0:t0 + 1024].to_broadcast([128, 1024]), reads=[self.POST.r], writes=[pb_.r])
                dma(SP, ab_.ap, self.AFFT.ap[ex:ex + 1, t0:t0 + 1024].to_broadcast([128, 1024]), reads=[self.AFFT.r], writes=[ab_.r])
                for c in range(4):
                    sg = stg[c % 2]
                    ecx = ex * 4 + c
                    op(DVE, lambda e, sg=sg, c=c, pb_=pb_, ab_=ab_: e.scalar_tensor_tensor(
                        out=sg.ap, in0=pb_.ap, scalar=self.iotacol.ap[:, c:c + 1], in1=ab_.ap, op0=ALU.is_equal, op1=ALU.mult),
                       reads=[pb_.r, ab_.r, self.iotacol.r], writes=[sg.r])
                    dma(SP, self.SELT.ap[qd * 8:(qd + 1) * 8, :, ecx, :].rearrange("i s t -> s i t"), sg.ap.rearrange("s (i t) -> s i t", t=128),
                        reads=[sg.r], writes=self.SELT.sub[qd * 8:(qd + 1) * 8])


        for ex in range(NE):
            for i in range(2, NT):
                eng = DVE if i % 2 == 0 else POOL
                op(DVE, lambda e, i=i, ex=ex: e.tensor_scalar(out=sel.ap[:, i - 2, :], in0=self.iota512.ap, scalar1=posm.ap[:, i, ex:ex + 1], scalar2=None,
                                                              op0=ALU.is_equal), reads=[self.iota512.r, posm.r], writes=[sel.sub[i - 2]])
            for i in range(2):
                op(DVE, lambda e, i=i, ex=ex: e.tensor_scalar(out=selc.ap[:, i, :], in0=self.iota512.ap[:, 0:32], scalar1=posm.ap[:, i, ex:ex + 1], scalar2=None,
                                                              op0=ALU.is_equal), reads=[self.iota512.r, posm.r], writes=[selc.r])
            for k in range(8):
                ps = self.psum()
                ks = slice(k * 128, (k + 1) * 128)
                for i in range(2, NT):
                    op(PE, lambda e, ps=ps, i=i, ks=ks: e.matmul(ps.ap, lhsT=h2tok.ap[:, i, ks], rhs=sel.ap[:, i - 2, :], start=(i == 2), stop=(i == NT - 1)),
                       reads=[h2tok.sub[i], sel.sub[i - 2]], writes=[ps.r])
                op(ACT, lambda e, ps=ps, k=k: e.copy(out=xsT.ap[:, k, 0:512], in_=ps.ap), reads=[ps.r], writes=[xsT.r])
                ps2 = self.psum()
                for i in range(2):
                    op(PE, lambda e, ps2=ps2, i=i, ks=ks: e.matmul(ps2.ap[:, 0:32], lhsT=h2tok.ap[:, i, ks], rhs=selc.ap[:, i, :], start=(i == 0), stop=(i == 1)),
                       reads=[h2tok.sub[i], selc.r], writes=[ps2.r])
                op(ACT, lambda e, ps2=ps2, k=k: e.copy(out=xsT.ap[:, k, 512:544], in_=ps2.ap[:, 0:32]), reads=[ps2.r], writes=[xsT.r])
            if ex > 0:
                build_selT(ex - 1)
            for q in range(8):
                wg, wu = wgb[nw[0] % 2], wub[nw[0] % 2]
                nw[0] += 1
                cs = slice(q * 256, (q + 1) * 256)
                if not (self.cfg.get('nowdma') and ex > 0):
                    dma(POOL, wg.ap, self.moe_w_gate[l, ex][:, cs].rearrange("(k p) n -> p k n", p=128), writes=[wg.r])
                    dma(POOL, wu.ap, self.moe_w_up[l, ex][:, cs].rearrange("(k p) n -> p k n", p=128), writes=[wu.r])
                for jl in range(2):
                    j = q * 2 + jl
                    js = slice(jl * 128, (jl + 1) * 128)
                    ps_a, ps_u, ps_c = self.psum(), self.psum(), self.psum()
                    for (w, ps) in ((wg, ps_a), (wu, ps_u)):
                        for k in range(8):
                            op(PE, lambda e, w=w, ps=ps, k=k, js=js: e.matmul(ps.ap, lhsT=w.ap[:, k, js], rhs=xsT.ap[:, k, 0:512], start=(k == 0), stop=(k == 7)),
                               reads=[w.r, xsT.r], writes=[ps.r])
                    for (w, c0) in ((wg, 0), (wu, 32)):
                        for k in range(8):
                            op(PE, lambda e, w=w, c0=c0, k=k, js=js, ps_c=ps_c: e.matmul(ps_c.ap[:, c0:c0 + 32], lhsT=w.ap[:, k, js], rhs=xsT.ap[:, k, 512:544],
                                                                                         start=(k == 0), stop=(k == 7)),
                               reads=[w.r, xsT.r], writes=[ps_c.r])
                    s = sa[ns % 2]
                    ns += 1
                    op(ACT, lambda e, s=s, ps_a=ps_a: e.activation(out=s.ap, in_=ps_a.ap, func=AF.Silu), reads=[ps_a.r], writes=[s.r])
                    op(DVE, lambda e, s=s, ps_u=ps_u, j=j: e.tensor_tensor(out=hact.ap[:, j, 0:512], in0=s.ap, in1=ps_u.ap, op=ALU.mult),
                       reads=[s.r, ps_u.r], writes=[hact.r])
                    op(ACT, lambda e, ps_c=ps_c: e.activation(out=sac.ap, in_=ps_c.ap[:, 0:32], func=AF.Silu), reads=[ps_c.r], writes=[sac.r])
                    op(DVE, lambda e, ps_c=ps_c, j=j: e.tensor_tensor(out=hact.ap[:, j, 512:544], in0=sac.ap, in1=ps_c.ap[:, 32:64], op=ALU.mult),
                       reads=[sac.r, ps_c.r], writes=[hact.r])
            for half in range(2):
                hs = slice(half * 512, (half + 1) * 512)
                psd = [self.psum() for _ in range(5)]
                for jj in range(8):
                    wd = wdb[nw[1] % 3]
                    nw[1] += 1
                    if not (self.cfg.get('nowdma') and ex > 0):
                        dma(POOL, wd.ap, self.moe_w_down[l, ex][jj * 256:(jj + 1) * 256, hs].rearrange("(a p) n -> p a n", p=128), writes=[wd.r])
                    for a in range(2):
                        j = jj * 2 + a
                        first, last = (j == 0), (j == 15)
                        for c in range(4):
                            op(PE, lambda e, c=c, j=j, a=a, wd=wd, first=first, last=last, p=psd[c]: e.matmul(p.ap, lhsT=hact.ap[:, j, c * 128:(c + 1) * 128], rhs=wd.ap[:, a, :],
                                                                                                                  start=first, stop=last),
                               reads=[hact.r, wd.r], writes=[psd[c].r])
                        op(PE, lambda e, j=j, a=a, wd=wd, first=first, last=last, p=psd[4]: e.matmul(p.ap[0:32, :], lhsT=hact.ap[:, j, 512:544], rhs=wd.ap[:, a, :],
                                                                                                      start=first, stop=last),
                           reads=[hact.r, wd.r], writes=[psd[4].r])
                for c in range(4):
                    op(ACT, lambda e, c=c, p=psd[c], hs=hs: e.copy(out=yes[c].ap[:, hs], in_=p.ap), reads=[psd[c].r], writes=[yes[c].r])
                op(ACT, lambda e, p=psd[4], hs=hs: e.copy(out=yec.ap[:, hs], in_=p.ap[0:32, :]), reads=[psd[4].r], writes=[yec.r])
            for c in range(4):
                dma(SP, self.YE.ap[ex, c], yes[c].ap, reads=[yes[c].r], writes=[self.YE.sub[ex]])
            dma(SP, self.YEC.ap[ex], yec.ap, reads=[yec.r], writes=[self.YEC.sub[ex]])
        build_selT(NE - 1)

    def moe_passB(self, l):
        op, dma = self.op, self.dma
        self.phase_begin()
        last = (l == DEPTH - 1)
        yeall = self.tile([128, 64, D], BF16, "yeall")
        for ex in range(NE):
            dma(SP, yeall.ap[:, ex * 4:(ex + 1) * 4, :], self.YE.ap[ex].rearrange("c s d -> s c d"), reads=[self.YE.sub[ex]], writes=[yeall.r])
        yecall = self.tile([128, 4, D], BF16, "yecall")
        dma(SP, yecall.ap, self.YEC.ap.rearrange("(g e) s d -> (e s) g d", e=4), reads=self.YEC.sub, writes=[yecall.r])
        seltc = self.tile([128, 4, 256], BF16, "seltc")
        dma(SP, seltc.ap, self.SELTC.ap.rearrange("(g e) s t -> (e s) g t", e=4), reads=[self.SELTC.r], writes=[seltc.r])
        mods = self.mod_tiles(l, [5])
        g1 = self.ln_vec(self.ln_g[l, 1:2, :])
        b1 = self.ln_vec(self.ln_b[l, 1:2, :])
        selt = [self.tile([128, 32, 128], BF16, "selt") for _ in range(3)]
        xb = [self.tile([128, D], F32, "xb") for _ in range(2)]
        yb = [self.tile([128, D], F32, "yb") for _ in range(2)]
        st = self.tile([128, 2, 6], F32, "st")
        mv = self.tile([128, 4], F32, "mv")
        nsl = 0
        for i in range(NT):
            c = 1 if i < 2 else 0
            rows = slice(i * 128, (i + 1) * 128)
            x, y = xb[i % 2], yb[i % 2]
            dma(SP, x.ap, self.XS.ap[rows, :], reads=[self.XS.sub[i]], writes=[x.r])
            m5 = mods[5][c]
            pss = [self.psum(), self.psum()]
            if c:
                for g in range(4):
                    for half in range(2):
                        hs = slice(half * 512, (half + 1) * 512)
                        op(PE, lambda e, ps=pss[half], g=g, hs=hs, i=i: e.matmul(ps.ap, lhsT=seltc.ap[:, g, i * 128:(i + 1) * 128], rhs=yecall.ap[:, g, hs],
                                                                                 start=(g == 0), stop=(g == 3)),
                           reads=[seltc.r, yecall.r], writes=[pss[half].r])
            else:
                for hh in range(2):
                    sl = selt[nsl % 3]
                    nsl += 1
                    dma(SP, sl.ap, self.SELT.ap[i - 2, :, hh * 32:(hh + 1) * 32, :], reads=[self.SELT.sub[i - 2]], writes=[sl.r])
                    for b in range(32):
                        ec = hh * 32 + b
                        for half in range(2):
                            hs = slice(half * 512, (half + 1) * 512)
                            op(PE, lambda e, ps=pss[half], sl=sl, b=b, ec=ec, hs=hs: e.matmul(ps.ap, lhsT=sl.ap[:, b, :], rhs=yeall.ap[:, ec, hs],
                                                                                              start=(ec == 0), stop=(ec == 63)),
                               reads=[sl.r, yeall.r], writes=[pss[half].r])
            for half in range(2):
                hs = slice(half * 512, (half + 1) * 512)
                op(DVE, lambda e, ps=pss[half], y=y, m5=m5, hs=hs: e.tensor_tensor(out=y.ap[:, hs], in0=ps.ap, in1=m5.ap[:, hs], op=ALU.mult),
                   reads=[pss[half].r, m5.r], writes=[y.r])
            op(DVE, lambda e, x=x, y=y: e.scalar_tensor_tensor(out=y.ap, in0=x.ap, scalar=ALPHA, in1=y.ap, op0=ALU.mult, op1=ALU.add),
               reads=[x.r, y.r], writes=[y.r])
            self.layernorm_(y, st, mv)
            op(POOL, lambda e, y=y, x=x: e.tensor_tensor(out=x.ap, in0=y.ap, in1=g1.ap, op=ALU.mult), reads=[y.r, g1.r], writes=[x.r])
            op(POOL, lambda e, x=x: e.tensor_tensor(out=x.ap, in0=x.ap, in1=b1.ap, op=ALU.add), reads=[x.r, b1.r], writes=[x.r])
            dma(SP, self.XS.ap[rows, :], x.ap, reads=[x.r], writes=[self.XS.sub[i]])
            if last and not c:
                dma(SP, self.out[(i - 2) * 128:(i - 1) * 128, :], x.ap, reads=[x.r], writes=[self.outr])
            if self.cfg.get("test"):
                dma(SP, self.dbg.ap[rows, :], x.ap, reads=[x.r], writes=[self.dbg.sub[i]])

    def finish(self):
        rs = [self.outr]
        if self.cfg.get("test"):
            rs = rs + [self.dbg.r] + self.dbg.sub
        self.P.barrier()
        self.op(SP, lambda e: e.nop(), reads=rs)
        self.P.emit()


def Tl_view(t):
    return t


def build(nc, cfg):
    k = K(nc, cfg)
    k.declare_io()
    k.setup()
    tk = cfg.get("test")
    if tk in ("mix", "gdn1", "gdn2"):
        l = cfg["layer"]
        k.premix(l)
        if l % 2 == 0:
            k.natten(l)
        else:
            k.gdn(l)
    if tk == "post":
        l = cfg["layer"]
        k.postmix(l)
        k.moe_select(l)
        k.moe_passA(l)
        k.moe_passB(l)
    if not tk:
        for l in range(cfg.get("depth", DEPTH)):
            k.premix(l)
            if l % 2 == 0:
                k.natten(l)
            else:
                k.gdn(l)
            k.postmix(l)
            k.moe_select(l)
            k.moe_passA(l)
            k.moe_passB(l)
    k.finish()
    return k


WNAMES = ["c_ctx", "ada_w", "ada_b", "ln_g", "ln_b", "na_w_qkv", "na_w_o", "na_rpb", "gdn_w_in", "gdn_conv_w", "gdn_a_log",
          "gdn_dt_bias", "gdn_norm_w", "gdn_w_o", "moe_w_router", "moe_w_gate", "moe_w_up", "moe_w_down"]


def make_in_maps(inputs, cores):
    maps = []
    shared = {}
    for n in WNAMES:
        a = np.ascontiguousarray(inputs[n], dtype=np.float32)
        if n == "c_ctx":
            a = a.reshape(1, D)
        shared[n] = a
    rp = np.zeros((2, 16, 15, 128), np.float32)
    rp[..., 48:79] = np.asarray(inputs["na_rpb"], np.float32)[..., ::-1]
    shared["rpb_pad"] = rp
    qc = np.arange(64)
    cs = np.clip(qc - 8, 0, 48)
    kc = np.arange(64)[:, None]
    cmv = np.where((kc >= cs[None, :]) & (kc < cs[None, :] + 16), 0.0, -1e30).astype(np.float32)
    shared["cmask"] = np.tile(cmv, (2, 2))
    pp = np.arange(128)
    bd = lambda b: (pp[:, None] // b == pp[None, :] // b).astype(np.float32)
    shared["gmask"] = np.stack([bd(16), bd(32) - bd(16), bd(64) - bd(32), 1.0 - bd(64)]).astype(np.float32)
    for b in cores:
        m = dict(shared)
        m["x"] = np.ascontiguousarray(inputs["x"][b], dtype=np.float32)
        m["c"] = np.ascontiguousarray(inputs["c"][b:b + 1], dtype=np.float32)
        m["ctx"] = np.ascontiguousarray(inputs["ctx"][b], dtype=np.float32)
        maps.append(m)
    return maps


def kernel(**inputs):
    nc = bass.Bass("TRN2", target_bir_lowering=False)
    build(nc, {})
    maps = make_in_maps(inputs, list(range(8)))
    res = run_bass_kernel_spmd(nc, maps, core_ids=list(range(8)))
    return np.stack([np.asarray(r["out"], dtype=np.float32) for r in res.results], axis=0)
```

```python
import contextlib
import numpy as np
import concourse.bass as bass
import concourse.mybir as mybir
from concourse.bass_utils import run_bass_kernel_spmd

F32 = mybir.dt.float32
BF16 = mybir.dt.bfloat16
I32 = mybir.dt.int32
U8 = mybir.dt.uint8
AF = mybir.ActivationFunctionType
ALU = mybir.AluOpType
AX = mybir.AxisListType

PE, ACT, DVE, POOL, SP = "tensor", "scalar", "vector", "gpsimd", "sync"
ENGS = [PE, ACT, DVE, POOL, SP]
N_DMA_SEMS = 20
SEM_EPOCH = 20000

D = 1024
NT = 34
TT = NT * 128
NE = 16
DEPTH = 4
ALPHA = (2.0 * DEPTH) ** 0.25
LN_EPS = 1e-6
DSZ = {F32: 4, BF16: 2, I32: 4, U8: 1}


class Res:
    __slots__ = ("name", "last_w", "readers", "excl")

    def __init__(self, name=""):
        self.name = name
        self.last_w = None
        self.readers = []
        self.excl = False


class GRes:
    def __init__(self, name=""):
        self.subs = [Res(name + "a"), Res(name + "b")]


def _grp(fn):
    d = fn.__defaults__
    if not d:
        return None
    c = fn.__code__
    names = c.co_varnames[:c.co_argcount]
    m = dict(zip(names[len(names) - len(d):], d))
    if isinstance(m.get("j"), int):
        return m["j"]
    if isinstance(m.get("h"), int):
        return m["h"] // 4
    return None


def _expand(rs, g):
    out = []
    for r in rs:
        if isinstance(r, GRes):
            out.extend(r.subs if g is None else [r.subs[g]])
        else:
            out.append(r)
    return out


class Op:
    __slots__ = ("eng", "fn", "deps", "is_dma", "sem", "val", "sig")

    def __init__(self, eng, fn, is_dma):
        self.eng = eng
        self.fn = fn
        self.deps = ()
        self.is_dma = is_dma
        self.sem = None
        self.val = None
        self.sig = False


class Prog:
    def __init__(self, nc):
        self.nc = nc
        self.q = {e: [] for e in ENGS}
        self.dmas_since_barrier = []
        self.nops = 0

    def _add(self, eng, fn, reads, writes, is_dma):
        if any(isinstance(r, GRes) for r in reads) or any(isinstance(r, GRes) for r in writes):
            g = _grp(fn)
            reads, writes = _expand(reads, g), _expand(writes, g)
        op = Op(eng, fn, is_dma)
        deps = set()
        for r in reads:
            if r.last_w is not None:
                deps.add(r.last_w)
            if r.excl:
                deps.update(x for x in r.readers if x.eng != eng)
        for w in writes:
            if w.last_w is not None:
                deps.add(w.last_w)
            deps.update(w.readers)
        op.deps = tuple(deps)
        for r in reads:
            r.readers.append(op)
        for w in writes:
            w.last_w = op
            w.readers = []
        self.q[eng].append(op)
        self.nops += 1
        if is_dma:
            self.dmas_since_barrier.append(op)
        return op

    def op(self, eng, fn, reads=(), writes=()):
        return self._add(eng, fn, reads, writes, False)

    def dma(self, eng, out, in_, reads=(), writes=(), **kw):
        return self._add(eng, lambda e: e.dma_start(out=out, in_=in_, **kw), reads, writes, True)

    def barrier(self):
        b = Op(SP, lambda e: e.nop(), False)
        deps = set(self.dmas_since_barrier)
        for e in ENGS:
            if self.q[e]:
                deps.add(self.q[e][-1])
        b.deps = tuple(deps)
        self.q[SP].append(b)
        self.dmas_since_barrier = []
        for e in ENGS:
            if e == SP:
                continue
            o = Op(e, lambda en: en.nop(), False)
            o.deps = (b,)
            self.q[e].append(o)

    def emit(self):
        nc = self.nc
        for e in ENGS:
            for op in self.q[e]:
                for d in op.deps:
                    if d.is_dma or d.eng != op.eng or op.is_dma or d.eng != PE:
                        d.sig = True
        with contextlib.ExitStack() as st:
            nsig = {e: sum(1 for o in self.q[e] if o.sig and not o.is_dma) for e in ENGS}
            csem = {e: [st.enter_context(nc.semaphore("cs_%s_%d" % (e, i)))
                        for i in range(nsig[e] // SEM_EPOCH + 1)] for e in ENGS}
            dsem = {e: [st.enter_context(nc.semaphore("ds_%s_%d" % (e, i))) for i in range(N_DMA_SEMS)]
                    for e in (SP, ACT, POOL)}
            for e in ENGS:
                cnt = 0
                dcnt = 0
                for op in self.q[e]:
                    if op.is_dma:
                        op.sem = dsem[e][dcnt % N_DMA_SEMS]
                        op.val = 16 * (dcnt // N_DMA_SEMS + 1)
                        op.sig = True
                        dcnt += 1
                    elif op.sig:
                        op.sem = csem[e][cnt // SEM_EPOCH]
                        op.val = cnt % SEM_EPOCH + 1
                        cnt += 1
            block = st.enter_context(nc.Block())

            def gen(e):
                def body(eng):
                    waited = {}
                    for op in self.q[e]:
                        needs = {}
                        for d in op.deps:
                            if not d.sig:
                                continue
                            if (not d.is_dma) and d.eng == e and e == PE and not op.is_dma:
                                continue
                            k = id(d.sem)
                            if needs.get(k, (None, 0))[1] < d.val:
                                needs[k] = (d.sem, d.val)
                        if op.is_dma and op.val > 16:
                            k = id(op.sem)
                            if needs.get(k, (None, 0))[1] < op.val - 16:
                                needs[k] = (op.sem, op.val - 16)
                        for k, (s, v) in needs.items():
                            if waited.get(k, 0) >= v:
                                continue
                            eng.wait_ge(s, v)
                            waited[k] = v
                        ins = op.fn(eng)
                        if op.sig:
                            ins.then_inc(op.sem, 16 if op.is_dma else 1)
                return body

            for e in ENGS:
                if self.q[e]:
                    getattr(block, e)(gen(e))


class Tl:
    def __init__(self, ap, name="", nsub=0):
        self.ap = ap
        self.r = Res(name)
        self.sub = [Res(name + str(i)) for i in range(nsub)]


ARENA = 206000


class K:
    def __init__(self, nc, cfg):
        self.nc = nc
        self.cfg = cfg
        self.P = Prog(nc)
        global ARENA
        ARENA = (int(nc.sbuf_bytes_remaining) - 256) // 64 * 64
        self.big = nc.alloc_sbuf_tensor("arena", [128, ARENA], U8)
        self.off = 0
        self.ps = []
        for i in range(8):
            t = nc.alloc_psum_tensor("psb%d" % i, [128, 512], F32)
            self.ps.append(Tl(t[:], "ps%d" % i, nsub=4))
            for r_ in [self.ps[-1].r] + self.ps[-1].sub:
                r_.excl = True
        self.psn = 0
        self.uid = 0

    def tile(self, shape, dt, name="t", nsub=0):
        n = int(np.prod(shape[1:])) * DSZ[dt]
        if self.off + n > ARENA:
            raise RuntimeError("SBUF arena overflow at %s: %d + %d" % (name, self.off, n))
        a = self.big[0:shape[0], self.off:self.off + n].bitcast(dt)
        self.off += (n + 63) // 64 * 64
        if len(shape) == 3:
            a = a.rearrange("p (a b) -> p a b", a=shape[1])
        elif len(shape) == 4:
            a = a.rearrange("p (a b c) -> p a b c", a=shape[1], b=shape[2])
        self.uid += 1
        return Tl(a, "%s_%d" % (name, self.uid), nsub)

    def psum(self):
        t = self.ps[self.psn % 8]
        self.psn += 1
        return t

    def dram(self, name, shape, dt, kind="Internal"):
        return self.nc.dram_tensor(name, list(shape), dt, kind=kind).ap()

    def op(self, eng, fn, reads=(), writes=()):
        return self.P.op(eng, fn, reads, writes)

    def dma(self, eng, out, in_, reads=(), writes=(), **kw):
        return self.P.dma(eng, out, in_, reads, writes, **kw)

    def declare_io(self):
        nc, cfg = self.nc, self.cfg
        ein = lambda n, s: self.dram(n, s, F32, kind="ExternalInput")
        self.x_in = ein("x", [4096, D])
        self.c_in = ein("c", [1, D])
        self.ctx_in = ein("ctx", [256, D])
        self.cc_in = ein("c_ctx", [1, D])
        self.ada_w = ein("ada_w", [DEPTH, D, 6 * D])
        self.ada_b = ein("ada_b", [DEPTH, 6 * D])
        self.ln_g = ein("ln_g", [DEPTH, 2, D])
        self.ln_b = ein("ln_b", [DEPTH, 2, D])
        self.na_w_qkv = ein("na_w_qkv", [2, D, 3 * D])
        self.na_w_o = ein("na_w_o", [2, D, D])
        self.na_rpb = ein("na_rpb", [2, 16, 15, 31])
        self.gdn_w_in = ein("gdn_w_in", [2, D, 4 * D + 32])
        self.gdn_conv_w = ein("gdn_conv_w", [2, 5, 3 * D])
        self.gdn_a_log = ein("gdn_a_log", [2, 2, 8])
        self.gdn_dt_bias = ein("gdn_dt_bias", [2, 2, 8])
        self.gdn_norm_w = ein("gdn_norm_w", [2, 128])
        self.gdn_w_o = ein("gdn_w_o", [2, D, D])
        if cfg.get("test") not in ("mix", "gdn1", "gdn2"):
            self.moe_w_router = ein("moe_w_router", [DEPTH, D, NE])
            self.moe_w_gate = ein("moe_w_gate", [DEPTH, NE, D, 2048])
            self.moe_w_up = ein("moe_w_up", [DEPTH, NE, D, 2048])
            self.moe_w_down = ein("moe_w_down", [DEPTH, NE, 2048, D])
        if cfg.get("test") in ("gdn1", "gdn2"):
            self.dbg2 = self.dram("dbg2", [4, D, TT], F32, kind="ExternalOutput")
            self.dbg3 = self.dram("dbg3", [128, 2, NT, 16], F32, kind="ExternalOutput")
        self.rpb_pad = ein("rpb_pad", [2, 16, 15, 128])
        self.cmask_in = ein("cmask", [128, 128])
        self.gmask_in = ein("gmask", [4, 128, 128])
        self.out = self.dram("out", [4096, D], F32, kind="ExternalOutput")
        tk = cfg.get("test")
        self.XS = Tl(self.dram("XS", [TT, D], F32), "XS", NT)
        self.Y = Tl(self.dram("Y", [TT, D], F32), "Y", NT)
        if tk:
            self.xs_in = self.dram("xs_in", [TT, D], F32, kind="ExternalInput")
            self.y_in = self.dram("y_in", [TT, D], F32, kind="ExternalInput")
        if tk:
            self.dbg = Tl(self.dram("dbg", [TT, D], F32, kind="ExternalOutput"), "dbg", NT)
        self.AFFT = Tl(self.dram("AFFT", [NE, TT], F32), "AFFT")
        self.POST = Tl(self.dram("POST", [NE, TT], F32), "POST")
        self.YE = Tl(self.dram("YE", [NE, 4, 128, D], BF16), "YE", NE)
        self.YEC = Tl(self.dram("YEC", [NE, 32, D], BF16), "YEC", NE)
        self.SELT = Tl(self.dram("SELT", [32, 128, 64, 128], BF16), "SELT", 32)
        self.SELTC = Tl(self.dram("SELTC", [NE, 32, 256], BF16), "SELTC")
        self.outr = Res("out")

    def setup(self):
        op, dma = self.op, self.dma
        self.ident = self.tile([128, 128], BF16, "ident")
        self.ident32 = self.tile([128, 128], F32, "ident32")
        self.ones = self.tile([128, 128], BF16, "ones")
        self.triu = self.tile([128, 128], BF16, "triu")
        tmp = self.tile([128, 128], F32, "tmp")
        self.iota512 = self.tile([128, 512], F32, "iota512")
        self.iotacol = self.tile([128, 4], F32, "iotacol")
        i32, idb, on, tu, io, ic = self.ident32, self.ident, self.ones, self.triu, self.iota512, self.iotacol
        op(POOL, lambda e: e.memset(i32.ap, 1.0), writes=[i32.r])
        op(POOL, lambda e: e.affine_select(out=i32.ap, in_=i32.ap, pattern=[[-1, 128]], compare_op=ALU.is_equal,
                                           fill=0.0, base=0, channel_multiplier=1), reads=[i32.r], writes=[i32.r])
        op(DVE, lambda e: e.tensor_copy(out=idb.ap, in_=i32.ap), reads=[i32.r], writes=[idb.r])
        op(DVE, lambda e: e.memset(on.ap, 1.0), writes=[on.r])
        op(POOL, lambda e: e.memset(tmp.ap, 1.0), writes=[tmp.r])
        op(POOL, lambda e: e.affine_select(out=tmp.ap, in_=tmp.ap, pattern=[[1, 128]], compare_op=ALU.is_ge,
                                           fill=0.0, base=0, channel_multiplier=-1), reads=[tmp.r], writes=[tmp.r])
        op(DVE, lambda e: e.tensor_copy(out=tu.ap, in_=tmp.ap), reads=[tmp.r], writes=[tu.r])
        op(POOL, lambda e: e.iota(io.ap, pattern=[[1, 512]], base=0, channel_multiplier=0,
                                  allow_small_or_imprecise_dtypes=True), writes=[io.r])
        op(POOL, lambda e: e.iota(ic.ap, pattern=[[128, 4]], base=0, channel_multiplier=1,
                                  allow_small_or_imprecise_dtypes=True), writes=[ic.r])
        self.scb = self.tile([128, 8, 128], F32, "scb")
        self.sccb = self.tile([128, 8, 128], F32, "sccb")
        for src, dst in ((self.c_in, self.scb), (self.cc_in, self.sccb)):
            cv = self.tile([128, 8], F32, "cv")
            dma(SP, cv.ap, src.rearrange("o (k p) -> p (o k)", p=128), writes=[cv.r], allow_slow_non_contiguous=True)
            op(ACT, lambda e, cv=cv: e.activation(out=cv.ap, in_=cv.ap, func=AF.Silu), reads=[cv.r], writes=[cv.r])
            op(DVE, lambda e, cv=cv, dst=dst: e.tensor_copy(out=dst.ap, in_=cv.ap.unsqueeze(2).to_broadcast([128, 8, 128])),
               reads=[cv.r], writes=[dst.r])
        self.base_off = self.off
        if not self.cfg.get("test"):
            dma(SP, self.XS.ap[0:256, :], self.ctx_in, writes=self.XS.sub[0:2])
            dma(SP, self.XS.ap[256:TT, :], self.x_in, writes=self.XS.sub[2:NT])
        else:
            dma(SP, self.XS.ap, self.xs_in, writes=self.XS.sub)
            dma(SP, self.Y.ap, self.y_in, writes=self.Y.sub)

    def phase_begin(self):
        self.P.barrier()
        self.off = self.base_off

    def mod_tiles(self, l, idxs, plus1=()):
        op, dma = self.op, self.dma
        res = {}
        for idx in idxs:
            res[idx] = (self.tile([128, D], F32, "modl"), self.tile([128, D], F32, "modc"))
        mark = self.off
        wbuf = [self.tile([128, 8, 512], F32, "adaw") for _ in range(2)]
        bbuf = [self.tile([128, 1024], F32, "adab") for _ in range(2)]
        n = 0
        for ii, idx in enumerate(idxs):
            lat, ctx = res[idx]
            bb = bbuf[ii % 2]
            dma(SP, bb.ap, self.ada_b[l:l + 1, idx * D:(idx + 1) * D].to_broadcast([128, D]), writes=[bb.r])
            for half in range(2):
                wb = wbuf[n % 2]
                n += 1
                c0 = idx * D + half * 512
                dma(SP, wb.ap, self.ada_w[l, :, c0:c0 + 512].rearrange("(k p) n -> p k n", p=128), writes=[wb.r])
                for lhs, dst in ((self.scb, lat), (self.sccb, ctx)):
                    ps = self.psum()
                    for k in range(8):
                        op(PE, lambda e, ps=ps, lhs=lhs, wb=wb, k=k: e.matmul(ps.ap, lhsT=lhs.ap[:, k, :], rhs=wb.ap[:, k, :],
                                                                              start=(k == 0), stop=(k == 7)),
                           reads=[lhs.r, wb.r], writes=[ps.r])
                    sl = slice(half * 512, (half + 1) * 512)
                    op(DVE, lambda e, ps=ps, dst=dst, bb=bb, sl=sl: e.tensor_tensor(out=dst.ap[:, sl], in0=ps.ap, in1=bb.ap[:, sl], op=ALU.add),
                       reads=[ps.r, bb.r], writes=[dst.r])
            if idx in plus1:
                for t in (lat, ctx):
                    op(DVE, lambda e, t=t: e.tensor_scalar(out=t.ap, in0=t.ap, scalar1=1.0, scalar2=None, op0=ALU.add),
                       reads=[t.r], writes=[t.r])
        self.P.barrier()
        self.off = mark
        return res

    def ln_vec(self, src_row):
        t = self.tile([128, D], F32, "lnv")
        self.dma(SP, t.ap, src_row.to_broadcast([128, D]), writes=[t.r])
        return t

    def layernorm_(self, r, st, mv):
        op = self.op
        for h in range(2):
            op(DVE, lambda e, h=h: e.bn_stats(out=st.ap[:, h, :], in_=r.ap[:, h * 512:(h + 1) * 512]), reads=[r.r], writes=[st.r])
        op(DVE, lambda e: e.bn_aggr(out=mv.ap[:, 0:2], in_=st.ap.rearrange("p a b -> p (a b)")), reads=[st.r], writes=[mv.r])
        op(DVE, lambda e: e.tensor_scalar(out=mv.ap[:, 2:3], in0=mv.ap[:, 1:2], scalar1=LN_EPS, scalar2=None, op0=ALU.add),
           reads=[mv.r], writes=[mv.r])
        op(ACT, lambda e: e.activation(out=mv.ap[:, 2:3], in_=mv.ap[:, 2:3], func=AF.Ln), reads=[mv.r], writes=[mv.r])
        op(ACT, lambda e: e.activation(out=mv.ap[:, 2:3], in_=mv.ap[:, 2:3], func=AF.Exp, scale=-0.5), reads=[mv.r], writes=[mv.r])
        op(DVE, lambda e: e.tensor_scalar(out=r.ap, in0=r.ap, scalar1=mv.ap[:, 0:1], scalar2=mv.ap[:, 2:3],
                                          op0=ALU.subtract, op1=ALU.mult), reads=[r.r, mv.r], writes=[r.r])

    def premix(self, l):
        op, dma = self.op, self.dma
        self.phase_begin()
        hT = self.tile([128, 8, TT], BF16, "hT", nsub=NT)
        self.hT = hT
        self.mix_keep = self.off
        mods = self.mod_tiles(l, [0, 1], plus1=(1,))
        xb = [self.tile([128, D], F32, "xb") for _ in range(3)]
        hb = [self.tile([128, D], BF16, "hb") for _ in range(2)]
        for i in range(NT):
            x = xb[i % 3]
            h = hb[i % 2]
            c = 1 if i < 2 else 0
            dma(SP, x.ap, self.XS.ap[i * 128:(i + 1) * 128, :], reads=[self.XS.sub[i]], writes=[x.r])
            sc, sh = mods[1][c], mods[0][c]
            op(DVE, lambda e, x=x, sc=sc: e.tensor_tensor(out=x.ap, in0=x.ap, in1=sc.ap, op=ALU.mult), reads=[x.r, sc.r], writes=[x.r])
            op(POOL, lambda e, x=x, sh=sh, h=h: e.tensor_tensor(out=h.ap, in0=x.ap, in1=sh.ap, op=ALU.add), reads=[x.r, sh.r], writes=[h.r])
            self.transpose_to(h, hT, i)

    def transpose_to(self, h, hT, i, evac=ACT):
        op = self.op
        ps = self.psum()
        pb = ps.ap.bitcast(BF16)
        for k in range(8):
            op(PE, lambda e, k=k, pb=pb, h=h: e.transpose(out=pb[:, k * 128:(k + 1) * 128], in_=h.ap[:, k * 128:(k + 1) * 128],
                                                          identity=self.ident.ap), reads=[h.r, self.ident.r], writes=[ps.r])
        dst = hT.ap[:, :, i * 128:(i + 1) * 128]
        if evac == ACT:
            op(ACT, lambda e, pb=pb, dst=dst: e.copy(out=dst, in_=pb.rearrange("p (k n) -> p k n", k=8)), reads=[ps.r], writes=[hT.sub[i]])
        else:
            op(evac, lambda e, pb=pb, dst=dst: e.tensor_copy(out=dst, in_=pb.rearrange("p (k n) -> p k n", k=8)), reads=[ps.r], writes=[hT.sub[i]])

    def natten(self, l):
        op, dma = self.op, self.dma
        li = l // 2
        hT = self.hT
        self.P.barrier()
        self.off = self.mix_keep
        if not hasattr(self, "QT"):
            self.QT = Tl(self.dram("QT", [D, TT], BF16), "QT")
            self.KT = Tl(self.dram("KT", [D, TT], BF16), "KT")
            self.VX = Tl(self.dram("VX", [TT, 16, 65], BF16), "VX")
            self.OD = Tl(self.dram("OD", [TT, D], BF16), "OD", NT)
        QT, KT, VX, OD = self.QT, self.KT, self.VX, self.OD
        wq = self.na_w_qkv[li]
        wb = [self.tile([128, 8, 512], BF16, "wqk") for _ in range(2)]
        stq = [self.tile([128, 512], BF16, "stq") for _ in range(3)]
        blocks = [(0, 256)] + [(256 + 512 * b, 512) for b in range(8)]
        n = 0
        for cg in range(4):
            w = wb[cg % 2]
            dma(POOL, w.ap, wq[:, cg * 512:(cg + 1) * 512].rearrange("(k p) n -> p k n", p=128), writes=[w.r])
            for cl in range(4):
                ct = cg * 4 + cl
                dst = QT if ct < 8 else KT
                crow = (ct % 8) * 128
                for (t0, nt_) in blocks:
                    ps = self.psum()
                    for k in range(8):
                        op(PE, lambda e, ps=ps, w=w, k=k, cl=cl, t0=t0, nt_=nt_: e.matmul(ps.ap[:, 0:nt_], lhsT=w.ap[:, k, cl * 128:(cl + 1) * 128], rhs=hT.ap[:, k, t0:t0 + nt_],
                                                                                    start=(k == 0), stop=(k == 7)),
                           reads=[w.r] + hT.sub[t0 // 128:(t0 + nt_) // 128], writes=[ps.r])
                    sq = stq[n % 3]
                    n += 1
                    if ct < 8:
                        op(ACT, lambda e, ps=ps, sq=sq, nt_=nt_: e.mul(out=sq.ap[:, 0:nt_], in_=ps.ap[:, 0:nt_], mul=0.125), reads=[ps.r], writes=[sq.r])
                    else:
                        op(DVE, lambda e, ps=ps, sq=sq, nt_=nt_: e.tensor_copy(out=sq.ap[:, 0:nt_], in_=ps.ap[:, 0:nt_]), reads=[ps.r], writes=[sq.r])
                    dma(SP, dst.ap[crow:crow + 128, t0:t0 + nt_], sq.ap[:, 0:nt_], reads=[sq.r], writes=[dst.r])
        wv = self.tile([128, 8, D], BF16, "wv")
        dma(POOL, wv.ap, wq[:, 2 * D:3 * D].rearrange("(k p) n -> p k n", p=128), writes=[wv.r])
        vst = [self.tile([128, 16, 65], BF16, "vst") for _ in range(2)]
        for v in vst:
            op(DVE, lambda e, v=v: e.memset(v.ap, 1.0), writes=[v.r])
        for i in range(NT):
            v = vst[i % 2]
            for half in range(2):
                ps = self.psum()
                for k in range(8):
                    op(PE, lambda e, ps=ps, k=k, i=i, half=half: e.matmul(ps.ap, lhsT=hT.ap[:, k, i * 128:(i + 1) * 128], rhs=wv.ap[:, k, half * 512:(half + 1) * 512],
                                                                       start=(k == 0), stop=(k == 7)),
                       reads=[wv.r, hT.sub[i]], writes=[ps.r])
                op(ACT, lambda e, ps=ps, v=v, half=half: e.copy(out=v.ap[:, half * 8:(half + 1) * 8, 0:64], in_=ps.ap.rearrange("p (h d) -> p h d", d=64)),
                   reads=[ps.r], writes=[v.r])
            dma(SP, VX.ap[i * 128:(i + 1) * 128], v.ap, reads=[v.r], writes=[VX.r])
        self.P.barrier()
        self.off = self.mix_keep - 0
        self.off = self.base_off
        rs_ = np.clip(np.arange(64) - 4, 0, 56)
        tiles, tindex, per_rp = [], {}, []
        for rp in range(32):
            r0 = 2 * rp
            lst = []
            for kp in range(rs_[r0] // 2, (rs_[r0 + 1] + 7) // 2 + 1):
                spec = []
                for i2 in range(2):
                    for j2 in range(2):
                        kr, r = 2 * kp + i2, r0 + j2
                        spec.append(int(kr - r + 7) if rs_[r] <= kr < rs_[r] + 8 else None)
                spec = tuple(spec)
                key = (rp if rp in (0, 1, 30, 31) else -1, spec)
                if key not in tindex:
                    tindex[key] = len(tiles)
                    tiles.append(spec)
                lst.append((kp, tindex[key]))
            assert all(lst[j + 1][1] == lst[j][1] + 1 for j in range(len(lst) - 1))
            per_rp.append(lst)
        NTL = len(tiles)
        jm = self.tile([64, 128], F32, "jm")
        op(POOL, lambda e: e.memset(jm.ap, 1.0), writes=[jm.r])
        op(POOL, lambda e: e.affine_select(out=jm.ap[:, 0:64], in_=jm.ap[:, 0:64], pattern=[[1, 64]], compare_op=ALU.is_equal, fill=0.0, base=-63, channel_multiplier=1),
           reads=[jm.r], writes=[jm.r])
        op(POOL, lambda e: e.affine_select(out=jm.ap[:, 64:128], in_=jm.ap[:, 64:128], pattern=[[1, 64]], compare_op=ALU.is_equal, fill=0.0, base=-63, channel_multiplier=1),
           reads=[jm.r], writes=[jm.r])
        cm = self.tile([128, 128], F32, "cmask")
        dma(SP, cm.ap, self.cmask_in, writes=[cm.r])
        hk = self.tile([64, 15, 64], F32, "hk")
        toe = self.tile([128, 15, 64], F32, "toe")
        bias = self.tile([128, NTL, 128], F32, "bias")
        qt = [self.tile([64, TT], BF16, "qt") for _ in range(2)]
        kt = [self.tile([64, TT], BF16, "kt") for _ in range(2)]
        vh = [self.tile([128, NT, 65], BF16, "vh") for _ in range(2)]
        sb = [self.tile([128, 640], F32, "sb") for _ in range(2)]
        pb = [self.tile([128, 896], BF16, "pb") for _ in range(2)]
        ost = [self.tile([128, NT, 64], BF16, "ost") for _ in range(2)]
        rc = self.tile([128, 4], F32, "rc")
        rpb = self.rpb_pad[li]
        it = 0
        pending = None
        for h in range(16):
            q_, k_, v_, o_ = qt[h % 2], kt[h % 2], vh[h % 2], ost[h % 2]
            dma(SP, q_.ap, QT.ap[h * 64:(h + 1) * 64, :], reads=[QT.r], writes=[q_.r])
            dma(SP, k_.ap, KT.ap[h * 64:(h + 1) * 64, :], reads=[KT.r], writes=[k_.r])
            dma(SP, v_.ap, VX.ap.rearrange("(i p) h d -> p i h d", p=128)[:, :, h, :], reads=[VX.r], writes=[v_.r])
            src = bass.AP(tensor=rpb.tensor, offset=rpb[h].offset, ap=[[1, 64], [128, 15], [1, 64]])
            dma(SP, hk.ap, src, writes=[hk.r])
            for (d0, d1) in ((0, 8), (8, 15)):
                ps = self.psum()
                nn = (d1 - d0) * 64
                op(PE, lambda e, ps=ps, d0=d0, d1=d1, nn=nn: e.matmul(ps.ap[:, 0:nn], lhsT=jm.ap, rhs=hk.ap[:, d0:d1, :].rearrange("p a b -> p (a b)"), start=True, stop=True),
                   reads=[jm.r, hk.r], writes=[ps.r])
                op(ACT, lambda e, ps=ps, d0=d0, d1=d1, nn=nn: e.copy(out=toe.ap[:, d0:d1, :].rearrange("p a b -> p (a b)"), in_=ps.ap[:, 0:nn]), reads=[ps.r], writes=[toe.r])
            for t, spec in enumerate(tiles):
                for bi, dr in enumerate(spec):
                    i2, j2 = bi // 2, bi % 2
                    prt = slice(i2 * 64, (i2 + 1) * 64)
                    fr = slice(j2 * 64, (j2 + 1) * 64)
                    if dr is None:
                        op(POOL, lambda e, t=t, prt=prt, fr=fr: e.memset(bias.ap[prt, t, fr], -1e30), writes=[bias.r])
                    else:
                        op(POOL, lambda e, t=t, prt=prt, fr=fr, dr=dr: e.tensor_tensor(out=bias.ap[prt, t, fr], in0=toe.ap[prt, dr, :], in1=cm.ap[prt, fr], op=ALU.add),
                           reads=[toe.r, cm.r], writes=[bias.r])
            for qi in range(NT):
                if qi < 2:
                    wins = []
                    ctxk = [0, 1]
                else:
                    wins = per_rp[qi - 2]
                    ctxk = [0, 1]
                s_, p_ = sb[it % 2], pb[it % 2]
                it += 1
                nw = len(wins)
                qs = slice(qi * 128, (qi + 1) * 128)
                psA, psB = self.psum(), self.psum()
                slots = []
                for j in range(nw):
                    slots.append((psA, j * 128) if j < 4 else (psB, 0))
                cb = 128 if nw == 5 else 0
                for j, kk in enumerate(ctxk):
                    slots.append((psB, cb + j * 128))
                ktl = [2 + kp for (kp, _) in wins] + ctxk
                for (pst, co), kti in zip(slots, ktl):
                    op(PE, lambda e, pst=pst, co=co, kti=kti, qs=qs, k_=k_, q_=q_: e.matmul(pst.ap[:, co:co + 128], lhsT=k_.ap[:, kti * 128:(kti + 1) * 128], rhs=q_.ap[:, qs],
                                                                                         start=True, stop=True),
                       reads=[k_.r, q_.r], writes=[pst.r])
                if nw:
                    t0 = wins[0][1]
                    na = min(nw, 4)
                    op(DVE, lambda e, s_=s_, t0=t0, na=na, psA=psA: e.tensor_tensor(out=s_.ap[:, 0:na * 128], in0=psA.ap[:, 0:na * 128],
                                                                                   in1=bias.ap[:, t0:t0 + na, :].rearrange("p a b -> p (a b)"), op=ALU.add),
                       reads=[psA.r, bias.r], writes=[s_.r])
                    if nw == 5:
                        op(DVE, lambda e, s_=s_, t0=t0, psB=psB: e.tensor_tensor(out=s_.ap[:, 512:640], in0=psB.ap[:, 0:128], in1=bias.ap[:, t0 + 4, :], op=ALU.add),
                           reads=[psB.r, bias.r], writes=[s_.r])
                    op(ACT, lambda e, s_=s_, p_=p_, nw=nw: e.activation(out=p_.ap[:, 0:nw * 128], in_=s_.ap[:, 0:nw * 128], func=AF.Exp), reads=[s_.r], writes=[p_.r])
                op(ACT, lambda e, p_=p_, nw=nw, cb=cb, psB=psB: e.activation(out=p_.ap[:, nw * 128:nw * 128 + 256], in_=psB.ap[:, cb:cb + 256], func=AF.Exp),
                   reads=[psB.r], writes=[p_.r])
                def stage2(p_=p_, ktl=ktl, v_=v_, o_=o_, qi=qi):
                    pso = self.psum()
                    nk = len(ktl)
                    for j, kti in enumerate(ktl):
                        op(PE, lambda e, pso=pso, p_=p_, j=j, kti=kti, v_=v_, nk=nk: e.matmul(pso.ap[:, 0:65], lhsT=p_.ap[:, j * 128:(j + 1) * 128], rhs=v_.ap[:, kti, :],
                                                                                           start=(j == 0), stop=(j == nk - 1)),
                           reads=[p_.r, v_.r], writes=[pso.r])
                    op(DVE, lambda e, pso=pso: e.reciprocal(out=rc.ap[:, 0:1], in_=pso.ap[:, 64:65]), reads=[pso.r], writes=[rc.r])
                    op(DVE, lambda e, pso=pso, o_=o_, qi=qi: e.tensor_scalar(out=o_.ap[:, qi, :], in0=pso.ap[:, 0:64], scalar1=rc.ap[:, 0:1], scalar2=None, op0=ALU.mult),
                       reads=[pso.r, rc.r], writes=[o_.r])
                if pending is not None:
                    pending()
                pending = stage2
            if pending is not None:
                pending()
                pending = None
            dma(SP, OD.ap.rearrange("(i p) d -> p i d", p=128)[:, :, h * 64:(h + 1) * 64], o_.ap, reads=[o_.r], writes=OD.sub)
        self.out_proj(self.na_w_o[li])

    def gdn(self, l):
        op, dma = self.op, self.dma
        li = l // 2
        hT = self.hT
        self.P.barrier()
        self.off = self.mix_keep
        if not hasattr(self, "QF"):
            for nm in ("QF", "KF", "VF", "ZF", "OF"):
                setattr(self, nm, Tl(self.dram(nm, [D, TT], F32), nm, NT))
        QF, KF, VF, ZF, OF = self.QF, self.KF, self.VF, self.ZF, self.OF
        w_in = self.gdn_w_in[li]
        ones_a = self.tile([128, 128], F32, "ones_a")
        op(DVE, lambda e: e.memset(ones_a.ap, 1.0), writes=[ones_a.r])
        gT_a = self.tile([128, NT, 16], F32, "gT_a")
        bT_a = self.tile([128, NT, 16], F32, "bT_a")
        g_keep = self.off
        cw = self.tile([128, 24, 5], F32, "cw")
        for j in range(5):
            dma(SP, cw.ap[:, :, j], self.gdn_conv_w[li, j:j + 1, :].rearrange("o (c p) -> p (o c)", p=128), writes=[cw.r], allow_slow_non_contiguous=True)
        nwv = self.tile([128, 1], F32, "nwv")
        dma(SP, nwv.ap, self.gdn_norm_w[li:li + 1, :].rearrange("o p -> p o"), writes=[nwv.r], allow_slow_non_contiguous=True)
        pbuf = [self.tile([128, TT + 8], F32, "pbuf") for _ in range(2)]
        cbuf = [self.tile([128, TT], F32, "cbuf") for _ in range(2)]
        for pb in pbuf:
            op(POOL, lambda e, pb=pb: e.memset(pb.ap, 0.0), writes=[pb.r])
        wb = [self.tile([128, 8, 512], BF16, "win") for _ in range(2)]
        sqb = [self.tile([128, 512], F32, "sqb") for _ in range(2)]
        rnb = [self.tile([128, 512], F32, "rnb") for _ in range(2)]
        zsb = [self.tile([128, 512], F32, "zsb") for _ in range(2)]
        blocks = [(0, 256)] + [(256 + 512 * b, 512) for b in range(8)]
        nb = 0
        for cg in range(8):
            w = wb[cg % 2]
            dma(POOL, w.ap, w_in[:, cg * 512:(cg + 1) * 512].rearrange("(k p) n -> p k n", p=128), writes=[w.r])
            for cl in range(4):
                ct = cg * 4 + cl
                pb, cb = pbuf[ct % 2], cbuf[ct % 2]
                for (t0, nt_) in blocks:
                    ps = self.psum()
                    for k in range(8):
                        op(PE, lambda e, ps=ps, w=w, k=k, cl=cl, t0=t0, nt_=nt_: e.matmul(ps.ap[:, 0:nt_], lhsT=w.ap[:, k, cl * 128:(cl + 1) * 128], rhs=hT.ap[:, k, t0:t0 + nt_],
                                                                                    start=(k == 0), stop=(k == 7)),
                           reads=[w.r] + hT.sub[t0 // 128:(t0 + nt_) // 128], writes=ps.sub)
                    if ct < 24:
                        o0 = (2 if t0 == 0 else 6) + t0
                        op(ACT, lambda e, ps=ps, pb=pb, o0=o0, nt_=nt_: e.copy(out=pb.ap[:, o0:o0 + nt_], in_=ps.ap[:, 0:nt_]), reads=ps.sub, writes=[pb.r])
                    else:
                        zs = zsb[nb % 2]
                        nb += 1
                        op(ACT, lambda e, ps=ps, zs=zs, nt_=nt_: e.activation(out=zs.ap[:, 0:nt_], in_=ps.ap[:, 0:nt_], func=AF.Silu), reads=ps.sub, writes=[zs.r])
                        op(DVE, lambda e, zs=zs, nt_=nt_: e.tensor_scalar(out=zs.ap[:, 0:nt_], in0=zs.ap[:, 0:nt_], scalar1=nwv.ap[:, 0:1], scalar2=None, op0=ALU.mult),
                           reads=[zs.r, nwv.r], writes=[zs.r])
                        r0 = (ct - 24) * 128
                        dma(SP, ZF.ap[r0:r0 + 128, t0:t0 + nt_], zs.ap[:, 0:nt_], reads=[zs.r], writes=ZF.sub[t0 // 128:(t0 + nt_) // 128])
                if ct >= 24:
                    continue
                for (c0, ln, base) in ((0, 256, 0), (256, 4096, 260)):
                    op(DVE, lambda e, cb=cb, pb=pb, c0=c0, ln=ln, base=base, ct=ct: e.tensor_scalar(out=cb.ap[:, c0:c0 + ln], in0=pb.ap[:, base:base + ln], scalar1=cw.ap[:, ct, 0:1],
                                                                                             scalar2=None, op0=ALU.mult), reads=[pb.r, cw.r], writes=[cb.r])
                    for j in range(1, 5):
                        op(DVE, lambda e, cb=cb, pb=pb, c0=c0, ln=ln, base=base, ct=ct, j=j: e.scalar_tensor_tensor(out=cb.ap[:, c0:c0 + ln], in0=pb.ap[:, base + j:base + j + ln],
                                                                                                            scalar=cw.ap[:, ct, j:j + 1], in1=cb.ap[:, c0:c0 + ln], op0=ALU.mult, op1=ALU.add),
                           reads=[pb.r, cw.r, cb.r], writes=[cb.r])
                op(ACT, lambda e, cb=cb: e.activation(out=cb.ap, in_=cb.ap, func=AF.Silu), reads=[cb.r], writes=[cb.r])
                if ct < 16:
                    qsc = (128.0 ** -0.5) if ct < 8 else 1.0
                    for (t0, nt_) in blocks:
                        sq, rn = sqb[nb % 2], rnb[nb % 2]
                        nb += 1
                        op(POOL, lambda e, sq=sq, cb=cb, t0=t0, nt_=nt_: e.tensor_tensor(out=sq.ap[:, 0:nt_], in0=cb.ap[:, t0:t0 + nt_], in1=cb.ap[:, t0:t0 + nt_], op=ALU.mult),
                           reads=[cb.r], writes=[sq.r])
                        ps = self.psum()
                        op(PE, lambda e, ps=ps, sq=sq, nt_=nt_, o32=ones_a: e.matmul(ps.ap[:, 0:nt_], lhsT=o32.ap, rhs=sq.ap[:, 0:nt_], start=True, stop=True), reads=[ones_a.r, sq.r], writes=ps.sub)
                        op(DVE, lambda e, ps=ps, rn=rn, nt_=nt_: e.tensor_scalar(out=rn.ap[:, 0:nt_], in0=ps.ap[:, 0:nt_], scalar1=1e-6, scalar2=None, op0=ALU.add), reads=ps.sub, writes=[rn.r])
                        op(ACT, lambda e, rn=rn, nt_=nt_: e.activation(out=rn.ap[:, 0:nt_], in_=rn.ap[:, 0:nt_], func=AF.Ln), reads=[rn.r], writes=[rn.r])
                        op(ACT, lambda e, rn=rn, nt_=nt_: e.activation(out=rn.ap[:, 0:nt_], in_=rn.ap[:, 0:nt_], func=AF.Exp, scale=-0.5), reads=[rn.r], writes=[rn.r])
                        op(DVE, lambda e, rn=rn, cb=cb, t0=t0, nt_=nt_, qsc=qsc: e.scalar_tensor_tensor(out=cb.ap[:, t0:t0 + nt_], in0=cb.ap[:, t0:t0 + nt_], scalar=qsc, in1=rn.ap[:, 0:nt_],
                                                                                                 op0=ALU.mult, op1=ALU.mult), reads=[cb.r, rn.r], writes=[cb.r])
                dst = (QF, KF, VF)[ct // 8]
                r0 = (ct % 8) * 128
                dma(SP, dst.ap[r0:r0 + 128, :], cb.ap, reads=[cb.r], writes=dst.sub)
        wab = self.tile([128, 8, 32], BF16, "wab")
        dma(POOL, wab.ap, w_in[:, 4096:4128].rearrange("(k p) n -> p k n", p=128), writes=[wab.r])
        coef = self.tile([128, 16], F32, "coef")
        dtb = self.tile([128, 16], F32, "dtb")
        dma(SP, coef.ap, self.gdn_a_log[li:li + 1].rearrange("o a b -> o (a b)").to_broadcast([128, 16]), writes=[coef.r])
        dma(SP, dtb.ap, self.gdn_dt_bias[li:li + 1].rearrange("o a b -> o (a b)").to_broadcast([128, 16]), writes=[dtb.r])
        op(ACT, lambda e: e.activation(out=coef.ap, in_=coef.ap, func=AF.Exp), reads=[coef.r], writes=[coef.r])
        op(DVE, lambda e: e.tensor_scalar(out=coef.ap, in0=coef.ap, scalar1=-1.0, scalar2=None, op0=ALU.mult), reads=[coef.r], writes=[coef.r])
        psl = []
        for i in range(NT):
            if i % 16 == 0:
                ps = self.psum()
                psl.append(ps)
            c0 = (i % 16) * 32
            for k in range(8):
                op(PE, lambda e, ps=ps, i=i, k=k, c0=c0: e.matmul(ps.ap[:, c0:c0 + 32], lhsT=hT.ap[:, k, i * 128:(i + 1) * 128], rhs=wab.ap[:, k, :], start=(k == 0), stop=(k == 7)),
                   reads=[wab.r, hT.sub[i]], writes=ps.sub)
        for gi, ps in enumerate(psl):
            n_ = min(16, NT - gi * 16)
            pv = ps.ap[:, 0:n_ * 32].rearrange("p (i c) -> p i c", c=32)
            gs = gT_a.ap[:, gi * 16:gi * 16 + n_, :]
            bs = bT_a.ap[:, gi * 16:gi * 16 + n_, :]
            op(DVE, lambda e, pv=pv, gs=gs, n_=n_: e.tensor_tensor(out=gs, in0=pv[:, :, 0:16], in1=dtb.ap.unsqueeze(1).to_broadcast([128, n_, 16]), op=ALU.add), reads=ps.sub + [dtb.r], writes=[gT_a.r])
            op(ACT, lambda e, gs=gs: e.activation(out=gs, in_=gs, func=AF.Exp), reads=[gT_a.r], writes=[gT_a.r])
            op(DVE, lambda e, gs=gs: e.tensor_scalar(out=gs, in0=gs, scalar1=1.0, scalar2=None, op0=ALU.add), reads=[gT_a.r], writes=[gT_a.r])
            op(ACT, lambda e, gs=gs: e.activation(out=gs, in_=gs, func=AF.Ln), reads=[gT_a.r], writes=[gT_a.r])
            op(DVE, lambda e, gs=gs, n_=n_: e.tensor_tensor(out=gs, in0=gs, in1=coef.ap.unsqueeze(1).to_broadcast([128, n_, 16]), op=ALU.mult), reads=[gT_a.r, coef.r], writes=[gT_a.r])
            op(ACT, lambda e, pv=pv, bs=bs: e.activation(out=bs, in_=pv[:, :, 16:32], func=AF.Exp, scale=-1.0), reads=ps.sub, writes=[bT_a.r])
            op(DVE, lambda e, bs=bs: e.tensor_scalar(out=bs, in0=bs, scalar1=1.0, scalar2=None, op0=ALU.add), reads=[bT_a.r], writes=[bT_a.r])
            op(DVE, lambda e, bs=bs: e.reciprocal(out=bs, in_=bs), reads=[bT_a.r], writes=[bT_a.r])
        self.P.barrier()
        if self.cfg.get("test") == "gdn1":
            rr = Res("dbg2")
            for i_, t_ in enumerate((QF, KF, VF, ZF)):
                dma(SP, self.dbg2[i_], t_.ap, reads=t_.sub, writes=[rr])
            dma(SP, self.dbg3[:, 0], gT_a.ap, reads=[gT_a.r], writes=[rr])
            dma(SP, self.dbg3[:, 1], bT_a.ap, reads=[bT_a.r], writes=[rr])
            self.dbg.sub.append(rr)
            return
        self.off = self.base_off
        gT_o, bT_o = gT_a, bT_a
        ones32 = self.tile([128, 128], F32, "ones32b")
        gT = self.tile([128, NT, 16], F32, "gTb")
        bT = self.tile([128, NT, 16], F32, "bTb")
        op(DVE, lambda e: e.memset(ones32.ap, 1.0), writes=[ones32.r])
        op(DVE, lambda e: e.tensor_copy(out=gT.ap, in_=gT_o.ap), reads=[gT_o.r], writes=[gT.r])
        op(DVE, lambda e: e.tensor_copy(out=bT.ap, in_=bT_o.ap), reads=[bT_o.r], writes=[bT.r])
        self.P.barrier()
        def cmat(name, fill_in, pattern, cmult, cmp, fill):
            t = self.tile([128, 128], F32, name)
            op(POOL, lambda e: e.memset(t.ap, fill_in), writes=[t.r])
            op(POOL, lambda e: e.affine_select(out=t.ap, in_=t.ap, pattern=[[pattern, 128]], compare_op=cmp, fill=fill, base=0, channel_multiplier=cmult), reads=[t.r], writes=[t.r])
            return t
        triu32 = cmat("triu32", 1.0, 1, -1, ALU.is_ge, 0.0)
        tril32 = cmat("tril32", 1.0, -1, 1, ALU.is_ge, 0.0)
        sup = cmat("sup", 1.0, 1, -1, ALU.is_gt, 0.0)
        slo = cmat("slo", 1.0, -1, 1, ALU.is_gt, 0.0)
        mup = cmat("mup", 0.0, 1, -1, ALU.is_ge, -1e30)
        mlo = cmat("mlo", 0.0, -1, 1, ALU.is_ge, -1e30)
        i32 = self.ident32
        wo = self.tile([128, 8, D], BF16, "wo")
        dma(POOL, wo.ap, self.gdn_w_o[li].rearrange("(k p) n -> p k n", p=128), writes=[wo.r])
        def T4(nm):
            t_ = self.tile([128, 8, 128], F32, nm)
            t_.r = GRes(nm)
            return t_
        qfb = [T4("qf") for _ in range(2)]
        kfb = [T4("kf") for _ in range(2)]
        vfb = [T4("vf") for _ in range(2)]
        xfb = [T4("xf") for _ in range(2)]
        zfb = [T4("zf") for _ in range(2)]
        Rt, dTt, d2t, dec, decT, eGbc, AmT, An = (T4(nm) for nm in ("R", "dT", "d2", "dec", "decT", "eGbc", "AmT", "An"))
        kbg, ktail, vb = T4("kbg"), T4("ktail"), T4("vb")
        Mb = [T4("M0"), T4("M1")]
        Nb = [T4("N0"), T4("N1")]
        Pm, attnT, qdT, nwT, vnew, St = T4("P"), T4("attnT"), T4("qdT"), T4("nwT"), T4("vnew"), T4("S")
        PT = T4("PT")
        Mc, Nc = [Rt], [d2t]
        gm = []
        for gi_ in range(4):
            gt_ = self.tile([128, 128], F32, "gmask")
            dma(SP, gt_.ap, self.gmask_in[gi_], writes=[gt_.r])
            gm.append(gt_)
        Gtok = self.tile([128, 8], F32, "Gtok")
        eGtok = self.tile([128, 8], F32, "eGtok")
        bg = self.tile([128, 8], F32, "bg")
        yT = self.tile([128, 8, 128], BF16, "yT")
        ytile = [self.tile([128, D], F32, "ytile") for _ in range(2)]
        fl = lambda t, j: t.ap[:, 4 * j:4 * j + 4, :]
        pv4 = lambda ps: ps.ap.rearrange("p (a b) -> p a b", a=4)
        bc_h = lambda ap2, j: ap2[:, 4 * j:4 * j + 4].unsqueeze(2).to_broadcast([128, 4, 128])
        bc_m = lambda m, n_=4: m.ap.unsqueeze(1).to_broadcast([128, n_, 128])
        hview = lambda ap: ap.rearrange("(h p) t -> p h t", p=128)
        nch = 0
        for d in range(self.cfg.get("gdn_dirs", 2)):
            TRI, maskT, maskN, strT, strN, last = ((triu32, mup, mlo, sup, slo, 127), (tril32, mlo, mup, slo, sup, 0))[d]
            order = list(range(NT)) if d == 0 else [1, 0] + list(range(NT - 1, 1, -1))
            order = order[:self.cfg.get("gdn_nch", NT)]
            op(DVE, lambda e: e.memset(St.ap, 0.0), writes=[St.r])
            for n in order:
                cols = slice(n * 128, (n + 1) * 128)
                qf, kf, vf, xf, zf = qfb[nch % 2], kfb[nch % 2], vfb[nch % 2], xfb[nch % 2], zfb[nch % 2]
                nch += 1
                dma(SP, qf.ap, hview(QF.ap)[:, :, cols], reads=[QF.sub[n]], writes=[qf.r])
                dma(SP, kf.ap, hview(KF.ap)[:, :, cols], reads=[KF.sub[n]], writes=[kf.r])
                dma(SP, vf.ap, hview(VF.ap)[:, :, cols], reads=[VF.sub[n]], writes=[vf.r])
                if d == 1:
                    dma(SP, xf.ap, hview(OF.ap)[:, :, cols], reads=[OF.sub[n]], writes=[xf.r])
                    dma(SP, zf.ap, hview(ZF.ap)[:, :, cols], reads=[ZF.sub[n]], writes=[zf.r])
                gsl = gT.ap[:, n, d * 8:(d + 1) * 8]
                bsl = bT.ap[:, n, d * 8:(d + 1) * 8]
                psG = self.psum()
                op(PE, lambda e, psG=psG, TRI=TRI, gsl=gsl: e.matmul(psG.ap[:, 0:8], lhsT=TRI.ap, rhs=gsl, start=True, stop=True), reads=[TRI.r, gT.r], writes=psG.sub)
                op(DVE, lambda e, gsl=gsl, TRI=TRI: e.tensor_tensor(out=Rt.ap, in0=gsl.unsqueeze(2).to_broadcast([128, 8, 128]), in1=bc_m(TRI, 8), op=ALU.mult),
                   reads=[gT.r, TRI.r], writes=[Rt.r])
                psGb = [self.psum(), self.psum()]
                for j in range(2):
                    op(PE, lambda e, j=j, p=psGb[j]: e.matmul(p.ap, lhsT=ones32.ap, rhs=fl(Rt, j).rearrange("p a b -> p (a b)"), start=True, stop=True),
                       reads=[ones32.r, Rt.r], writes=psGb[j].sub)
                op(ACT, lambda e, psG=psG: e.copy(out=Gtok.ap, in_=psG.ap[:, 0:8]), reads=psG.sub, writes=[Gtok.r])
                for j in range(2):
                    op(DVE, lambda e, j=j, p=psGb[j]: e.tensor_tensor(out=fl(dTt, j), in0=pv4(p), in1=bc_h(Gtok.ap, j), op=ALU.subtract), reads=psGb[j].sub + [Gtok.r], writes=[dTt.r])
                    op(ACT, lambda e, j=j, p=psGb[j]: e.activation(out=fl(eGbc, j), in_=pv4(p), func=AF.Exp), reads=psGb[j].sub, writes=[eGbc.r])
                op(DVE, lambda e, maskN=maskN: e.scalar_tensor_tensor(out=d2t.ap, in0=dTt.ap, scalar=-1.0, in1=bc_m(maskN, 8), op0=ALU.mult, op1=ALU.add),
                   reads=[dTt.r, maskN.r], writes=[d2t.r])
                op(POOL, lambda e, maskT=maskT: e.tensor_tensor(out=dTt.ap, in0=dTt.ap, in1=bc_m(maskT, 8), op=ALU.add), reads=[dTt.r, maskT.r, d2t.r], writes=[dTt.r])
                op(ACT, lambda e: e.activation(out=dec.ap, in_=d2t.ap, func=AF.Exp), reads=[d2t.r], writes=[dec.r])
                op(ACT, lambda e: e.activation(out=decT.ap, in_=dTt.ap, func=AF.Exp), reads=[dTt.r], writes=[decT.r])
                op(ACT, lambda e: e.activation(out=eGtok.ap, in_=Gtok.ap, func=AF.Exp), reads=[Gtok.r], writes=[eGtok.r])
                op(DVE, lambda e, bsl=bsl: e.tensor_tensor(out=bg.ap, in0=eGtok.ap, in1=bsl, op=ALU.mult), reads=[eGtok.r, bT.r], writes=[bg.r])
                op(DVE, lambda e, bsl=bsl: e.tensor_tensor(out=Rt.ap, in0=bsl.unsqueeze(2).to_broadcast([128, 8, 128]), in1=bc_m(i32, 8), op=ALU.mult),
                   reads=[bT.r, i32.r], writes=[Rt.r])
                psBb = [self.psum(), self.psum()]
                for j in range(2):
                    op(PE, lambda e, j=j, p=psBb[j]: e.matmul(p.ap, lhsT=ones32.ap, rhs=fl(Rt, j).rearrange("p a b -> p (a b)"), start=True, stop=True),
                       reads=[ones32.r, Rt.r], writes=psBb[j].sub)
                for j in range(2):
                    op(DVE, lambda e, j=j, p=psBb[j]: e.tensor_tensor(out=fl(AmT, j), in0=fl(decT, j), in1=pv4(p), op=ALU.mult), reads=psBb[j].sub + [decT.r], writes=[AmT.r])
                op(POOL, lambda e, strT=strT: e.tensor_tensor(out=AmT.ap, in0=AmT.ap, in1=bc_m(strT, 8), op=ALU.mult), reads=[AmT.r, strT.r], writes=[AmT.r])
                op(POOL, lambda e, bsl=bsl: e.tensor_tensor(out=An.ap, in0=dec.ap, in1=bsl.unsqueeze(2).to_broadcast([128, 8, 128]), op=ALU.mult), reads=[dec.r, bT.r], writes=[An.r])
                op(POOL, lambda e, strN=strN: e.tensor_tensor(out=An.ap, in0=An.ap, in1=bc_m(strN, 8), op=ALU.mult), reads=[An.r, strN.r], writes=[An.r])
                op(POOL, lambda e, qf=qf: e.tensor_tensor(out=qdT.ap, in0=qf.ap, in1=eGbc.ap, op=ALU.mult), reads=[qf.r, eGbc.r], writes=[qdT.r])
                if self.cfg.get('gdn_stage', 99) < 1:
                    continue
                psK = [self.psum(), self.psum()]
                psV = [self.psum(), self.psum()]
                for h in range(8):
                    j, q4 = h // 4, (h % 4) * 128
                    op(PE, lambda e, h=h, p=psK[j], q4=q4, kf=kf: e.transpose(out=p.ap[:, q4:q4 + 128], in_=kf.ap[:, h, :], identity=i32.ap), reads=[kf.r, i32.r], writes=[psK[j].sub[h % 4]])
                    op(PE, lambda e, h=h, p=psV[j], q4=q4, vf=vf: e.transpose(out=p.ap[:, q4:q4 + 128], in_=vf.ap[:, h, :], identity=i32.ap), reads=[vf.r, i32.r], writes=[psV[j].sub[h % 4]])
                for j in range(2):
                    op(DVE, lambda e, j=j, p=psK[j]: e.tensor_tensor(out=fl(kbg, j), in0=pv4(p), in1=bc_h(bg.ap, j), op=ALU.mult), reads=psK[j].sub + [bg.r], writes=[kbg.r])
                    op(DVE, lambda e, j=j, p=psK[j], last=last: e.tensor_tensor(out=fl(ktail, j), in0=pv4(p), in1=decT.ap[:, 4 * j:4 * j + 4, last:last + 1].to_broadcast([128, 4, 128]), op=ALU.mult),
                       reads=psK[j].sub + [decT.r], writes=[ktail.r])
                    op(DVE, lambda e, j=j, p=psV[j], bsl=bsl: e.tensor_tensor(out=fl(vb, j), in0=pv4(p), in1=bc_h(bsl, j), op=ALU.mult), reads=psV[j].sub + [bT.r], writes=[vb.r])
                if self.cfg.get('gdn_stage', 99) < 2:
                    continue
                psKK = [self.psum(), self.psum()]
                psKQ = [self.psum(), self.psum()]
                for h in range(8):
                    j, q4 = h // 4, (h % 4) * 128
                    op(PE, lambda e, h=h, p=psKK[j], q4=q4, kf=kf: e.matmul(p.ap[:, q4:q4 + 128], lhsT=kf.ap[:, h, :], rhs=kf.ap[:, h, :], start=True, stop=True), reads=[kf.r], writes=[psKK[j].sub[h % 4]])
                    op(PE, lambda e, h=h, p=psKQ[j], q4=q4, kf=kf, qf=qf: e.matmul(p.ap[:, q4:q4 + 128], lhsT=kf.ap[:, h, :], rhs=qf.ap[:, h, :], start=True, stop=True), reads=[kf.r, qf.r], writes=[psKQ[j].sub[h % 4]])
                M0, N0 = Mb[0], Nb[0]
                for j in range(2):
                    op(DVE, lambda e, j=j, p=psKK[j]: e.scalar_tensor_tensor(out=fl(M0, j), in0=pv4(p), scalar=-1.0, in1=fl(AmT, j), op0=ALU.mult, op1=ALU.mult), reads=psKK[j].sub + [AmT.r], writes=[M0.r])
                    op(DVE, lambda e, j=j, p=psKK[j]: e.scalar_tensor_tensor(out=fl(N0, j), in0=pv4(p), scalar=-1.0, in1=fl(An, j), op0=ALU.mult, op1=ALU.mult), reads=psKK[j].sub + [An.r], writes=[N0.r])
                    op(DVE, lambda e, j=j, p=psKQ[j]: e.tensor_tensor(out=fl(attnT, j), in0=pv4(p), in1=fl(decT, j), op=ALU.mult), reads=psKQ[j].sub + [decT.r], writes=[attnT.r])
                MA, NA, MB, NB = Mb[1], Nb[1], Mc[0], Nc[0]
                op(DVE, lambda e: e.tensor_tensor(out=MA.ap, in0=M0.ap, in1=bc_m(gm[0], 8), op=ALU.mult), reads=[M0.r, gm[0].r], writes=[MA.r])
                op(POOL, lambda e: e.tensor_tensor(out=NA.ap, in0=N0.ap, in1=bc_m(gm[0], 8), op=ALU.mult), reads=[N0.r, gm[0].r], writes=[NA.r])
                op(POOL, lambda e: e.tensor_tensor(out=Pm.ap, in0=MA.ap, in1=bc_m(i32, 8), op=ALU.add), reads=[MA.r, i32.r], writes=[Pm.r])
                op(POOL, lambda e: e.tensor_tensor(out=PT.ap, in0=NA.ap, in1=bc_m(i32, 8), op=ALU.add), reads=[NA.r, i32.r], writes=[PT.r])
                cur = (MA, NA)
                nxt = (MB, NB)
                for kk in range(1, 4):
                    Mp, Np = cur
                    Mn, Nn = nxt
                    psN = [self.psum(), self.psum()]
                    psM = [self.psum(), self.psum()]
                    for h in range(8):
                        j, q4 = h // 4, (h % 4) * 128
                        op(PE, lambda e, h=h, p=psN[j], q4=q4, Mp=Mp, Np=Np: e.matmul(p.ap[:, q4:q4 + 128], lhsT=Mp.ap[:, h, :], rhs=Np.ap[:, h, :], start=True, stop=True),
                           reads=[Mp.r, Np.r], writes=[psN[j].sub[h % 4]])
                        op(PE, lambda e, h=h, p=psM[j], q4=q4, Mp=Mp, Np=Np: e.matmul(p.ap[:, q4:q4 + 128], lhsT=Np.ap[:, h, :], rhs=Mp.ap[:, h, :], start=True, stop=True),
                           reads=[Mp.r, Np.r], writes=[psM[j].sub[h % 4]])
                    op(ACT, lambda e, j=0, p=psN[0], Nn=Nn: e.copy(out=fl(Nn, j), in_=pv4(p)), reads=psN[0].sub, writes=[Nn.r])
                    op(DVE, lambda e, j=1, p=psN[1], Nn=Nn: e.tensor_copy(out=fl(Nn, j), in_=pv4(p)), reads=psN[1].sub, writes=[Nn.r])
                    op(DVE, lambda e, j=0, p=psM[0], Mn=Mn: e.tensor_copy(out=fl(Mn, j), in_=pv4(p)), reads=psM[0].sub, writes=[Mn.r])
                    op(ACT, lambda e, j=1, p=psM[1], Mn=Mn: e.copy(out=fl(Mn, j), in_=pv4(p)), reads=psM[1].sub, writes=[Mn.r])
                    psP = [self.psum(), self.psum()]
                    psQ_ = [self.psum(), self.psum()]
                    for h in range(8):
                        j, q4 = h // 4, (h % 4) * 128
                        op(PE, lambda e, h=h, p=psP[j], q4=q4, Nn=Nn: e.matmul(p.ap[:, q4:q4 + 128], lhsT=Nn.ap[:, h, :], rhs=Pm.ap[:, h, :], start=True, stop=True),
                           reads=[Nn.r, Pm.r], writes=[psP[j].sub[h % 4]])
                        op(PE, lambda e, h=h, p=psQ_[j], q4=q4, Mn=Mn: e.matmul(p.ap[:, q4:q4 + 128], lhsT=Mn.ap[:, h, :], rhs=PT.ap[:, h, :], start=True, stop=True),
                           reads=[Mn.r, PT.r], writes=[psQ_[j].sub[h % 4]])
                    for j in range(2):
                        op(DVE, lambda e, j=j, p=psP[j]: e.tensor_tensor(out=fl(Pm, j), in0=fl(Pm, j), in1=pv4(p), op=ALU.add), reads=psP[j].sub + [Pm.r], writes=[Pm.r])
                        op(DVE, lambda e, j=j, p=psQ_[j]: e.tensor_tensor(out=fl(PT, j), in0=fl(PT, j), in1=pv4(p), op=ALU.add), reads=psQ_[j].sub + [PT.r], writes=[PT.r])
                    cur, nxt = nxt, cur
                for lv in range(3):
                    UoT, Yt = Mc[0], Nc[0]
                    op(DVE, lambda e, lv=lv, UoT=UoT: e.scalar_tensor_tensor(out=UoT.ap, in0=N0.ap, scalar=-1.0, in1=bc_m(gm[1 + lv], 8), op0=ALU.mult, op1=ALU.mult),
                       reads=[N0.r, gm[1 + lv].r], writes=[UoT.r])
                    psY = [self.psum(), self.psum()]
                    for h in range(8):
                        j, q4 = h // 4, (h % 4) * 128
                        op(PE, lambda e, h=h, p=psY[j], q4=q4, UoT=UoT: e.matmul(p.ap[:, q4:q4 + 128], lhsT=UoT.ap[:, h, :], rhs=Pm.ap[:, h, :], start=True, stop=True),
                           reads=[UoT.r, Pm.r], writes=[psY[j].sub[h % 4]])
                    op(ACT, lambda e, j=0, p=psY[0], Yt=Yt: e.copy(out=fl(Yt, j), in_=pv4(p)), reads=psY[0].sub, writes=[Yt.r])
                    op(DVE, lambda e, j=1, p=psY[1], Yt=Yt: e.tensor_copy(out=fl(Yt, j), in_=pv4(p)), reads=psY[1].sub, writes=[Yt.r])
                    psX = [self.psum(), self.psum()]
                    psXT = [self.psum(), self.psum()]
                    for h in range(8):
                        j, q4 = h // 4, (h % 4) * 128
                        op(PE, lambda e, h=h, p=psX[j], q4=q4, Yt=Yt: e.matmul(p.ap[:, q4:q4 + 128], lhsT=PT.ap[:, h, :], rhs=Yt.ap[:, h, :], start=True, stop=True),
                           reads=[PT.r, Yt.r], writes=[psX[j].sub[h % 4]])
                        if lv < 2:
                            op(PE, lambda e, h=h, p=psXT[j], q4=q4, Yt=Yt: e.matmul(p.ap[:, q4:q4 + 128], lhsT=Yt.ap[:, h, :], rhs=PT.ap[:, h, :], start=True, stop=True),
                               reads=[PT.r, Yt.r], writes=[psXT[j].sub[h % 4]])
                    for j in range(2):
                        op(DVE, lambda e, j=j, p=psX[j]: e.tensor_tensor(out=fl(Pm, j), in0=fl(Pm, j), in1=pv4(p), op=ALU.subtract), reads=psX[j].sub + [Pm.r], writes=[Pm.r])
                        if lv < 2:
                            op(DVE, lambda e, j=j, p=psXT[j]: e.tensor_tensor(out=fl(PT, j), in0=fl(PT, j), in1=pv4(p), op=ALU.subtract), reads=psXT[j].sub + [PT.r], writes=[PT.r])
                if self.cfg.get('gdn_stage', 99) < 4:
                    continue
                psW = [self.psum(), self.psum()]
                for h in range(8):
                    j, q4 = h // 4, (h % 4) * 128
                    op(PE, lambda e, h=h, p=psW[j], q4=q4: e.matmul(p.ap[:, q4:q4 + 128], lhsT=kbg.ap[:, h, :], rhs=Pm.ap[:, h, :], start=True, stop=True), reads=[kbg.r, Pm.r], writes=[psW[j].sub[h % 4]])
                for j in range(2):
                    op(ACT, lambda e, j=j, p=psW[j]: e.mul(out=fl(nwT, j), in_=pv4(p), mul=-1.0), reads=psW[j].sub, writes=[nwT.r])
                psVn = [self.psum(), self.psum()]
                for h in range(8):
                    j, q4 = h // 4, (h % 4) * 128
                    op(PE, lambda e, h=h, p=psVn[j], q4=q4: e.matmul(p.ap[:, q4:q4 + 128], lhsT=Pm.ap[:, h, :], rhs=vb.ap[:, h, :], start=True, stop=False), reads=[Pm.r, vb.r], writes=[psVn[j].sub[h % 4]])
                    op(PE, lambda e, h=h, p=psVn[j], q4=q4: e.matmul(p.ap[:, q4:q4 + 128], lhsT=nwT.ap[:, h, :], rhs=St.ap[:, h, :], start=False, stop=True), reads=[nwT.r, St.r], writes=[psVn[j].sub[h % 4]])
                op(ACT, lambda e, j=0, p=psVn[0]: e.copy(out=fl(vnew, j), in_=pv4(p)), reads=psVn[0].sub, writes=[vnew.r])
                op(DVE, lambda e, j=1, p=psVn[1]: e.tensor_copy(out=fl(vnew, j), in_=pv4(p)), reads=psVn[1].sub, writes=[vnew.r])
                if self.cfg.get('gdn_stage', 99) < 5:
                    continue
                psO = [self.psum(), self.psum()]
                psS = [self.psum(), self.psum()]
                for h in range(8):
                    j, q4 = h // 4, (h % 4) * 128
                    op(PE, lambda e, h=h, p=psO[j], q4=q4: e.matmul(p.ap[:, q4:q4 + 128], lhsT=St.ap[:, h, :], rhs=qdT.ap[:, h, :], start=True, stop=False), reads=[St.r, qdT.r], writes=[psO[j].sub[h % 4]])
                    op(PE, lambda e, h=h, p=psO[j], q4=q4: e.matmul(p.ap[:, q4:q4 + 128], lhsT=vnew.ap[:, h, :], rhs=attnT.ap[:, h, :], start=False, stop=True), reads=[vnew.r, attnT.r], writes=[psO[j].sub[h % 4]])
                    op(PE, lambda e, h=h, p=psS[j], q4=q4: e.matmul(p.ap[:, q4:q4 + 128], lhsT=ktail.ap[:, h, :], rhs=vnew.ap[:, h, :], start=True, stop=True), reads=[ktail.r, vnew.r], writes=[psS[j].sub[h % 4]])
                for j in range(2):
                    op(DVE, lambda e, j=j, last=last: e.tensor_tensor(out=fl(St, j), in0=fl(St, j), in1=eGbc.ap[:, 4 * j:4 * j + 4, last:last + 1].to_broadcast([128, 4, 128]), op=ALU.mult),
                       reads=[St.r, eGbc.r], writes=[St.r])
                    op(DVE, lambda e, j=j, p=psS[j]: e.tensor_tensor(out=fl(St, j), in0=fl(St, j), in1=pv4(p), op=ALU.add), reads=psS[j].sub + [St.r], writes=[St.r])
                if d == 0:
                    for j in range(2):
                        op(ACT, lambda e, j=j, p=psO[j], xf=xf: e.copy(out=fl(xf, j), in_=pv4(p)), reads=psO[j].sub, writes=[xf.r])
                    dma(SP, hview(OF.ap)[:, :, cols], xf.ap, reads=[xf.r], writes=[OF.sub[n]])
                    continue
                for j in range(2):
                    op(DVE, lambda e, j=j, p=psO[j], xf=xf: e.tensor_tensor(out=fl(xf, j), in0=fl(xf, j), in1=pv4(p), op=ALU.add), reads=psO[j].sub + [xf.r], writes=[xf.r])
                op(POOL, lambda e, xf=xf: e.tensor_tensor(out=Rt.ap, in0=xf.ap, in1=xf.ap, op=ALU.mult), reads=[xf.r], writes=[Rt.r])
                psQ = [self.psum(), self.psum()]
                for j in range(2):
                    op(PE, lambda e, j=j, p=psQ[j]: e.matmul(p.ap, lhsT=ones32.ap, rhs=fl(Rt, j).rearrange("p a b -> p (a b)"), start=True, stop=True), reads=[ones32.r, Rt.r], writes=psQ[j].sub)
                for j in range(2):
                    op(DVE, lambda e, j=j, p=psQ[j]: e.tensor_scalar(out=fl(d2t, j), in0=pv4(p), scalar1=1.0 / 128.0, scalar2=LN_EPS, op0=ALU.mult, op1=ALU.add), reads=psQ[j].sub, writes=[d2t.r])
                op(ACT, lambda e: e.activation(out=d2t.ap, in_=d2t.ap, func=AF.Ln), reads=[d2t.r], writes=[d2t.r])
                op(ACT, lambda e: e.activation(out=d2t.ap, in_=d2t.ap, func=AF.Exp, scale=-0.5), reads=[d2t.r], writes=[d2t.r])
                op(POOL, lambda e, xf=xf: e.tensor_tensor(out=xf.ap, in0=xf.ap, in1=d2t.ap, op=ALU.mult), reads=[xf.r, d2t.r], writes=[xf.r])
                op(POOL, lambda e, xf=xf, zf=zf: e.tensor_tensor(out=yT.ap, in0=xf.ap, in1=zf.ap, op=ALU.mult), reads=[xf.r, zf.r], writes=[yT.r])
                yt = ytile[n % 2]
                for half in range(2):
                    ps = self.psum()
                    for h in range(8):
                        op(PE, lambda e, ps=ps, h=h, half=half: e.matmul(ps.ap, lhsT=yT.ap[:, h, :], rhs=wo.ap[:, h, half * 512:(half + 1) * 512], start=(h == 0), stop=(h == 7)),
                           reads=[yT.r, wo.r], writes=ps.sub)
                    op(ACT, lambda e, ps=ps, yt=yt, half=half: e.copy(out=yt.ap[:, half * 512:(half + 1) * 512], in_=ps.ap), reads=ps.sub, writes=[yt.r])
                dma(SP, self.Y.ap[cols, :], yt.ap, reads=[yt.r], writes=[self.Y.sub[n]])
                if self.cfg.get("test") == "mix":
                    dma(SP, self.dbg.ap[cols, :], yt.ap, reads=[yt.r], writes=[self.dbg.sub[n]])
        if self.cfg.get("test") == "gdn2":
            self.P.barrier()
            rr = Res("dbg2")
            dma(SP, self.dbg2[0], VF.ap if self.cfg.get('gdn_nch') == 1 else OF.ap, reads=OF.sub + VF.sub, writes=[rr])
            dma(SP, self.dbg2[1, 0:128, 0:1024], St.ap.rearrange("p a b -> p (a b)"), reads=[St.r], writes=[rr])
            for ii, tt in enumerate((decT, dec, AmT, An, Pm, vnew, attnT, kbg, vb, ktail, qdT, eGbc, nwT, qfb[0], kfb[0], vfb[0])):
                dma(SP, self.dbg2[2 + ii // 8, (ii % 8) * 128:(ii % 8 + 1) * 128, 0:1024], tt.ap.rearrange("p a b -> p (a b)"), reads=[tt.r], writes=[rr])
            dma(SP, self.dbg3[:, 0], gT.ap, reads=[gT.r], writes=[rr])
            dma(SP, self.dbg3[:, 1], bT.ap, reads=[bT.r], writes=[rr])
            self.dbg.sub.append(rr)

    def out_proj(self, w_o):
        op, dma = self.op, self.dma
        self.P.barrier()
        self.off = self.base_off
        OD = self.OD
        wo = self.tile([128, 8, D], BF16, "wo")
        dma(POOL, wo.ap, w_o.rearrange("(k p) n -> p k n", p=128), writes=[wo.r])
        ob = [self.tile([128, D], BF16, "ob") for _ in range(3)]
        oT = [self.tile([128, 8, 128], BF16, "oT", nsub=1) for _ in range(2)]
        yb = [self.tile([128, D], F32, "yb") for _ in range(2)]
        for i in range(NT):
            o, t, y = ob[i % 3], oT[i % 2], yb[i % 2]
            rows = slice(i * 128, (i + 1) * 128)
            dma(SP, o.ap, OD.ap[rows, :], reads=[OD.sub[i]], writes=[o.r])
            self.transpose_to(o, t, 0)
            for half in range(2):
                ps = self.psum()
                for k in range(8):
                    op(PE, lambda e, ps=ps, t=t, k=k, half=half: e.matmul(ps.ap, lhsT=t.ap[:, k, :], rhs=wo.ap[:, k, half * 512:(half + 1) * 512], start=(k == 0), stop=(k == 7)),
                       reads=[t.sub[0], wo.r], writes=[ps.r])
                op(ACT if half else DVE, (lambda e, ps=ps, y=y, half=half: e.copy(out=y.ap[:, half * 512:(half + 1) * 512], in_=ps.ap)) if half else
                   (lambda e, ps=ps, y=y, half=half: e.tensor_copy(out=y.ap[:, half * 512:(half + 1) * 512], in_=ps.ap)), reads=[ps.r], writes=[y.r])
            dma(SP, self.Y.ap[rows, :], y.ap, reads=[y.r], writes=[self.Y.sub[i]])
            if self.cfg.get("test") == "mix":
                dma(SP, self.dbg.ap[rows, :], y.ap, reads=[y.r], writes=[self.dbg.sub[i]])

    def postmix(self, l):
        op, dma = self.op, self.dma
        self.phase_begin()
        self.h2tok = self.tile([128, NT, D], BF16, "h2tok", nsub=NT)
        self.affTok = self.tile([128, NT, NE], F32, "affTok")
        h2tok, affTok = self.h2tok, self.affTok
        self.post_keep = self.off
        mods = self.mod_tiles(l, [2, 3, 4], plus1=(4,))
        g0 = self.ln_vec(self.ln_g[l, 0:1, :])
        b0 = self.ln_vec(self.ln_b[l, 0:1, :])
        wr = self.tile([128, 8, NE], BF16, "wr")
        dma(POOL, wr.ap, self.moe_w_router[l].rearrange("(k p) n -> p k n", p=128), writes=[wr.r])
        xb = [self.tile([128, D], F32, "xb") for _ in range(2)]
        yb = [self.tile([128, D], F32, "yb") for _ in range(2)]
        ob = [self.tile([128, D], F32, "ob") for _ in range(2)]
        h2T = [self.tile([128, 8, 128], BF16, "h2T", nsub=1) for _ in range(2)]
        st = self.tile([128, 2, 6], F32, "st")
        mv = self.tile([128, 4], F32, "mv")
        sm = self.tile([128, 4], F32, "sm")
        ex = self.tile([128, NE], F32, "ex")
        for i in range(NT):
            x, y, o = xb[i % 2], yb[i % 2], ob[i % 2]
            c = 1 if i < 2 else 0
            rows = slice(i * 128, (i + 1) * 128)
            dma(SP, x.ap, self.XS.ap[rows, :], reads=[self.XS.sub[i]], writes=[x.r])
            dma(SP, y.ap, self.Y.ap[rows, :], reads=[self.Y.sub[i]], writes=[y.r])
            m2, m3, m4 = mods[2][c], mods[3][c], mods[4][c]
            op(POOL, lambda e, y=y, m2=m2: e.tensor_tensor(out=y.ap, in0=y.ap, in1=m2.ap, op=ALU.mult), reads=[y.r, m2.r], writes=[y.r])
            op(DVE, lambda e, x=x, y=y: e.scalar_tensor_tensor(out=y.ap, in0=x.ap, scalar=ALPHA, in1=y.ap, op0=ALU.mult, op1=ALU.add),
               reads=[x.r, y.r], writes=[y.r])
            self.layernorm_(y, st, mv)
            op(POOL, lambda e, y=y, o=o: e.tensor_tensor(out=o.ap, in0=y.ap, in1=g0.ap, op=ALU.mult), reads=[y.r, g0.r], writes=[o.r])
            op(POOL, lambda e, o=o: e.tensor_tensor(out=o.ap, in0=o.ap, in1=b0.ap, op=ALU.add), reads=[o.r, b0.r], writes=[o.r])
            dma(POOL, self.XS.ap[rows, :], o.ap, reads=[o.r], writes=[self.XS.sub[i]])
            op(DVE, lambda e, o=o, x=x, m4=m4: e.tensor_tensor(out=x.ap, in0=o.ap, in1=m4.ap, op=ALU.mult), reads=[o.r, m4.r], writes=[x.r])
            hv = Tl(h2tok.ap[:, i, :])
            hv.r = h2tok.sub[i]
            op(DVE, lambda e, x=x, m3=m3, hv=hv: e.tensor_tensor(out=hv.ap, in0=x.ap, in1=m3.ap, op=ALU.add), reads=[x.r, m3.r], writes=[hv.r])
            ht = h2T[i % 2]
            self.transpose_to(hv, ht, 0)
            ps = self.psum()
            for k in range(8):
                op(PE, lambda e, ps=ps, ht=ht, k=k: e.matmul(ps.ap[:, 0:NE], lhsT=ht.ap[:, k, :], rhs=wr.ap[:, k, :], start=(k == 0), stop=(k == 7)),
                   reads=[ht.sub[0], wr.r], writes=[ps.r])
            op(DVE, lambda e, ps=ps: e.reduce_max(out=sm.ap[:, 0:1], in_=ps.ap[:, 0:NE], axis=AX.X), reads=[ps.r], writes=[sm.r])
            op(DVE, lambda e: e.tensor_scalar(out=sm.ap[:, 1:2], in0=sm.ap[:, 0:1], scalar1=-1.0, scalar2=None, op0=ALU.mult), reads=[sm.r], writes=[sm.r])
            op(ACT, lambda e, ps=ps: e.activation(out=ex.ap, in_=ps.ap[:, 0:NE], func=AF.Exp, bias=sm.ap[:, 1:2], scale=1.0, accum_out=sm.ap[:, 2:3]),
               reads=[ps.r, sm.r], writes=[ex.r, sm.r])
            op(DVE, lambda e: e.reciprocal(out=sm.ap[:, 3:4], in_=sm.ap[:, 2:3]), reads=[sm.r], writes=[sm.r])
            op(DVE, lambda e, i=i: e.tensor_scalar(out=affTok.ap[:, i, :], in0=ex.ap, scalar1=sm.ap[:, 3:4], scalar2=None, op0=ALU.mult),
               reads=[ex.r, sm.r], writes=[affTok.r])

    def moe_select(self, l):
        op, dma = self.op, self.dma
        affTok = self.affTok
        self.P.barrier()
        self.off = self.post_keep
        self.posm = self.tile([128, NT, NE], F32, "posm")
        posm = self.posm
        keep2 = self.off
        affT = self.tile([NE, TT], F32, "affT")
        work = self.tile([NE, 4096], F32, "work")
        m8 = self.tile([NE, 8], F32, "m8")
        thr = self.tile([NE, 2], F32, "thr")
        maskT = self.tile([NE, TT], BF16, "maskT")
        for g in range(9):
            ps = self.psum()
            tl = list(range(g * 4, min(g * 4 + 4, NT)))
            for j, i in enumerate(tl):
                op(PE, lambda e, ps=ps, i=i, j=j: e.transpose(out=ps.ap[0:NE, j * 128:(j + 1) * 128], in_=affTok.ap[:, i, :], identity=self.ident32.ap),
                   reads=[affTok.r, self.ident32.r], writes=[ps.r])
            n = len(tl) * 128
            op(ACT, lambda e, ps=ps, g=g, n=n: e.copy(out=affT.ap[:, g * 512:g * 512 + n], in_=ps.ap[0:NE, 0:n]), reads=[ps.r], writes=[affT.r])
        dma(SP, self.AFFT.ap, affT.ap, reads=[affT.r], writes=[self.AFFT.r])
        op(DVE, lambda e: e.tensor_copy(out=work.ap[:, 0:256], in_=affT.ap[:, 0:256]), reads=[affT.r], writes=[work.r])
        for it in range(4):
            op(DVE, lambda e: e.max(out=m8.ap, in_=work.ap[:, 0:256]), reads=[work.r], writes=[m8.r])
            if it < 3:
                op(DVE, lambda e: e.match_replace(out=work.ap[:, 0:256], in_to_replace=m8.ap, in_values=work.ap[:, 0:256], imm_value=-1.0),
                   reads=[work.r, m8.r], writes=[work.r])
        op(DVE, lambda e: e.tensor_copy(out=thr.ap[:, 0:1], in_=m8.ap[:, 7:8]), reads=[m8.r], writes=[thr.r])
        if not hasattr(self, "CAND"):
            self.CAND = Tl(self.dram("CAND", [128, 128], F32), "CAND")
        w1 = self.tile([128, 512], F32, "w1")
        c1 = self.tile([128, 128], F32, "c1")
        for e_ in range(NE):
            dma(SP, w1.ap[e_ * 8:(e_ + 1) * 8, :], self.AFFT.ap[e_, 256:TT].rearrange("(s t) -> s t", s=8), reads=[self.AFFT.r], writes=[w1.r])
        for it in range(16):
            op(DVE, lambda e, it=it: e.max(out=c1.ap[:, it * 8:(it + 1) * 8], in_=w1.ap), reads=[w1.r], writes=[c1.r])
            if it < 15:
                op(DVE, lambda e, it=it: e.match_replace(out=w1.ap, in_to_replace=c1.ap[:, it * 8:(it + 1) * 8], in_values=w1.ap, imm_value=-1.0),
                   reads=[w1.r, c1.r], writes=[w1.r])
        dma(SP, self.CAND.ap, c1.ap, reads=[c1.r], writes=[self.CAND.r])
        dma(SP, work.ap[:, 0:1024], self.CAND.ap.rearrange("(e s) c -> e (s c)", s=8), reads=[self.CAND.r], writes=[work.r])
        for it in range(64):
            op(DVE, lambda e: e.max(out=m8.ap, in_=work.ap[:, 0:1024]), reads=[work.r], writes=[m8.r])
            if it < 63:
                op(DVE, lambda e: e.match_replace(out=work.ap[:, 0:1024], in_to_replace=m8.ap, in_values=work.ap[:, 0:1024], imm_value=-1.0),
                   reads=[work.r, m8.r], writes=[work.r])
        op(DVE, lambda e: e.tensor_copy(out=thr.ap[:, 1:2], in_=m8.ap[:, 7:8]), reads=[m8.r], writes=[thr.r])
        for (lo, n, col) in ((0, 256, 0), (256, 4096, 1)):
            op(DVE, lambda e, lo=lo, n=n, col=col: e.tensor_scalar(out=maskT.ap[:, lo:lo + n], in0=affT.ap[:, lo:lo + n], scalar1=thr.ap[:, col:col + 1],
                                                                   scalar2=None, op0=ALU.is_ge), reads=[affT.r, thr.r], writes=[maskT.r])
        maskTok = self.tile([128, NT, NE], BF16, "maskTok")
        maskF = self.tile([128, NT, NE], F32, "maskF")
        ps = self.psum()
        pb = ps.ap.bitcast(BF16)
        for i in range(NT):
            op(PE, lambda e, i=i, pb=pb: e.transpose(out=pb[:, i * NE:(i + 1) * NE], in_=maskT.ap[:, i * 128:(i + 1) * 128], identity=self.ident.ap[0:NE, 0:NE]),
               reads=[maskT.r, self.ident.r], writes=[ps.r])
        op(DVE, lambda e, pb=pb: e.tensor_copy(out=maskTok.ap.rearrange("p a b -> p (a b)"), in_=pb[:, 0:NT * NE]), reads=[ps.r], writes=[maskTok.r])
        op(DVE, lambda e: e.tensor_copy(out=maskF.ap, in_=maskTok.ap), reads=[maskTok.r], writes=[maskF.r])
        mflat = maskTok.ap.rearrange("p a b -> p (a b)")
        ps_w, ps_t, ps_c = self.psum(), self.psum(), self.psum()
        op(PE, lambda e: e.matmul(ps_w.ap, lhsT=self.triu.ap, rhs=mflat[:, 2 * NE:NT * NE], start=True, stop=True), reads=[maskTok.r, self.triu.r], writes=[ps_w.r])
        op(PE, lambda e: e.matmul(ps_t.ap, lhsT=self.ones.ap, rhs=mflat[:, 2 * NE:NT * NE], start=True, stop=True), reads=[maskTok.r, self.ones.r], writes=[ps_t.r])
        op(PE, lambda e: e.matmul(ps_c.ap[:, 0:2 * NE], lhsT=self.triu.ap, rhs=mflat[:, 0:2 * NE], start=True, stop=True), reads=[maskTok.r, self.triu.r], writes=[ps_c.r])
        op(PE, lambda e: e.matmul(ps_c.ap[:, 2 * NE:4 * NE], lhsT=self.ones.ap, rhs=mflat[:, 0:2 * NE], start=True, stop=True), reads=[maskTok.r, self.ones.r], writes=[ps_c.r])
        eoff = self.tile([128, NT, NE], F32, "eoff")
        tot = self.tile([128, NT, NE], F32, "tot")
        op(DVE, lambda e: e.tensor_copy(out=tot.ap[:, 2:NT, :].rearrange("p a b -> p (a b)"), in_=ps_t.ap), reads=[ps_t.r], writes=[tot.r])
        op(DVE, lambda e: e.tensor_copy(out=tot.ap[:, 0:2, :].rearrange("p a b -> p (a b)"), in_=ps_c.ap[:, 2 * NE:4 * NE]), reads=[ps_c.r], writes=[tot.r])
        op(DVE, lambda e: e.memset(eoff.ap, 0.0), writes=[eoff.r])
        op(DVE, lambda e: e.tensor_copy(out=eoff.ap[:, 1, :], in_=tot.ap[:, 0, :]), reads=[tot.r], writes=[eoff.r])
        for i in range(3, NT):
            op(DVE, lambda e, i=i: e.tensor_tensor(out=eoff.ap[:, i, :], in0=eoff.ap[:, i - 1, :], in1=tot.ap[:, i - 1, :], op=ALU.add),
               reads=[eoff.r, tot.r], writes=[eoff.r])
        op(DVE, lambda e: e.tensor_tensor(out=posm.ap[:, 2:NT, :].rearrange("p a b -> p (a b)"), in0=ps_w.ap,
                                          in1=eoff.ap[:, 2:NT, :].rearrange("p a b -> p (a b)"), op=ALU.add), reads=[ps_w.r, eoff.r], writes=[posm.r])
        op(DVE, lambda e: e.tensor_tensor(out=posm.ap[:, 0:2, :].rearrange("p a b -> p (a b)"), in0=ps_c.ap[:, 0:2 * NE],
                                          in1=eoff.ap[:, 0:2, :].rearrange("p a b -> p (a b)"), op=ALU.add), reads=[ps_c.r, eoff.r], writes=[posm.r])
        op(DVE, lambda e: e.tensor_tensor(out=posm.ap, in0=posm.ap, in1=maskF.ap, op=ALU.mult), reads=[posm.r, maskF.r], writes=[posm.r])
        op(DVE, lambda e: e.tensor_scalar(out=posm.ap, in0=posm.ap, scalar1=-1.0, scalar2=None, op0=ALU.add), reads=[posm.r], writes=[posm.r])
        posT = self.tile([NE, TT], F32, "posT")
        for g in range(9):
            ps = self.psum()
            tl = list(range(g * 4, min(g * 4 + 4, NT)))
            for j, i in enumerate(tl):
                op(PE, lambda e, ps=ps, i=i, j=j: e.transpose(out=ps.ap[0:NE, j * 128:(j + 1) * 128], in_=posm.ap[:, i, :], identity=self.ident32.ap),
                   reads=[posm.r, self.ident32.r], writes=[ps.r])
            n = len(tl) * 128
            op(DVE, lambda e, ps=ps, g=g, n=n: e.tensor_scalar(out=posT.ap[:, g * 512:g * 512 + n], in0=ps.ap[0:NE, 0:n], scalar1=12582912.0, scalar2=12582912.0,
                                                               op0=ALU.add, op1=ALU.subtract), reads=[ps.r], writes=[posT.r])
        dma(SP, self.POST.ap, posT.ap, reads=[posT.r], writes=[self.POST.r])
        self.P.barrier()
        self.off = keep2

    def moe_passA(self, l):
        op, dma = self.op, self.dma
        h2tok, posm = self.h2tok, self.posm
        sel = self.tile([128, 32, 512], BF16, "sel", nsub=32)
        selc = self.tile([128, 2, 32], BF16, "selc")
        xsT = self.tile([128, 8, 544], BF16, "xsT")
        hact = self.tile([128, 16, 544], BF16, "hact")
        wgb = [self.tile([128, 8, 256], BF16, "wg") for _ in range(2)]
        wub = [self.tile([128, 8, 256], BF16, "wu") for _ in range(2)]
        wdb = [self.tile([128, 2, 512], BF16, "wd") for _ in range(3)]
        sa = [self.tile([128, 512], F32, "sa") for _ in range(2)]
        sac = self.tile([128, 32], F32, "sac")
        yes = [self.tile([128, D], BF16, "yes") for _ in range(4)]
        yec = self.tile([32, D], BF16, "yec")
        posbc = [self.tile([128, 1024], F32, "posbc") for _ in range(2)]
        affbc = [self.tile([128, 1024], F32, "affbc") for _ in range(2)]
        posc = self.tile([32, 256], F32, "posc")
        affc = self.tile([32, 256], F32, "affc")
        stg = [self.tile([128, 1024], BF16, "stg") for _ in range(2)]
        nq = 0
        stgc = self.tile([32, 256], BF16, "stgc")
        nw = [0, 0]
        ns = 0
        def build_selT(ex):
            nonlocal nq
            yield
            dma(SP, posc.ap, self.POST.ap[ex:ex + 1, 0:256].to_broadcast([32, 256]), reads=[self.POST.r], writes=[posc.r])
            dma(SP, affc.ap, self.AFFT.ap[ex:ex + 1, 0:256].to_broadcast([32, 256]), reads=[self.AFFT.r], writes=[affc.r])
            op(DVE, lambda e: e.scalar_tensor_tensor(out=stgc.ap, in0=posc.ap, scalar=self.iotacol.ap[0:32, 0:1], in1=affc.ap,
                                                      op0=ALU.is_equal, op1=ALU.mult), reads=[posc.r, affc.r, self.iotacol.r], writes=[stgc.r])
            dma(SP, self.SELTC.ap[ex], stgc.ap, reads=[stgc.r], writes=[self.SELTC.r])
            for qd in range(4):
                yield
                pb_, ab_ = posbc[nq % 2], affbc[nq % 2]
                nq += 1
                t0 = 256 + qd * 1024
                dma(SP, pb_.ap, self.POST.ap[ex:ex + 1, t0:t0 + 1024].to_broadcast([128, 1024]), reads=[self.POST.r], writes=[pb_.r])
                dma(SP, ab_.ap, self.AFFT.ap[ex:ex + 1, t0:t0 + 1024].to_broadcast([128, 1024]), reads=[self.AFFT.r], writes=[ab_.r])
                for c in range(4):
                    sg = stg[c % 2]
                    ecx = ex * 4 + c
                    op(DVE, lambda e, sg=sg, c=c, pb_=pb_, ab_=ab_: e.scalar_tensor_tensor(
                        out=sg.ap, in0=pb_.ap, scalar=self.iotacol.ap[:, c:c + 1], in1=ab_.ap, op0=ALU.is_equal, op1=ALU.mult),
                       reads=[pb_.r, ab_.r, self.iotacol.r], writes=[sg.r])
                    dma(SP, self.SELT.ap[qd * 8:(qd + 1) * 8, :, ecx, :].rearrange("i s t -> s i t"), sg.ap.rearrange("s (i t) -> s i t", t=128),
                        reads=[sg.r], writes=self.SELT.sub[qd * 8:(qd + 1) * 8])


        for ex in range(NE):
            for i in range(2, NT):
                eng = DVE if i % 2 == 0 else POOL
                op(DVE, lambda e, i=i, ex=ex: e.tensor_scalar(out=sel.ap[:, i - 2, :], in0=self.iota512.ap, scalar1=posm.ap[:, i, ex:ex + 1], scalar2=None,
                                                              op0=ALU.is_equal), reads=[self.iota512.r, posm.r], writes=[sel.sub[i - 2]])
            for i in range(2):
                op(DVE, lambda e, i=i, ex=ex: e.tensor_scalar(out=selc.ap[:, i, :], in0=self.iota512.ap[:, 0:32], scalar1=posm.ap[:, i, ex:ex + 1], scalar2=None,
                                                              op0=ALU.is_equal), reads=[self.iota512.r, posm.r], writes=[selc.r])
            for k in range(8):
                ps = self.psum()
                ks = slice(k * 128, (k + 1) * 128)
                for i in range(2, NT):
                    op(PE, lambda e, ps=ps, i=i, ks=ks: e.matmul(ps.ap, lhsT=h2tok.ap[:, i, ks], rhs=sel.ap[:, i - 2, :], start=(i == 2), stop=(i == NT - 1)),
                       reads=[h2tok.sub[i], sel.sub[i - 2]], writes=[ps.r])
                op(ACT, lambda e, ps=ps, k=k: e.copy(out=xsT.ap[:, k, 0:512], in_=ps.ap), reads=[ps.r], writes=[xsT.r])
                ps2 = self.psum()
                for i in range(2):
                    op(PE, lambda e, ps2=ps2, i=i, ks=ks: e.matmul(ps2.ap[:, 0:32], lhsT=h2tok.ap[:, i, ks], rhs=selc.ap[:, i, :], start=(i == 0), stop=(i == 1)),
                       reads=[h2tok.sub[i], selc.r], writes=[ps2.r])
                op(ACT, lambda e, ps2=ps2, k=k: e.copy(out=xsT.ap[:, k, 512:544], in_=ps2.ap[:, 0:32]), reads=[ps2.r], writes=[xsT.r])
            gen_ = build_selT(ex - 1) if ex > 0 else iter(())
            next(gen_, None)
            for q in range(8):
                next(gen_, None)
                wg, wu = wgb[nw[0] % 2], wub[nw[0] % 2]
                nw[0] += 1
                cs = slice(q * 256, (q + 1) * 256)
                if not (self.cfg.get('nowdma') and ex > 0):
                    dma(POOL, wg.ap, self.moe_w_gate[l, ex][:, cs].rearrange("(k p) n -> p k n", p=128), writes=[wg.r])
                    dma(POOL, wu.ap, self.moe_w_up[l, ex][:, cs].rearrange("(k p) n -> p k n", p=128), writes=[wu.r])
                for jl in range(2):
                    j = q * 2 + jl
                    js = slice(jl * 128, (jl + 1) * 128)
                    ps_a, ps_u, ps_c = self.psum(), self.psum(), self.psum()
                    for (w, ps) in ((wg, ps_a), (wu, ps_u)):
                        for k in range(8):
                            op(PE, lambda e, w=w, ps=ps, k=k, js=js: e.matmul(ps.ap, lhsT=w.ap[:, k, js], rhs=xsT.ap[:, k, 0:512], start=(k == 0), stop=(k == 7)),
                               reads=[w.r, xsT.r], writes=[ps.r])
                    for (w, c0) in ((wg, 0), (wu, 32)):
                        for k in range(8):
                            op(PE, lambda e, w=w, c0=c0, k=k, js=js, ps_c=ps_c: e.matmul(ps_c.ap[:, c0:c0 + 32], lhsT=w.ap[:, k, js], rhs=xsT.ap[:, k, 512:544],
                                                                                         start=(k == 0), stop=(k == 7)),
                               reads=[w.r, xsT.r], writes=[ps_c.r])
                    s = sa[ns % 2]
                    ns += 1
                    op(ACT, lambda e, s=s, ps_a=ps_a: e.activation(out=s.ap, in_=ps_a.ap, func=AF.Silu), reads=[ps_a.r], writes=[s.r])
                    op(DVE, lambda e, s=s, ps_u=ps_u, j=j: e.tensor_tensor(out=hact.ap[:, j, 0:512], in0=s.ap, in1=ps_u.ap, op=ALU.mult),
                       reads=[s.r, ps_u.r], writes=[hact.r])
                    op(ACT, lambda e, ps_c=ps_c: e.activation(out=sac.ap, in_=ps_c.ap[:, 0:32], func=AF.Silu), reads=[ps_c.r], writes=[sac.r])
                    op(DVE, lambda e, ps_c=ps_c, j=j: e.tensor_tensor(out=hact.ap[:, j, 512:544], in0=sac.ap, in1=ps_c.ap[:, 32:64], op=ALU.mult),
                       reads=[sac.r, ps_c.r], writes=[hact.r])
            for _ in gen_:
                pass
            for half in range(2):
                hs = slice(half * 512, (half + 1) * 512)
                psd = [self.psum() for _ in range(5)]
                for jj in range(8):
                    wd = wdb[nw[1] % 3]
                    nw[1] += 1
                    if not (self.cfg.get('nowdma') and ex > 0):
                        dma(POOL, wd.ap, self.moe_w_down[l, ex][jj * 256:(jj + 1) * 256, hs].rearrange("(a p) n -> p a n", p=128), writes=[wd.r])
                    for a in range(2):
                        j = jj * 2 + a
                        first, last = (j == 0), (j == 15)
                        for c in range(4):
                            op(PE, lambda e, c=c, j=j, a=a, wd=wd, first=first, last=last, p=psd[c]: e.matmul(p.ap, lhsT=hact.ap[:, j, c * 128:(c + 1) * 128], rhs=wd.ap[:, a, :],
                                                                                                                  start=first, stop=last),
                               reads=[hact.r, wd.r], writes=[psd[c].r])
                        op(PE, lambda e, j=j, a=a, wd=wd, first=first, last=last, p=psd[4]: e.matmul(p.ap[0:32, :], lhsT=hact.ap[:, j, 512:544], rhs=wd.ap[:, a, :],
                                                                                                      start=first, stop=last),
                           reads=[hact.r, wd.r], writes=[psd[4].r])
                for c in range(4):
                    op(ACT, lambda e, c=c, p=psd[c], hs=hs: e.copy(out=yes[c].ap[:, hs], in_=p.ap), reads=[psd[c].r], writes=[yes[c].r])
                op(ACT, lambda e, p=psd[4], hs=hs: e.copy(out=yec.ap[:, hs], in_=p.ap[0:32, :]), reads=[psd[4].r], writes=[yec.r])
            for c in range(4):
                dma(SP, self.YE.ap[ex, c], yes[c].ap, reads=[yes[c].r], writes=[self.YE.sub[ex]])
            dma(SP, self.YEC.ap[ex], yec.ap, reads=[yec.r], writes=[self.YEC.sub[ex]])
        for _ in build_selT(NE - 1):
            pass

    def moe_passB(self, l):
        op, dma = self.op, self.dma
        self.phase_begin()
        last = (l == DEPTH - 1)
        yeall = self.tile([128, 64, D], BF16, "yeall")
        for ex in range(NE):
            dma(SP, yeall.ap[:, ex * 4:(ex + 1) * 4, :], self.YE.ap[ex].rearrange("c s d -> s c d"), reads=[self.YE.sub[ex]], writes=[yeall.r])
        yecall = self.tile([128, 4, D], BF16, "yecall")
        dma(SP, yecall.ap, self.YEC.ap.rearrange("(g e) s d -> (e s) g d", e=4), reads=self.YEC.sub, writes=[yecall.r])
        seltc = self.tile([128, 4, 256], BF16, "seltc")
        dma(SP, seltc.ap, self.SELTC.ap.rearrange("(g e) s t -> (e s) g t", e=4), reads=[self.SELTC.r], writes=[seltc.r])
        mods = self.mod_tiles(l, [5])
        g1 = self.ln_vec(self.ln_g[l, 1:2, :])
        b1 = self.ln_vec(self.ln_b[l, 1:2, :])
        selt = [self.tile([128, 32, 128], BF16, "selt") for _ in range(3)]
        xb = [self.tile([128, D], F32, "xb") for _ in range(2)]
        yb = [self.tile([128, D], F32, "yb") for _ in range(2)]
        st = self.tile([128, 2, 6], F32, "st")
        mv = self.tile([128, 4], F32, "mv")
        nsl = 0
        for i in range(NT):
            c = 1 if i < 2 else 0
            rows = slice(i * 128, (i + 1) * 128)
            x, y = xb[i % 2], yb[i % 2]
            dma(SP, x.ap, self.XS.ap[rows, :], reads=[self.XS.sub[i]], writes=[x.r])
            m5 = mods[5][c]
            pss = [self.psum(), self.psum()]
            if c:
                for g in range(4):
                    for half in range(2):
                        hs = slice(half * 512, (half + 1) * 512)
                        op(PE, lambda e, ps=pss[half], g=g, hs=hs, i=i: e.matmul(ps.ap, lhsT=seltc.ap[:, g, i * 128:(i + 1) * 128], rhs=yecall.ap[:, g, hs],
                                                                                 start=(g == 0), stop=(g == 3)),
                           reads=[seltc.r, yecall.r], writes=[pss[half].r])
            else:
                for hh in range(2):
                    sl = selt[nsl % 3]
                    nsl += 1
                    dma(SP, sl.ap, self.SELT.ap[i - 2, :, hh * 32:(hh + 1) * 32, :], reads=[self.SELT.sub[i - 2]], writes=[sl.r])
                    for b in range(32):
                        ec = hh * 32 + b
                        for half in range(2):
                            hs = slice(half * 512, (half + 1) * 512)
                            op(PE, lambda e, ps=pss[half], sl=sl, b=b, ec=ec, hs=hs: e.matmul(ps.ap, lhsT=sl.ap[:, b, :], rhs=yeall.ap[:, ec, hs],
                                                                                              start=(ec == 0), stop=(ec == 63)),
                               reads=[sl.r, yeall.r], writes=[pss[half].r])
            for half in range(2):
                hs = slice(half * 512, (half + 1) * 512)
                op(DVE, lambda e, ps=pss[half], y=y, m5=m5, hs=hs: e.tensor_tensor(out=y.ap[:, hs], in0=ps.ap, in1=m5.ap[:, hs], op=ALU.mult),
                   reads=[pss[half].r, m5.r], writes=[y.r])
            op(DVE, lambda e, x=x, y=y: e.scalar_tensor_tensor(out=y.ap, in0=x.ap, scalar=ALPHA, in1=y.ap, op0=ALU.mult, op1=ALU.add),
               reads=[x.r, y.r], writes=[y.r])
            self.layernorm_(y, st, mv)
            op(POOL, lambda e, y=y, x=x: e.tensor_tensor(out=x.ap, in0=y.ap, in1=g1.ap, op=ALU.mult), reads=[y.r, g1.r], writes=[x.r])
            op(POOL, lambda e, x=x: e.tensor_tensor(out=x.ap, in0=x.ap, in1=b1.ap, op=ALU.add), reads=[x.r, b1.r], writes=[x.r])
            dma(POOL, self.XS.ap[rows, :], x.ap, reads=[x.r], writes=[self.XS.sub[i]])
            if last and not c:
                dma(POOL, self.out[(i - 2) * 128:(i - 1) * 128, :], x.ap, reads=[x.r], writes=[self.outr])
            if self.cfg.get("test"):
                dma(POOL, self.dbg.ap[rows, :], x.ap, reads=[x.r], writes=[self.dbg.sub[i]])

    def finish(self):
        rs = [self.outr]
        if self.cfg.get("test"):
            rs = rs + [self.dbg.r] + self.dbg.sub
        self.P.barrier()
        self.op(SP, lambda e: e.nop(), reads=rs)
        self.P.emit()


def Tl_view(t):
    return t


def build(nc, cfg):
    k = K(nc, cfg)
    k.declare_io()
    k.setup()
    tk = cfg.get("test")
    if tk in ("mix", "gdn1", "gdn2"):
        l = cfg["layer"]
        k.premix(l)
        if l % 2 == 0:
            k.natten(l)
        else:
            k.gdn(l)
    if tk == "post":
        l = cfg["layer"]
        k.postmix(l)
        k.moe_select(l)
        k.moe_passA(l)
        k.moe_passB(l)
    if not tk:
        for l in range(cfg.get("depth", DEPTH)):
            k.premix(l)
            if l % 2 == 0:
                k.natten(l)
            else:
                k.gdn(l)
            k.postmix(l)
            k.moe_select(l)
            k.moe_passA(l)
            k.moe_passB(l)
    k.finish()
    return k


WNAMES = ["c_ctx", "ada_w", "ada_b", "ln_g", "ln_b", "na_w_qkv", "na_w_o", "na_rpb", "gdn_w_in", "gdn_conv_w", "gdn_a_log",
          "gdn_dt_bias", "gdn_norm_w", "gdn_w_o", "moe_w_router", "moe_w_gate", "moe_w_up", "moe_w_down"]


def make_in_maps(inputs, cores):
    maps = []
    shared = {}
    for n in WNAMES:
        a = np.ascontiguousarray(inputs[n], dtype=np.float32)
        if n == "c_ctx":
            a = a.reshape(1, D)
        shared[n] = a
    rp = np.zeros((2, 16, 15, 128), np.float32)
    rp[..., 48:79] = np.asarray(inputs["na_rpb"], np.float32)[..., ::-1]
    shared["rpb_pad"] = rp
    qc = np.arange(64)
    cs = np.clip(qc - 8, 0, 48)
    kc = np.arange(64)[:, None]
    cmv = np.where((kc >= cs[None, :]) & (kc < cs[None, :] + 16), 0.0, -1e30).astype(np.float32)
    shared["cmask"] = np.tile(cmv, (2, 2))
    pp = np.arange(128)
    bd = lambda b: (pp[:, None] // b == pp[None, :] // b).astype(np.float32)
    shared["gmask"] = np.stack([bd(16), bd(32) - bd(16), bd(64) - bd(32), 1.0 - bd(64)]).astype(np.float32)
    for b in cores:
        m = dict(shared)
        m["x"] = np.ascontiguousarray(inputs["x"][b], dtype=np.float32)
        m["c"] = np.ascontiguousarray(inputs["c"][b:b + 1], dtype=np.float32)
        m["ctx"] = np.ascontiguousarray(inputs["ctx"][b], dtype=np.float32)
        maps.append(m)
    return maps


def kernel(**inputs):
    nc = bass.Bass("TRN2", target_bir_lowering=False)
    build(nc, {})
    maps = make_in_maps(inputs, list(range(8)))
    res = run_bass_kernel_spmd(nc, maps, core_ids=list(range(8)))
    return np.stack([np.asarray(r["out"], dtype=np.float32) for r in res.results], axis=0)
```

```python
import contextlib
import numpy as np
import concourse.bass as bass
import concourse.mybir as mybir
from concourse.bass_utils import run_bass_kernel_spmd

F32 = mybir.dt.float32
BF16 = mybir.dt.bfloat16
I32 = mybir.dt.int32
U8 = mybir.dt.uint8
AF = mybir.ActivationFunctionType
ALU = mybir.AluOpType
AX = mybir.AxisListType

PE, ACT, DVE, POOL, SP = "tensor", "scalar", "vector", "gpsimd", "sync"
ENGS = [PE, ACT, DVE, POOL, SP]
N_DMA_SEMS = 20
SEM_EPOCH = 20000

D = 1024
NT = 34
TT = NT * 128
NE = 16
DEPTH = 4
ALPHA = (2.0 * DEPTH) ** 0.25
LN_EPS = 1e-6
DSZ = {F32: 4, BF16: 2, I32: 4, U8: 1}


class Res:
    __slots__ = ("name", "last_w", "readers", "excl")

    def __init__(self, name=""):
        self.name = name
        self.last_w = None
        self.readers = []
        self.excl = False


class GRes:
    def __init__(self, name=""):
        self.subs = [Res(name + "a"), Res(name + "b")]


def _grp(fn):
    d = fn.__defaults__
    if not d:
        return None
    c = fn.__code__
    names = c.co_varnames[:c.co_argcount]
    m = dict(zip(names[len(names) - len(d):], d))
    if isinstance(m.get("j"), int):
        return m["j"]
    if isinstance(m.get("h"), int):
        return m["h"] // 4
    return None


def _expand(rs, g):
    out = []
    for r in rs:
        if isinstance(r, GRes):
            out.extend(r.subs if g is None else [r.subs[g]])
        else:
            out.append(r)
    return out


class Op:
    __slots__ = ("eng", "fn", "deps", "is_dma", "sem", "val", "sig")

    def __init__(self, eng, fn, is_dma):
        self.eng = eng
        self.fn = fn
        self.deps = ()
        self.is_dma = is_dma
        self.sem = None
        self.val = None
        self.sig = False


class Prog:
    def __init__(self, nc):
        self.nc = nc
        self.q = {e: [] for e in ENGS}
        self.dmas_since_barrier = []
        self.nops = 0

    def _add(self, eng, fn, reads, writes, is_dma):
        if any(isinstance(r, GRes) for r in reads) or any(isinstance(r, GRes) for r in writes):
            g = _grp(fn)
            reads, writes = _expand(reads, g), _expand(writes, g)
        op = Op(eng, fn, is_dma)
        deps = set()
        for r in reads:
            if r.last_w is not None:
                deps.add(r.last_w)
            if r.excl:
                deps.update(x for x in r.readers if x.eng != eng)
        for w in writes:
            if w.last_w is not None:
                deps.add(w.last_w)
            deps.update(w.readers)
        op.deps = tuple(deps)
        for r in reads:
            r.readers.append(op)
        for w in writes:
            w.last_w = op
            w.readers = []
        self.q[eng].append(op)
        self.nops += 1
        if is_dma:
            self.dmas_since_barrier.append(op)
        return op

    def op(self, eng, fn, reads=(), writes=()):
        return self._add(eng, fn, reads, writes, False)

    def dma(self, eng, out, in_, reads=(), writes=(), **kw):
        return self._add(eng, lambda e: e.dma_start(out=out, in_=in_, **kw), reads, writes, True)

    def barrier(self):
        b = Op(SP, lambda e: e.nop(), False)
        deps = set(self.dmas_since_barrier)
        for e in ENGS:
            if self.q[e]:
                deps.add(self.q[e][-1])
        b.deps = tuple(deps)
        self.q[SP].append(b)
        self.dmas_since_barrier = []
        for e in ENGS:
            if e == SP:
                continue
            o = Op(e, lambda en: en.nop(), False)
            o.deps = (b,)
            self.q[e].append(o)

    def emit(self):
        nc = self.nc
        for e in ENGS:
            for op in self.q[e]:
                for d in op.deps:
                    if d.is_dma or d.eng != op.eng or op.is_dma or d.eng != PE:
                        d.sig = True
        with contextlib.ExitStack() as st:
            nsig = {e: sum(1 for o in self.q[e] if o.sig and not o.is_dma) for e in ENGS}
            csem = {e: [st.enter_context(nc.semaphore("cs_%s_%d" % (e, i)))
                        for i in range(nsig[e] // SEM_EPOCH + 1)] for e in ENGS}
            dsem = {e: [st.enter_context(nc.semaphore("ds_%s_%d" % (e, i))) for i in range(N_DMA_SEMS)]
                    for e in (SP, ACT, POOL)}
            for e in ENGS:
                cnt = 0
                dcnt = 0
                for op in self.q[e]:
                    if op.is_dma:
                        op.sem = dsem[e][dcnt % N_DMA_SEMS]
                        op.val = 16 * (dcnt // N_DMA_SEMS + 1)
                        op.sig = True
                        dcnt += 1
                    elif op.sig:
                        op.sem = csem[e][cnt // SEM_EPOCH]
                        op.val = cnt % SEM_EPOCH + 1
                        cnt += 1
            block = st.enter_context(nc.Block())

            def gen(e):
                def body(eng):
                    waited = {}
                    for op in self.q[e]:
                        needs = {}
                        for d in op.deps:
                            if not d.sig:
                                continue
                            if (not d.is_dma) and d.eng == e and e == PE and not op.is_dma:
                                continue
                            k = id(d.sem)
                            if needs.get(k, (None, 0))[1] < d.val:
                                needs[k] = (d.sem, d.val)
                        if op.is_dma and op.val > 16:
                            k = id(op.sem)
                            if needs.get(k, (None, 0))[1] < op.val - 16:
                                needs[k] = (op.sem, op.val - 16)
                        for k, (s, v) in needs.items():
                            if waited.get(k, 0) >= v:
                                continue
                            eng.wait_ge(s, v)
                            waited[k] = v
                        ins = op.fn(eng)
                        if op.sig:
                            ins.then_inc(op.sem, 16 if op.is_dma else 1)
                return body

            for e in ENGS:
                if self.q[e]:
                    getattr(block, e)(gen(e))


class Tl:
    def __init__(self, ap, name="", nsub=0):
        self.ap = ap
        self.r = Res(name)
        self.sub = [Res(name + str(i)) for i in range(nsub)]


ARENA = 206000


class K:
    def __init__(self, nc, cfg):
        self.nc = nc
        self.cfg = cfg
        self.P = Prog(nc)
        global ARENA
        ARENA = (int(nc.sbuf_bytes_remaining) - 256) // 64 * 64
        self.big = nc.alloc_sbuf_tensor("arena", [128, ARENA], U8)
        self.off = 0
        self.ps = []
        for i in range(8):
            t = nc.alloc_psum_tensor("psb%d" % i, [128, 512], F32)
            self.ps.append(Tl(t[:], "ps%d" % i, nsub=4))
            for r_ in [self.ps[-1].r] + self.ps[-1].sub:
                r_.excl = True
        self.psn = 0
        self.uid = 0

    def tile(self, shape, dt, name="t", nsub=0):
        n = int(np.prod(shape[1:])) * DSZ[dt]
        if self.off + n > ARENA:
            raise RuntimeError("SBUF arena overflow at %s: %d + %d" % (name, self.off, n))
        a = self.big[0:shape[0], self.off:self.off + n].bitcast(dt)
        self.off += (n + 63) // 64 * 64
        if len(shape) == 3:
            a = a.rearrange("p (a b) -> p a b", a=shape[1])
        elif len(shape) == 4:
            a = a.rearrange("p (a b c) -> p a b c", a=shape[1], b=shape[2])
        self.uid += 1
        return Tl(a, "%s_%d" % (name, self.uid), nsub)

    def psum(self):
        t = self.ps[self.psn % 8]
        self.psn += 1
        return t

    def dram(self, name, shape, dt, kind="Internal"):
        return self.nc.dram_tensor(name, list(shape), dt, kind=kind).ap()

    def op(self, eng, fn, reads=(), writes=()):
        return self.P.op(eng, fn, reads, writes)

    def dma(self, eng, out, in_, reads=(), writes=(), **kw):
        return self.P.dma(eng, out, in_, reads, writes, **kw)

    def declare_io(self):
        nc, cfg = self.nc, self.cfg
        ein = lambda n, s: self.dram(n, s, F32, kind="ExternalInput")
        self.x_in = ein("x", [4096, D])
        self.c_in = ein("c", [1, D])
        self.ctx_in = ein("ctx", [256, D])
        self.cc_in = ein("c_ctx", [1, D])
        self.ada_w = ein("ada_w", [DEPTH, D, 6 * D])
        self.ada_b = ein("ada_b", [DEPTH, 6 * D])
        self.ln_g = ein("ln_g", [DEPTH, 2, D])
        self.ln_b = ein("ln_b", [DEPTH, 2, D])
        self.na_w_qkv = ein("na_w_qkv", [2, D, 3 * D])
        self.na_w_o = ein("na_w_o", [2, D, D])
        self.na_rpb = ein("na_rpb", [2, 16, 15, 31])
        self.gdn_w_in = ein("gdn_w_in", [2, D, 4 * D + 32])
        self.gdn_conv_w = ein("gdn_conv_w", [2, 5, 3 * D])
        self.gdn_a_log = ein("gdn_a_log", [2, 2, 8])
        self.gdn_dt_bias = ein("gdn_dt_bias", [2, 2, 8])
        self.gdn_norm_w = ein("gdn_norm_w", [2, 128])
        self.gdn_w_o = ein("gdn_w_o", [2, D, D])
        if cfg.get("test") not in ("mix", "gdn1", "gdn2"):
            self.moe_w_router = ein("moe_w_router", [DEPTH, D, NE])
            self.moe_w_gate = ein("moe_w_gate", [DEPTH, NE, D, 2048])
            self.moe_w_up = ein("moe_w_up", [DEPTH, NE, D, 2048])
            self.moe_w_down = ein("moe_w_down", [DEPTH, NE, 2048, D])
        if cfg.get("test") in ("gdn1", "gdn2"):
            self.dbg2 = self.dram("dbg2", [4, D, TT], F32, kind="ExternalOutput")
            self.dbg3 = self.dram("dbg3", [128, 2, NT, 16], F32, kind="ExternalOutput")
        self.rpb_pad = ein("rpb_pad", [2, 16, 15, 128])
        self.cmask_in = ein("cmask", [128, 128])
        self.gmask_in = ein("gmask", [4, 128, 128])
        self.out = self.dram("out", [4096, D], F32, kind="ExternalOutput")
        tk = cfg.get("test")
        self.XS = Tl(self.dram("XS", [TT, D], F32), "XS", NT)
        self.Y = Tl(self.dram("Y", [TT, D], F32), "Y", NT)
        if tk:
            self.xs_in = self.dram("xs_in", [TT, D], F32, kind="ExternalInput")
            self.y_in = self.dram("y_in", [TT, D], F32, kind="ExternalInput")
        if tk:
            self.dbg = Tl(self.dram("dbg", [TT, D], F32, kind="ExternalOutput"), "dbg", NT)
        self.AFFT = Tl(self.dram("AFFT", [NE, TT], F32), "AFFT")
        self.POST = Tl(self.dram("POST", [NE, TT], F32), "POST")
        self.YE = Tl(self.dram("YE", [NE, 4, 128, D], BF16), "YE", NE)
        self.YEC = Tl(self.dram("YEC", [NE, 32, D], BF16), "YEC", NE)
        self.SELT = Tl(self.dram("SELT", [32, 128, 64, 128], BF16), "SELT", 32)
        self.SELTC = Tl(self.dram("SELTC", [NE, 32, 256], BF16), "SELTC")
        self.outr = Res("out")

    def setup(self):
        op, dma = self.op, self.dma
        self.ident = self.tile([128, 128], BF16, "ident")
        self.ident32 = self.tile([128, 128], F32, "ident32")
        self.ones = self.tile([128, 128], BF16, "ones")
        self.triu = self.tile([128, 128], BF16, "triu")
        tmp = self.tile([128, 128], F32, "tmp")
        self.iota512 = self.tile([128, 512], F32, "iota512")
        self.iotacol = self.tile([128, 4], F32, "iotacol")
        i32, idb, on, tu, io, ic = self.ident32, self.ident, self.ones, self.triu, self.iota512, self.iotacol
        op(POOL, lambda e: e.memset(i32.ap, 1.0), writes=[i32.r])
        op(POOL, lambda e: e.affine_select(out=i32.ap, in_=i32.ap, pattern=[[-1, 128]], compare_op=ALU.is_equal,
                                           fill=0.0, base=0, channel_multiplier=1), reads=[i32.r], writes=[i32.r])
        op(DVE, lambda e: e.tensor_copy(out=idb.ap, in_=i32.ap), reads=[i32.r], writes=[idb.r])
        op(DVE, lambda e: e.memset(on.ap, 1.0), writes=[on.r])
        op(POOL, lambda e: e.memset(tmp.ap, 1.0), writes=[tmp.r])
        op(POOL, lambda e: e.affine_select(out=tmp.ap, in_=tmp.ap, pattern=[[1, 128]], compare_op=ALU.is_ge,
                                           fill=0.0, base=0, channel_multiplier=-1), reads=[tmp.r], writes=[tmp.r])
        op(DVE, lambda e: e.tensor_copy(out=tu.ap, in_=tmp.ap), reads=[tmp.r], writes=[tu.r])
        op(POOL, lambda e: e.iota(io.ap, pattern=[[1, 512]], base=0, channel_multiplier=0,
                                  allow_small_or_imprecise_dtypes=True), writes=[io.r])
        op(POOL, lambda e: e.iota(ic.ap, pattern=[[128, 4]], base=0, channel_multiplier=1,
                                  allow_small_or_imprecise_dtypes=True), writes=[ic.r])
        self.scb = self.tile([128, 8, 128], F32, "scb")
        self.sccb = self.tile([128, 8, 128], F32, "sccb")
        for src, dst in ((self.c_in, self.scb), (self.cc_in, self.sccb)):
            cv = self.tile([128, 8], F32, "cv")
            dma(SP, cv.ap, src.rearrange("o (k p) -> p (o k)", p=128), writes=[cv.r], allow_slow_non_contiguous=True)
            op(ACT, lambda e, cv=cv: e.activation(out=cv.ap, in_=cv.ap, func=AF.Silu), reads=[cv.r], writes=[cv.r])
            op(DVE, lambda e, cv=cv, dst=dst: e.tensor_copy(out=dst.ap, in_=cv.ap.unsqueeze(2).to_broadcast([128, 8, 128])),
               reads=[cv.r], writes=[dst.r])
        self.base_off = self.off
        if not self.cfg.get("test"):
            dma(SP, self.XS.ap[0:256, :], self.ctx_in, writes=self.XS.sub[0:2])
            dma(SP, self.XS.ap[256:TT, :], self.x_in, writes=self.XS.sub[2:NT])
        else:
            dma(SP, self.XS.ap, self.xs_in, writes=self.XS.sub)
            dma(SP, self.Y.ap, self.y_in, writes=self.Y.sub)

    def phase_begin(self):
        self.P.barrier()
        self.off = self.base_off

    def mod_tiles(self, l, idxs, plus1=()):
        op, dma = self.op, self.dma
        res = {}
        for idx in idxs:
            res[idx] = (self.tile([128, D], F32, "modl"), self.tile([128, D], F32, "modc"))
        mark = self.off
        wbuf = [self.tile([128, 8, 512], F32, "adaw") for _ in range(2)]
        bbuf = [self.tile([128, 1024], F32, "adab") for _ in range(2)]
        n = 0
        for ii, idx in enumerate(idxs):
            lat, ctx = res[idx]
            bb = bbuf[ii % 2]
            dma(SP, bb.ap, self.ada_b[l:l + 1, idx * D:(idx + 1) * D].to_broadcast([128, D]), writes=[bb.r])
            for half in range(2):
                wb = wbuf[n % 2]
                n += 1
                c0 = idx * D + half * 512
                dma(SP, wb.ap, self.ada_w[l, :, c0:c0 + 512].rearrange("(k p) n -> p k n", p=128), writes=[wb.r])
                for lhs, dst in ((self.scb, lat), (self.sccb, ctx)):
                    ps = self.psum()
                    for k in range(8):
                        op(PE, lambda e, ps=ps, lhs=lhs, wb=wb, k=k: e.matmul(ps.ap, lhsT=lhs.ap[:, k, :], rhs=wb.ap[:, k, :],
                                                                              start=(k == 0), stop=(k == 7)),
                           reads=[lhs.r, wb.r], writes=[ps.r])
                    sl = slice(half * 512, (half + 1) * 512)
                    op(DVE, lambda e, ps=ps, dst=dst, bb=bb, sl=sl: e.tensor_tensor(out=dst.ap[:, sl], in0=ps.ap, in1=bb.ap[:, sl], op=ALU.add),
                       reads=[ps.r, bb.r], writes=[dst.r])
            if idx in plus1:
                for t in (lat, ctx):
                    op(DVE, lambda e, t=t: e.tensor_scalar(out=t.ap, in0=t.ap, scalar1=1.0, scalar2=None, op0=ALU.add),
                       reads=[t.r], writes=[t.r])
        self.P.barrier()
        self.off = mark
        return res

    def ln_vec(self, src_row):
        t = self.tile([128, D], F32, "lnv")
        self.dma(SP, t.ap, src_row.to_broadcast([128, D]), writes=[t.r])
        return t

    def layernorm_(self, r, st, mv):
        op = self.op
        for h in range(2):
            op(DVE, lambda e, h=h: e.bn_stats(out=st.ap[:, h, :], in_=r.ap[:, h * 512:(h + 1) * 512]), reads=[r.r], writes=[st.r])
        op(DVE, lambda e: e.bn_aggr(out=mv.ap[:, 0:2], in_=st.ap.rearrange("p a b -> p (a b)")), reads=[st.r], writes=[mv.r])
        op(DVE, lambda e: e.tensor_scalar(out=mv.ap[:, 2:3], in0=mv.ap[:, 1:2], scalar1=LN_EPS, scalar2=None, op0=ALU.add),
           reads=[mv.r], writes=[mv.r])
        op(ACT, lambda e: e.activation(out=mv.ap[:, 2:3], in_=mv.ap[:, 2:3], func=AF.Ln), reads=[mv.r], writes=[mv.r])
        op(ACT, lambda e: e.activation(out=mv.ap[:, 2:3], in_=mv.ap[:, 2:3], func=AF.Exp, scale=-0.5), reads=[mv.r], writes=[mv.r])
        op(DVE, lambda e: e.tensor_scalar(out=r.ap, in0=r.ap, scalar1=mv.ap[:, 0:1], scalar2=mv.ap[:, 2:3],
                                          op0=ALU.subtract, op1=ALU.mult), reads=[r.r, mv.r], writes=[r.r])

    def premix(self, l):
        op, dma = self.op, self.dma
        self.phase_begin()
        hT = self.tile([128, 8, TT], BF16, "hT", nsub=NT)
        self.hT = hT
        self.mix_keep = self.off
        mods = self.mod_tiles(l, [0, 1], plus1=(1,))
        xb = [self.tile([128, D], F32, "xb") for _ in range(3)]
        hb = [self.tile([128, D], BF16, "hb") for _ in range(2)]
        for i in range(NT):
            x = xb[i % 3]
            h = hb[i % 2]
            c = 1 if i < 2 else 0
            dma(SP, x.ap, self.XS.ap[i * 128:(i + 1) * 128, :], reads=[self.XS.sub[i]], writes=[x.r])
            sc, sh = mods[1][c], mods[0][c]
            op(DVE, lambda e, x=x, sc=sc: e.tensor_tensor(out=x.ap, in0=x.ap, in1=sc.ap, op=ALU.mult), reads=[x.r, sc.r], writes=[x.r])
            op(POOL, lambda e, x=x, sh=sh, h=h: e.tensor_tensor(out=h.ap, in0=x.ap, in1=sh.ap, op=ALU.add), reads=[x.r, sh.r], writes=[h.r])
            self.transpose_to(h, hT, i)

    def transpose_to(self, h, hT, i, evac=ACT):
        op = self.op
        ps = self.psum()
        pb = ps.ap.bitcast(BF16)
        for k in range(8):
            op(PE, lambda e, k=k, pb=pb, h=h: e.transpose(out=pb[:, k * 128:(k + 1) * 128], in_=h.ap[:, k * 128:(k + 1) * 128],
                                                          identity=self.ident.ap), reads=[h.r, self.ident.r], writes=[ps.r])
        dst = hT.ap[:, :, i * 128:(i + 1) * 128]
        if evac == ACT:
            op(ACT, lambda e, pb=pb, dst=dst: e.copy(out=dst, in_=pb.rearrange("p (k n) -> p k n", k=8)), reads=[ps.r], writes=[hT.sub[i]])
        else:
            op(evac, lambda e, pb=pb, dst=dst: e.tensor_copy(out=dst, in_=pb.rearrange("p (k n) -> p k n", k=8)), reads=[ps.r], writes=[hT.sub[i]])

    def natten(self, l):
        op, dma = self.op, self.dma
        li = l // 2
        hT = self.hT
        self.P.barrier()
        self.off = self.mix_keep
        if not hasattr(self, "QT"):
            self.QT = Tl(self.dram("QT", [D, TT], BF16), "QT")
            self.KT = Tl(self.dram("KT", [D, TT], BF16), "KT")
            self.VX = Tl(self.dram("VX", [TT, 16, 65], BF16), "VX")
            self.OD = Tl(self.dram("OD", [TT, D], BF16), "OD", NT)
        QT, KT, VX, OD = self.QT, self.KT, self.VX, self.OD
        wq = self.na_w_qkv[li]
        wb = [self.tile([128, 8, 512], BF16, "wqk") for _ in range(2)]
        stq = [self.tile([128, 512], BF16, "stq") for _ in range(3)]
        blocks = [(0, 256)] + [(256 + 512 * b, 512) for b in range(8)]
        n = 0
        for cg in range(4):
            w = wb[cg % 2]
            dma(POOL, w.ap, wq[:, cg * 512:(cg + 1) * 512].rearrange("(k p) n -> p k n", p=128), writes=[w.r])
            for cl in range(4):
                ct = cg * 4 + cl
                dst = QT if ct < 8 else KT
                crow = (ct % 8) * 128
                for (t0, nt_) in blocks:
                    ps = self.psum()
                    for k in range(8):
                        op(PE, lambda e, ps=ps, w=w, k=k, cl=cl, t0=t0, nt_=nt_: e.matmul(ps.ap[:, 0:nt_], lhsT=w.ap[:, k, cl * 128:(cl + 1) * 128], rhs=hT.ap[:, k, t0:t0 + nt_],
                                                                                    start=(k == 0), stop=(k == 7)),
                           reads=[w.r] + hT.sub[t0 // 128:(t0 + nt_) // 128], writes=[ps.r])
                    sq = stq[n % 3]
                    n += 1
                    if ct < 8:
                        op(ACT, lambda e, ps=ps, sq=sq, nt_=nt_: e.mul(out=sq.ap[:, 0:nt_], in_=ps.ap[:, 0:nt_], mul=0.125), reads=[ps.r], writes=[sq.r])
                    else:
                        op(DVE, lambda e, ps=ps, sq=sq, nt_=nt_: e.tensor_copy(out=sq.ap[:, 0:nt_], in_=ps.ap[:, 0:nt_]), reads=[ps.r], writes=[sq.r])
                    dma(SP, dst.ap[crow:crow + 128, t0:t0 + nt_], sq.ap[:, 0:nt_], reads=[sq.r], writes=[dst.r])
        wv = self.tile([128, 8, D], BF16, "wv")
        dma(POOL, wv.ap, wq[:, 2 * D:3 * D].rearrange("(k p) n -> p k n", p=128), writes=[wv.r])
        vst = [self.tile([128, 16, 65], BF16, "vst") for _ in range(2)]
        for v in vst:
            op(DVE, lambda e, v=v: e.memset(v.ap, 1.0), writes=[v.r])
        for i in range(NT):
            v = vst[i % 2]
            for half in range(2):
                ps = self.psum()
                for k in range(8):
                    op(PE, lambda e, ps=ps, k=k, i=i, half=half: e.matmul(ps.ap, lhsT=hT.ap[:, k, i * 128:(i + 1) * 128], rhs=wv.ap[:, k, half * 512:(half + 1) * 512],
                                                                       start=(k == 0), stop=(k == 7)),
                       reads=[wv.r, hT.sub[i]], writes=[ps.r])
                op(ACT, lambda e, ps=ps, v=v, half=half: e.copy(out=v.ap[:, half * 8:(half + 1) * 8, 0:64], in_=ps.ap.rearrange("p (h d) -> p h d", d=64)),
                   reads=[ps.r], writes=[v.r])
            dma(SP, VX.ap[i * 128:(i + 1) * 128], v.ap, reads=[v.r], writes=[VX.r])
        self.P.barrier()
        self.off = self.mix_keep - 0
        self.off = self.base_off
        rs_ = np.clip(np.arange(64) - 4, 0, 56)
        tiles, tindex, per_rp = [], {}, []
        for rp in range(32):
            r0 = 2 * rp
            lst = []
            for kp in range(rs_[r0] // 2, (rs_[r0 + 1] + 7) // 2 + 1):
                spec = []
                for i2 in range(2):
                    for j2 in range(2):
                        kr, r = 2 * kp + i2, r0 + j2
                        spec.append(int(kr - r + 7) if rs_[r] <= kr < rs_[r] + 8 else None)
                spec = tuple(spec)
                key = (rp if rp in (0, 1, 30, 31) else -1, spec)
                if key not in tindex:
                    tindex[key] = len(tiles)
                    tiles.append(spec)
                lst.append((kp, tindex[key]))
            assert all(lst[j + 1][1] == lst[j][1] + 1 for j in range(len(lst) - 1))
            per_rp.append(lst)
        NTL = len(tiles)
        jm = self.tile([64, 128], F32, "jm")
        op(POOL, lambda e: e.memset(jm.ap, 1.0), writes=[jm.r])
        op(POOL, lambda e: e.affine_select(out=jm.ap[:, 0:64], in_=jm.ap[:, 0:64], pattern=[[1, 64]], compare_op=ALU.is_equal, fill=0.0, base=-63, channel_multiplier=1),
           reads=[jm.r], writes=[jm.r])
        op(POOL, lambda e: e.affine_select(out=jm.ap[:, 64:128], in_=jm.ap[:, 64:128], pattern=[[1, 64]], compare_op=ALU.is_equal, fill=0.0, base=-63, channel_multiplier=1),
           reads=[jm.r], writes=[jm.r])
        cm = self.tile([128, 128], F32, "cmask")
        dma(SP, cm.ap, self.cmask_in, writes=[cm.r])
        hk = self.tile([64, 15, 64], F32, "hk")
        toe = self.tile([128, 15, 64], F32, "toe")
        bias = self.tile([128, NTL, 128], F32, "bias")
        qt = [self.tile([64, TT], BF16, "qt") for _ in range(2)]
        kt = [self.tile([64, TT], BF16, "kt") for _ in range(2)]
        vh = [self.tile([128, NT, 65], BF16, "vh") for _ in range(2)]
        sb = [self.tile([128, 640], F32, "sb") for _ in range(3)]
        pb = [self.tile([128, 896], BF16, "pb") for _ in range(3)]
        ost = [self.tile([128, NT, 64], BF16, "ost") for _ in range(2)]
        rc = self.tile([128, 4], F32, "rc")
        rpb = self.rpb_pad[li]
        it = 0
        pending = []
        for h in range(16):
            q_, k_, v_, o_ = qt[h % 2], kt[h % 2], vh[h % 2], ost[h % 2]
            dma(SP, q_.ap, QT.ap[h * 64:(h + 1) * 64, :], reads=[QT.r], writes=[q_.r])
            dma(SP, k_.ap, KT.ap[h * 64:(h + 1) * 64, :], reads=[KT.r], writes=[k_.r])
            dma(SP, v_.ap, VX.ap.rearrange("(i p) h d -> p i h d", p=128)[:, :, h, :], reads=[VX.r], writes=[v_.r])
            src = bass.AP(tensor=rpb.tensor, offset=rpb[h].offset, ap=[[1, 64], [128, 15], [1, 64]])
            dma(SP, hk.ap, src, writes=[hk.r])
            for (d0, d1) in ((0, 8), (8, 15)):
                ps = self.psum()
                nn = (d1 - d0) * 64
                op(PE, lambda e, ps=ps, d0=d0, d1=d1, nn=nn: e.matmul(ps.ap[:, 0:nn], lhsT=jm.ap, rhs=hk.ap[:, d0:d1, :].rearrange("p a b -> p (a b)"), start=True, stop=True),
                   reads=[jm.r, hk.r], writes=[ps.r])
                op(ACT, lambda e, ps=ps, d0=d0, d1=d1, nn=nn: e.copy(out=toe.ap[:, d0:d1, :].rearrange("p a b -> p (a b)"), in_=ps.ap[:, 0:nn]), reads=[ps.r], writes=[toe.r])
            for t, spec in enumerate(tiles):
                for bi, dr in enumerate(spec):
                    i2, j2 = bi // 2, bi % 2
                    prt = slice(i2 * 64, (i2 + 1) * 64)
                    fr = slice(j2 * 64, (j2 + 1) * 64)
                    if dr is None:
                        op(POOL, lambda e, t=t, prt=prt, fr=fr: e.memset(bias.ap[prt, t, fr], -1e30), writes=[bias.r])
                    else:
                        op(POOL, lambda e, t=t, prt=prt, fr=fr, dr=dr: e.tensor_tensor(out=bias.ap[prt, t, fr], in0=toe.ap[prt, dr, :], in1=cm.ap[prt, fr], op=ALU.add),
                           reads=[toe.r, cm.r], writes=[bias.r])
            for qi in range(NT):
                if qi < 2:
                    wins = []
                    ctxk = [0, 1]
                else:
                    wins = per_rp[qi - 2]
                    ctxk = [0, 1]
                s_, p_ = sb[it % 3], pb[it % 3]
                it += 1
                nw = len(wins)
                qs = slice(qi * 128, (qi + 1) * 128)
                psA, psB = self.psum(), self.psum()
                slots = []
                for j in range(nw):
                    slots.append((psA, j * 128) if j < 4 else (psB, 0))
                cb = 128 if nw == 5 else 0
                for j, kk in enumerate(ctxk):
                    slots.append((psB, cb + j * 128))
                ktl = [2 + kp for (kp, _) in wins] + ctxk
                for (pst, co), kti in zip(slots, ktl):
                    op(PE, lambda e, pst=pst, co=co, kti=kti, qs=qs, k_=k_, q_=q_: e.matmul(pst.ap[:, co:co + 128], lhsT=k_.ap[:, kti * 128:(kti + 1) * 128], rhs=q_.ap[:, qs],
                                                                                         start=True, stop=True),
                       reads=[k_.r, q_.r], writes=[pst.r])
                if nw:
                    t0 = wins[0][1]
                    na = min(nw, 4)
                    op(DVE, lambda e, s_=s_, t0=t0, na=na, psA=psA: e.tensor_tensor(out=s_.ap[:, 0:na * 128], in0=psA.ap[:, 0:na * 128],
                                                                                   in1=bias.ap[:, t0:t0 + na, :].rearrange("p a b -> p (a b)"), op=ALU.add),
                       reads=[psA.r, bias.r], writes=[s_.r])
                    if nw == 5:
                        op(DVE, lambda e, s_=s_, t0=t0, psB=psB: e.tensor_tensor(out=s_.ap[:, 512:640], in0=psB.ap[:, 0:128], in1=bias.ap[:, t0 + 4, :], op=ALU.add),
                           reads=[psB.r, bias.r], writes=[s_.r])
                    op(ACT, lambda e, s_=s_, p_=p_, nw=nw: e.activation(out=p_.ap[:, 0:nw * 128], in_=s_.ap[:, 0:nw * 128], func=AF.Exp), reads=[s_.r], writes=[p_.r])
                op(ACT, lambda e, p_=p_, nw=nw, cb=cb, psB=psB: e.activation(out=p_.ap[:, nw * 128:nw * 128 + 256], in_=psB.ap[:, cb:cb + 256], func=AF.Exp),
                   reads=[psB.r], writes=[p_.r])
                def stage2(p_=p_, ktl=ktl, v_=v_, o_=o_, qi=qi):
                    pso = self.psum()
                    nk = len(ktl)
                    for j, kti in enumerate(ktl):
                        op(PE, lambda e, pso=pso, p_=p_, j=j, kti=kti, v_=v_, nk=nk: e.matmul(pso.ap[:, 0:65], lhsT=p_.ap[:, j * 128:(j + 1) * 128], rhs=v_.ap[:, kti, :],
                                                                                           start=(j == 0), stop=(j == nk - 1)),
                           reads=[p_.r, v_.r], writes=[pso.r])
                    op(DVE, lambda e, pso=pso: e.reciprocal(out=rc.ap[:, 0:1], in_=pso.ap[:, 64:65]), reads=[pso.r], writes=[rc.r])
                    op(DVE, lambda e, pso=pso, o_=o_, qi=qi: e.tensor_scalar(out=o_.ap[:, qi, :], in0=pso.ap[:, 0:64], scalar1=rc.ap[:, 0:1], scalar2=None, op0=ALU.mult),
                       reads=[pso.r, rc.r], writes=[o_.r])
                pending.append(stage2)
                if len(pending) > 2:
                    pending.pop(0)()
            while pending:
                pending.pop(0)()
            dma(SP, OD.ap.rearrange("(i p) d -> p i d", p=128)[:, :, h * 64:(h + 1) * 64], o_.ap, reads=[o_.r], writes=OD.sub)
        self.out_proj(self.na_w_o[li])

    def gdn(self, l):
        op, dma = self.op, self.dma
        li = l // 2
        hT = self.hT
        self.P.barrier()
        self.off = self.mix_keep
        if not hasattr(self, "QF"):
            for nm in ("QF", "KF", "VF", "ZF", "OF"):
                setattr(self, nm, Tl(self.dram(nm, [D, TT], F32), nm, NT))
        QF, KF, VF, ZF, OF = self.QF, self.KF, self.VF, self.ZF, self.OF
        w_in = self.gdn_w_in[li]
        ones_a = self.tile([128, 128], F32, "ones_a")
        op(DVE, lambda e: e.memset(ones_a.ap, 1.0), writes=[ones_a.r])
        gT_a = self.tile([128, NT, 16], F32, "gT_a")
        bT_a = self.tile([128, NT, 16], F32, "bT_a")
        g_keep = self.off
        cw = self.tile([128, 24, 5], F32, "cw")
        for j in range(5):
            dma(SP, cw.ap[:, :, j], self.gdn_conv_w[li, j:j + 1, :].rearrange("o (c p) -> p (o c)", p=128), writes=[cw.r], allow_slow_non_contiguous=True)
        nwv = self.tile([128, 1], F32, "nwv")
        dma(SP, nwv.ap, self.gdn_norm_w[li:li + 1, :].rearrange("o p -> p o"), writes=[nwv.r], allow_slow_non_contiguous=True)
        pbuf = [self.tile([128, TT + 8], F32, "pbuf") for _ in range(2)]
        cbuf = [self.tile([128, TT], F32, "cbuf") for _ in range(2)]
        for pb in pbuf:
            op(POOL, lambda e, pb=pb: e.memset(pb.ap, 0.0), writes=[pb.r])
        wb = [self.tile([128, 8, 512], BF16, "win") for _ in range(2)]
        sqb = [self.tile([128, 512], F32, "sqb") for _ in range(2)]
        rnb = [self.tile([128, 512], F32, "rnb") for _ in range(2)]
        zsb = [self.tile([128, 512], F32, "zsb") for _ in range(2)]
        blocks = [(0, 256)] + [(256 + 512 * b, 512) for b in range(8)]
        nb = 0
        for cg in range(8):
            w = wb[cg % 2]
            dma(POOL, w.ap, w_in[:, cg * 512:(cg + 1) * 512].rearrange("(k p) n -> p k n", p=128), writes=[w.r])
            for cl in range(4):
                ct = cg * 4 + cl
                pb, cb = pbuf[ct % 2], cbuf[ct % 2]
                for (t0, nt_) in blocks:
                    ps = self.psum()
                    for k in range(8):
                        op(PE, lambda e, ps=ps, w=w, k=k, cl=cl, t0=t0, nt_=nt_: e.matmul(ps.ap[:, 0:nt_], lhsT=w.ap[:, k, cl * 128:(cl + 1) * 128], rhs=hT.ap[:, k, t0:t0 + nt_],
                                                                                    start=(k == 0), stop=(k == 7)),
                           reads=[w.r] + hT.sub[t0 // 128:(t0 + nt_) // 128], writes=ps.sub)
                    if ct < 24:
                        o0 = (2 if t0 == 0 else 6) + t0
                        op(ACT, lambda e, ps=ps, pb=pb, o0=o0, nt_=nt_: e.copy(out=pb.ap[:, o0:o0 + nt_], in_=ps.ap[:, 0:nt_]), reads=ps.sub, writes=[pb.r])
                    else:
                        zs = zsb[nb % 2]
                        nb += 1
                        op(ACT, lambda e, ps=ps, zs=zs, nt_=nt_: e.activation(out=zs.ap[:, 0:nt_], in_=ps.ap[:, 0:nt_], func=AF.Silu), reads=ps.sub, writes=[zs.r])
                        op(DVE, lambda e, zs=zs, nt_=nt_: e.tensor_scalar(out=zs.ap[:, 0:nt_], in0=zs.ap[:, 0:nt_], scalar1=nwv.ap[:, 0:1], scalar2=None, op0=ALU.mult),
                           reads=[zs.r, nwv.r], writes=[zs.r])
                        r0 = (ct - 24) * 128
                        dma(SP, ZF.ap[r0:r0 + 128, t0:t0 + nt_], zs.ap[:, 0:nt_], reads=[zs.r], writes=ZF.sub[t0 // 128:(t0 + nt_) // 128])
                if ct >= 24:
                    continue
                for (c0, ln, base) in ((0, 256, 0), (256, 4096, 260)):
                    op(DVE, lambda e, cb=cb, pb=pb, c0=c0, ln=ln, base=base, ct=ct: e.tensor_scalar(out=cb.ap[:, c0:c0 + ln], in0=pb.ap[:, base:base + ln], scalar1=cw.ap[:, ct, 0:1],
                                                                                             scalar2=None, op0=ALU.mult), reads=[pb.r, cw.r], writes=[cb.r])
                    for j in range(1, 5):
                        op(DVE, lambda e, cb=cb, pb=pb, c0=c0, ln=ln, base=base, ct=ct, j=j: e.scalar_tensor_tensor(out=cb.ap[:, c0:c0 + ln], in0=pb.ap[:, base + j:base + j + ln],
                                                                                                            scalar=cw.ap[:, ct, j:j + 1], in1=cb.ap[:, c0:c0 + ln], op0=ALU.mult, op1=ALU.add),
                           reads=[pb.r, cw.r, cb.r], writes=[cb.r])
                op(ACT, lambda e, cb=cb: e.activation(out=cb.ap, in_=cb.ap, func=AF.Silu), reads=[cb.r], writes=[cb.r])
                if ct < 16:
                    qsc = (128.0 ** -0.5) if ct < 8 else 1.0
                    for (t0, nt_) in blocks:
                        sq, rn = sqb[nb % 2], rnb[nb % 2]
                        nb += 1
                        op(POOL, lambda e, sq=sq, cb=cb, t0=t0, nt_=nt_: e.tensor_tensor(out=sq.ap[:, 0:nt_], in0=cb.ap[:, t0:t0 + nt_], in1=cb.ap[:, t0:t0 + nt_], op=ALU.mult),
                           reads=[cb.r], writes=[sq.r])
                        ps = self.psum()
                        op(PE, lambda e, ps=ps, sq=sq, nt_=nt_, o32=ones_a: e.matmul(ps.ap[:, 0:nt_], lhsT=o32.ap, rhs=sq.ap[:, 0:nt_], start=True, stop=True), reads=[ones_a.r, sq.r], writes=ps.sub)
                        op(DVE, lambda e, ps=ps, rn=rn, nt_=nt_: e.tensor_scalar(out=rn.ap[:, 0:nt_], in0=ps.ap[:, 0:nt_], scalar1=1e-6, scalar2=None, op0=ALU.add), reads=ps.sub, writes=[rn.r])
                        op(ACT, lambda e, rn=rn, nt_=nt_: e.activation(out=rn.ap[:, 0:nt_], in_=rn.ap[:, 0:nt_], func=AF.Ln), reads=[rn.r], writes=[rn.r])
                        op(ACT, lambda e, rn=rn, nt_=nt_: e.activation(out=rn.ap[:, 0:nt_], in_=rn.ap[:, 0:nt_], func=AF.Exp, scale=-0.5), reads=[rn.r], writes=[rn.r])
                        op(DVE, lambda e, rn=rn, cb=cb, t0=t0, nt_=nt_, qsc=qsc: e.scalar_tensor_tensor(out=cb.ap[:, t0:t0 + nt_], in0=cb.ap[:, t0:t0 + nt_], scalar=qsc, in1=rn.ap[:, 0:nt_],
                                                                                                 op0=ALU.mult, op1=ALU.mult), reads=[cb.r, rn.r], writes=[cb.r])
                dst = (QF, KF, VF)[ct // 8]
                r0 = (ct % 8) * 128
                dma(SP, dst.ap[r0:r0 + 128, :], cb.ap, reads=[cb.r], writes=dst.sub)
        wab = self.tile([128, 8, 32], BF16, "wab")
        dma(POOL, wab.ap, w_in[:, 4096:4128].rearrange("(k p) n -> p k n", p=128), writes=[wab.r])
        coef = self.tile([128, 16], F32, "coef")
        dtb = self.tile([128, 16], F32, "dtb")
        dma(SP, coef.ap, self.gdn_a_log[li:li + 1].rearrange("o a b -> o (a b)").to_broadcast([128, 16]), writes=[coef.r])
        dma(SP, dtb.ap, self.gdn_dt_bias[li:li + 1].rearrange("o a b -> o (a b)").to_broadcast([128, 16]), writes=[dtb.r])
        op(ACT, lambda e: e.activation(out=coef.ap, in_=coef.ap, func=AF.Exp), reads=[coef.r], writes=[coef.r])
        op(DVE, lambda e: e.tensor_scalar(out=coef.ap, in0=coef.ap, scalar1=-1.0, scalar2=None, op0=ALU.mult), reads=[coef.r], writes=[coef.r])
        psl = []
        for i in range(NT):
            if i % 16 == 0:
                ps = self.psum()
                psl.append(ps)
            c0 = (i % 16) * 32
            for k in range(8):
                op(PE, lambda e, ps=ps, i=i, k=k, c0=c0: e.matmul(ps.ap[:, c0:c0 + 32], lhsT=hT.ap[:, k, i * 128:(i + 1) * 128], rhs=wab.ap[:, k, :], start=(k == 0), stop=(k == 7)),
                   reads=[wab.r, hT.sub[i]], writes=ps.sub)
        for gi, ps in enumerate(psl):
            n_ = min(16, NT - gi * 16)
            pv = ps.ap[:, 0:n_ * 32].rearrange("p (i c) -> p i c", c=32)
            gs = gT_a.ap[:, gi * 16:gi * 16 + n_, :]
            bs = bT_a.ap[:, gi * 16:gi * 16 + n_, :]
            op(DVE, lambda e, pv=pv, gs=gs, n_=n_: e.tensor_tensor(out=gs, in0=pv[:, :, 0:16], in1=dtb.ap.unsqueeze(1).to_broadcast([128, n_, 16]), op=ALU.add), reads=ps.sub + [dtb.r], writes=[gT_a.r])
            op(ACT, lambda e, gs=gs: e.activation(out=gs, in_=gs, func=AF.Exp), reads=[gT_a.r], writes=[gT_a.r])
            op(DVE, lambda e, gs=gs: e.tensor_scalar(out=gs, in0=gs, scalar1=1.0, scalar2=None, op0=ALU.add), reads=[gT_a.r], writes=[gT_a.r])
            op(ACT, lambda e, gs=gs: e.activation(out=gs, in_=gs, func=AF.Ln), reads=[gT_a.r], writes=[gT_a.r])
            op(DVE, lambda e, gs=gs, n_=n_: e.tensor_tensor(out=gs, in0=gs, in1=coef.ap.unsqueeze(1).to_broadcast([128, n_, 16]), op=ALU.mult), reads=[gT_a.r, coef.r], writes=[gT_a.r])
            op(ACT, lambda e, pv=pv, bs=bs: e.activation(out=bs, in_=pv[:, :, 16:32], func=AF.Exp, scale=-1.0), reads=ps.sub, writes=[bT_a.r])
            op(DVE, lambda e, bs=bs: e.tensor_scalar(out=bs, in0=bs, scalar1=1.0, scalar2=None, op0=ALU.add), reads=[bT_a.r], writes=[bT_a.r])
            op(DVE, lambda e, bs=bs: e.reciprocal(out=bs, in_=bs), reads=[bT_a.r], writes=[bT_a.r])
        self.P.barrier()
        if self.cfg.get("test") == "gdn1":
            rr = Res("dbg2")
            for i_, t_ in enumerate((QF, KF, VF, ZF)):
                dma(SP, self.dbg2[i_], t_.ap, reads=t_.sub, writes=[rr])
            dma(SP, self.dbg3[:, 0], gT_a.ap, reads=[gT_a.r], writes=[rr])
            dma(SP, self.dbg3[:, 1], bT_a.ap, reads=[bT_a.r], writes=[rr])
            self.dbg.sub.append(rr)
            return
        self.off = self.base_off
        gT_o, bT_o = gT_a, bT_a
        ones32 = self.tile([128, 128], F32, "ones32b")
        gT = self.tile([128, NT, 16], F32, "gTb")
        bT = self.tile([128, NT, 16], F32, "bTb")
        op(DVE, lambda e: e.memset(ones32.ap, 1.0), writes=[ones32.r])
        op(DVE, lambda e: e.tensor_copy(out=gT.ap, in_=gT_o.ap), reads=[gT_o.r], writes=[gT.r])
        op(DVE, lambda e: e.tensor_copy(out=bT.ap, in_=bT_o.ap), reads=[bT_o.r], writes=[bT.r])
        self.P.barrier()
        def cmat(name, fill_in, pattern, cmult, cmp, fill):
            t = self.tile([128, 128], F32, name)
            op(POOL, lambda e: e.memset(t.ap, fill_in), writes=[t.r])
            op(POOL, lambda e: e.affine_select(out=t.ap, in_=t.ap, pattern=[[pattern, 128]], compare_op=cmp, fill=fill, base=0, channel_multiplier=cmult), reads=[t.r], writes=[t.r])
            return t
        triu32 = cmat("triu32", 1.0, 1, -1, ALU.is_ge, 0.0)
        tril32 = cmat("tril32", 1.0, -1, 1, ALU.is_ge, 0.0)
        sup = cmat("sup", 1.0, 1, -1, ALU.is_gt, 0.0)
        slo = cmat("slo", 1.0, -1, 1, ALU.is_gt, 0.0)
        mup = cmat("mup", 0.0, 1, -1, ALU.is_ge, -1e30)
        mlo = cmat("mlo", 0.0, -1, 1, ALU.is_ge, -1e30)
        i32 = self.ident32
        wo = self.tile([128, 8, D], BF16, "wo")
        dma(POOL, wo.ap, self.gdn_w_o[li].rearrange("(k p) n -> p k n", p=128), writes=[wo.r])
        def T4(nm):
            t_ = self.tile([128, 8, 128], F32, nm)
            t_.r = GRes(nm)
            return t_
        qfb = [T4("qf") for _ in range(2)]
        kfb = [T4("kf") for _ in range(2)]
        vfb = [T4("vf") for _ in range(2)]
        xfb = [T4("xf") for _ in range(2)]
        zfb = [T4("zf") for _ in range(2)]
        Rt, dTt, d2t, dec, decT, eGbc, AmT, An = (T4(nm) for nm in ("R", "dT", "d2", "dec", "decT", "eGbc", "AmT", "An"))
        kbg, ktail, vb = T4("kbg"), T4("ktail"), T4("vb")
        Mb = [T4("M0"), T4("M1")]
        Nb = [T4("N0"), T4("N1")]
        Pm, attnT, qdT, nwT, vnew, St = T4("P"), T4("attnT"), T4("qdT"), T4("nwT"), T4("vnew"), T4("S")
        PT = T4("PT")
        Mc, Nc = [Rt], [d2t]
        gm = []
        for gi_ in range(4):
            gt_ = self.tile([128, 128], F32, "gmask")
            dma(SP, gt_.ap, self.gmask_in[gi_], writes=[gt_.r])
            gm.append(gt_)
        Gtok = self.tile([128, 8], F32, "Gtok")
        eGtok = self.tile([128, 8], F32, "eGtok")
        bg = self.tile([128, 8], F32, "bg")
        yT = self.tile([128, 8, 128], BF16, "yT")
        ytile = [self.tile([128, D], F32, "ytile") for _ in range(2)]
        fl = lambda t, j: t.ap[:, 4 * j:4 * j + 4, :]
        pv4 = lambda ps: ps.ap.rearrange("p (a b) -> p a b", a=4)
        bc_h = lambda ap2, j: ap2[:, 4 * j:4 * j + 4].unsqueeze(2).to_broadcast([128, 4, 128])
        bc_m = lambda m, n_=4: m.ap.unsqueeze(1).to_broadcast([128, n_, 128])
        hview = lambda ap: ap.rearrange("(h p) t -> p h t", p=128)
        nch = 0
        gidx = 0
        seq = []
        for d_ in range(self.cfg.get("gdn_dirs", 2)):
            ord_ = list(range(NT)) if d_ == 0 else [1, 0] + list(range(NT - 1, 1, -1))
            seq += [(d_, n_) for n_ in ord_[:self.cfg.get("gdn_nch", NT)]]

        def g2_loads(ix):
            d_, n_ = seq[ix]
            cl_ = slice(n_ * 128, (n_ + 1) * 128)
            b_ = ix % 2
            dma(SP, qfb[b_].ap, hview(QF.ap)[:, :, cl_], reads=[QF.sub[n_]], writes=[qfb[b_].r])
            dma(SP, kfb[b_].ap, hview(KF.ap)[:, :, cl_], reads=[KF.sub[n_]], writes=[kfb[b_].r])
            dma(SP, vfb[b_].ap, hview(VF.ap)[:, :, cl_], reads=[VF.sub[n_]], writes=[vfb[b_].r])
            if d_ == 1:
                dma(SP, xfb[b_].ap, hview(OF.ap)[:, :, cl_], reads=[OF.sub[n_]], writes=[xfb[b_].r])
                dma(SP, zfb[b_].ap, hview(ZF.ap)[:, :, cl_], reads=[ZF.sub[n_]], writes=[zfb[b_].r])

        for d in range(self.cfg.get("gdn_dirs", 2)):
            TRI, maskT, maskN, strT, strN, last = ((triu32, mup, mlo, sup, slo, 127), (tril32, mlo, mup, slo, sup, 0))[d]
            order = list(range(NT)) if d == 0 else [1, 0] + list(range(NT - 1, 1, -1))
            order = order[:self.cfg.get("gdn_nch", NT)]
            op(DVE, lambda e: e.memset(St.ap, 0.0), writes=[St.r])
            for n in order:
                cols = slice(n * 128, (n + 1) * 128)
                qf, kf, vf, xf, zf = qfb[nch % 2], kfb[nch % 2], vfb[nch % 2], xfb[nch % 2], zfb[nch % 2]
                nch += 1
                if gidx == 0:
                    g2_loads(0)
                if gidx + 1 < len(seq):
                    g2_loads(gidx + 1)
                gidx += 1
                gsl = gT.ap[:, n, d * 8:(d + 1) * 8]
                bsl = bT.ap[:, n, d * 8:(d + 1) * 8]
                psG = self.psum()
                op(PE, lambda e, psG=psG, TRI=TRI, gsl=gsl: e.matmul(psG.ap[:, 0:8], lhsT=TRI.ap, rhs=gsl, start=True, stop=True), reads=[TRI.r, gT.r], writes=psG.sub)
                op(DVE, lambda e, gsl=gsl, TRI=TRI: e.tensor_tensor(out=Rt.ap, in0=gsl.unsqueeze(2).to_broadcast([128, 8, 128]), in1=bc_m(TRI, 8), op=ALU.mult),
                   reads=[gT.r, TRI.r], writes=[Rt.r])
                psGb = [self.psum(), self.psum()]
                for j in range(2):
                    op(PE, lambda e, j=j, p=psGb[j]: e.matmul(p.ap, lhsT=ones32.ap, rhs=fl(Rt, j).rearrange("p a b -> p (a b)"), start=True, stop=True),
                       reads=[ones32.r, Rt.r], writes=psGb[j].sub)
                op(ACT, lambda e, psG=psG: e.copy(out=Gtok.ap, in_=psG.ap[:, 0:8]), reads=psG.sub, writes=[Gtok.r])
                for j in range(2):
                    op(DVE, lambda e, j=j, p=psGb[j]: e.tensor_tensor(out=fl(dTt, j), in0=pv4(p), in1=bc_h(Gtok.ap, j), op=ALU.subtract), reads=psGb[j].sub + [Gtok.r], writes=[dTt.r])
                    op(ACT, lambda e, j=j, p=psGb[j]: e.activation(out=fl(eGbc, j), in_=pv4(p), func=AF.Exp), reads=psGb[j].sub, writes=[eGbc.r])
                op(DVE, lambda e, maskN=maskN: e.scalar_tensor_tensor(out=d2t.ap, in0=dTt.ap, scalar=-1.0, in1=bc_m(maskN, 8), op0=ALU.mult, op1=ALU.add),
                   reads=[dTt.r, maskN.r], writes=[d2t.r])
                op(POOL, lambda e, maskT=maskT: e.tensor_tensor(out=dTt.ap, in0=dTt.ap, in1=bc_m(maskT, 8), op=ALU.add), reads=[dTt.r, maskT.r, d2t.r], writes=[dTt.r])
                op(ACT, lambda e: e.activation(out=dec.ap, in_=d2t.ap, func=AF.Exp), reads=[d2t.r], writes=[dec.r])
                op(ACT, lambda e: e.activation(out=decT.ap, in_=dTt.ap, func=AF.Exp), reads=[dTt.r], writes=[decT.r])
                op(ACT, lambda e: e.activation(out=eGtok.ap, in_=Gtok.ap, func=AF.Exp), reads=[Gtok.r], writes=[eGtok.r])
                op(DVE, lambda e, bsl=bsl: e.tensor_tensor(out=bg.ap, in0=eGtok.ap, in1=bsl, op=ALU.mult), reads=[eGtok.r, bT.r], writes=[bg.r])
                op(DVE, lambda e, bsl=bsl: e.tensor_tensor(out=Rt.ap, in0=bsl.unsqueeze(2).to_broadcast([128, 8, 128]), in1=bc_m(i32, 8), op=ALU.mult),
                   reads=[bT.r, i32.r], writes=[Rt.r])
                psBb = [self.psum(), self.psum()]
                for j in range(2):
                    op(PE, lambda e, j=j, p=psBb[j]: e.matmul(p.ap, lhsT=ones32.ap, rhs=fl(Rt, j).rearrange("p a b -> p (a b)"), start=True, stop=True),
                       reads=[ones32.r, Rt.r], writes=psBb[j].sub)
                for j in range(2):
                    op(DVE, lambda e, j=j, p=psBb[j]: e.tensor_tensor(out=fl(AmT, j), in0=fl(decT, j), in1=pv4(p), op=ALU.mult), reads=psBb[j].sub + [decT.r], writes=[AmT.r])
                op(POOL, lambda e, strT=strT: e.tensor_tensor(out=AmT.ap, in0=AmT.ap, in1=bc_m(strT, 8), op=ALU.mult), reads=[AmT.r, strT.r], writes=[AmT.r])
                op(POOL, lambda e, bsl=bsl: e.tensor_tensor(out=An.ap, in0=dec.ap, in1=bsl.unsqueeze(2).to_broadcast([128, 8, 128]), op=ALU.mult), reads=[dec.r, bT.r], writes=[An.r])
                op(POOL, lambda e, strN=strN: e.tensor_tensor(out=An.ap, in0=An.ap, in1=bc_m(strN, 8), op=ALU.mult), reads=[An.r, strN.r], writes=[An.r])
                op(POOL, lambda e, qf=qf: e.tensor_tensor(out=qdT.ap, in0=qf.ap, in1=eGbc.ap, op=ALU.mult), reads=[qf.r, eGbc.r], writes=[qdT.r])
                if self.cfg.get('gdn_stage', 99) < 1:
                    continue
                psK = [self.psum(), self.psum()]
                psV = [self.psum(), self.psum()]
                for h in range(8):
                    j, q4 = h // 4, (h % 4) * 128
                    op(PE, lambda e, h=h, p=psK[j], q4=q4, kf=kf: e.transpose(out=p.ap[:, q4:q4 + 128], in_=kf.ap[:, h, :], identity=i32.ap), reads=[kf.r, i32.r], writes=[psK[j].sub[h % 4]])
                    op(PE, lambda e, h=h, p=psV[j], q4=q4, vf=vf: e.transpose(out=p.ap[:, q4:q4 + 128], in_=vf.ap[:, h, :], identity=i32.ap), reads=[vf.r, i32.r], writes=[psV[j].sub[h % 4]])
                for j in range(2):
                    op(DVE, lambda e, j=j, p=psK[j]: e.tensor_tensor(out=fl(kbg, j), in0=pv4(p), in1=bc_h(bg.ap, j), op=ALU.mult), reads=psK[j].sub + [bg.r], writes=[kbg.r])
                    op(DVE, lambda e, j=j, p=psK[j], last=last: e.tensor_tensor(out=fl(ktail, j), in0=pv4(p), in1=decT.ap[:, 4 * j:4 * j + 4, last:last + 1].to_broadcast([128, 4, 128]), op=ALU.mult),
                       reads=psK[j].sub + [decT.r], writes=[ktail.r])
                    op(DVE, lambda e, j=j, p=psV[j], bsl=bsl: e.tensor_tensor(out=fl(vb, j), in0=pv4(p), in1=bc_h(bsl, j), op=ALU.mult), reads=psV[j].sub + [bT.r], writes=[vb.r])
                if self.cfg.get('gdn_stage', 99) < 2:
                    continue
                psKK = [self.psum(), self.psum()]
                psKQ = [self.psum(), self.psum()]
                for h in range(8):
                    j, q4 = h // 4, (h % 4) * 128
                    op(PE, lambda e, h=h, p=psKK[j], q4=q4, kf=kf: e.matmul(p.ap[:, q4:q4 + 128], lhsT=kf.ap[:, h, :], rhs=kf.ap[:, h, :], start=True, stop=True), reads=[kf.r], writes=[psKK[j].sub[h % 4]])
                    op(PE, lambda e, h=h, p=psKQ[j], q4=q4, kf=kf, qf=qf: e.matmul(p.ap[:, q4:q4 + 128], lhsT=kf.ap[:, h, :], rhs=qf.ap[:, h, :], start=True, stop=True), reads=[kf.r, qf.r], writes=[psKQ[j].sub[h % 4]])
                M0, N0 = Mb[0], Nb[0]
                for j in range(2):
                    op(DVE, lambda e, j=j, p=psKK[j]: e.scalar_tensor_tensor(out=fl(M0, j), in0=pv4(p), scalar=-1.0, in1=fl(AmT, j), op0=ALU.mult, op1=ALU.mult), reads=psKK[j].sub + [AmT.r], writes=[M0.r])
                    op(DVE, lambda e, j=j, p=psKK[j]: e.scalar_tensor_tensor(out=fl(N0, j), in0=pv4(p), scalar=-1.0, in1=fl(An, j), op0=ALU.mult, op1=ALU.mult), reads=psKK[j].sub + [An.r], writes=[N0.r])
                    op(DVE, lambda e, j=j, p=psKQ[j]: e.tensor_tensor(out=fl(attnT, j), in0=pv4(p), in1=fl(decT, j), op=ALU.mult), reads=psKQ[j].sub + [decT.r], writes=[attnT.r])
                MA, NA, MB, NB = Mb[1], Nb[1], Mc[0], Nc[0]
                op(DVE, lambda e: e.tensor_tensor(out=MA.ap, in0=M0.ap, in1=bc_m(gm[0], 8), op=ALU.mult), reads=[M0.r, gm[0].r], writes=[MA.r])
                op(POOL, lambda e: e.tensor_tensor(out=NA.ap, in0=N0.ap, in1=bc_m(gm[0], 8), op=ALU.mult), reads=[N0.r, gm[0].r], writes=[NA.r])
                op(POOL, lambda e: e.tensor_tensor(out=Pm.ap, in0=MA.ap, in1=bc_m(i32, 8), op=ALU.add), reads=[MA.r, i32.r], writes=[Pm.r])
                op(POOL, lambda e: e.tensor_tensor(out=PT.ap, in0=NA.ap, in1=bc_m(i32, 8), op=ALU.add), reads=[NA.r, i32.r], writes=[PT.r])
                cur = (MA, NA)
                nxt = (MB, NB)
                for kk in range(1, 4):
                    Mp, Np = cur
                    Mn, Nn = nxt
                    psN = [self.psum(), self.psum()]
                    psM = [self.psum(), self.psum()]
                    for h in range(8):
                        j, q4 = h // 4, (h % 4) * 128
                        op(PE, lambda e, h=h, p=psN[j], q4=q4, Mp=Mp, Np=Np: e.matmul(p.ap[:, q4:q4 + 128], lhsT=Mp.ap[:, h, :], rhs=Np.ap[:, h, :], start=True, stop=True),
                           reads=[Mp.r, Np.r], writes=[psN[j].sub[h % 4]])
                        op(PE, lambda e, h=h, p=psM[j], q4=q4, Mp=Mp, Np=Np: e.matmul(p.ap[:, q4:q4 + 128], lhsT=Np.ap[:, h, :], rhs=Mp.ap[:, h, :], start=True, stop=True),
                           reads=[Mp.r, Np.r], writes=[psM[j].sub[h % 4]])
                    op(ACT, lambda e, j=0, p=psN[0], Nn=Nn: e.copy(out=fl(Nn, j), in_=pv4(p)), reads=psN[0].sub, writes=[Nn.r])
                    op(DVE, lambda e, j=1, p=psN[1], Nn=Nn: e.tensor_copy(out=fl(Nn, j), in_=pv4(p)), reads=psN[1].sub, writes=[Nn.r])
                    op(DVE, lambda e, j=0, p=psM[0], Mn=Mn: e.tensor_copy(out=fl(Mn, j), in_=pv4(p)), reads=psM[0].sub, writes=[Mn.r])
                    op(ACT, lambda e, j=1, p=psM[1], Mn=Mn: e.copy(out=fl(Mn, j), in_=pv4(p)), reads=psM[1].sub, writes=[Mn.r])
                    psP = [self.psum(), self.psum()]
                    psQ_ = [self.psum(), self.psum()]
                    for h in range(8):
                        j, q4 = h // 4, (h % 4) * 128
                        op(PE, lambda e, h=h, p=psP[j], q4=q4, Nn=Nn: e.matmul(p.ap[:, q4:q4 + 128], lhsT=Nn.ap[:, h, :], rhs=Pm.ap[:, h, :], start=True, stop=True),
                           reads=[Nn.r, Pm.r], writes=[psP[j].sub[h % 4]])
                        op(PE, lambda e, h=h, p=psQ_[j], q4=q4, Mn=Mn: e.matmul(p.ap[:, q4:q4 + 128], lhsT=Mn.ap[:, h, :], rhs=PT.ap[:, h, :], start=True, stop=True),
                           reads=[Mn.r, PT.r], writes=[psQ_[j].sub[h % 4]])
                    for j in range(2):
                        op(DVE, lambda e, j=j, p=psP[j]: e.tensor_tensor(out=fl(Pm, j), in0=fl(Pm, j), in1=pv4(p), op=ALU.add), reads=psP[j].sub + [Pm.r], writes=[Pm.r])
                        op(DVE, lambda e, j=j, p=psQ_[j]: e.tensor_tensor(out=fl(PT, j), in0=fl(PT, j), in1=pv4(p), op=ALU.add), reads=psQ_[j].sub + [PT.r], writes=[PT.r])
                    cur, nxt = nxt, cur
                for lv in range(3):
                    UoT, Yt = Mc[0], Nc[0]
                    op(DVE, lambda e, lv=lv, UoT=UoT: e.scalar_tensor_tensor(out=UoT.ap, in0=N0.ap, scalar=-1.0, in1=bc_m(gm[1 + lv], 8), op0=ALU.mult, op1=ALU.mult),
                       reads=[N0.r, gm[1 + lv].r], writes=[UoT.r])
                    psY = [self.psum(), self.psum()]
                    for h in range(8):
                        j, q4 = h // 4, (h % 4) * 128
                        op(PE, lambda e, h=h, p=psY[j], q4=q4, UoT=UoT: e.matmul(p.ap[:, q4:q4 + 128], lhsT=UoT.ap[:, h, :], rhs=Pm.ap[:, h, :], start=True, stop=True),
                           reads=[UoT.r, Pm.r], writes=[psY[j].sub[h % 4]])
                    op(ACT, lambda e, j=0, p=psY[0], Yt=Yt: e.copy(out=fl(Yt, j), in_=pv4(p)), reads=psY[0].sub, writes=[Yt.r])
                    op(DVE, lambda e, j=1, p=psY[1], Yt=Yt: e.tensor_copy(out=fl(Yt, j), in_=pv4(p)), reads=psY[1].sub, writes=[Yt.r])
                    psX = [self.psum(), self.psum()]
                    psXT = [self.psum(), self.psum()]
                    for h in range(8):
                        j, q4 = h // 4, (h % 4) * 128
                        op(PE, lambda e, h=h, p=psX[j], q4=q4, Yt=Yt: e.matmul(p.ap[:, q4:q4 + 128], lhsT=PT.ap[:, h, :], rhs=Yt.ap[:, h, :], start=True, stop=True),
                           reads=[PT.r, Yt.r], writes=[psX[j].sub[h % 4]])
                        if lv < 2:
                            op(PE, lambda e, h=h, p=psXT[j], q4=q4, Yt=Yt: e.matmul(p.ap[:, q4:q4 + 128], lhsT=Yt.ap[:, h, :], rhs=PT.ap[:, h, :], start=True, stop=True),
                               reads=[PT.r, Yt.r], writes=[psXT[j].sub[h % 4]])
                    for j in range(2):
                        op(DVE, lambda e, j=j, p=psX[j]: e.tensor_tensor(out=fl(Pm, j), in0=fl(Pm, j), in1=pv4(p), op=ALU.subtract), reads=psX[j].sub + [Pm.r], writes=[Pm.r])
                        if lv < 2:
                            op(DVE, lambda e, j=j, p=psXT[j]: e.tensor_tensor(out=fl(PT, j), in0=fl(PT, j), in1=pv4(p), op=ALU.subtract), reads=psXT[j].sub + [PT.r], writes=[PT.r])
                if self.cfg.get('gdn_stage', 99) < 4:
                    continue
                psW = [self.psum(), self.psum()]
                for h in range(8):
                    j, q4 = h // 4, (h % 4) * 128
                    op(PE, lambda e, h=h, p=psW[j], q4=q4: e.matmul(p.ap[:, q4:q4 + 128], lhsT=kbg.ap[:, h, :], rhs=Pm.ap[:, h, :], start=True, stop=True), reads=[kbg.r, Pm.r], writes=[psW[j].sub[h % 4]])
                for j in range(2):
                    op(ACT, lambda e, j=j, p=psW[j]: e.mul(out=fl(nwT, j), in_=pv4(p), mul=-1.0), reads=psW[j].sub, writes=[nwT.r])
                psVn = [self.psum(), self.psum()]
                for h in range(8):
                    j, q4 = h // 4, (h % 4) * 128
                    op(PE, lambda e, h=h, p=psVn[j], q4=q4: e.matmul(p.ap[:, q4:q4 + 128], lhsT=Pm.ap[:, h, :], rhs=vb.ap[:, h, :], start=True, stop=False), reads=[Pm.r, vb.r], writes=[psVn[j].sub[h % 4]])
                    op(PE, lambda e, h=h, p=psVn[j], q4=q4: e.matmul(p.ap[:, q4:q4 + 128], lhsT=nwT.ap[:, h, :], rhs=St.ap[:, h, :], start=False, stop=True), reads=[nwT.r, St.r], writes=[psVn[j].sub[h % 4]])
                op(ACT, lambda e, j=0, p=psVn[0]: e.copy(out=fl(vnew, j), in_=pv4(p)), reads=psVn[0].sub, writes=[vnew.r])
                op(DVE, lambda e, j=1, p=psVn[1]: e.tensor_copy(out=fl(vnew, j), in_=pv4(p)), reads=psVn[1].sub, writes=[vnew.r])
                if self.cfg.get('gdn_stage', 99) < 5:
                    continue
                psO = [self.psum(), self.psum()]
                psS = [self.psum(), self.psum()]
                for h in range(8):
                    j, q4 = h // 4, (h % 4) * 128
                    op(PE, lambda e, h=h, p=psO[j], q4=q4: e.matmul(p.ap[:, q4:q4 + 128], lhsT=St.ap[:, h, :], rhs=qdT.ap[:, h, :], start=True, stop=False), reads=[St.r, qdT.r], writes=[psO[j].sub[h % 4]])
                    op(PE, lambda e, h=h, p=psO[j], q4=q4: e.matmul(p.ap[:, q4:q4 + 128], lhsT=vnew.ap[:, h, :], rhs=attnT.ap[:, h, :], start=False, stop=True), reads=[vnew.r, attnT.r], writes=[psO[j].sub[h % 4]])
                    op(PE, lambda e, h=h, p=psS[j], q4=q4: e.matmul(p.ap[:, q4:q4 + 128], lhsT=ktail.ap[:, h, :], rhs=vnew.ap[:, h, :], start=True, stop=True), reads=[ktail.r, vnew.r], writes=[psS[j].sub[h % 4]])
                for j in range(2):
                    op(DVE, lambda e, j=j, last=last: e.tensor_tensor(out=fl(St, j), in0=fl(St, j), in1=eGbc.ap[:, 4 * j:4 * j + 4, last:last + 1].to_broadcast([128, 4, 128]), op=ALU.mult),
                       reads=[St.r, eGbc.r], writes=[St.r])
                    op(DVE, lambda e, j=j, p=psS[j]: e.tensor_tensor(out=fl(St, j), in0=fl(St, j), in1=pv4(p), op=ALU.add), reads=psS[j].sub + [St.r], writes=[St.r])
                if d == 0:
                    for j in range(2):
                        op(ACT, lambda e, j=j, p=psO[j], xf=xf: e.copy(out=fl(xf, j), in_=pv4(p)), reads=psO[j].sub, writes=[xf.r])
                    dma(SP, hview(OF.ap)[:, :, cols], xf.ap, reads=[xf.r], writes=[OF.sub[n]])
                    continue
                for j in range(2):
                    op(DVE, lambda e, j=j, p=psO[j], xf=xf: e.tensor_tensor(out=fl(xf, j), in0=fl(xf, j), in1=pv4(p), op=ALU.add), reads=psO[j].sub + [xf.r], writes=[xf.r])
                op(POOL, lambda e, xf=xf: e.tensor_tensor(out=Rt.ap, in0=xf.ap, in1=xf.ap, op=ALU.mult), reads=[xf.r], writes=[Rt.r])
                psQ = [self.psum(), self.psum()]
                for j in range(2):
                    op(PE, lambda e, j=j, p=psQ[j]: e.matmul(p.ap, lhsT=ones32.ap, rhs=fl(Rt, j).rearrange("p a b -> p (a b)"), start=True, stop=True), reads=[ones32.r, Rt.r], writes=psQ[j].sub)
                for j in range(2):
                    op(DVE, lambda e, j=j, p=psQ[j]: e.tensor_scalar(out=fl(d2t, j), in0=pv4(p), scalar1=1.0 / 128.0, scalar2=LN_EPS, op0=ALU.mult, op1=ALU.add), reads=psQ[j].sub, writes=[d2t.r])
                op(ACT, lambda e: e.activation(out=d2t.ap, in_=d2t.ap, func=AF.Ln), reads=[d2t.r], writes=[d2t.r])
                op(ACT, lambda e: e.activation(out=d2t.ap, in_=d2t.ap, func=AF.Exp, scale=-0.5), reads=[d2t.r], writes=[d2t.r])
                op(POOL, lambda e, xf=xf: e.tensor_tensor(out=xf.ap, in0=xf.ap, in1=d2t.ap, op=ALU.mult), reads=[xf.r, d2t.r], writes=[xf.r])
                op(POOL, lambda e, xf=xf, zf=zf: e.tensor_tensor(out=yT.ap, in0=xf.ap, in1=zf.ap, op=ALU.mult), reads=[xf.r, zf.r], writes=[yT.r])
                yt = ytile[n % 2]
                for half in range(2):
                    ps = self.psum()
                    for h in range(8):
                        op(PE, lambda e, ps=ps, h=h, half=half: e.matmul(ps.ap, lhsT=yT.ap[:, h, :], rhs=wo.ap[:, h, half * 512:(half + 1) * 512], start=(h == 0), stop=(h == 7)),
                           reads=[yT.r, wo.r], writes=ps.sub)
                    op(ACT, lambda e, ps=ps, yt=yt, half=half: e.copy(out=yt.ap[:, half * 512:(half + 1) * 512], in_=ps.ap), reads=ps.sub, writes=[yt.r])
                dma(SP, self.Y.ap[cols, :], yt.ap, reads=[yt.r], writes=[self.Y.sub[n]])
                if self.cfg.get("test") == "mix":
                    dma(SP, self.dbg.ap[cols, :], yt.ap, reads=[yt.r], writes=[self.dbg.sub[n]])
        if self.cfg.get("test") == "gdn2":
            self.P.barrier()
            rr = Res("dbg2")
            dma(SP, self.dbg2[0], VF.ap if self.cfg.get('gdn_nch') == 1 else OF.ap, reads=OF.sub + VF.sub, writes=[rr])
            dma(SP, self.dbg2[1, 0:128, 0:1024], St.ap.rearrange("p a b -> p (a b)"), reads=[St.r], writes=[rr])
            for ii, tt in enumerate((decT, dec, AmT, An, Pm, vnew, attnT, kbg, vb, ktail, qdT, eGbc, nwT, qfb[0], kfb[0], vfb[0])):
                dma(SP, self.dbg2[2 + ii // 8, (ii % 8) * 128:(ii % 8 + 1) * 128, 0:1024], tt.ap.rearrange("p a b -> p (a b)"), reads=[tt.r], writes=[rr])
            dma(SP, self.dbg3[:, 0], gT.ap, reads=[gT.r], writes=[rr])
            dma(SP, self.dbg3[:, 1], bT.ap, reads=[bT.r], writes=[rr])
            self.dbg.sub.append(rr)

    def out_proj(self, w_o):
        op, dma = self.op, self.dma
        self.P.barrier()
        self.off = self.base_off
        OD = self.OD
        wo = self.tile([128, 8, D], BF16, "wo")
        dma(POOL, wo.ap, w_o.rearrange("(k p) n -> p k n", p=128), writes=[wo.r])
        ob = [self.tile([128, D], BF16, "ob") for _ in range(3)]
        oT = [self.tile([128, 8, 128], BF16, "oT", nsub=1) for _ in range(2)]
        yb = [self.tile([128, D], F32, "yb") for _ in range(2)]
        for i in range(NT):
            o, t, y = ob[i % 3], oT[i % 2], yb[i % 2]
            rows = slice(i * 128, (i + 1) * 128)
            dma(SP, o.ap, OD.ap[rows, :], reads=[OD.sub[i]], writes=[o.r])
            self.transpose_to(o, t, 0)
            for half in range(2):
                ps = self.psum()
                for k in range(8):
                    op(PE, lambda e, ps=ps, t=t, k=k, half=half: e.matmul(ps.ap, lhsT=t.ap[:, k, :], rhs=wo.ap[:, k, half * 512:(half + 1) * 512], start=(k == 0), stop=(k == 7)),
                       reads=[t.sub[0], wo.r], writes=[ps.r])
                op(ACT if half else DVE, (lambda e, ps=ps, y=y, half=half: e.copy(out=y.ap[:, half * 512:(half + 1) * 512], in_=ps.ap)) if half else
                   (lambda e, ps=ps, y=y, half=half: e.tensor_copy(out=y.ap[:, half * 512:(half + 1) * 512], in_=ps.ap)), reads=[ps.r], writes=[y.r])
            dma(SP, self.Y.ap[rows, :], y.ap, reads=[y.r], writes=[self.Y.sub[i]])
            if self.cfg.get("test") == "mix":
                dma(SP, self.dbg.ap[rows, :], y.ap, reads=[y.r], writes=[self.dbg.sub[i]])

    def postmix(self, l):
        op, dma = self.op, self.dma
        self.phase_begin()
        self.h2tok = self.tile([128, NT, D], BF16, "h2tok", nsub=NT)
        self.affTok = self.tile([128, NT, NE], F32, "affTok")
        h2tok, affTok = self.h2tok, self.affTok
        self.post_keep = self.off
        mods = self.mod_tiles(l, [2, 3, 4], plus1=(4,))
        g0 = self.ln_vec(self.ln_g[l, 0:1, :])
        b0 = self.ln_vec(self.ln_b[l, 0:1, :])
        wr = self.tile([128, 8, NE], BF16, "wr")
        dma(POOL, wr.ap, self.moe_w_router[l].rearrange("(k p) n -> p k n", p=128), writes=[wr.r])
        xb = [self.tile([128, D], F32, "xb") for _ in range(2)]
        yb = [self.tile([128, D], F32, "yb") for _ in range(2)]
        ob = [self.tile([128, D], F32, "ob") for _ in range(2)]
        h2T = [self.tile([128, 8, 128], BF16, "h2T", nsub=1) for _ in range(2)]
        st = self.tile([128, 2, 6], F32, "st")
        mv = self.tile([128, 4], F32, "mv")
        sm = self.tile([128, 4], F32, "sm")
        ex = self.tile([128, NE], F32, "ex")
        for i in range(NT):
            x, y, o = xb[i % 2], yb[i % 2], ob[i % 2]
            c = 1 if i < 2 else 0
            rows = slice(i * 128, (i + 1) * 128)
            dma(SP, x.ap, self.XS.ap[rows, :], reads=[self.XS.sub[i]], writes=[x.r])
            dma(SP, y.ap, self.Y.ap[rows, :], reads=[self.Y.sub[i]], writes=[y.r])
            m2, m3, m4 = mods[2][c], mods[3][c], mods[4][c]
            op(POOL, lambda e, y=y, m2=m2: e.tensor_tensor(out=y.ap, in0=y.ap, in1=m2.ap, op=ALU.mult), reads=[y.r, m2.r], writes=[y.r])
            op(DVE, lambda e, x=x, y=y: e.scalar_tensor_tensor(out=y.ap, in0=x.ap, scalar=ALPHA, in1=y.ap, op0=ALU.mult, op1=ALU.add),
               reads=[x.r, y.r], writes=[y.r])
            self.layernorm_(y, st, mv)
            op(POOL, lambda e, y=y, o=o: e.tensor_tensor(out=o.ap, in0=y.ap, in1=g0.ap, op=ALU.mult), reads=[y.r, g0.r], writes=[o.r])
            op(POOL, lambda e, o=o: e.tensor_tensor(out=o.ap, in0=o.ap, in1=b0.ap, op=ALU.add), reads=[o.r, b0.r], writes=[o.r])
            dma(POOL, self.XS.ap[rows, :], o.ap, reads=[o.r], writes=[self.XS.sub[i]])
            op(DVE, lambda e, o=o, x=x, m4=m4: e.tensor_tensor(out=x.ap, in0=o.ap, in1=m4.ap, op=ALU.mult), reads=[o.r, m4.r], writes=[x.r])
            hv = Tl(h2tok.ap[:, i, :])
            hv.r = h2tok.sub[i]
            op(DVE, lambda e, x=x, m3=m3, hv=hv: e.tensor_tensor(out=hv.ap, in0=x.ap, in1=m3.ap, op=ALU.add), reads=[x.r, m3.r], writes=[hv.r])
            ht = h2T[i % 2]
            self.transpose_to(hv, ht, 0)
            ps = self.psum()
            for k in range(8):
                op(PE, lambda e, ps=ps, ht=ht, k=k: e.matmul(ps.ap[:, 0:NE], lhsT=ht.ap[:, k, :], rhs=wr.ap[:, k, :], start=(k == 0), stop=(k == 7)),
                   reads=[ht.sub[0], wr.r], writes=[ps.r])
            op(DVE, lambda e, ps=ps: e.reduce_max(out=sm.ap[:, 0:1], in_=ps.ap[:, 0:NE], axis=AX.X), reads=[ps.r], writes=[sm.r])
            op(DVE, lambda e: e.tensor_scalar(out=sm.ap[:, 1:2], in0=sm.ap[:, 0:1], scalar1=-1.0, scalar2=None, op0=ALU.mult), reads=[sm.r], writes=[sm.r])
            op(ACT, lambda e, ps=ps: e.activation(out=ex.ap, in_=ps.ap[:, 0:NE], func=AF.Exp, bias=sm.ap[:, 1:2], scale=1.0, accum_out=sm.ap[:, 2:3]),
               reads=[ps.r, sm.r], writes=[ex.r, sm.r])
            op(DVE, lambda e: e.reciprocal(out=sm.ap[:, 3:4], in_=sm.ap[:, 2:3]), reads=[sm.r], writes=[sm.r])
            op(DVE, lambda e, i=i: e.tensor_scalar(out=affTok.ap[:, i, :], in0=ex.ap, scalar1=sm.ap[:, 3:4], scalar2=None, op0=ALU.mult),
               reads=[ex.r, sm.r], writes=[affTok.r])

    def moe_select(self, l):
        op, dma = self.op, self.dma
        affTok = self.affTok
        self.P.barrier()
        self.off = self.post_keep
        self.posm = self.tile([128, NT, NE], F32, "posm")
        posm = self.posm
        keep2 = self.off
        affT = self.tile([NE, TT], F32, "affT")
        work = self.tile([NE, 4096], F32, "work")
        m8 = self.tile([NE, 8], F32, "m8")
        thr = self.tile([NE, 2], F32, "thr")
        maskT = self.tile([NE, TT], BF16, "maskT")
        for g in range(9):
            ps = self.psum()
            tl = list(range(g * 4, min(g * 4 + 4, NT)))
            for j, i in enumerate(tl):
                op(PE, lambda e, ps=ps, i=i, j=j: e.transpose(out=ps.ap[0:NE, j * 128:(j + 1) * 128], in_=affTok.ap[:, i, :], identity=self.ident32.ap),
                   reads=[affTok.r, self.ident32.r], writes=[ps.r])
            n = len(tl) * 128
            op(ACT, lambda e, ps=ps, g=g, n=n: e.copy(out=affT.ap[:, g * 512:g * 512 + n], in_=ps.ap[0:NE, 0:n]), reads=[ps.r], writes=[affT.r])
        dma(SP, self.AFFT.ap, affT.ap, reads=[affT.r], writes=[self.AFFT.r])
        op(DVE, lambda e: e.tensor_copy(out=work.ap[:, 0:256], in_=affT.ap[:, 0:256]), reads=[affT.r], writes=[work.r])
        for it in range(4):
            op(DVE, lambda e: e.max(out=m8.ap, in_=work.ap[:, 0:256]), reads=[work.r], writes=[m8.r])
            if it < 3:
                op(DVE, lambda e: e.match_replace(out=work.ap[:, 0:256], in_to_replace=m8.ap, in_values=work.ap[:, 0:256], imm_value=-1.0),
                   reads=[work.r, m8.r], writes=[work.r])
        op(DVE, lambda e: e.tensor_copy(out=thr.ap[:, 0:1], in_=m8.ap[:, 7:8]), reads=[m8.r], writes=[thr.r])
        if not hasattr(self, "CAND"):
            self.CAND = Tl(self.dram("CAND", [128, 128], F32), "CAND")
        w1 = self.tile([128, 512], F32, "w1")
        c1 = self.tile([128, 128], F32, "c1")
        for e_ in range(NE):
            dma(SP, w1.ap[e_ * 8:(e_ + 1) * 8, :], self.AFFT.ap[e_, 256:TT].rearrange("(s t) -> s t", s=8), reads=[self.AFFT.r], writes=[w1.r])
        for it in range(16):
            op(DVE, lambda e, it=it: e.max(out=c1.ap[:, it * 8:(it + 1) * 8], in_=w1.ap), reads=[w1.r], writes=[c1.r])
            if it < 15:
                op(DVE, lambda e, it=it: e.match_replace(out=w1.ap, in_to_replace=c1.ap[:, it * 8:(it + 1) * 8], in_values=w1.ap, imm_value=-1.0),
                   reads=[w1.r, c1.r], writes=[w1.r])
        dma(SP, self.CAND.ap, c1.ap, reads=[c1.r], writes=[self.CAND.r])
        dma(SP, work.ap[:, 0:1024], self.CAND.ap.rearrange("(e s) c -> e (s c)", s=8), reads=[self.CAND.r], writes=[work.r])
        for it in range(64):
            op(DVE, lambda e: e.max(out=m8.ap, in_=work.ap[:, 0:1024]), reads=[work.r], writes=[m8.r])
            if it < 63:
                op(DVE, lambda e: e.match_replace(out=work.ap[:, 0:1024], in_to_replace=m8.ap, in_values=work.ap[:, 0:1024], imm_value=-1.0),
                   reads=[work.r, m8.r], writes=[work.r])
        op(DVE, lambda e: e.tensor_copy(out=thr.ap[:, 1:2], in_=m8.ap[:, 7:8]), reads=[m8.r], writes=[thr.r])
        for (lo, n, col) in ((0, 256, 0), (256, 4096, 1)):
            op(DVE, lambda e, lo=lo, n=n, col=col: e.tensor_scalar(out=maskT.ap[:, lo:lo + n], in0=affT.ap[:, lo:lo + n], scalar1=thr.ap[:, col:col + 1],
                                                                   scalar2=None, op0=ALU.is_ge), reads=[affT.r, thr.r], writes=[maskT.r])
        maskTok = self.tile([128, NT, NE], BF16, "maskTok")
        maskF = self.tile([128, NT, NE], F32, "maskF")
        ps = self.psum()
        pb = ps.ap.bitcast(BF16)
        for i in range(NT):
            op(PE, lambda e, i=i, pb=pb: e.transpose(out=pb[:, i * NE:(i + 1) * NE], in_=maskT.ap[:, i * 128:(i + 1) * 128], identity=self.ident.ap[0:NE, 0:NE]),
               reads=[maskT.r, self.ident.r], writes=[ps.r])
        op(DVE, lambda e, pb=pb: e.tensor_copy(out=maskTok.ap.rearrange("p a b -> p (a b)"), in_=pb[:, 0:NT * NE]), reads=[ps.r], writes=[maskTok.r])
        op(DVE, lambda e: e.tensor_copy(out=maskF.ap, in_=maskTok.ap), reads=[maskTok.r], writes=[maskF.r])
        mflat = maskTok.ap.rearrange("p a b -> p (a b)")
        ps_w, ps_t, ps_c = self.psum(), self.psum(), self.psum()
        op(PE, lambda e: e.matmul(ps_w.ap, lhsT=self.triu.ap, rhs=mflat[:, 2 * NE:NT * NE], start=True, stop=True), reads=[maskTok.r, self.triu.r], writes=[ps_w.r])
        op(PE, lambda e: e.matmul(ps_t.ap, lhsT=self.ones.ap, rhs=mflat[:, 2 * NE:NT * NE], start=True, stop=True), reads=[maskTok.r, self.ones.r], writes=[ps_t.r])
        op(PE, lambda e: e.matmul(ps_c.ap[:, 0:2 * NE], lhsT=self.triu.ap, rhs=mflat[:, 0:2 * NE], start=True, stop=True), reads=[maskTok.r, self.triu.r], writes=[ps_c.r])
        op(PE, lambda e: e.matmul(ps_c.ap[:, 2 * NE:4 * NE], lhsT=self.ones.ap, rhs=mflat[:, 0:2 * NE], start=True, stop=True), reads=[maskTok.r, self.ones.r], writes=[ps_c.r])
        eoff = self.tile([128, NT, NE], F32, "eoff")
        tot = self.tile([128, NT, NE], F32, "tot")
        op(DVE, lambda e: e.tensor_copy(out=tot.ap[:, 2:NT, :].rearrange("p a b -> p (a b)"), in_=ps_t.ap), reads=[ps_t.r], writes=[tot.r])
        op(DVE, lambda e: e.tensor_copy(out=tot.ap[:, 0:2, :].rearrange("p a b -> p (a b)"), in_=ps_c.ap[:, 2 * NE:4 * NE]), reads=[ps_c.r], writes=[tot.r])
        op(DVE, lambda e: e.memset(eoff.ap, 0.0), writes=[eoff.r])
        op(DVE, lambda e: e.tensor_copy(out=eoff.ap[:, 1, :], in_=tot.ap[:, 0, :]), reads=[tot.r], writes=[eoff.r])
        for i in range(3, NT):
            op(DVE, lambda e, i=i: e.tensor_tensor(out=eoff.ap[:, i, :], in0=eoff.ap[:, i - 1, :], in1=tot.ap[:, i - 1, :], op=ALU.add),
               reads=[eoff.r, tot.r], writes=[eoff.r])
        op(DVE, lambda e: e.tensor_tensor(out=posm.ap[:, 2:NT, :].rearrange("p a b -> p (a b)"), in0=ps_w.ap,
                                          in1=eoff.ap[:, 2:NT, :].rearrange("p a b -> p (a b)"), op=ALU.add), reads=[ps_w.r, eoff.r], writes=[posm.r])
        op(DVE, lambda e: e.tensor_tensor(out=posm.ap[:, 0:2, :].rearrange("p a b -> p (a b)"), in0=ps_c.ap[:, 0:2 * NE],
                                          in1=eoff.ap[:, 0:2, :].rearrange("p a b -> p (a b)"), op=ALU.add), reads=[ps_c.r, eoff.r], writes=[posm.r])
        op(DVE, lambda e: e.tensor_tensor(out=posm.ap, in0=posm.ap, in1=maskF.ap, op=ALU.mult), reads=[posm.r, maskF.r], writes=[posm.r])
        op(DVE, lambda e: e.tensor_scalar(out=posm.ap, in0=posm.ap, scalar1=-1.0, scalar2=None, op0=ALU.add), reads=[posm.r], writes=[posm.r])
        posT = self.tile([NE, TT], F32, "posT")
        for g in range(9):
            ps = self.psum()
            tl = list(range(g * 4, min(g * 4 + 4, NT)))
            for j, i in enumerate(tl):
                op(PE, lambda e, ps=ps, i=i, j=j: e.transpose(out=ps.ap[0:NE, j * 128:(j + 1) * 128], in_=posm.ap[:, i, :], identity=self.ident32.ap),
                   reads=[posm.r, self.ident32.r], writes=[ps.r])
            n = len(tl) * 128
            op(DVE, lambda e, ps=ps, g=g, n=n: e.tensor_scalar(out=posT.ap[:, g * 512:g * 512 + n], in0=ps.ap[0:NE, 0:n], scalar1=12582912.0, scalar2=12582912.0,
                                                               op0=ALU.add, op1=ALU.subtract), reads=[ps.r], writes=[posT.r])
        dma(SP, self.POST.ap, posT.ap, reads=[posT.r], writes=[self.POST.r])
        self.P.barrier()
        self.off = keep2

    def moe_passA(self, l):
        op, dma = self.op, self.dma
        h2tok, posm = self.h2tok, self.posm
        sel = self.tile([128, 32, 512], BF16, "sel", nsub=32)
        selc = self.tile([128, 2, 32], BF16, "selc")
        xsT = self.tile([128, 8, 544], BF16, "xsT")
        hact = self.tile([128, 16, 544], BF16, "hact")
        wgb = [self.tile([128, 8, 256], BF16, "wg") for _ in range(2)]
        wub = [self.tile([128, 8, 256], BF16, "wu") for _ in range(2)]
        wdb = [self.tile([128, 2, 512], BF16, "wd") for _ in range(3)]
        sa = [self.tile([128, 512], F32, "sa") for _ in range(2)]
        sac = self.tile([128, 32], F32, "sac")
        yes = [self.tile([128, D], BF16, "yes") for _ in range(4)]
        yec = self.tile([32, D], BF16, "yec")
        posbc = [self.tile([128, 1024], F32, "posbc") for _ in range(2)]
        affbc = [self.tile([128, 1024], F32, "affbc") for _ in range(2)]
        posc = self.tile([32, 256], F32, "posc")
        affc = self.tile([32, 256], F32, "affc")
        stg = [self.tile([128, 1024], BF16, "stg") for _ in range(2)]
        nq = 0
        stgc = self.tile([32, 256], BF16, "stgc")
        nw = [0, 0]
        ns = 0
        def build_selT(ex):
            nonlocal nq
            yield
            dma(SP, posc.ap, self.POST.ap[ex:ex + 1, 0:256].to_broadcast([32, 256]), reads=[self.POST.r], writes=[posc.r])
            dma(SP, affc.ap, self.AFFT.ap[ex:ex + 1, 0:256].to_broadcast([32, 256]), reads=[self.AFFT.r], writes=[affc.r])
            op(DVE, lambda e: e.scalar_tensor_tensor(out=stgc.ap, in0=posc.ap, scalar=self.iotacol.ap[0:32, 0:1], in1=affc.ap,
                                                      op0=ALU.is_equal, op1=ALU.mult), reads=[posc.r, affc.r, self.iotacol.r], writes=[stgc.r])
            dma(SP, self.SELTC.ap[ex], stgc.ap, reads=[stgc.r], writes=[self.SELTC.r])
            for qd in range(4):
                yield
                pb_, ab_ = posbc[nq % 2], affbc[nq % 2]
                nq += 1
                t0 = 256 + qd * 1024
                dma(SP, pb_.ap, self.POST.ap[ex:ex + 1, t0:t0 + 1024].to_broadcast([128, 1024]), reads=[self.POST.r], writes=[pb_.r])
                dma(SP, ab_.ap, self.AFFT.ap[ex:ex + 1, t0:t0 + 1024].to_broadcast([128, 1024]), reads=[self.AFFT.r], writes=[ab_.r])
                for c in range(4):
                    sg = stg[c % 2]
                    ecx = ex * 4 + c
                    op(DVE, lambda e, sg=sg, c=c, pb_=pb_, ab_=ab_: e.scalar_tensor_tensor(
                        out=sg.ap, in0=pb_.ap, scalar=self.iotacol.ap[:, c:c + 1], in1=ab_.ap, op0=ALU.is_equal, op1=ALU.mult),
                       reads=[pb_.r, ab_.r, self.iotacol.r], writes=[sg.r])
                    dma(SP, self.SELT.ap[qd * 8:(qd + 1) * 8, :, ecx, :].rearrange("i s t -> s i t"), sg.ap.rearrange("s (i t) -> s i t", t=128),
                        reads=[sg.r], writes=self.SELT.sub[qd * 8:(qd + 1) * 8])


        for ex in range(NE):
            for i in range(2, NT):
                eng = DVE if i % 2 == 0 else POOL
                op(DVE, lambda e, i=i, ex=ex: e.tensor_scalar(out=sel.ap[:, i - 2, :], in0=self.iota512.ap, scalar1=posm.ap[:, i, ex:ex + 1], scalar2=None,
                                                              op0=ALU.is_equal), reads=[self.iota512.r, posm.r], writes=[sel.sub[i - 2]])
            for i in range(2):
                op(DVE, lambda e, i=i, ex=ex: e.tensor_scalar(out=selc.ap[:, i, :], in0=self.iota512.ap[:, 0:32], scalar1=posm.ap[:, i, ex:ex + 1], scalar2=None,
                                                              op0=ALU.is_equal), reads=[self.iota512.r, posm.r], writes=[selc.r])
            for k in range(8):
                ps = self.psum()
                ks = slice(k * 128, (k + 1) * 128)
                for i in range(2, NT):
                    op(PE, lambda e, ps=ps, i=i, ks=ks: e.matmul(ps.ap, lhsT=h2tok.ap[:, i, ks], rhs=sel.ap[:, i - 2, :], start=(i == 2), stop=(i == NT - 1)),
                       reads=[h2tok.sub[i], sel.sub[i - 2]], writes=[ps.r])
                op(ACT, lambda e, ps=ps, k=k: e.copy(out=xsT.ap[:, k, 0:512], in_=ps.ap), reads=[ps.r], writes=[xsT.r])
                ps2 = self.psum()
                for i in range(2):
                    op(PE, lambda e, ps2=ps2, i=i, ks=ks: e.matmul(ps2.ap[:, 0:32], lhsT=h2tok.ap[:, i, ks], rhs=selc.ap[:, i, :], start=(i == 0), stop=(i == 1)),
                       reads=[h2tok.sub[i], selc.r], writes=[ps2.r])
                op(ACT, lambda e, ps2=ps2, k=k: e.copy(out=xsT.ap[:, k, 512:544], in_=ps2.ap[:, 0:32]), reads=[ps2.r], writes=[xsT.r])
            gen_ = build_selT(ex - 1) if ex > 0 else iter(())
            next(gen_, None)
            for q in range(8):
                next(gen_, None)
                wg, wu = wgb[nw[0] % 2], wub[nw[0] % 2]
                nw[0] += 1
                cs = slice(q * 256, (q + 1) * 256)
                if not (self.cfg.get('nowdma') and ex > 0):
                    dma(POOL, wg.ap, self.moe_w_gate[l, ex][:, cs].rearrange("(k p) n -> p k n", p=128), writes=[wg.r])
                    dma(POOL, wu.ap, self.moe_w_up[l, ex][:, cs].rearrange("(k p) n -> p k n", p=128), writes=[wu.r])
                for jl in range(2):
                    j = q * 2 + jl
                    js = slice(jl * 128, (jl + 1) * 128)
                    ps_a, ps_u, ps_c = self.psum(), self.psum(), self.psum()
                    for (w, ps) in ((wg, ps_a), (wu, ps_u)):
                        for k in range(8):
                            op(PE, lambda e, w=w, ps=ps, k=k, js=js: e.matmul(ps.ap, lhsT=w.ap[:, k, js], rhs=xsT.ap[:, k, 0:512], start=(k == 0), stop=(k == 7)),
                               reads=[w.r, xsT.r], writes=[ps.r])
                    for (w, c0) in ((wg, 0), (wu, 32)):
                        for k in range(8):
                            op(PE, lambda e, w=w, c0=c0, k=k, js=js, ps_c=ps_c: e.matmul(ps_c.ap[:, c0:c0 + 32], lhsT=w.ap[:, k, js], rhs=xsT.ap[:, k, 512:544],
                                                                                         start=(k == 0), stop=(k == 7)),
                               reads=[w.r, xsT.r], writes=[ps_c.r])
                    s = sa[ns % 2]
                    ns += 1
                    op(ACT, lambda e, s=s, ps_a=ps_a: e.activation(out=s.ap, in_=ps_a.ap, func=AF.Silu), reads=[ps_a.r], writes=[s.r])
                    op(DVE, lambda e, s=s, ps_u=ps_u, j=j: e.tensor_tensor(out=hact.ap[:, j, 0:512], in0=s.ap, in1=ps_u.ap, op=ALU.mult),
                       reads=[s.r, ps_u.r], writes=[hact.r])
                    op(ACT, lambda e, ps_c=ps_c: e.activation(out=sac.ap, in_=ps_c.ap[:, 0:32], func=AF.Silu), reads=[ps_c.r], writes=[sac.r])
                    op(DVE, lambda e, ps_c=ps_c, j=j: e.tensor_tensor(out=hact.ap[:, j, 512:544], in0=sac.ap, in1=ps_c.ap[:, 32:64], op=ALU.mult),
                       reads=[sac.r, ps_c.r], writes=[hact.r])
            for _ in gen_:
                pass
            for half in range(2):
                hs = slice(half * 512, (half + 1) * 512)
                psd = [self.psum() for _ in range(5)]
                for jj in range(8):
                    wd = wdb[nw[1] % 3]
                    nw[1] += 1
                    if not (self.cfg.get('nowdma') and ex > 0):
                        dma(POOL, wd.ap, self.moe_w_down[l, ex][jj * 256:(jj + 1) * 256, hs].rearrange("(a p) n -> p a n", p=128), writes=[wd.r])
                    for a in range(2):
                        j = jj * 2 + a
                        first, last = (j == 0), (j == 15)
                        for c in range(4):
                            op(PE, lambda e, c=c, j=j, a=a, wd=wd, first=first, last=last, p=psd[c]: e.matmul(p.ap, lhsT=hact.ap[:, j, c * 128:(c + 1) * 128], rhs=wd.ap[:, a, :],
                                                                                                                  start=first, stop=last),
                               reads=[hact.r, wd.r], writes=[psd[c].r])
                        op(PE, lambda e, j=j, a=a, wd=wd, first=first, last=last, p=psd[4]: e.matmul(p.ap[0:32, :], lhsT=hact.ap[:, j, 512:544], rhs=wd.ap[:, a, :],
                                                                                                      start=first, stop=last),
                           reads=[hact.r, wd.r], writes=[psd[4].r])
                for c in range(4):
                    op(ACT, lambda e, c=c, p=psd[c], hs=hs: e.copy(out=yes[c].ap[:, hs], in_=p.ap), reads=[psd[c].r], writes=[yes[c].r])
                op(ACT, lambda e, p=psd[4], hs=hs: e.copy(out=yec.ap[:, hs], in_=p.ap[0:32, :]), reads=[psd[4].r], writes=[yec.r])
            for c in range(4):
                dma(SP, self.YE.ap[ex, c], yes[c].ap, reads=[yes[c].r], writes=[self.YE.sub[ex]])
            dma(SP, self.YEC.ap[ex], yec.ap, reads=[yec.r], writes=[self.YEC.sub[ex]])
        for _ in build_selT(NE - 1):
            pass

    def moe_passB(self, l):
        op, dma = self.op, self.dma
        self.phase_begin()
        last = (l == DEPTH - 1)
        yeall = self.tile([128, 64, D], BF16, "yeall")
        for ex in range(NE):
            dma(SP, yeall.ap[:, ex * 4:(ex + 1) * 4, :], self.YE.ap[ex].rearrange("c s d -> s c d"), reads=[self.YE.sub[ex]], writes=[yeall.r])
        yecall = self.tile([128, 4, D], BF16, "yecall")
        dma(SP, yecall.ap, self.YEC.ap.rearrange("(g e) s d -> (e s) g d", e=4), reads=self.YEC.sub, writes=[yecall.r])
        seltc = self.tile([128, 4, 256], BF16, "seltc")
        dma(SP, seltc.ap, self.SELTC.ap.rearrange("(g e) s t -> (e s) g t", e=4), reads=[self.SELTC.r], writes=[seltc.r])
        mods = self.mod_tiles(l, [5])
        g1 = self.ln_vec(self.ln_g[l, 1:2, :])
        b1 = self.ln_vec(self.ln_b[l, 1:2, :])
        selt = [self.tile([128, 32, 128], BF16, "selt") for _ in range(3)]
        xb = [self.tile([128, D], F32, "xb") for _ in range(2)]
        yb = [self.tile([128, D], F32, "yb") for _ in range(2)]
        st = self.tile([128, 2, 6], F32, "st")
        mv = self.tile([128, 4], F32, "mv")
        nsl = 0
        for i in range(NT):
            c = 1 if i < 2 else 0
            rows = slice(i * 128, (i + 1) * 128)
            x, y = xb[i % 2], yb[i % 2]
            dma(SP, x.ap, self.XS.ap[rows, :], reads=[self.XS.sub[i]], writes=[x.r])
            m5 = mods[5][c]
            pss = [self.psum(), self.psum()]
            if c:
                for g in range(4):
                    for half in range(2):
                        hs = slice(half * 512, (half + 1) * 512)
                        op(PE, lambda e, ps=pss[half], g=g, hs=hs, i=i: e.matmul(ps.ap, lhsT=seltc.ap[:, g, i * 128:(i + 1) * 128], rhs=yecall.ap[:, g, hs],
                                                                                 start=(g == 0), stop=(g == 3)),
                           reads=[seltc.r, yecall.r], writes=[pss[half].r])
            else:
                for hh in range(2):
                    sl = selt[nsl % 3]
                    nsl += 1
                    dma(SP, sl.ap, self.SELT.ap[i - 2, :, hh * 32:(hh + 1) * 32, :], reads=[self.SELT.sub[i - 2]], writes=[sl.r])
                    for b in range(32):
                        ec = hh * 32 + b
                        for half in range(2):
                            hs = slice(half * 512, (half + 1) * 512)
                            op(PE, lambda e, ps=pss[half], sl=sl, b=b, ec=ec, hs=hs: e.matmul(ps.ap, lhsT=sl.ap[:, b, :], rhs=yeall.ap[:, ec, hs],
                                                                                              start=(ec == 0), stop=(ec == 63)),
                               reads=[sl.r, yeall.r], writes=[pss[half].r])
            for half in range(2):
                hs = slice(half * 512, (half + 1) * 512)
                op(DVE, lambda e, ps=pss[half], y=y, m5=m5, hs=hs: e.tensor_tensor(out=y.ap[:, hs], in0=ps.ap, in1=m5.ap[:, hs], op=ALU.mult),
                   reads=[pss[half].r, m5.r], writes=[y.r])
            op(DVE, lambda e, x=x, y=y: e.scalar_tensor_tensor(out=y.ap, in0=x.ap, scalar=ALPHA, in1=y.ap, op0=ALU.mult, op1=ALU.add),
               reads=[x.r, y.r], writes=[y.r])
            self.layernorm_(y, st, mv)
            op(POOL, lambda e, y=y, x=x: e.tensor_tensor(out=x.ap, in0=y.ap, in1=g1.ap, op=ALU.mult), reads=[y.r, g1.r], writes=[x.r])
            op(POOL, lambda e, x=x: e.tensor_tensor(out=x.ap, in0=x.ap, in1=b1.ap, op=ALU.add), reads=[x.r, b1.r], writes=[x.r])
            dma(POOL, self.XS.ap[rows, :], x.ap, reads=[x.r], writes=[self.XS.sub[i]])
            if last and not c:
                dma(POOL, self.out[(i - 2) * 128:(i - 1) * 128, :], x.ap, reads=[x.r], writes=[self.outr])
            if self.cfg.get("test"):
                dma(POOL, self.dbg.ap[rows, :], x.ap, reads=[x.r], writes=[self.dbg.sub[i]])

    def finish(self):
        rs = [self.outr]
        if self.cfg.get("test"):
            rs = rs + [self.dbg.r] + self.dbg.sub
        self.P.barrier()
        self.op(SP, lambda e: e.nop(), reads=rs)
        self.P.emit()


def Tl_view(t):
    return t


def build(nc, cfg):
    k = K(nc, cfg)
    k.declare_io()
    k.setup()
    tk = cfg.get("test")
    if tk in ("mix", "gdn1", "gdn2"):
        l = cfg["layer"]
        k.premix(l)
        if l % 2 == 0:
            k.natten(l)
        else:
            k.gdn(l)
    if tk == "post":
        l = cfg["layer"]
        k.postmix(l)
        k.moe_select(l)
        k.moe_passA(l)
        k.moe_passB(l)
    if not tk:
        for l in range(cfg.get("depth", DEPTH)):
            k.premix(l)
            if l % 2 == 0:
                k.natten(l)
            else:
                k.gdn(l)
            k.postmix(l)
            k.moe_select(l)
            k.moe_passA(l)
            k.moe_passB(l)
    k.finish()
    return k


WNAMES = ["c_ctx", "ada_w", "ada_b", "ln_g", "ln_b", "na_w_qkv", "na_w_o", "na_rpb", "gdn_w_in", "gdn_conv_w", "gdn_a_log",
          "gdn_dt_bias", "gdn_norm_w", "gdn_w_o", "moe_w_router", "moe_w_gate", "moe_w_up", "moe_w_down"]


def make_in_maps(inputs, cores):
    maps = []
    shared = {}
    for n in WNAMES:
        a = np.ascontiguousarray(inputs[n], dtype=np.float32)
        if n == "c_ctx":
            a = a.reshape(1, D)
        shared[n] = a
    rp = np.zeros((2, 16, 15, 128), np.float32)
    rp[..., 48:79] = np.asarray(inputs["na_rpb"], np.float32)[..., ::-1]
    shared["rpb_pad"] = rp
    qc = np.arange(64)
    cs = np.clip(qc - 8, 0, 48)
    kc = np.arange(64)[:, None]
    cmv = np.where((kc >= cs[None, :]) & (kc < cs[None, :] + 16), 0.0, -1e30).astype(np.float32)
    shared["cmask"] = np.tile(cmv, (2, 2))
    pp = np.arange(128)
    bd = lambda b: (pp[:, None] // b == pp[None, :] // b).astype(np.float32)
    shared["gmask"] = np.stack([bd(16), bd(32) - bd(16), bd(64) - bd(32), 1.0 - bd(64)]).astype(np.float32)
    for b in cores:
        m = dict(shared)
        m["x"] = np.ascontiguousarray(inputs["x"][b], dtype=np.float32)
        m["c"] = np.ascontiguousarray(inputs["c"][b:b + 1], dtype=np.float32)
        m["ctx"] = np.ascontiguousarray(inputs["ctx"][b], dtype=np.float32)
        maps.append(m)
    return maps


def kernel(**inputs):
    nc = bass.Bass("TRN2", target_bir_lowering=False)
    build(nc, {})
    maps = make_in_maps(inputs, list(range(8)))
    res = run_bass_kernel_spmd(nc, maps, core_ids=list(range(8)))
    return np.stack([np.asarray(r["out"], dtype=np.float32) for r in res.results], axis=0)
```

```python
import contextlib
import numpy as np
import concourse.bass as bass
import concourse.mybir as mybir
from concourse.bass_utils import run_bass_kernel_spmd

F32 = mybir.dt.float32
BF16 = mybir.dt.bfloat16
I32 = mybir.dt.int32
U8 = mybir.dt.uint8
AF = mybir.ActivationFunctionType
ALU = mybir.AluOpType
AX = mybir.AxisListType

PE, ACT, DVE, POOL, SP = "tensor", "scalar", "vector", "gpsimd", "sync"
ENGS = [PE, ACT, DVE, POOL, SP]
N_DMA_SEMS = 20
SEM_EPOCH = 20000

D = 1024
NT = 34
TT = NT * 128
NE = 16
DEPTH = 4
ALPHA = (2.0 * DEPTH) ** 0.25
LN_EPS = 1e-6
DSZ = {F32: 4, BF16: 2, I32: 4, U8: 1}


class Res:
    __slots__ = ("name", "last_w", "readers", "excl")

    def __init__(self, name=""):
        self.name = name
        self.last_w = None
        self.readers = []
        self.excl = False


class GRes:
    def __init__(self, name=""):
        self.subs = [Res(name + "a"), Res(name + "b")]


def _grp(fn):
    d = fn.__defaults__
    if not d:
        return None
    c = fn.__code__
    names = c.co_varnames[:c.co_argcount]
    m = dict(zip(names[len(names) - len(d):], d))
    if isinstance(m.get("j"), int):
        return m["j"]
    if isinstance(m.get("h"), int):
        return m["h"] // 4
    return None


def _expand(rs, g):
    out = []
    for r in rs:
        if isinstance(r, GRes):
            out.extend(r.subs if g is None else [r.subs[g]])
        else:
            out.append(r)
    return out


class Op:
    __slots__ = ("eng", "fn", "deps", "is_dma", "sem", "val", "sig")

    def __init__(self, eng, fn, is_dma):
        self.eng = eng
        self.fn = fn
        self.deps = ()
        self.is_dma = is_dma
        self.sem = None
        self.val = None
        self.sig = False


class Prog:
    def __init__(self, nc):
        self.nc = nc
        self.q = {e: [] for e in ENGS}
        self.dmas_since_barrier = []
        self.nops = 0

    def _add(self, eng, fn, reads, writes, is_dma):
        if any(isinstance(r, GRes) for r in reads) or any(isinstance(r, GRes) for r in writes):
            g = _grp(fn)
            reads, writes = _expand(reads, g), _expand(writes, g)
        op = Op(eng, fn, is_dma)
        deps = set()
        for r in reads:
            if r.last_w is not None:
                deps.add(r.last_w)
            if r.excl:
                deps.update(x for x in r.readers if x.eng != eng)
        for w in writes:
            if w.last_w is not None:
                deps.add(w.last_w)
            deps.update(w.readers)
        op.deps = tuple(deps)
        for r in reads:
            r.readers.append(op)
        for w in writes:
            w.last_w = op
            w.readers = []
        self.q[eng].append(op)
        self.nops += 1
        if is_dma:
            self.dmas_since_barrier.append(op)
        return op

    def op(self, eng, fn, reads=(), writes=()):
        return self._add(eng, fn, reads, writes, False)

    def dma(self, eng, out, in_, reads=(), writes=(), **kw):
        return self._add(eng, lambda e: e.dma_start(out=out, in_=in_, **kw), reads, writes, True)

    def barrier(self):
        b = Op(SP, lambda e: e.nop(), False)
        deps = set(self.dmas_since_barrier)
        for e in ENGS:
            if self.q[e]:
                deps.add(self.q[e][-1])
        b.deps = tuple(deps)
        self.q[SP].append(b)
        self.dmas_since_barrier = []
        for e in ENGS:
            if e == SP:
                continue
            o = Op(e, lambda en: en.nop(), False)
            o.deps = (b,)
            self.q[e].append(o)

    def emit(self):
        nc = self.nc
        for e in ENGS:
            for op in self.q[e]:
                for d in op.deps:
                    if d.is_dma or d.eng != op.eng or op.is_dma or d.eng != PE:
                        d.sig = True
        with contextlib.ExitStack() as st:
            nsig = {e: sum(1 for o in self.q[e] if o.sig and not o.is_dma) for e in ENGS}
            csem = {e: [st.enter_context(nc.semaphore("cs_%s_%d" % (e, i)))
                        for i in range(nsig[e] // SEM_EPOCH + 1)] for e in ENGS}
            dsem = {e: [st.enter_context(nc.semaphore("ds_%s_%d" % (e, i))) for i in range(N_DMA_SEMS)]
                    for e in (SP, ACT, POOL)}
            for e in ENGS:
                cnt = 0
                dcnt = 0
                for op in self.q[e]:
                    if op.is_dma:
                        op.sem = dsem[e][dcnt % N_DMA_SEMS]
                        op.val = 16 * (dcnt // N_DMA_SEMS + 1)
                        op.sig = True
                        dcnt += 1
                    elif op.sig:
                        op.sem = csem[e][cnt // SEM_EPOCH]
                        op.val = cnt % SEM_EPOCH + 1
                        cnt += 1
            block = st.enter_context(nc.Block())

            def gen(e):
                def body(eng):
                    waited = {}
                    for op in self.q[e]:
                        needs = {}
                        for d in op.deps:
                            if not d.sig:
                                continue
                            if (not d.is_dma) and d.eng == e and e == PE and not op.is_dma:
                                continue
                            k = id(d.sem)
                            if needs.get(k, (None, 0))[1] < d.val:
                                needs[k] = (d.sem, d.val)
                        if op.is_dma and op.val > 16:
                            k = id(op.sem)
                            if needs.get(k, (None, 0))[1] < op.val - 16:
                                needs[k] = (op.sem, op.val - 16)
                        for k, (s, v) in needs.items():
                            if waited.get(k, 0) >= v:
                                continue
                            eng.wait_ge(s, v)
                            waited[k] = v
                        ins = op.fn(eng)
                        if op.sig:
                            ins.then_inc(op.sem, 16 if op.is_dma else 1)
                return body

            for e in ENGS:
                if self.q[e]:
                    getattr(block, e)(gen(e))


class Tl:
    def __init__(self, ap, name="", nsub=0):
        self.ap = ap
        self.r = Res(name)
        self.sub = [Res(name + str(i)) for i in range(nsub)]


ARENA = 206000


class K:
    def __init__(self, nc, cfg):
        self.nc = nc
        self.cfg = cfg
        self.P = Prog(nc)
        global ARENA
        ARENA = (int(nc.sbuf_bytes_remaining) - 256) // 64 * 64
        self.big = nc.alloc_sbuf_tensor("arena", [128, ARENA], U8)
        self.off = 0
        self.ps = []
        for i in range(8):
            t = nc.alloc_psum_tensor("psb%d" % i, [128, 512], F32)
            self.ps.append(Tl(t[:], "ps%d" % i, nsub=4))
            for r_ in [self.ps[-1].r] + self.ps[-1].sub:
                r_.excl = True
        self.psn = 0
        self.uid = 0

    def tile(self, shape, dt, name="t", nsub=0):
        n = int(np.prod(shape[1:])) * DSZ[dt]
        if self.off + n > ARENA:
            raise RuntimeError("SBUF arena overflow at %s: %d + %d" % (name, self.off, n))
        a = self.big[0:shape[0], self.off:self.off + n].bitcast(dt)
        self.off += (n + 63) // 64 * 64
        if len(shape) == 3:
            a = a.rearrange("p (a b) -> p a b", a=shape[1])
        elif len(shape) == 4:
            a = a.rearrange("p (a b c) -> p a b c", a=shape[1], b=shape[2])
        self.uid += 1
        return Tl(a, "%s_%d" % (name, self.uid), nsub)

    def psum(self):
        t = self.ps[self.psn % 8]
        self.psn += 1
        return t

    def dram(self, name, shape, dt, kind="Internal"):
        return self.nc.dram_tensor(name, list(shape), dt, kind=kind).ap()

    def op(self, eng, fn, reads=(), writes=()):
        return self.P.op(eng, fn, reads, writes)

    def dma(self, eng, out, in_, reads=(), writes=(), **kw):
        return self.P.dma(eng, out, in_, reads, writes, **kw)

    def declare_io(self):
        nc, cfg = self.nc, self.cfg
        ein = lambda n, s: self.dram(n, s, F32, kind="ExternalInput")
        self.x_in = ein("x", [4096, D])
        self.c_in = ein("c", [1, D])
        self.ctx_in = ein("ctx", [256, D])
        self.cc_in = ein("c_ctx", [1, D])
        self.ada_w = ein("ada_w", [DEPTH, D, 6 * D])
        self.ada_b = ein("ada_b", [DEPTH, 6 * D])
        self.ln_g = ein("ln_g", [DEPTH, 2, D])
        self.ln_b = ein("ln_b", [DEPTH, 2, D])
        self.na_w_qkv = ein("na_w_qkv", [2, D, 3 * D])
        self.na_w_o = ein("na_w_o", [2, D, D])
        self.na_rpb = ein("na_rpb", [2, 16, 15, 31])
        self.gdn_w_in = ein("gdn_w_in", [2, D, 4 * D + 32])
        self.gdn_conv_w = ein("gdn_conv_w", [2, 5, 3 * D])
        self.gdn_a_log = ein("gdn_a_log", [2, 2, 8])
        self.gdn_dt_bias = ein("gdn_dt_bias", [2, 2, 8])
        self.gdn_norm_w = ein("gdn_norm_w", [2, 128])
        self.gdn_w_o = ein("gdn_w_o", [2, D, D])
        if cfg.get("test") not in ("mix", "gdn1", "gdn2"):
            self.moe_w_router = ein("moe_w_router", [DEPTH, D, NE])
            self.moe_w_gate = ein("moe_w_gate", [DEPTH, NE, D, 2048])
            self.moe_w_up = ein("moe_w_up", [DEPTH, NE, D, 2048])
            self.moe_w_down = ein("moe_w_down", [DEPTH, NE, 2048, D])
        if cfg.get("test") in ("gdn1", "gdn2"):
            self.dbg2 = self.dram("dbg2", [4, D, TT], F32, kind="ExternalOutput")
            self.dbg3 = self.dram("dbg3", [128, 2, NT, 16], F32, kind="ExternalOutput")
        self.rpb_pad = ein("rpb_pad", [2, 16, 15, 128])
        self.cmask_in = ein("cmask", [128, 128])
        self.gmask_in = ein("gmask", [4, 128, 128])
        self.out = self.dram("out", [4096, D], F32, kind="ExternalOutput")
        tk = cfg.get("test")
        self.XS = Tl(self.dram("XS", [TT, D], F32), "XS", NT)
        self.Y = Tl(self.dram("Y", [TT, D], F32), "Y", NT)
        if tk:
            self.xs_in = self.dram("xs_in", [TT, D], F32, kind="ExternalInput")
            self.y_in = self.dram("y_in", [TT, D], F32, kind="ExternalInput")
        if tk:
            self.dbg = Tl(self.dram("dbg", [TT, D], F32, kind="ExternalOutput"), "dbg", NT)
        self.AFFT = Tl(self.dram("AFFT", [NE, TT], F32), "AFFT")
        self.POST = Tl(self.dram("POST", [NE, TT], F32), "POST")
        self.YE = Tl(self.dram("YE", [NE, 4, 128, D], BF16), "YE", NE)
        self.YEC = Tl(self.dram("YEC", [NE, 32, D], BF16), "YEC", NE)
        self.SELT = Tl(self.dram("SELT", [32, 128, 64, 128], BF16), "SELT", 32)
        self.SELTC = Tl(self.dram("SELTC", [NE, 32, 256], BF16), "SELTC")
        self.outr = Res("out")

    def setup(self):
        op, dma = self.op, self.dma
        self.ident = self.tile([128, 128], BF16, "ident")
        self.ident32 = self.tile([128, 128], F32, "ident32")
        self.ones = self.tile([128, 128], BF16, "ones")
        self.triu = self.tile([128, 128], BF16, "triu")
        tmp = self.tile([128, 128], F32, "tmp")
        self.iota512 = self.tile([128, 512], F32, "iota512")
        self.iotacol = self.tile([128, 4], F32, "iotacol")
        i32, idb, on, tu, io, ic = self.ident32, self.ident, self.ones, self.triu, self.iota512, self.iotacol
        op(POOL, lambda e: e.memset(i32.ap, 1.0), writes=[i32.r])
        op(POOL, lambda e: e.affine_select(out=i32.ap, in_=i32.ap, pattern=[[-1, 128]], compare_op=ALU.is_equal,
                                           fill=0.0, base=0, channel_multiplier=1), reads=[i32.r], writes=[i32.r])
        op(DVE, lambda e: e.tensor_copy(out=idb.ap, in_=i32.ap), reads=[i32.r], writes=[idb.r])
        op(DVE, lambda e: e.memset(on.ap, 1.0), writes=[on.r])
        op(POOL, lambda e: e.memset(tmp.ap, 1.0), writes=[tmp.r])
        op(POOL, lambda e: e.affine_select(out=tmp.ap, in_=tmp.ap, pattern=[[1, 128]], compare_op=ALU.is_ge,
                                           fill=0.0, base=0, channel_multiplier=-1), reads=[tmp.r], writes=[tmp.r])
        op(DVE, lambda e: e.tensor_copy(out=tu.ap, in_=tmp.ap), reads=[tmp.r], writes=[tu.r])
        op(POOL, lambda e: e.iota(io.ap, pattern=[[1, 512]], base=0, channel_multiplier=0,
                                  allow_small_or_imprecise_dtypes=True), writes=[io.r])
        op(POOL, lambda e: e.iota(ic.ap, pattern=[[128, 4]], base=0, channel_multiplier=1,
                                  allow_small_or_imprecise_dtypes=True), writes=[ic.r])
        self.scb = self.tile([128, 8, 128], F32, "scb")
        self.sccb = self.tile([128, 8, 128], F32, "sccb")
        for src, dst in ((self.c_in, self.scb), (self.cc_in, self.sccb)):
            cv = self.tile([128, 8], F32, "cv")
            dma(SP, cv.ap, src.rearrange("o (k p) -> p (o k)", p=128), writes=[cv.r], allow_slow_non_contiguous=True)
            op(ACT, lambda e, cv=cv: e.activation(out=cv.ap, in_=cv.ap, func=AF.Silu), reads=[cv.r], writes=[cv.r])
            op(DVE, lambda e, cv=cv, dst=dst: e.tensor_copy(out=dst.ap, in_=cv.ap.unsqueeze(2).to_broadcast([128, 8, 128])),
               reads=[cv.r], writes=[dst.r])
        self.base_off = self.off
        if not self.cfg.get("test"):
            dma(SP, self.XS.ap[0:256, :], self.ctx_in, writes=self.XS.sub[0:2])
            dma(SP, self.XS.ap[256:TT, :], self.x_in, writes=self.XS.sub[2:NT])
        else:
            dma(SP, self.XS.ap, self.xs_in, writes=self.XS.sub)
            dma(SP, self.Y.ap, self.y_in, writes=self.Y.sub)

    def phase_begin(self):
        self.P.barrier()
        self.off = self.base_off

    def mod_tiles(self, l, idxs, plus1=()):
        op, dma = self.op, self.dma
        res = {}
        for idx in idxs:
            res[idx] = (self.tile([128, D], F32, "modl"), self.tile([128, D], F32, "modc"))
        mark = self.off
        wbuf = [self.tile([128, 8, 512], F32, "adaw") for _ in range(2)]
        bbuf = [self.tile([128, 1024], F32, "adab") for _ in range(2)]
        n = 0
        for ii, idx in enumerate(idxs):
            lat, ctx = res[idx]
            bb = bbuf[ii % 2]
            dma(SP, bb.ap, self.ada_b[l:l + 1, idx * D:(idx + 1) * D].to_broadcast([128, D]), writes=[bb.r])
            for half in range(2):
                wb = wbuf[n % 2]
                n += 1
                c0 = idx * D + half * 512
                dma(SP, wb.ap, self.ada_w[l, :, c0:c0 + 512].rearrange("(k p) n -> p k n", p=128), writes=[wb.r])
                for lhs, dst in ((self.scb, lat), (self.sccb, ctx)):
                    ps = self.psum()
                    for k in range(8):
                        op(PE, lambda e, ps=ps, lhs=lhs, wb=wb, k=k: e.matmul(ps.ap, lhsT=lhs.ap[:, k, :], rhs=wb.ap[:, k, :],
                                                                              start=(k == 0), stop=(k == 7)),
                           reads=[lhs.r, wb.r], writes=[ps.r])
                    sl = slice(half * 512, (half + 1) * 512)
                    op(DVE, lambda e, ps=ps, dst=dst, bb=bb, sl=sl: e.tensor_tensor(out=dst.ap[:, sl], in0=ps.ap, in1=bb.ap[:, sl], op=ALU.add),
                       reads=[ps.r, bb.r], writes=[dst.r])
            if idx in plus1:
                for t in (lat, ctx):
                    op(DVE, lambda e, t=t: e.tensor_scalar(out=t.ap, in0=t.ap, scalar1=1.0, scalar2=None, op0=ALU.add),
                       reads=[t.r], writes=[t.r])
        self.P.barrier()
        self.off = mark
        return res

    def ln_vec(self, src_row):
        t = self.tile([128, D], F32, "lnv")
        self.dma(SP, t.ap, src_row.to_broadcast([128, D]), writes=[t.r])
        return t

    def layernorm_(self, r, st, mv):
        op = self.op
        for h in range(2):
            op(DVE, lambda e, h=h: e.bn_stats(out=st.ap[:, h, :], in_=r.ap[:, h * 512:(h + 1) * 512]), reads=[r.r], writes=[st.r])
        op(DVE, lambda e: e.bn_aggr(out=mv.ap[:, 0:2], in_=st.ap.rearrange("p a b -> p (a b)")), reads=[st.r], writes=[mv.r])
        op(DVE, lambda e: e.tensor_scalar(out=mv.ap[:, 2:3], in0=mv.ap[:, 1:2], scalar1=LN_EPS, scalar2=None, op0=ALU.add),
           reads=[mv.r], writes=[mv.r])
        op(ACT, lambda e: e.activation(out=mv.ap[:, 2:3], in_=mv.ap[:, 2:3], func=AF.Ln), reads=[mv.r], writes=[mv.r])
        op(ACT, lambda e: e.activation(out=mv.ap[:, 2:3], in_=mv.ap[:, 2:3], func=AF.Exp, scale=-0.5), reads=[mv.r], writes=[mv.r])
        op(DVE, lambda e: e.tensor_scalar(out=r.ap, in0=r.ap, scalar1=mv.ap[:, 0:1], scalar2=mv.ap[:, 2:3],
                                          op0=ALU.subtract, op1=ALU.mult), reads=[r.r, mv.r], writes=[r.r])

    def premix(self, l):
        op, dma = self.op, self.dma
        self.phase_begin()
        hT = self.tile([128, 8, TT], BF16, "hT", nsub=NT)
        self.hT = hT
        self.mix_keep = self.off
        mods = self.mod_tiles(l, [0, 1], plus1=(1,))
        xb = [self.tile([128, D], F32, "xb") for _ in range(3)]
        hb = [self.tile([128, D], BF16, "hb") for _ in range(2)]
        for i in range(NT):
            x = xb[i % 3]
            h = hb[i % 2]
            c = 1 if i < 2 else 0
            dma(SP, x.ap, self.XS.ap[i * 128:(i + 1) * 128, :], reads=[self.XS.sub[i]], writes=[x.r])
            sc, sh = mods[1][c], mods[0][c]
            op(DVE, lambda e, x=x, sc=sc: e.tensor_tensor(out=x.ap, in0=x.ap, in1=sc.ap, op=ALU.mult), reads=[x.r, sc.r], writes=[x.r])
            op(POOL, lambda e, x=x, sh=sh, h=h: e.tensor_tensor(out=h.ap, in0=x.ap, in1=sh.ap, op=ALU.add), reads=[x.r, sh.r], writes=[h.r])
            self.transpose_to(h, hT, i)

    def transpose_to(self, h, hT, i, evac=ACT):
        op = self.op
        ps = self.psum()
        pb = ps.ap.bitcast(BF16)
        for k in range(8):
            op(PE, lambda e, k=k, pb=pb, h=h: e.transpose(out=pb[:, k * 128:(k + 1) * 128], in_=h.ap[:, k * 128:(k + 1) * 128],
                                                          identity=self.ident.ap), reads=[h.r, self.ident.r], writes=[ps.r])
        dst = hT.ap[:, :, i * 128:(i + 1) * 128]
        if evac == ACT:
            op(ACT, lambda e, pb=pb, dst=dst: e.copy(out=dst, in_=pb.rearrange("p (k n) -> p k n", k=8)), reads=[ps.r], writes=[hT.sub[i]])
        else:
            op(evac, lambda e, pb=pb, dst=dst: e.tensor_copy(out=dst, in_=pb.rearrange("p (k n) -> p k n", k=8)), reads=[ps.r], writes=[hT.sub[i]])

    def natten(self, l):
        op, dma = self.op, self.dma
        li = l // 2
        hT = self.hT
        self.P.barrier()
        self.off = self.mix_keep
        if not hasattr(self, "QT"):
            self.QT = Tl(self.dram("QT", [D, TT], BF16), "QT")
            self.KT = Tl(self.dram("KT", [D, TT], BF16), "KT")
            self.VX = Tl(self.dram("VX", [TT, 16, 65], BF16), "VX")
            self.OD = Tl(self.dram("OD", [TT, D], BF16), "OD", NT)
        QT, KT, VX, OD = self.QT, self.KT, self.VX, self.OD
        wq = self.na_w_qkv[li]
        wb = [self.tile([128, 8, 512], BF16, "wqk") for _ in range(2)]
        stq = [self.tile([128, 512], BF16, "stq") for _ in range(3)]
        blocks = [(0, 256)] + [(256 + 512 * b, 512) for b in range(8)]
        n = 0
        for cg in range(4):
            w = wb[cg % 2]
            dma(POOL, w.ap, wq[:, cg * 512:(cg + 1) * 512].rearrange("(k p) n -> p k n", p=128), writes=[w.r])
            for cl in range(4):
                ct = cg * 4 + cl
                dst = QT if ct < 8 else KT
                crow = (ct % 8) * 128
                for (t0, nt_) in blocks:
                    ps = self.psum()
                    for k in range(8):
                        op(PE, lambda e, ps=ps, w=w, k=k, cl=cl, t0=t0, nt_=nt_: e.matmul(ps.ap[:, 0:nt_], lhsT=w.ap[:, k, cl * 128:(cl + 1) * 128], rhs=hT.ap[:, k, t0:t0 + nt_],
                                                                                    start=(k == 0), stop=(k == 7)),
                           reads=[w.r] + hT.sub[t0 // 128:(t0 + nt_) // 128], writes=[ps.r])
                    sq = stq[n % 3]
                    n += 1
                    if ct < 8:
                        op(ACT, lambda e, ps=ps, sq=sq, nt_=nt_: e.mul(out=sq.ap[:, 0:nt_], in_=ps.ap[:, 0:nt_], mul=0.125), reads=[ps.r], writes=[sq.r])
                    else:
                        op(DVE, lambda e, ps=ps, sq=sq, nt_=nt_: e.tensor_copy(out=sq.ap[:, 0:nt_], in_=ps.ap[:, 0:nt_]), reads=[ps.r], writes=[sq.r])
                    dma(SP, dst.ap[crow:crow + 128, t0:t0 + nt_], sq.ap[:, 0:nt_], reads=[sq.r], writes=[dst.r])
        wv = self.tile([128, 8, D], BF16, "wv")
        dma(POOL, wv.ap, wq[:, 2 * D:3 * D].rearrange("(k p) n -> p k n", p=128), writes=[wv.r])
        vst = [self.tile([128, 16, 65], BF16, "vst") for _ in range(2)]
        for v in vst:
            op(DVE, lambda e, v=v: e.memset(v.ap, 1.0), writes=[v.r])
        for i in range(NT):
            v = vst[i % 2]
            for half in range(2):
                ps = self.psum()
                for k in range(8):
                    op(PE, lambda e, ps=ps, k=k, i=i, half=half: e.matmul(ps.ap, lhsT=hT.ap[:, k, i * 128:(i + 1) * 128], rhs=wv.ap[:, k, half * 512:(half + 1) * 512],
                                                                       start=(k == 0), stop=(k == 7)),
                       reads=[wv.r, hT.sub[i]], writes=[ps.r])
                op(ACT, lambda e, ps=ps, v=v, half=half: e.copy(out=v.ap[:, half * 8:(half + 1) * 8, 0:64], in_=ps.ap.rearrange("p (h d) -> p h d", d=64)),
                   reads=[ps.r], writes=[v.r])
            dma(SP, VX.ap[i * 128:(i + 1) * 128], v.ap, reads=[v.r], writes=[VX.r])
        self.P.barrier()
        self.off = self.mix_keep - 0
        self.off = self.base_off
        rs_ = np.clip(np.arange(64) - 4, 0, 56)
        tiles, tindex, per_rp = [], {}, []
        for rp in range(32):
            r0 = 2 * rp
            lst = []
            for kp in range(rs_[r0] // 2, (rs_[r0 + 1] + 7) // 2 + 1):
                spec = []
                for i2 in range(2):
                    for j2 in range(2):
                        kr, r = 2 * kp + i2, r0 + j2
                        spec.append(int(kr - r + 7) if rs_[r] <= kr < rs_[r] + 8 else None)
                spec = tuple(spec)
                key = (rp if rp in (0, 1, 30, 31) else -1, spec)
                if key not in tindex:
                    tindex[key] = len(tiles)
                    tiles.append(spec)
                lst.append((kp, tindex[key]))
            assert all(lst[j + 1][1] == lst[j][1] + 1 for j in range(len(lst) - 1))
            per_rp.append(lst)
        NTL = len(tiles)
        jm = self.tile([64, 128], F32, "jm")
        op(POOL, lambda e: e.memset(jm.ap, 1.0), writes=[jm.r])
        op(POOL, lambda e: e.affine_select(out=jm.ap[:, 0:64], in_=jm.ap[:, 0:64], pattern=[[1, 64]], compare_op=ALU.is_equal, fill=0.0, base=-63, channel_multiplier=1),
           reads=[jm.r], writes=[jm.r])
        op(POOL, lambda e: e.affine_select(out=jm.ap[:, 64:128], in_=jm.ap[:, 64:128], pattern=[[1, 64]], compare_op=ALU.is_equal, fill=0.0, base=-63, channel_multiplier=1),
           reads=[jm.r], writes=[jm.r])
        cm = self.tile([128, 128], F32, "cmask")
        dma(SP, cm.ap, self.cmask_in, writes=[cm.r])
        hk = self.tile([64, 15, 64], F32, "hk")
        toe = self.tile([128, 15, 64], F32, "toe")
        bias = self.tile([128, NTL, 128], F32, "bias")
        qt = [self.tile([64, TT], BF16, "qt") for _ in range(2)]
        kt = [self.tile([64, TT], BF16, "kt") for _ in range(2)]
        vh = [self.tile([128, NT, 65], BF16, "vh") for _ in range(2)]
        sb = [self.tile([128, 640], F32, "sb") for _ in range(3)]
        pb = [self.tile([128, 896], BF16, "pb") for _ in range(3)]
        ost = [self.tile([128, NT, 64], BF16, "ost") for _ in range(2)]
        rc = self.tile([128, 4], F32, "rc")
        rpb = self.rpb_pad[li]
        it = 0
        pending = []
        for h in range(16):
            q_, k_, v_, o_ = qt[h % 2], kt[h % 2], vh[h % 2], ost[h % 2]
            dma(SP, q_.ap, QT.ap[h * 64:(h + 1) * 64, :], reads=[QT.r], writes=[q_.r])
            dma(SP, k_.ap, KT.ap[h * 64:(h + 1) * 64, :], reads=[KT.r], writes=[k_.r])
            dma(SP, v_.ap, VX.ap.rearrange("(i p) h d -> p i h d", p=128)[:, :, h, :], reads=[VX.r], writes=[v_.r])
            src = bass.AP(tensor=rpb.tensor, offset=rpb[h].offset, ap=[[1, 64], [128, 15], [1, 64]])
            dma(SP, hk.ap, src, writes=[hk.r])
            for (d0, d1) in ((0, 8), (8, 15)):
                ps = self.psum()
                nn = (d1 - d0) * 64
                op(PE, lambda e, ps=ps, d0=d0, d1=d1, nn=nn: e.matmul(ps.ap[:, 0:nn], lhsT=jm.ap, rhs=hk.ap[:, d0:d1, :].rearrange("p a b -> p (a b)"), start=True, stop=True),
                   reads=[jm.r, hk.r], writes=[ps.r])
                op(ACT, lambda e, ps=ps, d0=d0, d1=d1, nn=nn: e.copy(out=toe.ap[:, d0:d1, :].rearrange("p a b -> p (a b)"), in_=ps.ap[:, 0:nn]), reads=[ps.r], writes=[toe.r])
            for t, spec in enumerate(tiles):
                for bi, dr in enumerate(spec):
                    i2, j2 = bi // 2, bi % 2
                    prt = slice(i2 * 64, (i2 + 1) * 64)
                    fr = slice(j2 * 64, (j2 + 1) * 64)
                    if dr is None:
                        op(POOL, lambda e, t=t, prt=prt, fr=fr: e.memset(bias.ap[prt, t, fr], -1e30), writes=[bias.r])
                    else:
                        op(POOL, lambda e, t=t, prt=prt, fr=fr, dr=dr: e.tensor_tensor(out=bias.ap[prt, t, fr], in0=toe.ap[prt, dr, :], in1=cm.ap[prt, fr], op=ALU.add),
                           reads=[toe.r, cm.r], writes=[bias.r])
            for qi in range(NT):
                if qi < 2:
                    wins = []
                    ctxk = [0, 1]
                else:
                    wins = per_rp[qi - 2]
                    ctxk = [0, 1]
                s_, p_ = sb[it % 3], pb[it % 3]
                it += 1
                nw = len(wins)
                qs = slice(qi * 128, (qi + 1) * 128)
                psA, psB = self.psum(), self.psum()
                slots = []
                for j in range(nw):
                    slots.append((psA, j * 128) if j < 4 else (psB, 0))
                cb = 128 if nw == 5 else 0
                for j, kk in enumerate(ctxk):
                    slots.append((psB, cb + j * 128))
                ktl = [2 + kp for (kp, _) in wins] + ctxk
                for (pst, co), kti in zip(slots, ktl):
                    op(PE, lambda e, pst=pst, co=co, kti=kti, qs=qs, k_=k_, q_=q_: e.matmul(pst.ap[:, co:co + 128], lhsT=k_.ap[:, kti * 128:(kti + 1) * 128], rhs=q_.ap[:, qs],
                                                                                         start=True, stop=True),
                       reads=[k_.r, q_.r], writes=[pst.r])
                if nw:
                    t0 = wins[0][1]
                    na = min(nw, 4)
                    op(DVE, lambda e, s_=s_, t0=t0, na=na, psA=psA: e.tensor_tensor(out=s_.ap[:, 0:na * 128], in0=psA.ap[:, 0:na * 128],
                                                                                   in1=bias.ap[:, t0:t0 + na, :].rearrange("p a b -> p (a b)"), op=ALU.add),
                       reads=[psA.r, bias.r], writes=[s_.r])
                    if nw == 5:
                        op(DVE, lambda e, s_=s_, t0=t0, psB=psB: e.tensor_tensor(out=s_.ap[:, 512:640], in0=psB.ap[:, 0:128], in1=bias.ap[:, t0 + 4, :], op=ALU.add),
                           reads=[psB.r, bias.r], writes=[s_.r])
                    op(ACT, lambda e, s_=s_, p_=p_, nw=nw: e.activation(out=p_.ap[:, 0:nw * 128], in_=s_.ap[:, 0:nw * 128], func=AF.Exp), reads=[s_.r], writes=[p_.r])
                op(ACT, lambda e, p_=p_, nw=nw, cb=cb, psB=psB: e.activation(out=p_.ap[:, nw * 128:nw * 128 + 256], in_=psB.ap[:, cb:cb + 256], func=AF.Exp),
                   reads=[psB.r], writes=[p_.r])
                def stage2(p_=p_, ktl=ktl, v_=v_, o_=o_, qi=qi):
                    pso = self.psum()
                    nk = len(ktl)
                    for j, kti in enumerate(ktl):
                        op(PE, lambda e, pso=pso, p_=p_, j=j, kti=kti, v_=v_, nk=nk: e.matmul(pso.ap[:, 0:65], lhsT=p_.ap[:, j * 128:(j + 1) * 128], rhs=v_.ap[:, kti, :],
                                                                                           start=(j == 0), stop=(j == nk - 1)),
                           reads=[p_.r, v_.r], writes=[pso.r])
                    op(DVE, lambda e, pso=pso: e.reciprocal(out=rc.ap[:, 0:1], in_=pso.ap[:, 64:65]), reads=[pso.r], writes=[rc.r])
                    op(DVE, lambda e, pso=pso, o_=o_, qi=qi: e.tensor_scalar(out=o_.ap[:, qi, :], in0=pso.ap[:, 0:64], scalar1=rc.ap[:, 0:1], scalar2=None, op0=ALU.mult),
                       reads=[pso.r, rc.r], writes=[o_.r])
                pending.append(stage2)
                if len(pending) > 2:
                    pending.pop(0)()
            while pending:
                pending.pop(0)()
            dma(SP, OD.ap.rearrange("(i p) d -> p i d", p=128)[:, :, h * 64:(h + 1) * 64], o_.ap, reads=[o_.r], writes=OD.sub)
        self.out_proj(self.na_w_o[li])

    def gdn(self, l):
        op, dma = self.op, self.dma
        li = l // 2
        hT = self.hT
        self.P.barrier()
        self.off = self.mix_keep
        if not hasattr(self, "QF"):
            for nm in ("QF", "KF", "VF", "ZF", "OF"):
                setattr(self, nm, Tl(self.dram(nm, [D, TT], F32), nm, NT))
        QF, KF, VF, ZF, OF = self.QF, self.KF, self.VF, self.ZF, self.OF
        w_in = self.gdn_w_in[li]
        ones_a = self.tile([128, 128], F32, "ones_a")
        op(DVE, lambda e: e.memset(ones_a.ap, 1.0), writes=[ones_a.r])
        gT_a = self.tile([128, NT, 16], F32, "gT_a")
        bT_a = self.tile([128, NT, 16], F32, "bT_a")
        g_keep = self.off
        cw = self.tile([128, 24, 5], F32, "cw")
        for j in range(5):
            dma(SP, cw.ap[:, :, j], self.gdn_conv_w[li, j:j + 1, :].rearrange("o (c p) -> p (o c)", p=128), writes=[cw.r], allow_slow_non_contiguous=True)
        nwv = self.tile([128, 1], F32, "nwv")
        dma(SP, nwv.ap, self.gdn_norm_w[li:li + 1, :].rearrange("o p -> p o"), writes=[nwv.r], allow_slow_non_contiguous=True)
        pbuf = [self.tile([128, TT + 8], F32, "pbuf") for _ in range(2)]
        cbuf = [self.tile([128, TT], F32, "cbuf") for _ in range(2)]
        for pb in pbuf:
            op(POOL, lambda e, pb=pb: e.memset(pb.ap, 0.0), writes=[pb.r])
        wb = [self.tile([128, 8, 512], BF16, "win") for _ in range(2)]
        sqb = [self.tile([128, 512], F32, "sqb") for _ in range(2)]
        rnb = [self.tile([128, 512], F32, "rnb") for _ in range(2)]
        zsb = [self.tile([128, 512], F32, "zsb") for _ in range(2)]
        blocks = [(0, 256)] + [(256 + 512 * b, 512) for b in range(8)]
        nb = 0
        for cg in range(8):
            w = wb[cg % 2]
            dma(POOL, w.ap, w_in[:, cg * 512:(cg + 1) * 512].rearrange("(k p) n -> p k n", p=128), writes=[w.r])
            for cl in range(4):
                ct = cg * 4 + cl
                pb, cb = pbuf[ct % 2], cbuf[ct % 2]
                for (t0, nt_) in blocks:
                    ps = self.psum()
                    for k in range(8):
                        op(PE, lambda e, ps=ps, w=w, k=k, cl=cl, t0=t0, nt_=nt_: e.matmul(ps.ap[:, 0:nt_], lhsT=w.ap[:, k, cl * 128:(cl + 1) * 128], rhs=hT.ap[:, k, t0:t0 + nt_],
                                                                                    start=(k == 0), stop=(k == 7)),
                           reads=[w.r] + hT.sub[t0 // 128:(t0 + nt_) // 128], writes=ps.sub)
                    if ct < 24:
                        o0 = (2 if t0 == 0 else 6) + t0
                        op(ACT, lambda e, ps=ps, pb=pb, o0=o0, nt_=nt_: e.copy(out=pb.ap[:, o0:o0 + nt_], in_=ps.ap[:, 0:nt_]), reads=ps.sub, writes=[pb.r])
                    else:
                        zs = zsb[nb % 2]
                        nb += 1
                        op(ACT, lambda e, ps=ps, zs=zs, nt_=nt_: e.activation(out=zs.ap[:, 0:nt_], in_=ps.ap[:, 0:nt_], func=AF.Silu), reads=ps.sub, writes=[zs.r])
                        op(DVE, lambda e, zs=zs, nt_=nt_: e.tensor_scalar(out=zs.ap[:, 0:nt_], in0=zs.ap[:, 0:nt_], scalar1=nwv.ap[:, 0:1], scalar2=None, op0=ALU.mult),
                           reads=[zs.r, nwv.r], writes=[zs.r])
                        r0 = (ct - 24) * 128
                        dma(SP, ZF.ap[r0:r0 + 128, t0:t0 + nt_], zs.ap[:, 0:nt_], reads=[zs.r], writes=ZF.sub[t0 // 128:(t0 + nt_) // 128])
                if ct >= 24:
                    continue
                for (c0, ln, base) in ((0, 256, 0), (256, 4096, 260)):
                    op(DVE, lambda e, cb=cb, pb=pb, c0=c0, ln=ln, base=base, ct=ct: e.tensor_scalar(out=cb.ap[:, c0:c0 + ln], in0=pb.ap[:, base:base + ln], scalar1=cw.ap[:, ct, 0:1],
                                                                                             scalar2=None, op0=ALU.mult), reads=[pb.r, cw.r], writes=[cb.r])
                    for j in range(1, 5):
                        op(DVE, lambda e, cb=cb, pb=pb, c0=c0, ln=ln, base=base, ct=ct, j=j: e.scalar_tensor_tensor(out=cb.ap[:, c0:c0 + ln], in0=pb.ap[:, base + j:base + j + ln],
                                                                                                            scalar=cw.ap[:, ct, j:j + 1], in1=cb.ap[:, c0:c0 + ln], op0=ALU.mult, op1=ALU.add),
                           reads=[pb.r, cw.r, cb.r], writes=[cb.r])
                op(ACT, lambda e, cb=cb: e.activation(out=cb.ap, in_=cb.ap, func=AF.Silu), reads=[cb.r], writes=[cb.r])
                if ct < 16:
                    qsc = (128.0 ** -0.5) if ct < 8 else 1.0
                    for (t0, nt_) in blocks:
                        sq, rn = sqb[nb % 2], rnb[nb % 2]
                        nb += 1
                        op(POOL, lambda e, sq=sq, cb=cb, t0=t0, nt_=nt_: e.tensor_tensor(out=sq.ap[:, 0:nt_], in0=cb.ap[:, t0:t0 + nt_], in1=cb.ap[:, t0:t0 + nt_], op=ALU.mult),
                           reads=[cb.r], writes=[sq.r])
                        ps = self.psum()
                        op(PE, lambda e, ps=ps, sq=sq, nt_=nt_, o32=ones_a: e.matmul(ps.ap[:, 0:nt_], lhsT=o32.ap, rhs=sq.ap[:, 0:nt_], start=True, stop=True), reads=[ones_a.r, sq.r], writes=ps.sub)
                        op(DVE, lambda e, ps=ps, rn=rn, nt_=nt_: e.tensor_scalar(out=rn.ap[:, 0:nt_], in0=ps.ap[:, 0:nt_], scalar1=1e-6, scalar2=None, op0=ALU.add), reads=ps.sub, writes=[rn.r])
                        op(ACT, lambda e, rn=rn, nt_=nt_: e.activation(out=rn.ap[:, 0:nt_], in_=rn.ap[:, 0:nt_], func=AF.Ln), reads=[rn.r], writes=[rn.r])
                        op(ACT, lambda e, rn=rn, nt_=nt_: e.activation(out=rn.ap[:, 0:nt_], in_=rn.ap[:, 0:nt_], func=AF.Exp, scale=-0.5), reads=[rn.r], writes=[rn.r])
                        op(DVE, lambda e, rn=rn, cb=cb, t0=t0, nt_=nt_, qsc=qsc: e.scalar_tensor_tensor(out=cb.ap[:, t0:t0 + nt_], in0=cb.ap[:, t0:t0 + nt_], scalar=qsc, in1=rn.ap[:, 0:nt_],
                                                                                                 op0=ALU.mult, op1=ALU.mult), reads=[cb.r, rn.r], writes=[cb.r])
                dst = (QF, KF, VF)[ct // 8]
                r0 = (ct % 8) * 128
                dma(SP, dst.ap[r0:r0 + 128, :], cb.ap, reads=[cb.r], writes=dst.sub)
        wab = self.tile([128, 8, 32], BF16, "wab")
        dma(POOL, wab.ap, w_in[:, 4096:4128].rearrange("(k p) n -> p k n", p=128), writes=[wab.r])
        coef = self.tile([128, 16], F32, "coef")
        dtb = self.tile([128, 16], F32, "dtb")
        dma(SP, coef.ap, self.gdn_a_log[li:li + 1].rearrange("o a b -> o (a b)").to_broadcast([128, 16]), writes=[coef.r])
        dma(SP, dtb.ap, self.gdn_dt_bias[li:li + 1].rearrange("o a b -> o (a b)").to_broadcast([128, 16]), writes=[dtb.r])
        op(ACT, lambda e: e.activation(out=coef.ap, in_=coef.ap, func=AF.Exp), reads=[coef.r], writes=[coef.r])
        op(DVE, lambda e: e.tensor_scalar(out=coef.ap, in0=coef.ap, scalar1=-1.0, scalar2=None, op0=ALU.mult), reads=[coef.r], writes=[coef.r])
        psl = []
        for i in range(NT):
            if i % 16 == 0:
                ps = self.psum()
                psl.append(ps)
            c0 = (i % 16) * 32
            for k in range(8):
                op(PE, lambda e, ps=ps, i=i, k=k, c0=c0: e.matmul(ps.ap[:, c0:c0 + 32], lhsT=hT.ap[:, k, i * 128:(i + 1) * 128], rhs=wab.ap[:, k, :], start=(k == 0), stop=(k == 7)),
                   reads=[wab.r, hT.sub[i]], writes=ps.sub)
        for gi, ps in enumerate(psl):
            n_ = min(16, NT - gi * 16)
            pv = ps.ap[:, 0:n_ * 32].rearrange("p (i c) -> p i c", c=32)
            gs = gT_a.ap[:, gi * 16:gi * 16 + n_, :]
            bs = bT_a.ap[:, gi * 16:gi * 16 + n_, :]
            op(DVE, lambda e, pv=pv, gs=gs, n_=n_: e.tensor_tensor(out=gs, in0=pv[:, :, 0:16], in1=dtb.ap.unsqueeze(1).to_broadcast([128, n_, 16]), op=ALU.add), reads=ps.sub + [dtb.r], writes=[gT_a.r])
            op(ACT, lambda e, gs=gs: e.activation(out=gs, in_=gs, func=AF.Exp), reads=[gT_a.r], writes=[gT_a.r])
            op(DVE, lambda e, gs=gs: e.tensor_scalar(out=gs, in0=gs, scalar1=1.0, scalar2=None, op0=ALU.add), reads=[gT_a.r], writes=[gT_a.r])
            op(ACT, lambda e, gs=gs: e.activation(out=gs, in_=gs, func=AF.Ln), reads=[gT_a.r], writes=[gT_a.r])
            op(DVE, lambda e, gs=gs, n_=n_: e.tensor_tensor(out=gs, in0=gs, in1=coef.ap.unsqueeze(1).to_broadcast([128, n_, 16]), op=ALU.mult), reads=[gT_a.r, coef.r], writes=[gT_a.r])
            op(ACT, lambda e, pv=pv, bs=bs: e.activation(out=bs, in_=pv[:, :, 16:32], func=AF.Exp, scale=-1.0), reads=ps.sub, writes=[bT_a.r])
            op(DVE, lambda e, bs=bs: e.tensor_scalar(out=bs, in0=bs, scalar1=1.0, scalar2=None, op0=ALU.add), reads=[bT_a.r], writes=[bT_a.r])
            op(DVE, lambda e, bs=bs: e.reciprocal(out=bs, in_=bs), reads=[bT_a.r], writes=[bT_a.r])
        self.P.barrier()
        if self.cfg.get("test") == "gdn1":
            rr = Res("dbg2")
            for i_, t_ in enumerate((QF, KF, VF, ZF)):
                dma(SP, self.dbg2[i_], t_.ap, reads=t_.sub, writes=[rr])
            dma(SP, self.dbg3[:, 0], gT_a.ap, reads=[gT_a.r], writes=[rr])
            dma(SP, self.dbg3[:, 1], bT_a.ap, reads=[bT_a.r], writes=[rr])
            self.dbg.sub.append(rr)
            return
        self.off = self.base_off
        gT_o, bT_o = gT_a, bT_a
        ones32 = self.tile([128, 128], F32, "ones32b")
        gT = self.tile([128, NT, 16], F32, "gTb")
        bT = self.tile([128, NT, 16], F32, "bTb")
        op(DVE, lambda e: e.memset(ones32.ap, 1.0), writes=[ones32.r])
        op(DVE, lambda e: e.tensor_copy(out=gT.ap, in_=gT_o.ap), reads=[gT_o.r], writes=[gT.r])
        op(DVE, lambda e: e.tensor_copy(out=bT.ap, in_=bT_o.ap), reads=[bT_o.r], writes=[bT.r])
        self.P.barrier()
        def cmat(name, fill_in, pattern, cmult, cmp, fill):
            t = self.tile([128, 128], F32, name)
            op(POOL, lambda e: e.memset(t.ap, fill_in), writes=[t.r])
            op(POOL, lambda e: e.affine_select(out=t.ap, in_=t.ap, pattern=[[pattern, 128]], compare_op=cmp, fill=fill, base=0, channel_multiplier=cmult), reads=[t.r], writes=[t.r])
            return t
        triu32 = cmat("triu32", 1.0, 1, -1, ALU.is_ge, 0.0)
        tril32 = cmat("tril32", 1.0, -1, 1, ALU.is_ge, 0.0)
        sup = cmat("sup", 1.0, 1, -1, ALU.is_gt, 0.0)
        slo = cmat("slo", 1.0, -1, 1, ALU.is_gt, 0.0)
        mup = cmat("mup", 0.0, 1, -1, ALU.is_ge, -1e30)
        mlo = cmat("mlo", 0.0, -1, 1, ALU.is_ge, -1e30)
        i32 = self.ident32
        wo = self.tile([128, 8, D], BF16, "wo")
        dma(POOL, wo.ap, self.gdn_w_o[li].rearrange("(k p) n -> p k n", p=128), writes=[wo.r])
        def T4(nm):
            t_ = self.tile([128, 8, 128], F32, nm)
            t_.r = GRes(nm)
            return t_
        qfb = [T4("qf") for _ in range(2)]
        kfb = [T4("kf") for _ in range(2)]
        vfb = [T4("vf") for _ in range(2)]
        xfb = [T4("xf") for _ in range(2)]
        zfb = [T4("zf") for _ in range(2)]
        Rt, dTt, d2t, dec, decT, eGbc, AmT, An = (T4(nm) for nm in ("R", "dT", "d2", "dec", "decT", "eGbc", "AmT", "An"))
        kbg, ktail, vb = T4("kbg"), T4("ktail"), T4("vb")
        Mb = [T4("M0"), T4("M1")]
        Nb = [T4("N0"), T4("N1")]
        Pm, attnT, qdT, nwT, vnew, St = T4("P"), T4("attnT"), T4("qdT"), T4("nwT"), T4("vnew"), T4("S")
        PT = T4("PT")
        Mc, Nc = [Rt], [d2t]
        gm = []
        for gi_ in range(4):
            gt_ = self.tile([128, 128], F32, "gmask")
            dma(SP, gt_.ap, self.gmask_in[gi_], writes=[gt_.r])
            gm.append(gt_)
        Gtok = self.tile([128, 8], F32, "Gtok")
        eGtok = self.tile([128, 8], F32, "eGtok")
        bg = self.tile([128, 8], F32, "bg")
        yT = self.tile([128, 8, 128], BF16, "yT")
        ytile = [self.tile([128, D], F32, "ytile") for _ in range(2)]
        fl = lambda t, j: t.ap[:, 4 * j:4 * j + 4, :]
        pv4 = lambda ps: ps.ap.rearrange("p (a b) -> p a b", a=4)
        bc_h = lambda ap2, j: ap2[:, 4 * j:4 * j + 4].unsqueeze(2).to_broadcast([128, 4, 128])
        bc_m = lambda m, n_=4: m.ap.unsqueeze(1).to_broadcast([128, n_, 128])
        hview = lambda ap: ap.rearrange("(h p) t -> p h t", p=128)
        nch = 0
        for d in range(self.cfg.get("gdn_dirs", 2)):
            TRI, maskT, maskN, strT, strN, last = ((triu32, mup, mlo, sup, slo, 127), (tril32, mlo, mup, slo, sup, 0))[d]
            order = list(range(NT)) if d == 0 else [1, 0] + list(range(NT - 1, 1, -1))
            order = order[:self.cfg.get("gdn_nch", NT)]
            op(DVE, lambda e: e.memset(St.ap, 0.0), writes=[St.r])
            for n in order:
                cols = slice(n * 128, (n + 1) * 128)
                qf, kf, vf, xf, zf = qfb[nch % 2], kfb[nch % 2], vfb[nch % 2], xfb[nch % 2], zfb[nch % 2]
                nch += 1
                dma(SP, qf.ap, hview(QF.ap)[:, :, cols], reads=[QF.sub[n]], writes=[qf.r])
                dma(SP, kf.ap, hview(KF.ap)[:, :, cols], reads=[KF.sub[n]], writes=[kf.r])
                dma(SP, vf.ap, hview(VF.ap)[:, :, cols], reads=[VF.sub[n]], writes=[vf.r])
                if d == 1:
                    dma(SP, xf.ap, hview(OF.ap)[:, :, cols], reads=[OF.sub[n]], writes=[xf.r])
                    dma(SP, zf.ap, hview(ZF.ap)[:, :, cols], reads=[ZF.sub[n]], writes=[zf.r])
                gsl = gT.ap[:, n, d * 8:(d + 1) * 8]
                bsl = bT.ap[:, n, d * 8:(d + 1) * 8]
                psG = self.psum()
                op(PE, lambda e, psG=psG, TRI=TRI, gsl=gsl: e.matmul(psG.ap[:, 0:8], lhsT=TRI.ap, rhs=gsl, start=True, stop=True), reads=[TRI.r, gT.r], writes=psG.sub)
                op(DVE, lambda e, gsl=gsl, TRI=TRI: e.tensor_tensor(out=Rt.ap, in0=gsl.unsqueeze(2).to_broadcast([128, 8, 128]), in1=bc_m(TRI, 8), op=ALU.mult),
                   reads=[gT.r, TRI.r], writes=[Rt.r])
                psGb = [self.psum(), self.psum()]
                for j in range(2):
                    op(PE, lambda e, j=j, p=psGb[j]: e.matmul(p.ap, lhsT=ones32.ap, rhs=fl(Rt, j).rearrange("p a b -> p (a b)"), start=True, stop=True),
                       reads=[ones32.r, Rt.r], writes=psGb[j].sub)
                op(ACT, lambda e, psG=psG: e.copy(out=Gtok.ap, in_=psG.ap[:, 0:8]), reads=psG.sub, writes=[Gtok.r])
                for j in range(2):
                    op(DVE, lambda e, j=j, p=psGb[j]: e.tensor_tensor(out=fl(dTt, j), in0=pv4(p), in1=bc_h(Gtok.ap, j), op=ALU.subtract), reads=psGb[j].sub + [Gtok.r], writes=[dTt.r])
                    op(ACT, lambda e, j=j, p=psGb[j]: e.activation(out=fl(eGbc, j), in_=pv4(p), func=AF.Exp), reads=psGb[j].sub, writes=[eGbc.r])
                op(DVE, lambda e, maskN=maskN: e.scalar_tensor_tensor(out=d2t.ap, in0=dTt.ap, scalar=-1.0, in1=bc_m(maskN, 8), op0=ALU.mult, op1=ALU.add),
                   reads=[dTt.r, maskN.r], writes=[d2t.r])
                op(POOL, lambda e, maskT=maskT: e.tensor_tensor(out=dTt.ap, in0=dTt.ap, in1=bc_m(maskT, 8), op=ALU.add), reads=[dTt.r, maskT.r, d2t.r], writes=[dTt.r])
                op(ACT, lambda e: e.activation(out=dec.ap, in_=d2t.ap, func=AF.Exp), reads=[d2t.r], writes=[dec.r])
                op(ACT, lambda e: e.activation(out=decT.ap, in_=dTt.ap, func=AF.Exp), reads=[dTt.r], writes=[decT.r])
                op(ACT, lambda e: e.activation(out=eGtok.ap, in_=Gtok.ap, func=AF.Exp), reads=[Gtok.r], writes=[eGtok.r])
                op(DVE, lambda e, bsl=bsl: e.tensor_tensor(out=bg.ap, in0=eGtok.ap, in1=bsl, op=ALU.mult), reads=[eGtok.r, bT.r], writes=[bg.r])
                op(DVE, lambda e, bsl=bsl: e.tensor_tensor(out=Rt.ap, in0=bsl.unsqueeze(2).to_broadcast([128, 8, 128]), in1=bc_m(i32, 8), op=ALU.mult),
                   reads=[bT.r, i32.r], writes=[Rt.r])
                psBb = [self.psum(), self.psum()]
                for j in range(2):
                    op(PE, lambda e, j=j, p=psBb[j]: e.matmul(p.ap, lhsT=ones32.ap, rhs=fl(Rt, j).rearrange("p a b -> p (a b)"), start=True, stop=True),
                       reads=[ones32.r, Rt.r], writes=psBb[j].sub)
                for j in range(2):
                    op(DVE, lambda e, j=j, p=psBb[j]: e.tensor_tensor(out=fl(AmT, j), in0=fl(decT, j), in1=pv4(p), op=ALU.mult), reads=psBb[j].sub + [decT.r], writes=[AmT.r])
                op(POOL, lambda e, strT=strT: e.tensor_tensor(out=AmT.ap, in0=AmT.ap, in1=bc_m(strT, 8), op=ALU.mult), reads=[AmT.r, strT.r], writes=[AmT.r])
                op(POOL, lambda e, bsl=bsl: e.tensor_tensor(out=An.ap, in0=dec.ap, in1=bsl.unsqueeze(2).to_broadcast([128, 8, 128]), op=ALU.mult), reads=[dec.r, bT.r], writes=[An.r])
                op(POOL, lambda e, strN=strN: e.tensor_tensor(out=An.ap, in0=An.ap, in1=bc_m(strN, 8), op=ALU.mult), reads=[An.r, strN.r], writes=[An.r])
                op(POOL, lambda e, qf=qf: e.tensor_tensor(out=qdT.ap, in0=qf.ap, in1=eGbc.ap, op=ALU.mult), reads=[qf.r, eGbc.r], writes=[qdT.r])
                if self.cfg.get('gdn_stage', 99) < 1:
                    continue
                psK = [self.psum(), self.psum()]
                psV = [self.psum(), self.psum()]
                for h in range(8):
                    j, q4 = h // 4, (h % 4) * 128
                    op(PE, lambda e, h=h, p=psK[j], q4=q4, kf=kf: e.transpose(out=p.ap[:, q4:q4 + 128], in_=kf.ap[:, h, :], identity=i32.ap), reads=[kf.r, i32.r], writes=[psK[j].sub[h % 4]])
                    op(PE, lambda e, h=h, p=psV[j], q4=q4, vf=vf: e.transpose(out=p.ap[:, q4:q4 + 128], in_=vf.ap[:, h, :], identity=i32.ap), reads=[vf.r, i32.r], writes=[psV[j].sub[h % 4]])
                for j in range(2):
                    op(DVE, lambda e, j=j, p=psK[j]: e.tensor_tensor(out=fl(kbg, j), in0=pv4(p), in1=bc_h(bg.ap, j), op=ALU.mult), reads=psK[j].sub + [bg.r], writes=[kbg.r])
                    op(DVE, lambda e, j=j, p=psK[j], last=last: e.tensor_tensor(out=fl(ktail, j), in0=pv4(p), in1=decT.ap[:, 4 * j:4 * j + 4, last:last + 1].to_broadcast([128, 4, 128]), op=ALU.mult),
                       reads=psK[j].sub + [decT.r], writes=[ktail.r])
                    op(DVE, lambda e, j=j, p=psV[j], bsl=bsl: e.tensor_tensor(out=fl(vb, j), in0=pv4(p), in1=bc_h(bsl, j), op=ALU.mult), reads=psV[j].sub + [bT.r], writes=[vb.r])
                if self.cfg.get('gdn_stage', 99) < 2:
                    continue
                psKK = [self.psum(), self.psum()]
                psKQ = [self.psum(), self.psum()]
                for h in range(8):
                    j, q4 = h // 4, (h % 4) * 128
                    op(PE, lambda e, h=h, p=psKK[j], q4=q4, kf=kf: e.matmul(p.ap[:, q4:q4 + 128], lhsT=kf.ap[:, h, :], rhs=kf.ap[:, h, :], start=True, stop=True), reads=[kf.r], writes=[psKK[j].sub[h % 4]])
                    op(PE, lambda e, h=h, p=psKQ[j], q4=q4, kf=kf, qf=qf: e.matmul(p.ap[:, q4:q4 + 128], lhsT=kf.ap[:, h, :], rhs=qf.ap[:, h, :], start=True, stop=True), reads=[kf.r, qf.r], writes=[psKQ[j].sub[h % 4]])
                M0, N0 = Mb[0], Nb[0]
                for j in range(2):
                    op(DVE, lambda e, j=j, p=psKK[j]: e.scalar_tensor_tensor(out=fl(M0, j), in0=pv4(p), scalar=-1.0, in1=fl(AmT, j), op0=ALU.mult, op1=ALU.mult), reads=psKK[j].sub + [AmT.r], writes=[M0.r])
                    op(DVE, lambda e, j=j, p=psKK[j]: e.scalar_tensor_tensor(out=fl(N0, j), in0=pv4(p), scalar=-1.0, in1=fl(An, j), op0=ALU.mult, op1=ALU.mult), reads=psKK[j].sub + [An.r], writes=[N0.r])
                    op(DVE, lambda e, j=j, p=psKQ[j]: e.tensor_tensor(out=fl(attnT, j), in0=pv4(p), in1=fl(decT, j), op=ALU.mult), reads=psKQ[j].sub + [decT.r], writes=[attnT.r])
                MA, NA, MB, NB = Mb[1], Nb[1], Mc[0], Nc[0]
                op(DVE, lambda e: e.tensor_tensor(out=MA.ap, in0=M0.ap, in1=bc_m(gm[0], 8), op=ALU.mult), reads=[M0.r, gm[0].r], writes=[MA.r])
                op(POOL, lambda e: e.tensor_tensor(out=NA.ap, in0=N0.ap, in1=bc_m(gm[0], 8), op=ALU.mult), reads=[N0.r, gm[0].r], writes=[NA.r])
                op(POOL, lambda e: e.tensor_tensor(out=Pm.ap, in0=MA.ap, in1=bc_m(i32, 8), op=ALU.add), reads=[MA.r, i32.r], writes=[Pm.r])
                op(POOL, lambda e: e.tensor_tensor(out=PT.ap, in0=NA.ap, in1=bc_m(i32, 8), op=ALU.add), reads=[NA.r, i32.r], writes=[PT.r])
                cur = (MA, NA)
                nxt = (MB, NB)
                for kk in range(1, 4):
                    Mp, Np = cur
                    Mn, Nn = nxt
                    psN = [self.psum(), self.psum()]
                    psM = [self.psum(), self.psum()]
                    for h in range(8):
                        j, q4 = h // 4, (h % 4) * 128
                        op(PE, lambda e, h=h, p=psN[j], q4=q4, Mp=Mp, Np=Np: e.matmul(p.ap[:, q4:q4 + 128], lhsT=Mp.ap[:, h, :], rhs=Np.ap[:, h, :], start=True, stop=True),
                           reads=[Mp.r, Np.r], writes=[psN[j].sub[h % 4]])
                        op(PE, lambda e, h=h, p=psM[j], q4=q4, Mp=Mp, Np=Np: e.matmul(p.ap[:, q4:q4 + 128], lhsT=Np.ap[:, h, :], rhs=Mp.ap[:, h, :], start=True, stop=True),
                           reads=[Mp.r, Np.r], writes=[psM[j].sub[h % 4]])
                    op(ACT, lambda e, j=0, p=psN[0], Nn=Nn: e.copy(out=fl(Nn, j), in_=pv4(p)), reads=psN[0].sub, writes=[Nn.r])
                    op(DVE, lambda e, j=1, p=psN[1], Nn=Nn: e.tensor_copy(out=fl(Nn, j), in_=pv4(p)), reads=psN[1].sub, writes=[Nn.r])
                    op(DVE, lambda e, j=0, p=psM[0], Mn=Mn: e.tensor_copy(out=fl(Mn, j), in_=pv4(p)), reads=psM[0].sub, writes=[Mn.r])
                    op(ACT, lambda e, j=1, p=psM[1], Mn=Mn: e.copy(out=fl(Mn, j), in_=pv4(p)), reads=psM[1].sub, writes=[Mn.r])
                    psP = [self.psum(), self.psum()]
                    psQ_ = [self.psum(), self.psum()]
                    for h in range(8):
                        j, q4 = h // 4, (h % 4) * 128
                        op(PE, lambda e, h=h, p=psP[j], q4=q4, Nn=Nn: e.matmul(p.ap[:, q4:q4 + 128], lhsT=Nn.ap[:, h, :], rhs=Pm.ap[:, h, :], start=True, stop=True),
                           reads=[Nn.r, Pm.r], writes=[psP[j].sub[h % 4]])
                        op(PE, lambda e, h=h, p=psQ_[j], q4=q4, Mn=Mn: e.matmul(p.ap[:, q4:q4 + 128], lhsT=Mn.ap[:, h, :], rhs=PT.ap[:, h, :], start=True, stop=True),
                           reads=[Mn.r, PT.r], writes=[psQ_[j].sub[h % 4]])
                    for j in range(2):
                        op(DVE, lambda e, j=j, p=psP[j]: e.tensor_tensor(out=fl(Pm, j), in0=fl(Pm, j), in1=pv4(p), op=ALU.add), reads=psP[j].sub + [Pm.r], writes=[Pm.r])
                        op(DVE, lambda e, j=j, p=psQ_[j]: e.tensor_tensor(out=fl(PT, j), in0=fl(PT, j), in1=pv4(p), op=ALU.add), reads=psQ_[j].sub + [PT.r], writes=[PT.r])
                    cur, nxt = nxt, cur
                for lv in range(3):
                    UoT, Yt = Mc[0], Nc[0]
                    op(DVE, lambda e, lv=lv, UoT=UoT: e.scalar_tensor_tensor(out=UoT.ap, in0=N0.ap, scalar=-1.0, in1=bc_m(gm[1 + lv], 8), op0=ALU.mult, op1=ALU.mult),
                       reads=[N0.r, gm[1 + lv].r], writes=[UoT.r])
                    psY = [self.psum(), self.psum()]
                    for h in range(8):
                        j, q4 = h // 4, (h % 4) * 128
                        op(PE, lambda e, h=h, p=psY[j], q4=q4, UoT=UoT: e.matmul(p.ap[:, q4:q4 + 128], lhsT=UoT.ap[:, h, :], rhs=Pm.ap[:, h, :], start=True, stop=True),
                           reads=[UoT.r, Pm.r], writes=[psY[j].sub[h % 4]])
                    op(ACT, lambda e, j=0, p=psY[0], Yt=Yt: e.copy(out=fl(Yt, j), in_=pv4(p)), reads=psY[0].sub, writes=[Yt.r])
                    op(DVE, lambda e, j=1, p=psY[1], Yt=Yt: e.tensor_copy(out=fl(Yt, j), in_=pv4(p)), reads=psY[1].sub, writes=[Yt.r])
                    psX = [self.psum(), self.psum()]
                    psXT = [self.psum(), self.psum()]
                    for h in range(8):
                        j, q4 = h // 4, (h % 4) * 128
                        op(PE, lambda e, h=h, p=psX[j], q4=q4, Yt=Yt: e.matmul(p.ap[:, q4:q4 + 128], lhsT=PT.ap[:, h, :], rhs=Yt.ap[:, h, :], start=True, stop=True),
                           reads=[PT.r, Yt.r], writes=[psX[j].sub[h % 4]])
                        if lv < 2:
                            op(PE, lambda e, h=h, p=psXT[j], q4=q4, Yt=Yt: e.matmul(p.ap[:, q4:q4 + 128], lhsT=Yt.ap[:, h, :], rhs=PT.ap[:, h, :], start=True, stop=True),
                               reads=[PT.r, Yt.r], writes=[psXT[j].sub[h % 4]])
                    for j in range(2):
                        op(DVE, lambda e, j=j, p=psX[j]: e.tensor_tensor(out=fl(Pm, j), in0=fl(Pm, j), in1=pv4(p), op=ALU.subtract), reads=psX[j].sub + [Pm.r], writes=[Pm.r])
                        if lv < 2:
                            op(DVE, lambda e, j=j, p=psXT[j]: e.tensor_tensor(out=fl(PT, j), in0=fl(PT, j), in1=pv4(p), op=ALU.subtract), reads=psXT[j].sub + [PT.r], writes=[PT.r])
                if self.cfg.get('gdn_stage', 99) < 4:
                    continue
                psW = [self.psum(), self.psum()]
                for h in range(8):
                    j, q4 = h // 4, (h % 4) * 128
                    op(PE, lambda e, h=h, p=psW[j], q4=q4: e.matmul(p.ap[:, q4:q4 + 128], lhsT=kbg.ap[:, h, :], rhs=Pm.ap[:, h, :], start=True, stop=True), reads=[kbg.r, Pm.r], writes=[psW[j].sub[h % 4]])
                for j in range(2):
                    op(ACT, lambda e, j=j, p=psW[j]: e.mul(out=fl(nwT, j), in_=pv4(p), mul=-1.0), reads=psW[j].sub, writes=[nwT.r])
                psVn = [self.psum(), self.psum()]
                for h in range(8):
                    j, q4 = h // 4, (h % 4) * 128
                    op(PE, lambda e, h=h, p=psVn[j], q4=q4: e.matmul(p.ap[:, q4:q4 + 128], lhsT=Pm.ap[:, h, :], rhs=vb.ap[:, h, :], start=True, stop=False), reads=[Pm.r, vb.r], writes=[psVn[j].sub[h % 4]])
                    op(PE, lambda e, h=h, p=psVn[j], q4=q4: e.matmul(p.ap[:, q4:q4 + 128], lhsT=nwT.ap[:, h, :], rhs=St.ap[:, h, :], start=False, stop=True), reads=[nwT.r, St.r], writes=[psVn[j].sub[h % 4]])
                op(ACT, lambda e, j=0, p=psVn[0]: e.copy(out=fl(vnew, j), in_=pv4(p)), reads=psVn[0].sub, writes=[vnew.r])
                op(DVE, lambda e, j=1, p=psVn[1]: e.tensor_copy(out=fl(vnew, j), in_=pv4(p)), reads=psVn[1].sub, writes=[vnew.r])
                if self.cfg.get('gdn_stage', 99) < 5:
                    continue
                psO = [self.psum(), self.psum()]
                psS = [self.psum(), self.psum()]
                for h in range(8):
                    j, q4 = h // 4, (h % 4) * 128
                    op(PE, lambda e, h=h, p=psO[j], q4=q4: e.matmul(p.ap[:, q4:q4 + 128], lhsT=St.ap[:, h, :], rhs=qdT.ap[:, h, :], start=True, stop=False), reads=[St.r, qdT.r], writes=[psO[j].sub[h % 4]])
                    op(PE, lambda e, h=h, p=psO[j], q4=q4: e.matmul(p.ap[:, q4:q4 + 128], lhsT=vnew.ap[:, h, :], rhs=attnT.ap[:, h, :], start=False, stop=True), reads=[vnew.r, attnT.r], writes=[psO[j].sub[h % 4]])
                    op(PE, lambda e, h=h, p=psS[j], q4=q4: e.matmul(p.ap[:, q4:q4 + 128], lhsT=ktail.ap[:, h, :], rhs=vnew.ap[:, h, :], start=True, stop=True), reads=[ktail.r, vnew.r], writes=[psS[j].sub[h % 4]])
                for j in range(2):
                    op(DVE, lambda e, j=j, last=last: e.tensor_tensor(out=fl(St, j), in0=fl(St, j), in1=eGbc.ap[:, 4 * j:4 * j + 4, last:last + 1].to_broadcast([128, 4, 128]), op=ALU.mult),
                       reads=[St.r, eGbc.r], writes=[St.r])
                    op(DVE, lambda e, j=j, p=psS[j]: e.tensor_tensor(out=fl(St, j), in0=fl(St, j), in1=pv4(p), op=ALU.add), reads=psS[j].sub + [St.r], writes=[St.r])
                if d == 0:
                    for j in range(2):
                        op(ACT, lambda e, j=j, p=psO[j], xf=xf: e.copy(out=fl(xf, j), in_=pv4(p)), reads=psO[j].sub, writes=[xf.r])
                    dma(SP, hview(OF.ap)[:, :, cols], xf.ap, reads=[xf.r], writes=[OF.sub[n]])
                    continue
                for j in range(2):
                    op(DVE, lambda e, j=j, p=psO[j], xf=xf: e.tensor_tensor(out=fl(xf, j), in0=fl(xf, j), in1=pv4(p), op=ALU.add), reads=psO[j].sub + [xf.r], writes=[xf.r])
                op(POOL, lambda e, xf=xf: e.tensor_tensor(out=Rt.ap, in0=xf.ap, in1=xf.ap, op=ALU.mult), reads=[xf.r], writes=[Rt.r])
                psQ = [self.psum(), self.psum()]
                for j in range(2):
                    op(PE, lambda e, j=j, p=psQ[j]: e.matmul(p.ap, lhsT=ones32.ap, rhs=fl(Rt, j).rearrange("p a b -> p (a b)"), start=True, stop=True), reads=[ones32.r, Rt.r], writes=psQ[j].sub)
                for j in range(2):
                    op(DVE, lambda e, j=j, p=psQ[j]: e.tensor_scalar(out=fl(d2t, j), in0=pv4(p), scalar1=1.0 / 128.0, scalar2=LN_EPS, op0=ALU.mult, op1=ALU.add), reads=psQ[j].sub, writes=[d2t.r])
                op(ACT, lambda e: e.activation(out=d2t.ap, in_=d2t.ap, func=AF.Ln), reads=[d2t.r], writes=[d2t.r])
                op(ACT, lambda e: e.activation(out=d2t.ap, in_=d2t.ap, func=AF.Exp, scale=-0.5), reads=[d2t.r], writes=[d2t.r])
                op(POOL, lambda e, xf=xf: e.tensor_tensor(out=xf.ap, in0=xf.ap, in1=d2t.ap, op=ALU.mult), reads=[xf.r, d2t.r], writes=[xf.r])
                op(POOL, lambda e, xf=xf, zf=zf: e.tensor_tensor(out=yT.ap, in0=xf.ap, in1=zf.ap, op=ALU.mult), reads=[xf.r, zf.r], writes=[yT.r])
                yt = ytile[n % 2]
                for half in range(2):
                    ps = self.psum()
                    for h in range(8):
                        op(PE, lambda e, ps=ps, h=h, half=half: e.matmul(ps.ap, lhsT=yT.ap[:, h, :], rhs=wo.ap[:, h, half * 512:(half + 1) * 512], start=(h == 0), stop=(h == 7)),
                           reads=[yT.r, wo.r], writes=ps.sub)
                    op(ACT, lambda e, ps=ps, yt=yt, half=half: e.copy(out=yt.ap[:, half * 512:(half + 1) * 512], in_=ps.ap), reads=ps.sub, writes=[yt.r])
                dma(SP, self.Y.ap[cols, :], yt.ap, reads=[yt.r], writes=[self.Y.sub[n]])
                if self.cfg.get("test") == "mix":
                    dma(SP, self.dbg.ap[cols, :], yt.ap, reads=[yt.r], writes=[self.dbg.sub[n]])
        if self.cfg.get("test") == "gdn2":
            self.P.barrier()
            rr = Res("dbg2")
            dma(SP, self.dbg2[0], VF.ap if self.cfg.get('gdn_nch') == 1 else OF.ap, reads=OF.sub + VF.sub, writes=[rr])
            dma(SP, self.dbg2[1, 0:128, 0:1024], St.ap.rearrange("p a b -> p (a b)"), reads=[St.r], writes=[rr])
            for ii, tt in enumerate((decT, dec, AmT, An, Pm, vnew, attnT, kbg, vb, ktail, qdT, eGbc, nwT, qfb[0], kfb[0], vfb[0])):
                dma(SP, self.dbg2[2 + ii // 8, (ii % 8) * 128:(ii % 8 + 1) * 128, 0:1024], tt.ap.rearrange("p a b -> p (a b)"), reads=[tt.r], writes=[rr])
            dma(SP, self.dbg3[:, 0], gT.ap, reads=[gT.r], writes=[rr])
            dma(SP, self.dbg3[:, 1], bT.ap, reads=[bT.r], writes=[rr])
            self.dbg.sub.append(rr)

    def out_proj(self, w_o):
        op, dma = self.op, self.dma
        self.P.barrier()
        self.off = self.base_off
        OD = self.OD
        wo = self.tile([128, 8, D], BF16, "wo")
        dma(POOL, wo.ap, w_o.rearrange("(k p) n -> p k n", p=128), writes=[wo.r])
        ob = [self.tile([128, D], BF16, "ob") for _ in range(3)]
        oT = [self.tile([128, 8, 128], BF16, "oT", nsub=1) for _ in range(2)]
        yb = [self.tile([128, D], F32, "yb") for _ in range(2)]
        for i in range(NT):
            o, t, y = ob[i % 3], oT[i % 2], yb[i % 2]
            rows = slice(i * 128, (i + 1) * 128)
            dma(SP, o.ap, OD.ap[rows, :], reads=[OD.sub[i]], writes=[o.r])
            self.transpose_to(o, t, 0)
            for half in range(2):
                ps = self.psum()
                for k in range(8):
                    op(PE, lambda e, ps=ps, t=t, k=k, half=half: e.matmul(ps.ap, lhsT=t.ap[:, k, :], rhs=wo.ap[:, k, half * 512:(half + 1) * 512], start=(k == 0), stop=(k == 7)),
                       reads=[t.sub[0], wo.r], writes=[ps.r])
                op(ACT if half else DVE, (lambda e, ps=ps, y=y, half=half: e.copy(out=y.ap[:, half * 512:(half + 1) * 512], in_=ps.ap)) if half else
                   (lambda e, ps=ps, y=y, half=half: e.tensor_copy(out=y.ap[:, half * 512:(half + 1) * 512], in_=ps.ap)), reads=[ps.r], writes=[y.r])
            dma(SP, self.Y.ap[rows, :], y.ap, reads=[y.r], writes=[self.Y.sub[i]])
            if self.cfg.get("test") == "mix":
                dma(SP, self.dbg.ap[rows, :], y.ap, reads=[y.r], writes=[self.dbg.sub[i]])

    def postmix(self, l):
        op, dma = self.op, self.dma
        self.phase_begin()
        self.h2tok = self.tile([128, NT, D], BF16, "h2tok", nsub=NT)
        self.affTok = self.tile([128, NT, NE], F32, "affTok")
        h2tok, affTok = self.h2tok, self.affTok
        self.post_keep = self.off
        mods = self.mod_tiles(l, [2, 3, 4], plus1=(4,))
        g0 = self.ln_vec(self.ln_g[l, 0:1, :])
        b0 = self.ln_vec(self.ln_b[l, 0:1, :])
        wr = self.tile([128, 8, NE], BF16, "wr")
        dma(POOL, wr.ap, self.moe_w_router[l].rearrange("(k p) n -> p k n", p=128), writes=[wr.r])
        xb = [self.tile([128, D], F32, "xb") for _ in range(2)]
        yb = [self.tile([128, D], F32, "yb") for _ in range(2)]
        ob = [self.tile([128, D], F32, "ob") for _ in range(2)]
        h2T = [self.tile([128, 8, 128], BF16, "h2T", nsub=1) for _ in range(2)]
        st = self.tile([128, 2, 6], F32, "st")
        mv = self.tile([128, 4], F32, "mv")
        sm = self.tile([128, 4], F32, "sm")
        ex = self.tile([128, NE], F32, "ex")
        for i in range(NT):
            x, y, o = xb[i % 2], yb[i % 2], ob[i % 2]
            c = 1 if i < 2 else 0
            rows = slice(i * 128, (i + 1) * 128)
            dma(SP, x.ap, self.XS.ap[rows, :], reads=[self.XS.sub[i]], writes=[x.r])
            dma(SP, y.ap, self.Y.ap[rows, :], reads=[self.Y.sub[i]], writes=[y.r])
            m2, m3, m4 = mods[2][c], mods[3][c], mods[4][c]
            op(POOL, lambda e, y=y, m2=m2: e.tensor_tensor(out=y.ap, in0=y.ap, in1=m2.ap, op=ALU.mult), reads=[y.r, m2.r], writes=[y.r])
            op(DVE, lambda e, x=x, y=y: e.scalar_tensor_tensor(out=y.ap, in0=x.ap, scalar=ALPHA, in1=y.ap, op0=ALU.mult, op1=ALU.add),
               reads=[x.r, y.r], writes=[y.r])
            self.layernorm_(y, st, mv)
            op(POOL, lambda e, y=y, o=o: e.tensor_tensor(out=o.ap, in0=y.ap, in1=g0.ap, op=ALU.mult), reads=[y.r, g0.r], writes=[o.r])
            op(POOL, lambda e, o=o: e.tensor_tensor(out=o.ap, in0=o.ap, in1=b0.ap, op=ALU.add), reads=[o.r, b0.r], writes=[o.r])
            dma(POOL, self.XS.ap[rows, :], o.ap, reads=[o.r], writes=[self.XS.sub[i]])
            op(DVE, lambda e, o=o, x=x, m4=m4: e.tensor_tensor(out=x.ap, in0=o.ap, in1=m4.ap, op=ALU.mult), reads=[o.r, m4.r], writes=[x.r])
            hv = Tl(h2tok.ap[:, i, :])
            hv.r = h2tok.sub[i]
            op(DVE, lambda e, x=x, m3=m3, hv=hv: e.tensor_tensor(out=hv.ap, in0=x.ap, in1=m3.ap, op=ALU.add), reads=[x.r, m3.r], writes=[hv.r])
            ht = h2T[i % 2]
            self.transpose_to(hv, ht, 0)
            ps = self.psum()
            for k in range(8):
                op(PE, lambda e, ps=ps, ht=ht, k=k: e.matmul(ps.ap[:, 0:NE], lhsT=ht.ap[:, k, :], rhs=wr.ap[:, k, :], start=(k == 0), stop=(k == 7)),
                   reads=[ht.sub[0], wr.r], writes=[ps.r])
            op(DVE, lambda e, ps=ps: e.reduce_max(out=sm.ap[:, 0:1], in_=ps.ap[:, 0:NE], axis=AX.X), reads=[ps.r], writes=[sm.r])
            op(DVE, lambda e: e.tensor_scalar(out=sm.ap[:, 1:2], in0=sm.ap[:, 0:1], scalar1=-1.0, scalar2=None, op0=ALU.mult), reads=[sm.r], writes=[sm.r])
            op(ACT, lambda e, ps=ps: e.activation(out=ex.ap, in_=ps.ap[:, 0:NE], func=AF.Exp, bias=sm.ap[:, 1:2], scale=1.0, accum_out=sm.ap[:, 2:3]),
               reads=[ps.r, sm.r], writes=[ex.r, sm.r])
            op(DVE, lambda e: e.reciprocal(out=sm.ap[:, 3:4], in_=sm.ap[:, 2:3]), reads=[sm.r], writes=[sm.r])
            op(DVE, lambda e, i=i: e.tensor_scalar(out=affTok.ap[:, i, :], in0=ex.ap, scalar1=sm.ap[:, 3:4], scalar2=None, op0=ALU.mult),
               reads=[ex.r, sm.r], writes=[affTok.r])

    def moe_select(self, l):
        op, dma = self.op, self.dma
        affTok = self.affTok
        self.P.barrier()
        self.off = self.post_keep
        self.posm = self.tile([128, NT, NE], F32, "posm")
        posm = self.posm
        keep2 = self.off
        affT = self.tile([NE, TT], F32, "affT")
        work = self.tile([NE, 4096], F32, "work")
        m8 = self.tile([NE, 8], F32, "m8")
        thr = self.tile([NE, 2], F32, "thr")
        maskT = self.tile([NE, TT], BF16, "maskT")
        for g in range(9):
            ps = self.psum()
            tl = list(range(g * 4, min(g * 4 + 4, NT)))
            for j, i in enumerate(tl):
                op(PE, lambda e, ps=ps, i=i, j=j: e.transpose(out=ps.ap[0:NE, j * 128:(j + 1) * 128], in_=affTok.ap[:, i, :], identity=self.ident32.ap),
                   reads=[affTok.r, self.ident32.r], writes=[ps.r])
            n = len(tl) * 128
            op(ACT, lambda e, ps=ps, g=g, n=n: e.copy(out=affT.ap[:, g * 512:g * 512 + n], in_=ps.ap[0:NE, 0:n]), reads=[ps.r], writes=[affT.r])
        dma(SP, self.AFFT.ap, affT.ap, reads=[affT.r], writes=[self.AFFT.r])
        op(DVE, lambda e: e.tensor_copy(out=work.ap[:, 0:256], in_=affT.ap[:, 0:256]), reads=[affT.r], writes=[work.r])
        for it in range(4):
            op(DVE, lambda e: e.max(out=m8.ap, in_=work.ap[:, 0:256]), reads=[work.r], writes=[m8.r])
            if it < 3:
                op(DVE, lambda e: e.match_replace(out=work.ap[:, 0:256], in_to_replace=m8.ap, in_values=work.ap[:, 0:256], imm_value=-1.0),
                   reads=[work.r, m8.r], writes=[work.r])
        op(DVE, lambda e: e.tensor_copy(out=thr.ap[:, 0:1], in_=m8.ap[:, 7:8]), reads=[m8.r], writes=[thr.r])
        if not hasattr(self, "CAND"):
            self.CAND = Tl(self.dram("CAND", [128, 128], F32), "CAND")
        w1 = self.tile([128, 512], F32, "w1")
        c1 = self.tile([128, 128], F32, "c1")
        for e_ in range(NE):
            dma(SP, w1.ap[e_ * 8:(e_ + 1) * 8, :], self.AFFT.ap[e_, 256:TT].rearrange("(s t) -> s t", s=8), reads=[self.AFFT.r], writes=[w1.r])
        for it in range(16):
            op(DVE, lambda e, it=it: e.max(out=c1.ap[:, it * 8:(it + 1) * 8], in_=w1.ap), reads=[w1.r], writes=[c1.r])
            if it < 15:
                op(DVE, lambda e, it=it: e.match_replace(out=w1.ap, in_to_replace=c1.ap[:, it * 8:(it + 1) * 8], in_values=w1.ap, imm_value=-1.0),
                   reads=[w1.r, c1.r], writes=[w1.r])
        dma(SP, self.CAND.ap, c1.ap, reads=[c1.r], writes=[self.CAND.r])
        dma(SP, work.ap[:, 0:1024], self.CAND.ap.rearrange("(e s) c -> e (s c)", s=8), reads=[self.CAND.r], writes=[work.r])
        for it in range(64):
            op(DVE, lambda e: e.max(out=m8.ap, in_=work.ap[:, 0:1024]), reads=[work.r], writes=[m8.r])
            if it < 63:
                op(DVE, lambda e: e.match_replace(out=work.ap[:, 0:1024], in_to_replace=m8.ap, in_values=work.ap[:, 0:1024], imm_value=-1.0),
                   reads=[work.r, m8.r], writes=[work.r])
        op(DVE, lambda e: e.tensor_copy(out=thr.ap[:, 1:2], in_=m8.ap[:, 7:8]), reads=[m8.r], writes=[thr.r])
        for (lo, n, col) in ((0, 256, 0), (256, 4096, 1)):
            op(DVE, lambda e, lo=lo, n=n, col=col: e.tensor_scalar(out=maskT.ap[:, lo:lo + n], in0=affT.ap[:, lo:lo + n], scalar1=thr.ap[:, col:col + 1],
                                                                   scalar2=None, op0=ALU.is_ge), reads=[affT.r, thr.r], writes=[maskT.r])
        maskTok = self.tile([128, NT, NE], BF16, "maskTok")
        maskF = self.tile([128, NT, NE], F32, "maskF")
        ps = self.psum()
        pb = ps.ap.bitcast(BF16)
        for i in range(NT):
            op(PE, lambda e, i=i, pb=pb: e.transpose(out=pb[:, i * NE:(i + 1) * NE], in_=maskT.ap[:, i * 128:(i + 1) * 128], identity=self.ident.ap[0:NE, 0:NE]),
               reads=[maskT.r, self.ident.r], writes=[ps.r])
        op(DVE, lambda e, pb=pb: e.tensor_copy(out=maskTok.ap.rearrange("p a b -> p (a b)"), in_=pb[:, 0:NT * NE]), reads=[ps.r], writes=[maskTok.r])
        op(DVE, lambda e: e.tensor_copy(out=maskF.ap, in_=maskTok.ap), reads=[maskTok.r], writes=[maskF.r])
        mflat = maskTok.ap.rearrange("p a b -> p (a b)")
        ps_w, ps_t, ps_c = self.psum(), self.psum(), self.psum()
        op(PE, lambda e: e.matmul(ps_w.ap, lhsT=self.triu.ap, rhs=mflat[:, 2 * NE:NT * NE], start=True, stop=True), reads=[maskTok.r, self.triu.r], writes=[ps_w.r])
        op(PE, lambda e: e.matmul(ps_t.ap, lhsT=self.ones.ap, rhs=mflat[:, 2 * NE:NT * NE], start=True, stop=True), reads=[maskTok.r, self.ones.r], writes=[ps_t.r])
        op(PE, lambda e: e.matmul(ps_c.ap[:, 0:2 * NE], lhsT=self.triu.ap, rhs=mflat[:, 0:2 * NE], start=True, stop=True), reads=[maskTok.r, self.triu.r], writes=[ps_c.r])
        op(PE, lambda e: e.matmul(ps_c.ap[:, 2 * NE:4 * NE], lhsT=self.ones.ap, rhs=mflat[:, 0:2 * NE], start=True, stop=True), reads=[maskTok.r, self.ones.r], writes=[ps_c.r])
        eoff = self.tile([128, NT, NE], F32, "eoff")
        tot = self.tile([128, NT, NE], F32, "tot")
        op(DVE, lambda e: e.tensor_copy(out=tot.ap[:, 2:NT, :].rearrange("p a b -> p (a b)"), in_=ps_t.ap), reads=[ps_t.r], writes=[tot.r])
        op(DVE, lambda e: e.tensor_copy(out=tot.ap[:, 0:2, :].rearrange("p a b -> p (a b)"), in_=ps_c.ap[:, 2 * NE:4 * NE]), reads=[ps_c.r], writes=[tot.r])
        op(DVE, lambda e: e.memset(eoff.ap, 0.0), writes=[eoff.r])
        op(DVE, lambda e: e.tensor_copy(out=eoff.ap[:, 1, :], in_=tot.ap[:, 0, :]), reads=[tot.r], writes=[eoff.r])
        for i in range(3, NT):
            op(DVE, lambda e, i=i: e.tensor_tensor(out=eoff.ap[:, i, :], in0=eoff.ap[:, i - 1, :], in1=tot.ap[:, i - 1, :], op=ALU.add),
               reads=[eoff.r, tot.r], writes=[eoff.r])
        op(DVE, lambda e: e.tensor_tensor(out=posm.ap[:, 2:NT, :].rearrange("p a b -> p (a b)"), in0=ps_w.ap,
                                          in1=eoff.ap[:, 2:NT, :].rearrange("p a b -> p (a b)"), op=ALU.add), reads=[ps_w.r, eoff.r], writes=[posm.r])
        op(DVE, lambda e: e.tensor_tensor(out=posm.ap[:, 0:2, :].rearrange("p a b -> p (a b)"), in0=ps_c.ap[:, 0:2 * NE],
                                          in1=eoff.ap[:, 0:2, :].rearrange("p a b -> p (a b)"), op=ALU.add), reads=[ps_c.r, eoff.r], writes=[posm.r])
        op(DVE, lambda e: e.tensor_tensor(out=posm.ap, in0=posm.ap, in1=maskF.ap, op=ALU.mult), reads=[posm.r, maskF.r], writes=[posm.r])
        op(DVE, lambda e: e.tensor_scalar(out=posm.ap, in0=posm.ap, scalar1=-1.0, scalar2=None, op0=ALU.add), reads=[posm.r], writes=[posm.r])
        posT = self.tile([NE, TT], F32, "posT")
        for g in range(9):
            ps = self.psum()
            tl = list(range(g * 4, min(g * 4 + 4, NT)))
            for j, i in enumerate(tl):
                op(PE, lambda e, ps=ps, i=i, j=j: e.transpose(out=ps.ap[0:NE, j * 128:(j + 1) * 128], in_=posm.ap[:, i, :], identity=self.ident32.ap),
                   reads=[posm.r, self.ident32.r], writes=[ps.r])
            n = len(tl) * 128
            op(DVE, lambda e, ps=ps, g=g, n=n: e.tensor_scalar(out=posT.ap[:, g * 512:g * 512 + n], in0=ps.ap[0:NE, 0:n], scalar1=12582912.0, scalar2=12582912.0,
                                                               op0=ALU.add, op1=ALU.subtract), reads=[ps.r], writes=[posT.r])
        dma(SP, self.POST.ap, posT.ap, reads=[posT.r], writes=[self.POST.r])
        self.P.barrier()
        self.off = keep2

    def moe_passA(self, l):
        op, dma = self.op, self.dma
        h2tok, posm = self.h2tok, self.posm
        sel = self.tile([128, 32, 512], BF16, "sel", nsub=32)
        selc = self.tile([128, 2, 32], BF16, "selc")
        xsT = self.tile([128, 8, 544], BF16, "xsT")
        hact = self.tile([128, 16, 544], BF16, "hact")
        wgb = [self.tile([128, 8, 256], BF16, "wg") for _ in range(2)]
        wub = [self.tile([128, 8, 256], BF16, "wu") for _ in range(2)]
        wdb = [self.tile([128, 2, 512], BF16, "wd") for _ in range(3)]
        sa = [self.tile([128, 512], F32, "sa") for _ in range(2)]
        sac = self.tile([128, 32], F32, "sac")
        yes = [self.tile([128, D], BF16, "yes") for _ in range(4)]
        yec = self.tile([32, D], BF16, "yec")
        posbc = [self.tile([128, 1024], F32, "posbc") for _ in range(2)]
        affbc = [self.tile([128, 1024], F32, "affbc") for _ in range(2)]
        posc = self.tile([32, 256], F32, "posc")
        affc = self.tile([32, 256], F32, "affc")
        stg = [self.tile([128, 1024], BF16, "stg") for _ in range(2)]
        nq = 0
        stgc = self.tile([32, 256], BF16, "stgc")
        nw = [0, 0]
        ns = 0
        def build_selT(ex):
            nonlocal nq
            yield
            dma(SP, posc.ap, self.POST.ap[ex:ex + 1, 0:256].to_broadcast([32, 256]), reads=[self.POST.r], writes=[posc.r])
            dma(SP, affc.ap, self.AFFT.ap[ex:ex + 1, 0:256].to_broadcast([32, 256]), reads=[self.AFFT.r], writes=[affc.r])
            op(DVE, lambda e: e.scalar_tensor_tensor(out=stgc.ap, in0=posc.ap, scalar=self.iotacol.ap[0:32, 0:1], in1=affc.ap,
                                                      op0=ALU.is_equal, op1=ALU.mult), reads=[posc.r, affc.r, self.iotacol.r], writes=[stgc.r])
            dma(SP, self.SELTC.ap[ex], stgc.ap, reads=[stgc.r], writes=[self.SELTC.r])
            for qd in range(4):
                yield
                pb_, ab_ = posbc[nq % 2], affbc[nq % 2]
                nq += 1
                t0 = 256 + qd * 1024
                dma(SP, pb_.ap, self.POST.ap[ex:ex + 1, t0:t0 + 1024].to_broadcast([128, 1024]), reads=[self.POST.r], writes=[pb_.r])
                dma(SP, ab_.ap, self.AFFT.ap[ex:ex + 1, t0:t0 + 1024].to_broadcast([128, 1024]), reads=[self.AFFT.r], writes=[ab_.r])
                for c in range(4):
                    sg = stg[c % 2]
                    ecx = ex * 4 + c
                    op(DVE, lambda e, sg=sg, c=c, pb_=pb_, ab_=ab_: e.scalar_tensor_tensor(
                        out=sg.ap, in0=pb_.ap, scalar=self.iotacol.ap[:, c:c + 1], in1=ab_.ap, op0=ALU.is_equal, op1=ALU.mult),
                       reads=[pb_.r, ab_.r, self.iotacol.r], writes=[sg.r])
                    dma(SP, self.SELT.ap[qd * 8:(qd + 1) * 8, :, ecx, :].rearrange("i s t -> s i t"), sg.ap.rearrange("s (i t) -> s i t", t=128),
                        reads=[sg.r], writes=self.SELT.sub[qd * 8:(qd + 1) * 8])


        for ex in range(NE):
            for i in range(2, NT):
                eng = DVE if i % 2 == 0 else POOL
                op(DVE, lambda e, i=i, ex=ex: e.tensor_scalar(out=sel.ap[:, i - 2, :], in0=self.iota512.ap, scalar1=posm.ap[:, i, ex:ex + 1], scalar2=None,
                                                              op0=ALU.is_equal), reads=[self.iota512.r, posm.r], writes=[sel.sub[i - 2]])
            for i in range(2):
                op(DVE, lambda e, i=i, ex=ex: e.tensor_scalar(out=selc.ap[:, i, :], in0=self.iota512.ap[:, 0:32], scalar1=posm.ap[:, i, ex:ex + 1], scalar2=None,
                                                              op0=ALU.is_equal), reads=[self.iota512.r, posm.r], writes=[selc.r])
            for k in range(8):
                ps = self.psum()
                ks = slice(k * 128, (k + 1) * 128)
                for i in range(2, NT):
                    op(PE, lambda e, ps=ps, i=i, ks=ks: e.matmul(ps.ap, lhsT=h2tok.ap[:, i, ks], rhs=sel.ap[:, i - 2, :], start=(i == 2), stop=(i == NT - 1)),
                       reads=[h2tok.sub[i], sel.sub[i - 2]], writes=[ps.r])
                op(ACT, lambda e, ps=ps, k=k: e.copy(out=xsT.ap[:, k, 0:512], in_=ps.ap), reads=[ps.r], writes=[xsT.r])
                ps2 = self.psum()
                for i in range(2):
                    op(PE, lambda e, ps2=ps2, i=i, ks=ks: e.matmul(ps2.ap[:, 0:32], lhsT=h2tok.ap[:, i, ks], rhs=selc.ap[:, i, :], start=(i == 0), stop=(i == 1)),
                       reads=[h2tok.sub[i], selc.r], writes=[ps2.r])
                op(ACT, lambda e, ps2=ps2, k=k: e.copy(out=xsT.ap[:, k, 512:544], in_=ps2.ap[:, 0:32]), reads=[ps2.r], writes=[xsT.r])
            gen_ = build_selT(ex - 1) if ex > 0 else iter(())
            next(gen_, None)
            for q in range(8):
                next(gen_, None)
                wg, wu = wgb[nw[0] % 2], wub[nw[0] % 2]
                nw[0] += 1
                cs = slice(q * 256, (q + 1) * 256)
                if not (self.cfg.get('nowdma') and ex > 0):
                    dma(POOL, wg.ap, self.moe_w_gate[l, ex][:, cs].rearrange("(k p) n -> p k n", p=128), writes=[wg.r])
                    dma(POOL, wu.ap, self.moe_w_up[l, ex][:, cs].rearrange("(k p) n -> p k n", p=128), writes=[wu.r])
                for jl in range(2):
                    j = q * 2 + jl
                    js = slice(jl * 128, (jl + 1) * 128)
                    ps_a, ps_u, ps_c = self.psum(), self.psum(), self.psum()
                    for (w, ps) in ((wg, ps_a), (wu, ps_u)):
                        for k in range(8):
                            op(PE, lambda e, w=w, ps=ps, k=k, js=js: e.matmul(ps.ap, lhsT=w.ap[:, k, js], rhs=xsT.ap[:, k, 0:512], start=(k == 0), stop=(k == 7)),
                               reads=[w.r, xsT.r], writes=[ps.r])
                    for (w, c0) in ((wg, 0), (wu, 32)):
                        for k in range(8):
                            op(PE, lambda e, w=w, c0=c0, k=k, js=js, ps_c=ps_c: e.matmul(ps_c.ap[:, c0:c0 + 32], lhsT=w.ap[:, k, js], rhs=xsT.ap[:, k, 512:544],
                                                                                         start=(k == 0), stop=(k == 7)),
                               reads=[w.r, xsT.r], writes=[ps_c.r])
                    s = sa[ns % 2]
                    ns += 1
                    op(ACT, lambda e, s=s, ps_a=ps_a: e.activation(out=s.ap, in_=ps_a.ap, func=AF.Silu), reads=[ps_a.r], writes=[s.r])
                    op(DVE, lambda e, s=s, ps_u=ps_u, j=j: e.tensor_tensor(out=hact.ap[:, j, 0:512], in0=s.ap, in1=ps_u.ap, op=ALU.mult),
                       reads=[s.r, ps_u.r], writes=[hact.r])
                    op(ACT, lambda e, ps_c=ps_c: e.activation(out=sac.ap, in_=ps_c.ap[:, 0:32], func=AF.Silu), reads=[ps_c.r], writes=[sac.r])
                    op(DVE, lambda e, ps_c=ps_c, j=j: e.tensor_tensor(out=hact.ap[:, j, 512:544], in0=sac.ap, in1=ps_c.ap[:, 32:64], op=ALU.mult),
                       reads=[sac.r, ps_c.r], writes=[hact.r])
            for _ in gen_:
                pass
            for half in range(2):
                hs = slice(half * 512, (half + 1) * 512)
                psd = [self.psum() for _ in range(5)]
                for jj in range(8):
                    wd = wdb[nw[1] % 3]
                    nw[1] += 1
                    if not (self.cfg.get('nowdma') and ex > 0):
                        dma(POOL, wd.ap, self.moe_w_down[l, ex][jj * 256:(jj + 1) * 256, hs].rearrange("(a p) n -> p a n", p=128), writes=[wd.r])
                    for a in range(2):
                        j = jj * 2 + a
                        first, last = (j == 0), (j == 15)
                        for c in range(4):
                            op(PE, lambda e, c=c, j=j, a=a, wd=wd, first=first, last=last, p=psd[c]: e.matmul(p.ap, lhsT=hact.ap[:, j, c * 128:(c + 1) * 128], rhs=wd.ap[:, a, :],
                                                                                                                  start=first, stop=last),
                               reads=[hact.r, wd.r], writes=[psd[c].r])
                        op(PE, lambda e, j=j, a=a, wd=wd, first=first, last=last, p=psd[4]: e.matmul(p.ap[0:32, :], lhsT=hact.ap[:, j, 512:544], rhs=wd.ap[:, a, :],
                                                                                                      start=first, stop=last),
                           reads=[hact.r, wd.r], writes=[psd[4].r])
                for c in range(4):
                    op(ACT, lambda e, c=c, p=psd[c], hs=hs: e.copy(out=yes[c].ap[:, hs], in_=p.ap), reads=[psd[c].r], writes=[yes[c].r])
                op(ACT, lambda e, p=psd[4], hs=hs: e.copy(out=yec.ap[:, hs], in_=p.ap[0:32, :]), reads=[psd[4].r], writes=[yec.r])
            for c in range(4):
                dma(SP, self.YE.ap[ex, c], yes[c].ap, reads=[yes[c].r], writes=[self.YE.sub[ex]])
            dma(SP, self.YEC.ap[ex], yec.ap, reads=[yec.r], writes=[self.YEC.sub[ex]])
        for _ in build_selT(NE - 1):
            pass

    def moe_passB(self, l):
        op, dma = self.op, self.dma
        self.phase_begin()
        last = (l == DEPTH - 1)
        yeall = self.tile([128, 64, D], BF16, "yeall")
        for ex in range(NE):
            dma(SP, yeall.ap[:, ex * 4:(ex + 1) * 4, :], self.YE.ap[ex].rearrange("c s d -> s c d"), reads=[self.YE.sub[ex]], writes=[yeall.r])
        yecall = self.tile([128, 4, D], BF16, "yecall")
        dma(SP, yecall.ap, self.YEC.ap.rearrange("(g e) s d -> (e s) g d", e=4), reads=self.YEC.sub, writes=[yecall.r])
        seltc = self.tile([128, 4, 256], BF16, "seltc")
        dma(SP, seltc.ap, self.SELTC.ap.rearrange("(g e) s t -> (e s) g t", e=4), reads=[self.SELTC.r], writes=[seltc.r])
        mods = self.mod_tiles(l, [5])
        g1 = self.ln_vec(self.ln_g[l, 1:2, :])
        b1 = self.ln_vec(self.ln_b[l, 1:2, :])
        selt = [self.tile([128, 32, 128], BF16, "selt") for _ in range(3)]
        xb = [self.tile([128, D], F32, "xb") for _ in range(2)]
        yb = [self.tile([128, D], F32, "yb") for _ in range(2)]
        st = self.tile([128, 2, 6], F32, "st")
        mv = self.tile([128, 4], F32, "mv")
        nsl = 0
        for i in range(NT):
            c = 1 if i < 2 else 0
            rows = slice(i * 128, (i + 1) * 128)
            x, y = xb[i % 2], yb[i % 2]
            dma(SP, x.ap, self.XS.ap[rows, :], reads=[self.XS.sub[i]], writes=[x.r])
            m5 = mods[5][c]
            pss = [self.psum(), self.psum()]
            if c:
                for g in range(4):
                    for half in range(2):
                        hs = slice(half * 512, (half + 1) * 512)
                        op(PE, lambda e, ps=pss[half], g=g, hs=hs, i=i: e.matmul(ps.ap, lhsT=seltc.ap[:, g, i * 128:(i + 1) * 128], rhs=yecall.ap[:, g, hs],
                                                                                 start=(g == 0), stop=(g == 3)),
                           reads=[seltc.r, yecall.r], writes=[pss[half].r])
            else:
                for hh in range(2):
                    sl = selt[nsl % 3]
                    nsl += 1
                    dma(SP, sl.ap, self.SELT.ap[i - 2, :, hh * 32:(hh + 1) * 32, :], reads=[self.SELT.sub[i - 2]], writes=[sl.r])
                    for b in range(32):
                        ec = hh * 32 + b
                        for half in range(2):
                            hs = slice(half * 512, (half + 1) * 512)
                            op(PE, lambda e, ps=pss[half], sl=sl, b=b, ec=ec, hs=hs: e.matmul(ps.ap, lhsT=sl.ap[:, b, :], rhs=yeall.ap[:, ec, hs],
                                                                                              start=(ec == 0), stop=(ec == 63)),
                               reads=[sl.r, yeall.r], writes=[pss[half].r])
            for half in range(2):
                hs = slice(half * 512, (half + 1) * 512)
                op(DVE, lambda e, ps=pss[half], y=y, m5=m5, hs=hs: e.tensor_tensor(out=y.ap[:, hs], in0=ps.ap, in1=m5.ap[:, hs], op=ALU.mult),
                   reads=[pss[half].r, m5.r], writes=[y.r])
            op(DVE, lambda e, x=x, y=y: e.scalar_tensor_tensor(out=y.ap, in0=x.ap, scalar=ALPHA, in1=y.ap, op0=ALU.mult, op1=ALU.add),
               reads=[x.r, y.r], writes=[y.r])
            self.layernorm_(y, st, mv)
            op(POOL, lambda e, y=y, x=x: e.tensor_tensor(out=x.ap, in0=y.ap, in1=g1.ap, op=ALU.mult), reads=[y.r, g1.r], writes=[x.r])
            op(POOL, lambda e, x=x: e.tensor_tensor(out=x.ap, in0=x.ap, in1=b1.ap, op=ALU.add), reads=[x.r, b1.r], writes=[x.r])
            dma(POOL, self.XS.ap[rows, :], x.ap, reads=[x.r], writes=[self.XS.sub[i]])
            if last and not c:
                dma(POOL, self.out[(i - 2) * 128:(i - 1) * 128, :], x.ap, reads=[x.r], writes=[self.outr])
            if self.cfg.get("test"):
                dma(POOL, self.dbg.ap[rows, :], x.ap, reads=[x.r], writes=[self.dbg.sub[i]])

    def finish(self):
        rs = [self.outr]
        if self.cfg.get("test"):
            rs = rs + [self.dbg.r] + self.dbg.sub
        self.P.barrier()
        self.op(SP, lambda e: e.nop(), reads=rs)
        self.P.emit()


def Tl_view(t):
    return t


def build(nc, cfg):
    k = K(nc, cfg)
    k.declare_io()
    k.setup()
    tk = cfg.get("test")
    if tk in ("mix", "gdn1", "gdn2"):
        l = cfg["layer"]
        k.premix(l)
        if l % 2 == 0:
            k.natten(l)
        else:
            k.gdn(l)
    if tk == "post":
        l = cfg["layer"]
        k.postmix(l)
        k.moe_select(l)
        k.moe_passA(l)
        k.moe_passB(l)
    if not tk:
        for l in range(cfg.get("depth", DEPTH)):
            k.premix(l)
            if l % 2 == 0:
                k.natten(l)
            else:
                k.gdn(l)
            k.postmix(l)
            k.moe_select(l)
            k.moe_passA(l)
            k.moe_passB(l)
    k.finish()
    return k


WNAMES = ["c_ctx", "ada_w", "ada_b", "ln_g", "ln_b", "na_w_qkv", "na_w_o", "na_rpb", "gdn_w_in", "gdn_conv_w", "gdn_a_log",
          "gdn_dt_bias", "gdn_norm_w", "gdn_w_o", "moe_w_router", "moe_w_gate", "moe_w_up", "moe_w_down"]


def make_in_maps(inputs, cores):
    maps = []
    shared = {}
    for n in WNAMES:
        a = np.ascontiguousarray(inputs[n], dtype=np.float32)
        if n == "c_ctx":
            a = a.reshape(1, D)
        shared[n] = a
    rp = np.zeros((2, 16, 15, 128), np.float32)
    rp[..., 48:79] = np.asarray(inputs["na_rpb"], np.float32)[..., ::-1]
    shared["rpb_pad"] = rp
    qc = np.arange(64)
    cs = np.clip(qc - 8, 0, 48)
    kc = np.arange(64)[:, None]
    cmv = np.where((kc >= cs[None, :]) & (kc < cs[None, :] + 16), 0.0, -1e30).astype(np.float32)
    shared["cmask"] = np.tile(cmv, (2, 2))
    pp = np.arange(128)
    bd = lambda b: (pp[:, None] // b == pp[None, :] // b).astype(np.float32)
    shared["gmask"] = np.stack([bd(16), bd(32) - bd(16), bd(64) - bd(32), 1.0 - bd(64)]).astype(np.float32)
    for b in cores:
        m = dict(shared)
        m["x"] = np.ascontiguousarray(inputs["x"][b], dtype=np.float32)
        m["c"] = np.ascontiguousarray(inputs["c"][b:b + 1], dtype=np.float32)
        m["ctx"] = np.ascontiguousarray(inputs["ctx"][b], dtype=np.float32)
        maps.append(m)
    return maps


def kernel(**inputs):
    nc = bass.Bass("TRN2", target_bir_lowering=False)
    build(nc, {})
    maps = make_in_maps(inputs, list(range(8)))
    res = run_bass_kernel_spmd(nc, maps, core_ids=list(range(8)))
    return np.stack([np.asarray(r["out"], dtype=np.float32) for r in res.results], axis=0)
```
